# Optimizing a Trainium2 kernel written in Bass

```python
import jax, jax.numpy as jnp
from jax import lax
import numpy as np

D_MODEL = 2048
BATCH = 4
SEQ = 4096
DEPTH = 1
DEC_BATCH = 128
DEC_SEQ = 1
PAST_LEN = 16384
PAGE_SIZE = 128

N_HEADS = 16
N_KV_HEADS = 4
HEAD_DIM = 64
GROUP = N_HEADS // N_KV_HEADS
Q_DIM = N_HEADS * HEAD_DIM
KV_DIM = N_KV_HEADS * HEAD_DIM
ROT_DIM = HEAD_DIM // 4
ROPE_THETA = 500000.0
WINDOW = 128
CONV_DIM = D_MODEL // 2
CONV_WIDTH = 3
IN_COLS = Q_DIM + 2 * KV_DIM + 3 * CONV_DIM + 2 * D_MODEL
N_EXPERTS = 64
N_GROUPS = 8
TOPK_GROUPS = 4
TOP_K = 8
EXPERT_DIM = 512
SHARED_DIM = 512
ROUTED_SCALE = 2.5
EXPERT_BLOCK = 128
ALPHA = (2 * DEPTH) ** 0.25
BETA = (8 * DEPTH) ** -0.25
LN_EPS = 1e-5

kernel_name = "hybrid_swa_sink_shortconv_moe_decode_step"


def layer_norm(x, g, b):
    xf = x.astype(jnp.float32)
    mu = xf.mean(-1, keepdims=True)
    var = jnp.mean(jnp.square(xf - mu), -1, keepdims=True)
    return ((xf - mu) * lax.rsqrt(var + LN_EPS) * g.astype(jnp.float32) + b.astype(jnp.float32)).astype(x.dtype)


def apply_rope(x, pos):
    half = ROT_DIM // 2
    inv_freq = jnp.power(jnp.float32(ROPE_THETA), -jnp.arange(half, dtype=jnp.float32) * (2.0 / ROT_DIM))
    ang = pos.astype(jnp.float32)[:, None] * inv_freq[None, :]
    cos = jnp.cos(ang)[:, None, :]
    sin = jnp.sin(ang)[:, None, :]
    xr = x[..., :ROT_DIM].astype(jnp.float32)
    x1, x2 = xr[..., :half], xr[..., half:]
    rot = jnp.concatenate([x1 * cos - x2 * sin, x2 * cos + x1 * sin], axis=-1).astype(x.dtype)
    return jnp.concatenate([rot, x[..., ROT_DIM:]], axis=-1)


def split_projection(z):
    sizes = (Q_DIM, KV_DIM, KV_DIM, CONV_DIM, CONV_DIM, CONV_DIM, D_MODEL, D_MODEL)
    return jnp.split(z, np.cumsum(sizes)[:-1].tolist(), axis=-1)


def mixer_inputs(x, w_in, pos):
    n, s = x.shape[:2]
    q, k, v, b_g, c_g, h_c, g_a, g_c = split_projection(x @ w_in)
    q = apply_rope(q.reshape(n, s, N_HEADS, HEAD_DIM), pos)
    k = apply_rope(k.reshape(n, s, N_KV_HEADS, HEAD_DIM), pos)
    v = v.reshape(n, s, N_KV_HEADS, HEAD_DIM)
    u = c_g * h_c
    return q, k, v, u, b_g, g_a, g_c


def sink_attention(q, k, v, valid, sinks):
    s = jnp.einsum('...qkgd,...ckd->...kgqc', q, k).astype(jnp.float32) * (HEAD_DIM ** -0.5)
    s = jnp.where(valid[..., None, None, :, :], s, -jnp.inf)
    sink = sinks.astype(jnp.float32).reshape(N_KV_HEADS, GROUP, 1, 1)
    m = jnp.maximum(s.max(-1, keepdims=True), sink)
    p = jnp.exp(s - m)
    p = p / (p.sum(-1, keepdims=True) + jnp.exp(sink - m))
    return jnp.einsum('...kgqc,...ckd->...qkgd', p.astype(v.dtype), v)


def window_attention_prompt(q, k, v, sinks):
    n, s = q.shape[:2]
    nb = s // WINDOW
    qb = q.reshape(n, nb, WINDOW, N_KV_HEADS, GROUP, HEAD_DIM)

    def band(t):
        tb = t.reshape(n, nb, WINDOW, N_KV_HEADS, HEAD_DIM)
        prev = jnp.concatenate([jnp.zeros_like(tb[:, :1]), tb[:, :-1]], axis=1)
        return jnp.concatenate([prev, tb], axis=2)

    a = jnp.arange(WINDOW)[:, None]
    c = jnp.arange(2 * WINDOW)[None, :]
    blk = jnp.arange(nb)[:, None, None]
    valid = (c > a) & (c <= a + WINDOW) & ((blk > 0) | (c >= WINDOW))
    out = sink_attention(qb, band(k), band(v), valid, sinks)
    return out.reshape(n, s, Q_DIM)


def window_attention_sample(q, k_all, v_all, sinks):
    n, s = q.shape[:2]
    l = k_all.shape[1]
    qpos = PAST_LEN + jnp.arange(s)
    kpos = PAST_LEN + s - l + jnp.arange(l)
    valid = (kpos[None, :] <= qpos[:, None]) & (kpos[None, :] > qpos[:, None] - WINDOW)
    out = sink_attention(q.reshape(n, s, N_KV_HEADS, GROUP, HEAD_DIM), k_all, v_all, valid, sinks)
    return out.reshape(n, s, Q_DIM)


def causal_conv(u_ext, w):
    l = u_ext.shape[1] - (CONV_WIDTH - 1)
    out = w[0] * u_ext[:, 0:l]
    for j in range(1, CONV_WIDTH):
        out = out + w[j] * u_ext[:, j:j + l]
    return out


def route(h, w_router, router_bias):
    t = h.shape[0]
    per_group = N_EXPERTS // N_GROUPS
    scores = jax.nn.sigmoid((h @ w_router).astype(jnp.float32))
    choice = scores + router_bias.astype(jnp.float32)
    group_score = lax.top_k(choice.reshape(t, N_GROUPS, per_group), 2)[0].sum(-1)
    _, top_groups = lax.top_k(group_score, TOPK_GROUPS)
    group_keep = (top_groups[:, :, None] == jnp.arange(N_GROUPS)[None, None, :]).any(axis=1)
    expert_keep = jnp.repeat(group_keep, per_group, axis=1)
    _, idx = lax.top_k(jnp.where(expert_keep, choice, -jnp.inf), TOP_K)
    w = jnp.take_along_axis(scores, idx, axis=1)
    w = w / w.sum(-1, keepdims=True) * ROUTED_SCALE
    return idx.astype(jnp.int32), w


def routed_experts(h, idx, wts, w_gate, w_up, w_down):
    t, d = h.shape
    m = t * TOP_K
    flat_e = idx.reshape(m)
    order = jnp.argsort(flat_e)
    sorted_e = flat_e[order]
    counts = jnp.zeros((N_EXPERTS,), jnp.int32).at[flat_e].add(1)
    padded = (counts + EXPERT_BLOCK - 1) // EXPERT_BLOCK * EXPERT_BLOCK
    pad_end = jnp.cumsum(padded)
    pad_start = pad_end - padded
    start = jnp.cumsum(counts) - counts
    dest = pad_start[sorted_e] + jnp.arange(m, dtype=jnp.int32) - start[sorted_e]
    n_blocks = (m + N_EXPERTS * (EXPERT_BLOCK - 1) + EXPERT_BLOCK - 1) // EXPERT_BLOCK
    p = n_blocks * EXPERT_BLOCK
    tok_buf = jnp.full((p,), t, jnp.int32).at[dest].set((order // TOP_K).astype(jnp.int32))
    w_buf = jnp.zeros((p,), h.dtype).at[dest].set(wts.reshape(m)[order].astype(h.dtype))
    block_e = jnp.minimum(jnp.searchsorted(pad_end, jnp.arange(n_blocks, dtype=jnp.int32) * EXPERT_BLOCK, side='right'), N_EXPERTS - 1)
    h_pad = jnp.concatenate([h, jnp.zeros((1, d), h.dtype)], axis=0)

    def one_block(args):
        tok, e, w = args
        xb = h_pad[tok]
        act = jax.nn.silu(xb @ w_gate[e]) * (xb @ w_up[e])
        return (act @ w_down[e]) * w[:, None]

    ys = lax.map(one_block, (tok_buf.reshape(n_blocks, EXPERT_BLOCK), block_e, w_buf.reshape(n_blocks, EXPERT_BLOCK)))
    return jnp.zeros_like(h_pad).at[tok_buf].add(ys.reshape(p, d))[:t]


def merge_and_channel_mix(x, attn, conv_branch, g_a, g_c, w_attn_out, w_conv_out, w_o, ln1_g, ln1_b,
                          w_router, router_bias, w_exp_gate, w_exp_up, w_exp_down,
                          w_sh_gate, w_sh_up, w_sh_down, ln2_g, ln2_b):
    mixed = (jax.nn.sigmoid(g_a) * (attn @ w_attn_out) + jax.nn.sigmoid(g_c) * (conv_branch @ w_conv_out)) @ w_o
    h = layer_norm(ALPHA * x + mixed, ln1_g, ln1_b)
    n, s, d = h.shape
    hf = h.reshape(n * s, d)
    idx, wts = route(hf, w_router, router_bias)
    shared = (jax.nn.silu(hf @ w_sh_gate) * (hf @ w_sh_up)) @ w_sh_down
    ffn = routed_experts(hf, idx, wts, w_exp_gate, w_exp_up, w_exp_down) + shared
    return layer_norm(ALPHA * h + ffn.reshape(n, s, d), ln2_g, ln2_b)


def setup_inputs(seed: int = 0) -> dict:
    key = jax.random.key(seed)
    ks = jax.random.split(key, 24)
    f32 = jnp.float32

    def nrm(k, shape, scale):
        return jax.random.normal(k, shape, f32) * scale

    win_buf = min(WINDOW, PAST_LEN)
    return {
        'x_prompt': nrm(ks[0], (BATCH, SEQ, D_MODEL), 1.0),
        'x_sample': nrm(ks[1], (DEC_BATCH, DEC_SEQ, D_MODEL), 1.0),
        'cache_k': nrm(ks[2], (DEPTH, DEC_BATCH, win_buf, N_KV_HEADS, HEAD_DIM), 1.0),
        'cache_v': nrm(ks[3], (DEPTH, DEC_BATCH, win_buf, N_KV_HEADS, HEAD_DIM), 1.0),
        'state_conv': nrm(ks[4], (DEPTH, DEC_BATCH, CONV_WIDTH - 1, CONV_DIM), 1.0),
        'w_in': nrm(ks[5], (DEPTH, D_MODEL, IN_COLS), D_MODEL ** -0.5),
        'attn_sinks': nrm(ks[6], (DEPTH, N_HEADS), 0.5),
        'conv_w': nrm(ks[7], (DEPTH, CONV_WIDTH, CONV_DIM), CONV_WIDTH ** -0.5),
        'w_attn_out': nrm(ks[8], (DEPTH, Q_DIM, D_MODEL), Q_DIM ** -0.5 * BETA),
        'w_conv_out': nrm(ks[9], (DEPTH, CONV_DIM, D_MODEL), CONV_DIM ** -0.5 * BETA),
        'w_o': nrm(ks[10], (DEPTH, D_MODEL, D_MODEL), D_MODEL ** -0.5 * BETA),
        'ln1_g': 1.0 + nrm(ks[11], (DEPTH, D_MODEL), 0.02),
        'ln1_b': nrm(ks[12], (DEPTH, D_MODEL), 0.02),
        'w_router': nrm(ks[13], (DEPTH, D_MODEL, N_EXPERTS), D_MODEL ** -0.5),
        'router_bias': nrm(ks[14], (DEPTH, N_EXPERTS), 0.01),
        'w_exp_gate': nrm(ks[15], (DEPTH, N_EXPERTS, D_MODEL, EXPERT_DIM), D_MODEL ** -0.5),
        'w_exp_up': nrm(ks[16], (DEPTH, N_EXPERTS, D_MODEL, EXPERT_DIM), D_MODEL ** -0.5),
        'w_exp_down': nrm(ks[17], (DEPTH, N_EXPERTS, EXPERT_DIM, D_MODEL), EXPERT_DIM ** -0.5 * BETA),
        'w_sh_gate': nrm(ks[18], (DEPTH, D_MODEL, SHARED_DIM), D_MODEL ** -0.5),
        'w_sh_up': nrm(ks[19], (DEPTH, D_MODEL, SHARED_DIM), D_MODEL ** -0.5),
        'w_sh_down': nrm(ks[20], (DEPTH, SHARED_DIM, D_MODEL), SHARED_DIM ** -0.5 * BETA),
        'ln2_g': 1.0 + nrm(ks[21], (DEPTH, D_MODEL), 0.02),
        'ln2_b': nrm(ks[22], (DEPTH, D_MODEL), 0.02),
    }


def reference(x_prompt, x_sample, cache_k, cache_v, state_conv, w_in, attn_sinks, conv_w,
              w_attn_out, w_conv_out, w_o, ln1_g, ln1_b, w_router, router_bias,
              w_exp_gate, w_exp_up, w_exp_down, w_sh_gate, w_sh_up, w_sh_down, ln2_g, ln2_b):
    yp, ys = x_prompt, x_sample
    p_k, p_v, p_c, s_k, s_v, s_c = [], [], [], [], [], []
    for l in range(DEPTH):
        tail = (w_attn_out[l], w_conv_out[l], w_o[l], ln1_g[l], ln1_b[l], w_router[l], router_bias[l],
                w_exp_gate[l], w_exp_up[l], w_exp_down[l], w_sh_gate[l], w_sh_up[l], w_sh_down[l],
                ln2_g[l], ln2_b[l])
        n, s = yp.shape[:2]
        q, k, v, u, b_g, g_a, g_c = mixer_inputs(yp, w_in[l], jnp.arange(s))
        attn = window_attention_prompt(q, k, v, attn_sinks[l])
        u_ext = jnp.concatenate([jnp.zeros((n, CONV_WIDTH - 1, CONV_DIM), u.dtype), u], axis=1)
        conv_branch = b_g * causal_conv(u_ext, conv_w[l])
        keep = min(WINDOW, s)
        p_k.append(k[:, s - keep:])
        p_v.append(v[:, s - keep:])
        p_c.append(u_ext[:, -(CONV_WIDTH - 1):])
        yp_next = merge_and_channel_mix(yp, attn, conv_branch, g_a, g_c, *tail)
        sd = ys.shape[1]
        q, k, v, u, b_g, g_a, g_c = mixer_inputs(ys, w_in[l], PAST_LEN + jnp.arange(sd))
        k_all = jnp.concatenate([cache_k[l], k], axis=1)
        v_all = jnp.concatenate([cache_v[l], v], axis=1)
        attn = window_attention_sample(q, k_all, v_all, attn_sinks[l])
        u_ext = jnp.concatenate([state_conv[l], u], axis=1)
        conv_branch = b_g * causal_conv(u_ext, conv_w[l])
        buf = cache_k.shape[2]
        s_k.append(k_all[:, -buf:])
        s_v.append(v_all[:, -buf:])
        s_c.append(u_ext[:, -(CONV_WIDTH - 1):])
        ys = merge_and_channel_mix(ys, attn, conv_branch, g_a, g_c, *tail)
        yp = yp_next
    return (yp, ys, jnp.stack(p_k), jnp.stack(p_v), jnp.stack(p_c), jnp.stack(s_k), jnp.stack(s_v), jnp.stack(s_c))
```

```python
import contextlib
import numpy as np
import concourse.bass as bass
import concourse.mybir as mybir
from concourse.bass_utils import run_bass_kernel_spmd

F32 = mybir.dt.float32
BF16 = mybir.dt.bfloat16
I32 = mybir.dt.int32
AF = mybir.ActivationFunctionType
ALU = mybir.AluOpType
AX = mybir.AxisListType

ENGS = ("pe", "act", "dve", "pool", "sp")


class _Ctr:
    def __init__(self):
        self.n = 0

    def next(self):
        self.n += 1
        return self.n


DBGC = _Ctr()

P = 128
D = 2048
NCORES = 8
SEQ = 4096
TOK = 2048
NS = 16
NT = 16
GT = 4
NG = NT // GT
NE = 64
CAP = 384
NBLK = CAP // P
NSLOT = NE * CAP
ALPHA = 2.0 ** 0.25
LN_EPS = 1e-5
PAST = 16384
OQ, OK_, OV, OB, OC, OH, OGA, OGC = 0, 1024, 1280, 1536, 2560, 3584, 4608, 6656
NEG = -30000.0


class Op:
    __slots__ = ("eng", "fn", "deps", "is_dma", "chan", "idx", "signal", "semval", "gidx")

    def __init__(self, eng, fn, is_dma, chan):
        self.eng = eng
        self.fn = fn
        self.deps = {}
        self.is_dma = is_dma
        self.chan = chan
        self.signal = False
        self.semval = None


def _slot(op):
    return ("ch", op.chan) if op.is_dma else ("eng", op.eng)


class Sched:
    def __init__(self, nc):
        self.nc = nc
        self.ops = {e: [] for e in ENGS}
        self.last_writer = {}
        self.readers = {}
        self.all_ops = []
        self.planning = False

    def _add(self, eng, fn, reads, writes, is_dma=False, chan=None):
        if self.planning:
            return None
        import os
        mx = int(os.environ.get("KMAXOPS", "0"))
        if mx and len(self.all_ops) >= mx:
            return None
        psr = [k for k in reads if isinstance(k, tuple) and k[0] == "ps"]
        if psr:
            writes = list(writes) + psr
        op = Op(eng, fn, is_dma, chan)
        deps = {}

        def add(d):
            if d is op:
                return
            s = _slot(d)
            o = deps.get(s)
            if o is None or d.gidx > o.gidx:
                deps[s] = d

        for k in reads:
            w = self.last_writer.get(k)
            if w is not None:
                add(w)
        for k in writes:
            w = self.last_writer.get(k)
            if w is not None:
                add(w)
            for r in self.readers.get(k, {}).values():
                add(r)
        op.deps = deps
        op.gidx = len(self.all_ops)
        for k in reads:
            self.readers.setdefault(k, {})[_slot(op)] = op
        for k in writes:
            self.last_writer[k] = op
            self.readers[k] = {}
        self.ops[eng].append(op)
        self.all_ops.append(op)
        return op

    def op(self, eng, fn, reads=(), writes=()):
        return self._add(eng, fn, reads, writes)

    def dma(self, eng, chan, fn, reads=(), writes=()):
        return self._add(eng, fn, reads, writes, is_dma=True, chan=chan)

    def barrier(self):
        if self.planning:
            return
        lasts = {}
        for op in self.all_ops:
            if op.fn is not None:
                lasts[_slot(op)] = op
        for e in ENGS:
            op = Op(e, None, False, None)
            op.deps = {s: d for s, d in lasts.items()}
            op.gidx = len(self.all_ops)
            self.ops[e].append(op)
            self.all_ops.append(op)

    def finalize(self, es):
        nc = self.nc
        for op in self.all_ops:
            for d in op.deps.values():
                if d.is_dma:
                    continue
                if d.eng == "pe" and op.eng == "pe" and not op.is_dma:
                    continue
                d.signal = True
        self.sem = {e: es.enter_context(nc.semaphore("sem_" + e)) for e in ENGS}
        chans = []
        for op in self.all_ops:
            if op.is_dma and op.chan not in chans:
                chans.append(op.chan)
        self.chsem = {c: es.enter_context(nc.semaphore("ch_" + str(c))) for c in chans}
        cnt = {e: 0 for e in ENGS}
        chcnt = {c: 0 for c in chans}
        for op in self.all_ops:
            if op.is_dma:
                chcnt[op.chan] += 16
                op.semval = chcnt[op.chan]
            elif op.signal:
                cnt[op.eng] += 1
                op.semval = cnt[op.eng]
        for op in self.all_ops:
            if op.is_dma and str(op.chan).startswith("cst"):
                op.semval = chcnt[op.chan]
        self.chfinal = chcnt

    def emit(self, ename, eng):
        seen = {}
        for op in self.ops[ename]:
            for s, d in op.deps.items():
                if (not d.is_dma) and d.eng == "pe" and op.eng == "pe" and not op.is_dma:
                    continue
                v = d.semval
                if v is None:
                    continue
                if seen.get(s, 0) >= v:
                    continue
                sem = self.chsem[s[1]] if s[0] == "ch" else self.sem[s[1]]
                eng.wait_ge(sem, v)
                seen[s] = v
            if op.fn is None:
                continue
            inst = op.fn(eng)
            if op.is_dma:
                inst.then_inc(self.chsem[op.chan], 16)
            elif op.signal:
                inst.then_inc(self.sem[op.eng], 1)

    def run_block(self, block):
        S = self

        def mk(ename):
            def body(eng):
                S.emit(ename, eng)
                if ename == "sp":
                    for c, v in S.chfinal.items():
                        if v > 0:
                            eng.wait_ge(S.chsem[c], v)
            return body

        block.tensor(mk("pe"))
        block.scalar(mk("act"))
        block.vector(mk("dve"))
        block.gpsimd(mk("pool"))
        block.sync(mk("sp"))


def fap(t, offset, dims, parts=None, p0=0):
    base = t if isinstance(t, bass.AP) else t[:]
    pst = base.ap[0][0]
    npart = base.ap[0][1] if parts is None else parts
    return bass.AP(tensor=base.tensor, offset=base.offset + p0 * pst + offset,
                   ap=[[pst, npart]] + [list(d) for d in dims])


class Builder:
    def __init__(self, stop_after=None, debug=False):
        self.stop_after = stop_after
        self.debug = debug
        self.nc = bass.Bass("TRN2", target_bir_lowering=False)
        self.S = Sched(self.nc)
        self.bank_rr = 0
        self.wplan = []
        self.wpos = 0
        self.tmp_rr = {}

    def bcreg(self, e):
        if getattr(self, "_bcreg", None) is None:
            self._bcreg = e.to_reg(NSLOT - 1)
        return self._bcreg

    def din(self, name, shape, dt=F32):
        return self.nc.dram_tensor(name, list(shape), dt, kind="ExternalInput").ap()

    def dout(self, name, shape, dt=F32):
        return self.nc.dram_tensor(name, list(shape), dt, kind="ExternalOutput").ap()

    def dscr(self, name, shape, dt=F32):
        return self.nc.dram_tensor(name, list(shape), dt, kind="Internal").ap()

    def bank(self, n=1):
        if n == 2 and self.bank_rr % 2 == 1:
            self.bank_rr += 1
        b = self.bank_rr % 6
        self.bank_rr += n
        return b

    def bk(self, b, n=1):
        return [("ps", b + i) for i in range(n)]

    def mm(self, out, lhsT, rhs, start, stop, reads, writes):
        self.S.op("pe", lambda e: e.matmul(out, lhsT=lhsT, rhs=rhs, start=start, stop=stop), reads, writes)

    def tr(self, out, in_, ident, reads, writes):
        self.S.op("pe", lambda e: e.transpose(out=out, in_=in_, identity=ident), reads, writes)

    def act(self, out, in_, func, reads, writes, scale=None, bias=None, accum_out=None):
        kw = {}
        if scale is not None:
            kw["scale"] = scale
        if bias is not None:
            kw["bias"] = bias
        if accum_out is not None:
            kw["accum_out"] = accum_out
        self.S.op("act", lambda e: e.activation(out=out, in_=in_, func=func, **kw), reads, writes)

    def tt(self, eng, out, in0, in1, op, reads, writes):
        self.S.op(eng, lambda e: e.tensor_tensor(out=out, in0=in0, in1=in1, op=op), reads, writes)

    def ts(self, eng, out, in0, s1, op0, reads, writes, s2=None, op1=None, accum_out=None):
        kw = {}
        if op1 is not None:
            kw["op1"] = op1
        if accum_out is not None:
            kw["accum_out"] = accum_out
        self.S.op(eng, lambda e: e.tensor_scalar(out=out, in0=in0, scalar1=s1, scalar2=s2, op0=op0, **kw), reads, writes)

    def stt(self, out, in0, scalar, in1, op0, op1, reads, writes, accum_out=None):
        kw = {}
        if accum_out is not None:
            kw["accum_out"] = accum_out
        self.S.op("dve", lambda e: e.scalar_tensor_tensor(out=out, in0=in0, scalar=scalar, in1=in1, op0=op0, op1=op1, **kw), reads, writes)

    def red(self, out, in_, op, reads, writes, axis=AX.X):
        self.S.op("dve", lambda e: e.tensor_reduce(out=out, in_=in_, axis=axis, op=op), reads, writes)

    def cp(self, eng, out, in_, reads, writes):
        if eng == "act":
            self.S.op("act", lambda e: e.activation(out=out, in_=in_, func=AF.Copy), reads, writes)
        else:
            self.S.op(eng, lambda e: e.tensor_copy(out=out, in_=in_), reads, writes)

    def dma(self, eng, chan, out, in_, reads, writes, **kw):
        return self.S.dma(eng, chan, lambda e: e.dma_start(out=out, in_=in_, **kw), reads, writes)

    def wget(self, spec):
        S = self.S
        i = self.wpos
        self.wpos += 1
        if S.planning:
            self.wplan.append(spec)
        slot = i % self.NB
        nk, ncols, parts = spec
        while self.wissued < min(len(self.wplan), i + self.NB - 2):
            self._wissue(self.wissued)
            self.wissued += 1
        ap = fap(self.wring, slot * self.WSLOT, [[ncols, nk], [1, ncols]])
        return ap, [("w", slot)]

    def _wissue(self, j):
        if self.S.planning:
            return
        nk, ncols, parts = self.wplan[j]
        slot = j % self.NB
        for (c0, n, src) in parts:
            dst = fap(self.wring, slot * self.WSLOT + c0, [[ncols, nk], [1, n]])
            self.dma("pool", "w%d" % slot, dst, src.rearrange("(k p) n -> p k n", p=P), [], [("w", slot)])

    def areset(self):
        self.apos = 0

    def aalloc(self, nbytes):
        nbytes = (nbytes + 63) // 64 * 64
        off = self.apos
        self.apos += nbytes
        assert self.apos <= self.ARENA_BYTES, ("arena overflow", self.apos)
        keys = [("ar", b) for b in range(off // 2048, (off + nbytes - 1) // 2048 + 1)]
        return off, keys

    def af32(self, n, dims=None):
        off, keys = self.aalloc(n * 4)
        ap = fap(self.arena, off // 4, dims if dims is not None else [[1, n]])
        return ap, keys, off // 4

    def abf(self, n, dims=None):
        off, keys = self.aalloc(n * 2)
        ap = fap(self.arena_bf, off // 2, dims if dims is not None else [[1, n]])
        return ap, keys, off // 2


def build_program(stop_after=None, debug=False):
    B = Builder(stop_after, debug)
    nc = B.nc
    S = B.S

    xp = B.din("xp", [TOK + P, D])
    xs = B.din("xs", [NS, D])
    ck = B.din("ck", [NS, P, 256])
    cv = B.din("cv", [NS, P, 256])
    scv = B.din("sc", [NS, 2, 1024])
    w_in = B.din("w_in", [D, 8704])
    sinks = B.din("sinks", [1, 16])
    conv_w = B.din("conv_w", [3, 1024])
    w_ao = B.din("w_ao", [1024, D])
    w_co = B.din("w_co", [1024, D])
    w_o = B.din("w_o", [D, D])
    ln1_g = B.din("ln1_g", [1, D])
    ln1_b = B.din("ln1_b", [1, D])
    w_r = B.din("w_r", [D, NE])
    r_bias = B.din("r_bias", [1, NE])
    if stop_after is None:
        w_eg = B.din("w_eg", [NE, D, 512])
        w_eu = B.din("w_eu", [NE, D, 512])
        w_ed = B.din("w_ed", [NE, 512, D])
    w_sg = B.din("w_sg", [D, 512])
    w_su = B.din("w_su", [D, 512])
    w_sd = B.din("w_sd", [512, D])
    ln2_g = B.din("ln2_g", [1, D])
    ln2_b = B.din("ln2_b", [1, D])
    cosd = B.din("cosT", [P, 2192])
    sind = B.din("sinT", [P, 2192])
    cst = B.din("cst", [P, 1024])
    mskd = B.din("msk", [P, 512])
    c16 = B.din("c16", [P, 64 + 64 + 64 + 8 + 32])

    yp = B.dout("yp", [TOK, D])
    ys = B.dout("ys", [NS, D])
    pck = B.dout("pck", [P, 256])
    pcv = B.dout("pcv", [P, 256])
    psc = B.dout("psc", [2, 1024])
    sck = B.dout("sck", [NS, P, 256])
    scvo = B.dout("scvo", [NS, P, 256])
    ssc = B.dout("ssc", [NS, 2, 1024])
    if debug:
        dbg = {k: B.dout("dbg_" + k, shp) for k, shp in [("h", [TOK + NS, D]), ("cnt", [P, NE]), ("attn", [P, 8, 640]), ("conv", [P, 8, 640]), ("d8", [P, 17, 8]), ("w8", [P, 17, 8])]}

    XG = B.dscr("XG", [NSLOT + P, D], BF16)
    Y = B.dscr("Y", [NSLOT, D], BF16)
    BASE = B.dscr("BASE", [TOK + P, D], F32)

    es = contextlib.ExitStack()
    with es:
        def sb(name, shape, dt):
            return es.enter_context(nc.sbuf_tensor(name, list(shape), dt))

        ps = es.enter_context(nc.psum_tensor("ps", [P, 8, 512], F32))
        psb = ps.bitcast(BF16)

        ident_f = sb("ident_f", [P, P], F32)
        ident = sb("ident", [P, P], BF16)
        rotT = sb("rotT", [P, P], BF16)
        triU = sb("triU", [P, P], BF16)
        ones = sb("ones", [P, P], BF16)
        msk = sb("mskt", [P, 512], F32)
        c16t = sb("c16t", [P, 232], F32)
        sink8 = sb("sink8", [P, 16], F32)
        sinkraw = sb("sinkraw", [P, 16], F32)
        rbias = sb("rbias", [P, NE], F32)
        convw = sb("convw", [P, 8, 3], F32)
        lng = sb("lng", [P, D], F32)
        lnb = sb("lnb", [P, D], F32)
        d8all = sb("d8all", [P, 17, 8], I32)
        w8all = sb("w8all", [P, 17, 8], F32)
        cbase = sb("cbase", [P, NE], F32)
        epsT = sb("epsT", [P, 1], F32)
        sinkcol = sb("sinkcol", [P, 1], F32)
        rsel_t = sb("rsel", [P, 5, NE], BF16)

        def load_consts():
            B.dma("sp", "cst", ident_f[:], cst[:, 0:128], [], ["ident_f"])
            B.dma("pool", "cstp", ident[:], cst[:, 0:128], [], ["ident"])
            B.dma("pool", "cstp", rotT[:], cst[:, 128:256], [], ["rotT"])
            B.dma("pool", "cstp", triU[:], cst[:, 256:384], [], ["triU"])
            B.dma("pool", "cstp", ones[:], cst[:, 384:512], [], ["ones"])
            B.dma("sp", "cst", msk[:], mskd[:, :], [], ["msk"])
            B.dma("sp", "cst", c16t[:], c16[:, :], [], ["c16t"])
            B.dma("sp", "cst", sinkraw[:], fap_dram_bcast(sinks, 16), [], ["sinkraw"])
            B.dma("sp", "cst", rbias[:], fap_dram_bcast(r_bias, NE), [], ["rbias"])
            for j in range(3):
                B.dma("sp", "cst", convw[:, :, j], conv_w[j, :].rearrange("(c p) -> p c", p=P), [], ["convw%d" % j], allow_slow_non_contiguous=True)
            B.ts("dve", fap(sink8, 0, [[4, 4], [2, 2], [1, 2]]), fap(sinkraw, 0, [[4, 4], [1, 2], [2, 2]]), 8.0, ALU.mult, ["sinkraw"], ["sink8"])
            S.op("dve", lambda e: e.memset(cbase[:], 0.0), [], ["cbase"])
            S.op("dve", lambda e: e.memset(w8all[:], 0.0), [], [("w8", t) for t in range(17)])
            S.op("dve", lambda e: e.memset(epsT[:], LN_EPS), [], ["epsT"])
            B.dma("sp", "cst", sinkcol[0:16, :], sinks.rearrange("a h -> h a"), [], ["sinkcol"], allow_slow_non_contiguous=True)
            B.ts("dve", sinkcol[0:16, :], sinkcol[0:16, :], 8.0, ALU.mult, ["sinkcol"], ["sinkcol"])

        def fap_dram_bcast(src, n):
            return bass.AP(tensor=src.tensor, offset=src.offset, ap=[[0, P], [1, n]])

        load_consts()

        pa = contextlib.ExitStack()
        with pa:
            def sba(name, shape, dt):
                return pa.enter_context(nc.sbuf_tensor(name, list(shape), dt))

            NCMAX = 640
            bigT = sba("bigT", [P, 16, NCMAX], BF16)
            attnT = sba("attnT", [P, 8, NCMAX], BF16)
            convT = sba("convT", [P, 8, NCMAX], BF16)
            kTl = sba("kTl", [P, 4, P + NCMAX], BF16)
            kT32 = sba("kT32", [P, 2, 144], F32)
            vl = sba("vl", [P, 6, 256], BF16)
            v32 = sba("v32", [P, 2, 256], F32)
            cosl = sba("cosl", [P, NCMAX], F32)
            sinl = sba("sinl", [P, NCMAX], F32)
            uprev = sba("uprev", [P, 8, 2], F32)
            hres = sba("hres", [P, 5, D], F32)
            B.NB = 6
            B.WSLOT = 4096
            B.wring = sba("wring", [P, B.NB * B.WSLOT], BF16)
            B.ARENA_BYTES = 38 * 1024
            B.arena = sba("arena", [P, B.ARENA_BYTES // 4], F32)
            B.arena_bf = B.arena.bitcast(BF16)

            hres_bf = hres.bitcast(BF16)
            def zero_fill_xg():
                zt = hres_bf[:, 4, 0:D]
                S.op("dve", lambda e: e.memset(zt, 0.0), [], [("hres", 4)])
                nrow_total = NSLOT + P
                r = 0
                while r < nrow_total:
                    n = min(4 * P, nrow_total - r)
                    B.dma("act", "zf", XG[r:r + n, :].rearrange("(a p) d -> p a d", p=P), fap(hres_bf, 4 * 4096, [[0, n // P], [1, D]]), [("hres", 4)], ["XG"])
                    r += n

            B.dma("sp", "cst", lng[:], fap_dram_bcast(ln1_g, D), [], ["lng"])
            B.dma("sp", "cst", lnb[:], fap_dram_bcast(ln1_b, D), [], ["lnb"])

            if debug:
                S.op("dve", lambda e: e.memset(attnT[:], 0.0), [], ["attnT"])
                S.op("dve", lambda e: e.memset(convT[:], 0.0), [], ["convT"])

            def phase_a():
                B.wpos = 0
                B.wissued = 0
                for g in range(NG):
                    group(g)

            def group(g):
                halo = (g == 0)
                samp = (g == NG - 1)
                moff = P if halo else 0
                NC = moff + GT * P + (NS if samp else 0)
                soff = moff + GT * P
                absb = (P + GT * P * g) - moff
                own_segs = [(moff, GT * P)] + ([(soff, NS)] if samp else [])
                all_segs = ([(0, P)] if halo else []) + own_segs
                tiles = []
                if halo:
                    tiles.append((0, P, xp[0:P, :], "halo", None))
                for t in range(GT):
                    tt_ = g * GT + t
                    tiles.append((moff + t * P, P, xp[P + tt_ * P:P + (tt_ + 1) * P, :], "main", tt_))
                if samp:
                    tiles.append((soff, NS, xs[:, :], "samp", None))
                own_tiles = [tl for tl in tiles if tl[3] != "halo"]

                B.areset()
                xin = [B.af32(D) for _ in range(2)]
                xbf = [B.abf(D) for _ in range(2)]
                B.dma("sp", "tabc", cosl[:, 0:NC], cosd[:, absb:absb + NC], [], ["cosl"])
                B.dma("sp", "tabs", sinl[:, 0:NC], sind[:, absb:absb + NC], [], ["sinl"])
                for i, (c0, nr, src, kind, tt_) in enumerate(tiles):
                    xi, xik, _ = xin[i % 2]
                    xb, xbk, _ = xbf[i % 2]
                    B.dma("sp", "xin%d" % (i % 2), fap(xi, 0, [[1, D]], parts=nr), src, [], xik)
                    B.cp("act" if i % 2 == 0 else "dve", fap(xb, 0, [[1, D]], parts=nr), fap(xi, 0, [[1, D]], parts=nr), xik, xbk)
                    b2 = B.bank(2)
                    for k in range(16):
                        o = fap(psb, (b2 + k // 8) * 1024 + (k % 8) * nr, [[1, nr]])
                        B.tr(o, fap(xb, k * P, [[1, P]], parts=nr), ident[0:nr, 0:nr], xbk + ["ident"], B.bk(b2 + k // 8))
                    for hh in range(2):
                        src_ap = fap(psb, (b2 + hh) * 1024, [[nr, 8], [1, nr]])
                        B.cp("dve" if hh == 0 else "act", bigT[:, hh * 8:(hh + 1) * 8, c0:c0 + nr], src_ap, B.bk(b2 + hh), ["bigT"])

                if B.stop_after == "S0":
                    return
                def proj(wt, wk, nk, col_lo, segs, rhs_t, rhs_key, consumer):
                    for (c0, n) in segs:
                        b = B.bank()
                        for k in range(nk):
                            B.mm(ps[:, b, 0:n], wt[:, k, col_lo:col_lo + P], rhs_t[:, k, c0:c0 + n], k == 0, k == nk - 1,
                                 wk + [rhs_key], B.bk(b))
                        consumer(b, c0, n)

                def win_spec(col0, ncols=256):
                    return (16, ncols, [(0, ncols, w_in[:, col0:col0 + ncols])])

                B.areset()
                qT, qTk, _ = B.abf(8 * NCMAX, [[NCMAX, 8], [1, NCMAX]])
                qmark = B.apos
                qb = [B.abf(512) for _ in range(2)]
                t1 = [B.af32(512) for _ in range(2)]
                t2 = [B.af32(512) for _ in range(2)]
                rr = [0]

                def rope(b, c0, n, out_bf, out_keys, out_f32=None, out_f32_keys=None):
                    i = rr[0] % 2
                    rr[0] += 1
                    q_b, qbk, _ = qb[i]
                    a1, a1k, _ = t1[i]
                    a2, a2k, _ = t2[i]
                    B.cp("act", q_b[:, 0:n], ps[:, b, 0:n], B.bk(b), qbk)
                    B.tt("dve", a1[:, 0:n], ps[:, b, 0:n], cosl[:, c0:c0 + n], ALU.mult, B.bk(b) + ["cosl"], a1k)
                    b2 = B.bank()
                    B.mm(ps[:, b2, 0:n], rotT[:], q_b[:, 0:n], True, True, qbk + ["rotT"], B.bk(b2))
                    B.tt("dve", a2[:, 0:n], ps[:, b2, 0:n], sinl[:, c0:c0 + n], ALU.mult, B.bk(b2) + ["sinl"], a2k)
                    B.tt("dve", out_bf, a1[:, 0:n], a2[:, 0:n], ALU.add, a1k + a2k, out_keys)
                    if out_f32 is not None:
                        B.tt("dve", out_f32, a1[:, 0:n], a2[:, 0:n], ALU.add, a1k + a2k, out_f32_keys)

                for qs in range(4):
                    wt, wk = B.wget(win_spec(OQ + qs * 256))
                    for cc in range(2):
                        c = qs * 2 + cc
                        proj(wt, wk, 16, cc * P, own_segs, bigT, "bigT",
                             lambda b, c0, n, c=c: rope(b, c0, n, qT[:, c, c0:c0 + n], qTk))
                if B.stop_after == "S1q":
                    return
                if not halo:
                    ksrc = (P if g == 1 else 0) + GT * P
                    B.cp("act", kTl[:, :, 0:P], kTl[:, :, ksrc:ksrc + P], ["kTl"], ["kTl"])
                    B.cp("act", vl[:, 0, :], vl[:, (GT + 1) if g == 1 else GT, :], ["vl"], ["vl"])
                wt, wk = B.wget(win_spec(OK_))
                krt = [B.af32(512) for _ in range(2)]
                kri = [0]
                ksegs = list(all_segs)
                if samp:
                    ksegs = [(moff, (GT - 1) * P), (moff + (GT - 1) * P, P), (soff, NS)]
                for kp in range(2):
                    def kcons(b, c0, n, kp=kp):
                        kr, krk, _ = krt[kri[0] % 2]
                        kri[0] += 1
                        rope(b, c0, n, kr[:, 0:n], krk)
                        for j in range(2):
                            kv = 2 * kp + j
                            for dup in range(2):
                                B.cp("act" if dup == 0 else "dve", kTl[64 * dup:64 * dup + 64, kv, P + c0:P + c0 + n],
                                     kr[64 * j:64 * j + 64, 0:n], krk, ["kTl"])
                        if samp and c0 == soff:
                            B.cp("act", kT32[:, kp, P:P + NS], kr[:, 0:n], krk, ["kT32"])
                        elif samp and c0 == moff + (GT - 1) * P:
                            B.cp("dve", kT32[:, kp, 0:P], kr[:, 0:n], krk, ["kT32"])
                    proj(wt, wk, 16, kp * P, ksegs, bigT, "bigT", kcons)
                if B.stop_after == "S1k":
                    return
                wt, wk = B.wget(win_spec(OV))
                for li, (c0, nr, src, kind, tt_) in enumerate(tiles):
                    b = B.bank()
                    for k in range(16):
                        B.mm(ps[0:nr, b, 0:256], bigT[:, k, c0:c0 + nr], wt[:, k, :], k == 0, k == 15, wk + ["bigT"], B.bk(b))
                    B.cp("act", vl[0:nr, 1 + li, :], ps[0:nr, b, 0:256], B.bk(b), ["vl"])
                    if samp and kind == "samp":
                        B.cp("dve", v32[0:nr, 1, :], ps[0:nr, b, 0:256], B.bk(b), ["v32"])
                    if samp and kind == "main" and tt_ == NT - 1:
                        B.cp("dve", v32[:, 0, :], ps[:, b, 0:256], B.bk(b), ["v32"])

                if B.stop_after == "S1a":
                    return
                B.apos = qmark
                Sm = [B.af32(1024, [[256, 4], [1, 256]]) for _ in range(2)]
                Pn = [B.abf(1024, [[256, 4], [1, 256]]) for _ in range(2)]
                PTt = [B.abf(1024, [[128, 8], [1, 128]]) for _ in range(2)]
                sm_ = [B.af32(32) for _ in range(2)]
                items = []
                for li, (c0, nr, src_, kind, tt_) in enumerate(tiles):
                    if kind == "main":
                        for kv in range(4):
                            items.append((li, c0, tt_, kv))
                bpv = 6
                sc_bank = {}

                def a_scores(idx):
                    li, c0, tt_, kv = items[idx]
                    b2 = B.bank(2)
                    sc_bank[idx] = b2
                    for hh in range(4):
                        c = 2 * kv + hh // 2
                        half = hh % 2
                        B.mm(ps[:, b2 + half, (hh // 2) * 256:(hh // 2) * 256 + 256],
                             qT[64 * half:64 * half + 64, c, c0:c0 + P],
                             kTl[64 * half:64 * half + 64, kv, c0:c0 + 256], True, True,
                             qTk + ["kTl"], B.bk(b2 + half))

                def a_ctx(idx):
                    li, c0, tt_, kv = items[idx]
                    i = idx % 2
                    Sx, Sk, _ = Sm[i]
                    Px, Pk, _ = Pn[i]
                    PT, PTk, _ = PTt[i]
                    sx, sk_, _ = sm_[i]
                    return li, c0, tt_, kv, Sx, Sk, Px, Pk, PT, PTk, sx, sk_

                def a_A(idx):
                    li, c0, tt_, kv, Sx, Sk, Px, Pk, PT, PTk, sx, sk_ = a_ctx(idx)
                    b2 = sc_bank[idx]
                    mk_off = 256 if tt_ == 0 else 0
                    pin = fap(ps, b2 * 512, [[256, 4], [1, 256]])
                    B.tt("dve", Sx, pin, fap(msk, mk_off, [[0, 4], [1, 256]]), ALU.add, B.bk(b2, 2) + ["msk"], Sk)
                    mx = sx[:, 0:4]
                    m8 = sx[:, 4:8]
                    sm = sx[:, 8:12]
                    tq = sx[:, 12:16]
                    es_ = sx[:, 16:20]
                    rv = sx[:, 20:24]
                    nm = sx[:, 24:28]
                    B.red(mx, Sx, ALU.max, Sk, sk_)
                    B.tt("dve", m8, mx, sink8[:, 4 * kv:4 * kv + 4], ALU.max, sk_ + ["sink8"], sk_)
                    B.ts("dve", nm, m8, -0.125, ALU.mult, sk_, sk_)
                    B.tt("dve", tq, sink8[:, 4 * kv:4 * kv + 4], m8, ALU.subtract, sk_ + ["sink8"], sk_)

                def a_E(idx):
                    li, c0, tt_, kv, Sx, Sk, Px, Pk, PT, PTk, sx, sk_ = a_ctx(idx)
                    for s in range(4):
                        B.act(Sx[:, s, :], Sx[:, s, :], AF.Exp, Sk + sk_, Sk + sk_, scale=0.125, bias=sx[:, 24 + s:25 + s], accum_out=sx[:, 8 + s:9 + s])

                def a_B(idx):
                    li, c0, tt_, kv, Sx, Sk, Px, Pk, PT, PTk, sx, sk_ = a_ctx(idx)
                    sm = sx[:, 8:12]
                    tq = sx[:, 12:16]
                    es_ = sx[:, 16:20]
                    rv = sx[:, 20:24]
                    B.act(es_, tq, AF.Exp, sk_, sk_, scale=0.125)
                    B.tt("dve", rv, sm, es_, ALU.add, sk_, sk_)
                    S.op("dve", lambda e, rv=rv: e.reciprocal(out=rv, in_=rv), sk_, sk_)
                    for s in range(4):
                        B.act(Px[:, s, :], Sx[:, s, :], AF.Copy, Sk + sk_, Pk, scale=sx[:, 20 + s:21 + s])

                def a_C(idx):
                    li, c0, tt_, kv, Sx, Sk, Px, Pk, PT, PTk, sx, sk_ = a_ctx(idx)
                    bt = B.bank()
                    for hh in range(4):
                        for kb in range(2):
                            B.tr(psb[:, bt, (hh * 2 + kb) * P:(hh * 2 + kb + 1) * P], Px[:, (hh % 2) * 2 + hh // 2, kb * P:(kb + 1) * P], ident[:],
                                 Pk + ["ident"], B.bk(bt))
                    B.cp("act", PT, fap(psb, bt * 1024, [[128, 8], [1, 128]]), B.bk(bt), PTk)
                    for hh in range(4):
                        c = 2 * kv + hh // 2
                        half = hh % 2
                        o = ps[64 * half:64 * half + 64, bpv + c // 4, (c % 4) * P:(c % 4 + 1) * P]
                        for kb in range(2):
                            B.mm(o, vl[:, li + kb, kv * 64:(kv + 1) * 64], PT[:, hh * 2 + kb, :], kb == 0, kb == 1,
                                 ["vl"] + PTk, B.bk(bpv + c // 4))
                    if kv == 3:
                        for hh in range(2):
                            B.cp("act" if hh == 0 else "dve", attnT[:, hh * 4:(hh + 1) * 4, c0:c0 + P],
                                 fap(ps, (bpv + hh) * 512, [[128, 4], [1, 128]]), B.bk(bpv + hh), ["attnT"])

                if items:
                    n_it = len(items)
                    a_scores(0)
                    if n_it > 1:
                        a_scores(1)
                    a_A(0)
                    for idx in range(n_it):
                        a_E(idx)
                        if idx + 2 < n_it:
                            a_scores(idx + 2)
                        if idx + 1 < n_it:
                            a_A(idx + 1)
                        a_B(idx)
                        a_C(idx)

                if B.stop_after == "S1b":
                    return
                if samp:
                    B.apos = qmark
                    sample_attention(qT, qTk, soff)

                if B.stop_after == "S1":
                    return

                if g == 0:
                    zero_fill_xg()
                B.areset()
                NU = 2 + NCMAX
                hs = [B.af32(512) for _ in range(2)]
                bs = [B.af32(512) for _ in range(2)]
                ub = [B.af32(NU) for _ in range(2)]
                tb = [B.af32(512) for _ in range(2)]
                if samp:
                    stT, stTk, _ = B.af32(8 * 32, [[32, 8], [1, 32]])
                    st_in, st_ink, _ = B.af32(1024)
                    us_all, us_allk, _ = B.af32(8 * NS, [[NS, 8], [1, NS]])
                    ul_all, ul_allk, _ = B.af32(8 * 2, [[2, 8], [1, 2]])
                    B.dma("sp", "st", fap(st_in, 0, [[1, 1024]], parts=32), scv.rearrange("b j c -> (b j) c"), [], st_ink)
                    bq = B.bank()
                    for cc in range(8):
                        B.tr(ps[:, bq, cc * 32:(cc + 1) * 32], fap(st_in, cc * P, [[1, P]], parts=32), ident_f[0:32, 0:32],
                             st_ink + ["ident_f"], B.bk(bq))
                    B.cp("dve", stT, fap(ps, bq * 512, [[32, 8], [1, 32]]), B.bk(bq), stTk)
                for cc in range(8):
                    if cc % 2 == 0:
                        wb_, wbk_ = B.wget(win_spec(OB + (cc // 2) * 256))
                        wc_, wck_ = B.wget(win_spec(OC + (cc // 2) * 256))
                        wh_, whk_ = B.wget(win_spec(OH + (cc // 2) * 256))
                    cj = (cc % 2) * P
                    i = cc % 2
                    u, uk, _ = ub[i]
                    if not halo:
                        B.cp("act", u[:, 0:2], uprev[:, cc, :], ["uprev"], uk)
                    for (c0, n) in all_segs:
                        hx, hk, _ = hs[i]
                        bx, bxk, _ = bs[i]
                        tx, txk, _ = tb[i]
                        is_own = (c0, n) in own_segs
                        bh = B.bank()
                        for k in range(16):
                            B.mm(ps[:, bh, 0:n], wh_[:, k, cj:cj + P], bigT[:, k, c0:c0 + n], k == 0, k == 15, whk_ + ["bigT"], B.bk(bh))
                        B.cp("act", hx[:, 0:n], ps[:, bh, 0:n], B.bk(bh), hk)
                        bc = B.bank()
                        for k in range(16):
                            B.mm(ps[:, bc, 0:n], wc_[:, k, cj:cj + P], bigT[:, k, c0:c0 + n], k == 0, k == 15, wck_ + ["bigT"], B.bk(bc))
                        B.tt("dve", u[:, 2 + c0:2 + c0 + n], ps[:, bc, 0:n], hx[:, 0:n], ALU.mult, B.bk(bc) + hk, uk)
                        if not is_own:
                            continue
                        bb = B.bank()
                        for k in range(16):
                            B.mm(ps[:, bb, 0:n], wb_[:, k, cj:cj + P], bigT[:, k, c0:c0 + n], k == 0, k == 15, wbk_ + ["bigT"], B.bk(bb))
                        B.cp("act", bx[:, 0:n], ps[:, bb, 0:n], B.bk(bb), bxk)
                        if c0 == soff and samp:
                            s0 = fap(stT, cc * 32, [[2, NS]])
                            s1 = fap(stT, cc * 32 + 1, [[2, NS]])
                            B.ts("dve", tx[:, 0:n], u[:, 2 + c0:2 + c0 + n], convw[:, cc, 2:3], ALU.mult, uk + ["convw0", "convw1", "convw2"], txk)
                            B.stt(tx[:, 0:n], s1, convw[:, cc, 1:2], tx[:, 0:n], ALU.mult, ALU.add, stTk + ["convw0", "convw1", "convw2"] + txk, txk)
                            B.stt(tx[:, 0:n], s0, convw[:, cc, 0:1], tx[:, 0:n], ALU.mult, ALU.add, stTk + ["convw0", "convw1", "convw2"] + txk, txk)
                            B.cp("act", us_all[:, cc, :], u[:, 2 + c0:2 + c0 + n], uk, us_allk)
                        else:
                            B.ts("dve", tx[:, 0:n], u[:, 2 + c0:2 + c0 + n], convw[:, cc, 2:3], ALU.mult, uk + ["convw0", "convw1", "convw2"], txk)
                            B.stt(tx[:, 0:n], u[:, 1 + c0:1 + c0 + n], convw[:, cc, 1:2], tx[:, 0:n], ALU.mult, ALU.add, uk + ["convw0", "convw1", "convw2"] + txk, txk)
                            B.stt(tx[:, 0:n], u[:, c0:c0 + n], convw[:, cc, 0:1], tx[:, 0:n], ALU.mult, ALU.add, uk + ["convw0", "convw1", "convw2"] + txk, txk)
                            B.cp("act", uprev[:, cc, :], u[:, 2 + c0 + n - 2:2 + c0 + n], uk, ["uprev"])
                            if samp:
                                B.cp("act", ul_all[:, cc, :], u[:, 2 + c0 + n - 2:2 + c0 + n], uk, ul_allk)
                        B.tt("dve", convT[:, cc, c0:c0 + n], tx[:, 0:n], bx[:, 0:n], ALU.mult, txk + bxk, ["convT"])
                if samp:
                    def fm_to_tm(src3, srck, n):
                        b2 = B.bank(2)
                        for cc in range(8):
                            B.tr(ps[0:n, b2 + cc // 4, (cc % 4) * P:(cc % 4 + 1) * P], src3[:, cc, :], ident_f[:], srck + ["ident_f"], B.bk(b2 + cc // 4))
                        o, ok_, _ = B.af32(1024)
                        B.cp("dve", fap(o, 0, [[1, 1024]], parts=n), fap(ps, b2 * 512, [[1, 1024]], parts=n), B.bk(b2, 2), ok_)
                        return o, ok_
                    uo, uok = fm_to_tm(us_all, us_allk, NS)
                    B.dma("sp", "o_ssc", ssc[:, 1, :], fap(uo, 0, [[1, 1024]], parts=NS), uok, ["ssc"])
                    B.dma("sp", "o_ssc0", ssc[:, 0, :], scv[:, 1, :], [], ["ssc0"])
                    ul, ulk = fm_to_tm(ul_all, ul_allk, 2)
                    B.dma("sp", "o_psc", psc[:, :], fap(ul, 0, [[1, 1024]], parts=2), ulk, ["psc"])
                    kc, kck, _ = B.af32(256)
                    bq5 = B.bank()
                    for kp in range(2):
                        B.tr(ps[:, bq5, kp * P:(kp + 1) * P], kT32[:, kp, 0:P], ident_f[:], ["kT32", "ident_f"], B.bk(bq5))
                    B.cp("dve", kc, ps[:, bq5, 0:256], B.bk(bq5), kck)
                    B.dma("sp", "o_pck", pck[:, :], kc, kck, ["pck"])
                    B.dma("sp", "o_pcv", pcv[:, :], v32[:, 0, :], ["v32"], ["pcv"])

                if debug and g == 0:
                    B.dma("pool", "dbgp0_" + str(DBGC.next()), dbg["attn"][:, :, :], attnT[:], ["attnT"], [])
                    B.dma("pool", "dbgp1_" + str(DBGC.next()), dbg["conv"][:, :, :], convT[:], ["convT"], [])
                if B.stop_after == "S2":
                    return

                B.areset()
                mixT, mixk, _ = B.abf(16 * NCMAX, [[NCMAX, 16], [1, NCMAX]])
                sg = [B.af32(512) for _ in range(2)]
                ta = [B.af32(512) for _ in range(2)]
                for jb in range(8):
                    wga, wgak = B.wget(win_spec(OGA + jb * 256))
                    wgc, wgck = B.wget(win_spec(OGC + jb * 256))
                    wao, waok = B.wget((8, 512, [(0, 256, w_ao[:, jb * 256:(jb + 1) * 256]), (256, 256, w_co[:, jb * 256:(jb + 1) * 256])]))
                    for jj in range(2):
                        j = jb * 2 + jj
                        for (c0, n) in own_segs:
                            i = j % 2
                            sgx, sgk, _ = sg[i]
                            tax, tak, _ = ta[i]
                            b = B.bank()
                            for k in range(16):
                                B.mm(ps[:, b, 0:n], wga[:, k, jj * P:(jj + 1) * P], bigT[:, k, c0:c0 + n], k == 0, k == 15, wgak + ["bigT"], B.bk(b))
                            B.act(sgx[:, 0:n], ps[:, b, 0:n], AF.Sigmoid, B.bk(b), sgk)
                            b = B.bank()
                            for k in range(8):
                                B.mm(ps[:, b, 0:n], wao[:, k, jj * P:(jj + 1) * P], attnT[:, k, c0:c0 + n], k == 0, k == 7, waok + ["attnT"], B.bk(b))
                            B.tt("dve", tax[:, 0:n], ps[:, b, 0:n], sgx[:, 0:n], ALU.mult, B.bk(b) + sgk, tak)
                            b = B.bank()
                            for k in range(16):
                                B.mm(ps[:, b, 0:n], wgc[:, k, jj * P:(jj + 1) * P], bigT[:, k, c0:c0 + n], k == 0, k == 15, wgck + ["bigT"], B.bk(b))
                            B.act(sgx[:, 0:n], ps[:, b, 0:n], AF.Sigmoid, B.bk(b), sgk)
                            b = B.bank()
                            for k in range(8):
                                B.mm(ps[:, b, 0:n], wao[:, k, 256 + jj * P:256 + (jj + 1) * P], convT[:, k, c0:c0 + n], k == 0, k == 7, waok + ["convT"], B.bk(b))
                            B.tt("dve", sgx[:, 0:n], ps[:, b, 0:n], sgx[:, 0:n], ALU.mult, B.bk(b) + sgk, sgk)
                            B.tt("dve", mixT[:, j, c0:c0 + n], tax[:, 0:n], sgx[:, 0:n], ALU.add, tak + sgk, mixk)

                for li, (c0, nr, src, kind, tt_) in enumerate(own_tiles):
                    B.dma("sp", "xres%d" % li, hres[0:nr, li, :], src, [], [("hres", li)])
                for n4 in range(4):
                    wlo, wlok = B.wget((8, 512, [(0, 512, w_o[0:1024, n4 * 512:(n4 + 1) * 512])]))
                    whi, whik = B.wget((8, 512, [(0, 512, w_o[1024:2048, n4 * 512:(n4 + 1) * 512])]))
                    for li, (c0, nr, src, kind, tt_) in enumerate(own_tiles):
                        b = B.bank()
                        for k in range(16):
                            w_, wk_ = (wlo, wlok) if k < 8 else (whi, whik)
                            B.mm(ps[0:nr, b, :], mixT[:, k, c0:c0 + nr], w_[:, k % 8, :], k == 0, k == 15, mixk + wk_, B.bk(b))
                        hr = hres[0:nr, li, n4 * 512:(n4 + 1) * 512]
                        B.stt(hr, hr, ALPHA, ps[0:nr, b, :], ALU.mult, ALU.add, [("hres", li)] + B.bk(b), [("hres", li)])
                if B.stop_after == "S3":
                    return

                B.areset()
                lnt = [B.af32(32) for _ in range(2)]
                hbf = [B.abf(D) for _ in range(2)]
                rts = [B.af32(64 * 8) for _ in range(len(own_tiles))]
                actT, actk, _ = B.abf(4 * NCMAX, [[NCMAX, 4], [1, NCMAX]])
                sgs = [B.af32(512) for _ in range(2)]
                wrt, wrk = B.wget((16, 64, [(0, 64, w_r[:, :])]))
                for li, (c0, nr, src, kind, tt_) in enumerate(own_tiles):
                    layer_norm(hres[0:nr, li, :], [("hres", li)], nr, lnt[li % 2])
                    if debug:
                        r0 = TOK if kind == "samp" else tt_ * P
                        B.dma("sp", "dbg0_" + str(DBGC.next()), dbg["h"][r0:r0 + nr, :], hres[0:nr, li, :], [("hres", li)], [])
                    hb, hbk, _ = hbf[li % 2]
                    B.cp("act", fap(hb, 0, [[1, D]], parts=nr), hres[0:nr, li, :], [("hres", li)], hbk)
                    b2 = B.bank(2)
                    for k in range(16):
                        o = fap(psb, (b2 + k // 8) * 1024 + (k % 8) * nr, [[1, nr]])
                        B.tr(o, fap(hb, k * P, [[1, P]], parts=nr), ident[0:nr, 0:nr], hbk + ["ident"], B.bk(b2 + k // 8))
                    for hh in range(2):
                        src_ap = fap(psb, (b2 + hh) * 1024, [[nr, 8], [1, nr]])
                        B.cp("dve" if hh == 0 else "act", bigT[:, hh * 8:(hh + 1) * 8, c0:c0 + nr], src_ap, B.bk(b2 + hh), ["bigT"])
                    tix = NT if kind == "samp" else tt_
                    routing(1, li, c0, nr, tix, wrt, wrk, rts[li], None, None)
                r2_done = 0
                for cb in range(2):
                    wg_, wgk_ = B.wget((16, 256, [(0, 256, w_sg[:, cb * 256:(cb + 1) * 256])]))
                    wu_, wuk_ = B.wget((16, 256, [(0, 256, w_su[:, cb * 256:(cb + 1) * 256])]))
                    for jj in range(2):
                        c = cb * 2 + jj
                        for _ in range(2):
                            if r2_done < len(own_tiles) and (r2_done <= c + 1 or c == 3):
                                li = r2_done
                                (c0_, nr_, src_, kind_, tt2) = own_tiles[li]
                                routing(2, li, c0_, nr_, NT if kind_ == "samp" else tt2, wrt, wrk, rts[li], None, None)
                                r2_done += 1
                        for (c0, n) in own_segs:
                            sx_, sxk, _ = sgs[c % 2]
                            b = B.bank()
                            for k in range(16):
                                B.mm(ps[:, b, 0:n], wg_[:, k, jj * P:(jj + 1) * P], bigT[:, k, c0:c0 + n], k == 0, k == 15, wgk_ + ["bigT"], B.bk(b))
                            B.act(sx_[:, 0:n], ps[:, b, 0:n], AF.Silu, B.bk(b), sxk)
                            b = B.bank()
                            for k in range(16):
                                B.mm(ps[:, b, 0:n], wu_[:, k, jj * P:(jj + 1) * P], bigT[:, k, c0:c0 + n], k == 0, k == 15, wuk_ + ["bigT"], B.bk(b))
                            B.tt("dve", actT[:, c, c0:c0 + n], ps[:, b, 0:n], sx_[:, 0:n], ALU.mult, B.bk(b) + sxk, actk)
                while r2_done < len(own_tiles):
                    li = r2_done
                    (c0_, nr_, src_, kind_, tt2) = own_tiles[li]
                    routing(2, li, c0_, nr_, NT if kind_ == "samp" else tt2, wrt, wrk, rts[li], None, None)
                    r2_done += 1
                for li, (c0, nr, src, kind, tt_) in enumerate(own_tiles):
                    hb, hbk, _ = hbf[li % 2]
                    if kind == "samp":
                        S.op("dve", lambda e, hb=hb: e.memset(hb, 0.0), [], hbk)
                    B.cp("act", fap(hb, 0, [[1, D]], parts=nr), hres[0:nr, li, :], [("hres", li)], hbk)
                    tix = NT if kind == "samp" else tt_
                    routing(3, li, c0, nr, tix, wrt, wrk, rts[li], hb, hbk)
                for nb in range(2):
                    wd_, wdk_ = B.wget((4, 1024, [(0, 1024, w_sd[:, nb * 1024:(nb + 1) * 1024])]))
                    for n2 in range(2):
                        n4 = nb * 2 + n2
                        for li, (c0, nr, src, kind, tt_) in enumerate(own_tiles):
                            b = B.bank()
                            for k in range(4):
                                B.mm(ps[0:nr, b, :], actT[:, k, c0:c0 + nr], wd_[:, k, n2 * 512:(n2 + 1) * 512], k == 0, k == 3, actk + wdk_, B.bk(b))
                            hr = hres[0:nr, li, n4 * 512:(n4 + 1) * 512]
                            B.stt(hr, hr, ALPHA, ps[0:nr, b, :], ALU.mult, ALU.add, [("hres", li)] + B.bk(b), [("hres", li)])
                for li, (c0, nr, src, kind, tt_) in enumerate(own_tiles):
                    r0 = TOK if kind == "samp" else tt_ * P
                    B.dma("sp", "base%d" % li, BASE[r0:r0 + nr, :], hres[0:nr, li, :], [("hres", li)], ["BASE"])

            def layer_norm(xap, xkeys, nr, tmp, out=None, out_keys=None):
                st, stk, _ = tmp
                for q in range(4):
                    S.op("dve", lambda e, q=q: e.bn_stats(out=fap(st, q * 6, [[1, 6]], parts=nr), in_=fap(xap, q * 512, [[1, 512]], parts=nr)), xkeys, stk)
                mv = fap(st, 24, [[1, 2]], parts=nr)
                S.op("dve", lambda e: e.bn_aggr(out=mv, in_=fap(st, 0, [[1, 24]], parts=nr)), stk, stk)
                rs = fap(st, 26, [[1, 1]], parts=nr)
                B.act(rs, fap(st, 25, [[1, 1]], parts=nr), AF.Sqrt, stk + ["epsT"], stk, bias=epsT[0:nr, :])
                S.op("dve", lambda e: e.reciprocal(out=rs, in_=rs), stk, stk)
                o = xap if out is None else out
                ok_ = xkeys if out is None else out_keys
                B.ts("dve", o, xap, fap(st, 24, [[1, 1]], parts=nr), ALU.subtract, xkeys + stk, ok_, s2=rs, op1=ALU.mult)
                B.tt("dve", o, o, lng[0:nr, :], ALU.mult, ok_ + ["lng"], ok_)
                B.tt("dve", o, o, lnb[0:nr, :], ALU.add, ok_ + ["lnb"], ok_)

            def routing(phase, li, c0, nr, tix, wrt, wrk, rt, hb, hbk):
                r, rk, _ = rt
                def rv(i, n=NE, parts=nr):
                    return fap(r, i * 64, [[1, n]], parts=parts)
                sc_ = rv(0)
                ch = rv(1)
                tmp = rv(2)
                sel = rv(3, parts=P)
                wd = rv(4)
                key = rv(5, parts=P)
                sm = fap(r, 6 * 64, [[1, 64]], parts=nr)
                m1 = fap(r, 6 * 64, [[1, 8]], parts=nr)
                m2 = fap(r, 6 * 64 + 8, [[1, 8]], parts=nr)
                gs = fap(r, 6 * 64 + 16, [[1, 8]], parts=nr)
                g8 = fap(r, 6 * 64 + 24, [[1, 8]], parts=nr)
                pen = fap(r, 6 * 64 + 32, [[1, 8]], parts=nr)
                c8 = fap(r, 6 * 64 + 40, [[1, 8]], parts=nr)
                ssum = fap(r, 6 * 64 + 48, [[1, 1]], parts=nr)
                ch3 = fap(r, 1 * 64, [[8, 8], [1, 8]], parts=nr)
                tmp3 = fap(r, 2 * 64, [[8, 8], [1, 8]], parts=nr)
                selb, selbk = rsel_t[:, li, :], [("rsel", li)]
                pos = rv(7, parts=P)
                d8f = fap(r, 6 * 64 + 56, [[1, 8]], parts=P)
                BIGC = float(2 * NSLOT)
                if phase == 1:
                    b = B.bank()
                    for k in range(16):
                        B.mm(ps[0:nr, b, 0:NE], bigT[:, k, c0:c0 + nr], wrt[:, k, :], k == 0, k == 15, ["bigT"] + wrk, B.bk(b))

                    B.act(sc_, ps[0:nr, b, 0:NE], AF.Sigmoid, B.bk(b), rk)
                    return
                if phase == 2:
                    B.tt("dve", ch, sc_, rbias[0:nr, :], ALU.add, rk + ["rbias"], rk)
                    B.red(m1, ch3, ALU.max, rk, rk)
                    B.tt("dve", tmp3, ch3, fap(r, 6 * 64, [[1, 8], [0, 8]], parts=nr), ALU.is_equal, rk, rk)
                    B.stt(tmp, tmp, -1e9, ch, ALU.mult, ALU.add, rk, rk)
                    B.red(m2, tmp3, ALU.max, rk, rk)
                    B.tt("dve", gs, m1, m2, ALU.add, rk, rk)
                    S.op("dve", lambda e: e.max(out=g8, in_=gs), rk, rk)
                    B.ts("dve", pen, gs, fap(r, 6 * 64 + 24 + 3, [[1, 1]], parts=nr), ALU.is_ge, rk, rk, s2=-1.0, op1=ALU.add)
                    B.ts("dve", pen, pen, 1e9, ALU.mult, rk, rk)
                    B.tt("dve", tmp3, ch3, fap(r, 6 * 64 + 32, [[1, 8], [0, 8]], parts=nr), ALU.add, rk, rk)
                    S.op("dve", lambda e: e.max(out=c8, in_=tmp), rk, rk)
                    if nr < P:
                        S.op("dve", lambda e: e.memset(sel, 0.0), rk, rk)
                    B.ts("dve", rv(3), tmp, fap(r, 6 * 64 + 40 + 7, [[1, 1]], parts=nr), ALU.is_ge, rk, rk)
                    B.stt(wd, rv(3), 1.0, sc_, ALU.mult, ALU.mult, rk, rk, accum_out=ssum)
                    S.op("dve", lambda e: e.reciprocal(out=ssum, in_=ssum), rk, rk)
                    B.ts("dve", wd, wd, ssum, ALU.mult, rk, rk, s2=2.5, op1=ALU.mult)
                    B.cp("dve", selb, sel, rk, selbk)
                    return
                bp = B.bank()
                B.mm(ps[:, bp, 0:NE], triU[:], selb, True, True, ["triU"] + selbk, B.bk(bp))
                B.tt("dve", pos, ps[:, bp, 0:NE], cbase[:], ALU.add, B.bk(bp) + ["cbase"], rk)
                bp2 = B.bank()
                B.mm(ps[:, bp2, 0:NE], ones[:], selb, True, True, ["ones"] + selbk, B.bk(bp2))
                B.tt("dve", cbase[:], cbase[:], ps[:, bp2, 0:NE], ALU.add, B.bk(bp2) + ["cbase"], ["cbase"])
                B.ts("dve", key, pos, float(CAP), ALU.is_lt, rk, rk)
                B.tt("dve", key, key, sel, ALU.mult, rk, rk)
                B.tt("dve", pos, pos, c16t[:, 64:128], ALU.add, rk + ["c16t"], rk)
                B.ts("dve", pos, pos, -1.0, ALU.mult, rk, rk, s2=BIGC, op1=ALU.add)
                B.tt("dve", key, key, pos, ALU.mult, rk, rk)
                S.op("dve", lambda e: e.max(out=d8f, in_=key), rk, rk)
                for j in range(8):
                    B.stt(rv(2, parts=nr), fap(r, 5 * 64, [[1, NE]], parts=nr), fap(r, 6 * 64 + 56 + j, [[1, 1]], parts=nr), wd,
                          ALU.is_equal, ALU.mult, rk, rk + [("w8", tix)], accum_out=w8all[0:nr, tix, j:j + 1])
                B.ts("dve", fap(r, 2 * 64, [[1, 8]], parts=nr), fap(r, 6 * 64 + 56, [[1, 8]], parts=nr), 0.0, ALU.is_gt, rk, rk)
                B.tt("dve", w8all[0:nr, tix, :], w8all[0:nr, tix, :], fap(r, 2 * 64, [[1, 8]], parts=nr), ALU.mult, rk + [("w8", tix)], [("w8", tix)])
                B.ts("dve", d8f, d8f, -1.0, ALU.mult, rk, rk, s2=BIGC, op1=ALU.add)
                B.cp("dve", d8all[:, tix, :], d8f, rk, [("d8", tix)])
                for j in range(8):
                    S.dma("pool", "scat%d" % (li % 2), lambda e, j=j: e.indirect_dma_start(
                        out=XG[:, :], out_offset=bass.IndirectOffsetOnAxis(ap=d8all[:, tix, j:j + 1], axis=0),
                        in_=hb, in_offset=None, bounds_check=B.bcreg(e), oob_is_err=False),
                        hbk + [("d8", tix), "XG"], [("XGs", tix, j)])


            def sample_attention(qT, qTk, soff):
                kn, knk, _ = B.af32(256)
                bq = B.bank()
                for kp in range(2):
                    B.tr(ps[0:NS, bq, kp * P:(kp + 1) * P], kT32[:, kp, P:P + NS], ident_f[:], ["kT32", "ident_f"], B.bk(bq))
                B.cp("dve", fap(kn, 0, [[1, 256]], parts=NS), ps[0:NS, bq, 0:256], B.bk(bq), knk)
                B.dma("sp", "sck", sck[:, 0:P - 1, :], ck[:, 1:P, :], [], ["sck"])
                B.dma("sp", "sck", sck[:, P - 1, :], fap(kn, 0, [[1, 256]], parts=NS), knk, ["sck"])
                B.dma("sp", "scv", scvo[:, 0:P - 1, :], cv[:, 1:P, :], [], ["scvo"])
                B.dma("sp", "scv", scvo[:, P - 1, :], v32[0:NS, 1, :], ["v32"], ["scvo"])
                Ke, Kek = fap(hres_bf, 0 * 4096, [[256, NS], [1, 256]]), [("hres", 0)]
                Ve, Vek = fap(hres_bf, 1 * 4096, [[256, NS], [1, 256]]), [("hres", 1)]
                stg = fap(hres, 2 * D, [[256, NS], [1, 256]])
                stgk = [("hres", 2), ("hres", 3)]
                B.dma("sp", "ske", stg, sck.rearrange("b k d -> k b d"), ["sck"], stgk)
                B.cp("act", Ke, stg, stgk, Kek)
                B.dma("sp", "ske", stg, scvo.rearrange("b k d -> k b d"), ["scvo"], stgk)
                B.cp("dve", Ve, stg, stgk, Vek)
                QM, QMk, _ = B.abf(64, [[16, 4], [1, 16]])
                KT2, KT2k, _ = B.abf(4 * P, [[P, 4], [1, P]])
                Ssb, Ssk = fap(hres, 2 * D, [[P, NS], [1, P]]), [("hres", 2)]
                for b_ in range(NS):
                    qin = fap(qT, soff + b_, [[0, 4], [640, 8], [0, 2]])
                    B.tt("dve", fap(QM, 0, [[16, 4], [2, 8], [1, 2]]), qin, fap(c16t, 0, [[16, 4], [2, 8], [1, 2]]), ALU.mult, qTk + ["c16t"], QMk)
                    bt = B.bank()
                    for kv in range(4):
                        for dup in range(2):
                            B.tr(psb[64 * dup:64 * dup + 64, bt, kv * P:(kv + 1) * P], Ke[:, b_, kv * 64:(kv + 1) * 64], ident[:], Kek + ["ident"], B.bk(bt))
                    B.cp("act", KT2, fap(psb, bt * 1024, [[P, 4], [1, P]]), B.bk(bt), KT2k)
                    bsc = B.bank()
                    for kv in range(4):
                        B.mm(ps[0:16, bsc, 0:P], QM[:, kv, :], KT2[:, kv, :], kv == 0, kv == 3, QMk + KT2k, B.bk(bsc))
                    B.cp("dve", fap(Ssb, b_ * P, [[1, P]], parts=16), ps[0:16, bsc, 0:P], B.bk(bsc), Ssk)
                sx, sxk, _ = B.af32(5 * NS)
                S16 = fap(Ssb, 0, [[P, NS], [1, P]], parts=16)
                mx = fap(sx, 0, [[1, NS]], parts=16)
                sm = fap(sx, NS, [[1, NS]], parts=16)
                tq = fap(sx, 2 * NS, [[1, NS]], parts=16)
                rv_ = fap(sx, 3 * NS, [[1, NS]], parts=16)
                B.red(mx, S16, ALU.max, Ssk, sxk)
                B.ts("dve", mx, mx, sinkcol[0:16, 0:1], ALU.max, sxk + ["sinkcol"], sxk)
                B.tt("dve", S16, S16, fap(sx, 0, [[1, NS], [0, P]], parts=16), ALU.subtract, Ssk + sxk, Ssk)
                B.act(S16, S16, AF.Exp, Ssk, Ssk, scale=0.125)
                B.red(sm, S16, ALU.add, Ssk, sxk)
                B.ts("dve", tq, mx, -1.0, ALU.mult, sxk, sxk, s2=sinkcol[0:16, 0:1], op1=ALU.add)
                B.act(tq, tq, AF.Exp, sxk, sxk, scale=0.125)
                B.tt("dve", rv_, sm, tq, ALU.add, sxk, sxk)
                S.op("dve", lambda e: e.reciprocal(out=rv_, in_=rv_), sxk, sxk)
                Pb, Pbk = fap(hres_bf, 4 * 4096, [[P, NS], [1, P]]), [("hres", 4)]
                B.tt("dve", fap(Pb, 0, [[P, NS], [1, P]], parts=16), S16, fap(sx, 3 * NS, [[1, NS], [0, P]], parts=16), ALU.mult, Ssk + sxk, Pbk)
                bt = B.bank()
                for b_ in range(NS):
                    B.tr(psb[:, bt, b_ * 16:(b_ + 1) * 16], fap(Pb, b_ * P, [[1, P]], parts=16), ident[0:16, 0:16], Pbk + ["ident"], B.bk(bt))
                PTs, PTsk, _ = B.abf(NS * 16)
                B.cp("act", PTs, psb[:, bt, 0:NS * 16], B.bk(bt), PTsk)
                As, Ask = fap(hres, 4 * D + 1024, [[64, NS], [1, 64]]), [("hres", 4)]
                tmpv, tmpvk, _ = B.af32(256)
                for b_ in range(NS):
                    bo = B.bank()
                    B.mm(ps[0:16, bo, 0:256], PTs[:, b_ * 16:(b_ + 1) * 16], Ve[:, b_, :], True, True, PTsk + Vek, B.bk(bo))
                    B.tt("dve", fap(tmpv, 0, [[64, 4], [1, 64]], parts=16), fap(ps, bo * 512, [[64, 4], [1, 64]], parts=16),
                         fap(c16t, 192, [[1, 4], [0, 64]], parts=16), ALU.mult, B.bk(bo) + ["c16t"], tmpvk)
                    B.red(fap(As, b_ * 64, [[1, 64]], parts=16), fap(tmpv, 0, [[1, 64], [64, 4]], parts=16), ALU.add, tmpvk, Ask)
                A2, A2k = fap(hres, 3 * D, [[P, NS], [1, P]]), [("hres", 3)]
                B.tt("dve", fap(A2, 0, [[P, NS], [64, 2], [1, 64]], parts=16), fap(As, 0, [[64, NS], [0, 2], [1, 64]], parts=16),
                     fap(c16t, 200, [[0, NS], [1, 2], [0, 64]], parts=16), ALU.mult, Ask + ["c16t"], A2k)
                bt2 = B.bank()
                for b_ in range(NS):
                    B.tr(ps[:, bt2, b_ * 16:(b_ + 1) * 16], fap(A2, b_ * P, [[1, P]], parts=16), ident_f[0:16, 0:16], A2k + ["ident_f"], B.bk(bt2))
                af_, afk, _ = B.af32(8 * NS, [[NS, 8], [1, NS]])
                B.red(af_, fap(ps, bt2 * 512, [[2, 8], [16, NS], [1, 2]]), ALU.add, B.bk(bt2), afk)
                B.cp("dve", fap(attnT, soff, [[640, 8], [1, NS]]), af_, afk, ["attnT"])


            S.planning = True
            phase_a()
            S.planning = False
            B.bank_rr = 0
            phase_a()
        S.barrier()

        if stop_after is None:
            pb = contextlib.ExitStack()
            with pb:
                def sbb(name, shape, dt):
                    return pb.enter_context(nc.sbuf_tensor(name, list(shape), dt))
                wg = [sbb("wg%d" % i, [P, 16, 512], BF16) for i in range(2)]
                wu = [sbb("wu%d" % i, [P, 16, 512], BF16) for i in range(2)]
                wdn = [sbb("wd%d" % i, [P, 4, D], BF16) for i in range(2)]
                xb_ = [sbb("xb%d" % i, [P, NBLK, D], BF16) for i in range(2)]
                xbT = [sbb("xbT%d" % i, [P, 16, CAP], BF16) for i in range(2)]
                acT = [sbb("acT%d" % i, [P, 4, CAP], BF16) for i in range(2)]
                sgt = [sbb("sgt%d" % i, [P, CAP], F32) for i in range(2)]
                yo = [sbb("yo%d" % i, [P, D], BF16) for i in range(2)]

                def load_expert(e):
                    i = e % 2
                    for k0 in range(0, 16, 8):
                        B.dma("pool", "eg%d" % i, wg[i][:, k0:k0 + 8, :], w_eg[e, k0 * P:(k0 + 8) * P, :].rearrange("(k p) n -> p k n", p=P), [], [("wg", i)])
                        B.dma("pool", "eu%d" % i, wu[i][:, k0:k0 + 8, :], w_eu[e, k0 * P:(k0 + 8) * P, :].rearrange("(k p) n -> p k n", p=P), [], [("wu", i)])
                    for k in range(0, 4, 2):
                        B.dma("pool", "ed%d" % i, wdn[i][:, k:k + 2, :], w_ed[e, k * P:(k + 2) * P, :].rearrange("(k p) n -> p k n", p=P), [], [("wdn", i)])
                    B.dma("sp", "xb%d" % i, xb_[i][:], XG[e * CAP:(e + 1) * CAP, :].rearrange("(b p) d -> p b d", p=P), ["XG"] + [("XGs", t, j) for t in range(17) for j in range(8)], [("xb", i)])

                load_expert(0)
                yi = 0
                for e in range(NE):
                    i = e % 2
                    if e + 1 < NE:
                        load_expert(e + 1)
                    for blk in range(NBLK):
                        for h2 in range(2):
                            bt = B.bank()
                            for k8 in range(8):
                                k = h2 * 8 + k8
                                B.tr(psb[:, bt, k8 * P:(k8 + 1) * P], xb_[i][:, blk, k * P:(k + 1) * P], ident[:], [("xb", i), "ident"], B.bk(bt))
                            B.cp("act" if (blk * 2 + h2) % 2 == 0 else "dve", xbT[i][:, h2 * 8:(h2 + 1) * 8, blk * P:(blk + 1) * P],
                                 fap(psb, bt * 1024, [[P, 8], [1, P]]), B.bk(bt), [("xbT", i)])
                    for c in range(4):
                        bg = B.bank()
                        for k in range(16):
                            B.mm(ps[:, bg, 0:CAP], wg[i][:, k, c * P:(c + 1) * P], xbT[i][:, k, :], k == 0, k == 15, [("wg", i), ("xbT", i)], B.bk(bg))
                        B.act(sgt[c % 2][:], ps[:, bg, 0:CAP], AF.Silu, B.bk(bg), [("sgt", c % 2)])
                        bu = B.bank()
                        for k in range(16):
                            B.mm(ps[:, bu, 0:CAP], wu[i][:, k, c * P:(c + 1) * P], xbT[i][:, k, :], k == 0, k == 15, [("wu", i), ("xbT", i)], B.bk(bu))
                        B.tt("dve", acT[i][:, c, :], ps[:, bu, 0:CAP], sgt[c % 2][:], ALU.mult, B.bk(bu) + [("sgt", c % 2)], [("acT", i)])
                    for blk in range(NBLK):
                        y_ = yo[yi % 2]
                        yk = [("yo", yi % 2)]
                        for n4 in range(4):
                            b = B.bank()
                            for k in range(4):
                                B.mm(ps[:, b, :], acT[i][:, k, blk * P:(blk + 1) * P], wdn[i][:, k, n4 * 512:(n4 + 1) * 512], k == 0, k == 3, [("acT", i), ("wdn", i)], B.bk(b))
                            B.cp("act" if n4 % 2 == 0 else "dve", y_[:, n4 * 512:(n4 + 1) * 512], ps[:, b, :], B.bk(b), yk)
                        r0 = e * CAP + blk * P
                        B.dma("sp", "yst%d" % (yi % 2), Y[r0:r0 + P, :], y_[:], yk, ["Y"])
                        yi += 1
            S.barrier()

            pc = contextlib.ExitStack()
            with pc:
                def sbc(name, shape, dt):
                    return pc.enter_context(nc.sbuf_tensor(name, list(shape), dt))
                acc = [sbc("acc%d" % i, [P, D], F32) for i in range(2)]
                yg = [sbc("yg%d" % i, [P, D], BF16) for i in range(6)]
                lnt2 = [sbc("lnt2%d" % i, [P, 32], F32) for i in range(2)]
                B.dma("sp", "cst2", lng[:], fap_dram_bcast(ln2_g, D), ["lng"], ["lng"])
                B.dma("sp", "cst2", lnb[:], fap_dram_bcast(ln2_b, D), ["lnb"], ["lnb"])
                dgt = [sbc("dg%d" % i, [P, 16, P], BF16) for i in range(2)]
                wsm = [sbc("wsm%d" % i, [P, 32], F32) for i in range(2)]
                wbf = [sbc("wbf%d" % i, [P, 8], BF16) for i in range(2)]
                gi = [0]

                def f_ctx(tix):
                    nr = P if tix < NT else NS
                    r0 = tix * P if tix < NT else TOK
                    par = tix % 2
                    bks = [4 * par + n4 for n4 in range(4)]
                    return nr, r0, acc[par], [("acc", par)], dgt[par], [("dg", par)], wsm[par], [("wsm", par)], wbf[par], bks

                def f_prep(tix):
                    nr, r0, a, ak, dg, dgk, ws, wsk, wb16, bks = f_ctx(tix)
                    B.dma("sp", "bl%d" % (tix % 2), a[0:nr, :], BASE[r0:r0 + nr, :], ["BASE"], ak)
                    B.cp("dve", wb16[0:nr, :], w8all[0:nr, tix, :], [("w8", tix)], wsk)
                    B.cp("dve", ws[0:nr, 0:8], wb16[0:nr, :], wsk, wsk)
                    B.tt("dve", ws[0:nr, 8:16], w8all[0:nr, tix, :], ws[0:nr, 0:8], ALU.subtract, wsk + [("w8", tix)], wsk)
                    for j in range(8):
                        B.act(dg[0:nr, 2 * j, 0:nr], ident_f[0:nr, 0:nr], AF.Copy, wsk + ["ident_f"], dgk, scale=ws[0:nr, j:j + 1])
                        B.act(dg[0:nr, 2 * j + 1, 0:nr], ident_f[0:nr, 0:nr], AF.Copy, wsk + ["ident_f"], dgk, scale=ws[0:nr, 8 + j:9 + j])
                    for j in range(8):
                        g_ = yg[gi[0] % 6]
                        gk = [("yg", gi[0] % 6)]
                        S.dma("pool", "yg%d" % (gi[0] % 6), lambda e, g_=g_, tix=tix, j=j: e.indirect_dma_start(
                            out=g_[:], out_offset=None, in_=Y[:, :],
                            in_offset=bass.IndirectOffsetOnAxis(ap=d8all[:, tix, j:j + 1], axis=0),
                            bounds_check=B.bcreg(e), oob_is_err=False), ["Y", ("d8", tix)], gk)
                        for part in range(2):
                            for n4 in range(4):
                                B.mm(ps[0:nr, bks[n4], :], dg[0:nr, 2 * j + part, 0:nr], g_[0:nr, n4 * 512:(n4 + 1) * 512],
                                     j == 0 and part == 0, j == 7 and part == 1, gk + dgk, B.bk(bks[n4]))
                        gi[0] += 1

                def f_finish(tix):
                    nr, r0, a, ak, dg, dgk, ws, wsk, wb16, bks = f_ctx(tix)
                    for n4 in range(4):
                        B.tt("dve", a[0:nr, n4 * 512:(n4 + 1) * 512], a[0:nr, n4 * 512:(n4 + 1) * 512], ps[0:nr, bks[n4], :], ALU.add,
                             ak + B.bk(bks[n4]), ak)
                    layer_norm(a[0:nr, :], ak, nr, (lnt2[tix % 2][:], [("lnt2", tix % 2)], None))
                    if tix < NT:
                        B.dma("sp", "yout%d" % (tix % 2), yp[r0:r0 + nr, :], a[0:nr, :], ak, ["yp"])
                    else:
                        B.dma("sp", "yout%d" % (tix % 2), ys[:, :], a[0:nr, :], ak, ["ys"])

                f_prep(0)
                for tix in range(17):
                    if tix + 1 < 17:
                        f_prep(tix + 1)
                    f_finish(tix)
                if debug:
                    B.dma("sp", "dbg1_" + str(DBGC.next()), dbg["cnt"][:, :], cbase[:], ["cbase"], [])
                    dd = sbc("ddbg", [P, 17, 8], F32)
                    B.cp("dve", dd[:], d8all[:], [("d8", t) for t in range(17)], ["ddbg"])
                    B.dma("sp", "dbg2_" + str(DBGC.next()), dbg["d8"][:, :, :], dd[:], ["ddbg"], [])
                    B.dma("sp", "dbg3_" + str(DBGC.next()), dbg["w8"][:, :, :], w8all[:], [("w8", t) for t in range(17)], [])

        S.finalize(es)
        with nc.Block() as block:
            S.run_block(block)
    return nc


def _consts(core):
    hh = core % 2
    half = 8
    inv_freq = np.power(np.float32(500000.0), -np.arange(half, dtype=np.float32) * np.float32(2.0 / 16)).astype(np.float32)
    pos = np.zeros(2192, np.float32)
    pos[0:P] = hh * TOK - P + np.arange(P)
    pos[P:P + TOK] = hh * TOK + np.arange(TOK)
    pos[P + TOK:] = PAST
    pos = np.maximum(pos, 0).astype(np.float32)
    ang = (pos[:, None] * inv_freq[None, :]).astype(np.float32)
    cosv = np.cos(ang).astype(np.float32)
    sinv = np.sin(ang).astype(np.float32)
    cosT = np.ones((P, 2192), np.float32)
    sinT = np.zeros((P, 2192), np.float32)
    for p in range(P):
        d = p % 64
        if d < 16:
            cosT[p] = cosv[:, d % 8]
            sinT[p] = sinv[:, d % 8]
    ident = np.eye(P, dtype=np.float32)
    R = np.zeros((P, P), np.float32)
    for m in range(P):
        d = m % 64
        if d < 8:
            R[m, m + 8] = -1.0
        elif d < 16:
            R[m, m - 8] = 1.0
    rotT = R.T.copy()
    triU = np.triu(np.ones((P, P), np.float32), 1)
    ones = np.ones((P, P), np.float32)
    cst = np.concatenate([ident, rotT, triU, ones, np.zeros((P, 512), np.float32)], axis=1)
    a = np.arange(P)[:, None]
    c = np.arange(2 * P)[None, :]
    valid = (c > a) & (c <= a + P)
    mask_gen = np.where(valid, 0.0, NEG).astype(np.float32)
    if hh == 0:
        mask_first = np.where(valid & (c >= P), 0.0, NEG).astype(np.float32)
    else:
        mask_first = mask_gen
    msk = np.concatenate([mask_gen, mask_first], axis=1)
    c16 = np.zeros((P, 232), np.float32)
    for p in range(P):
        for kv in range(4):
            for h in range(16):
                c16[p, kv * 16 + h] = 1.0 if ((p // 64) == (h % 2) and (h // 4) == kv) else 0.0
    c16[:, 64:128] = (np.arange(NE) * CAP)[None, :]
    c16[:, 128:192] = np.arange(NE)[None, :]
    for h in range(16):
        for kv in range(4):
            c16[h, 192 + kv] = 1.0 if h // 4 == kv else 0.0
        for par in range(2):
            c16[h, 200 + par] = 1.0 if h % 2 == par else 0.0
    return dict(cosT=cosT, sinT=sinT, cst=cst, msk=msk, c16=c16)


_NC_CACHE = {}


def kernel(x_prompt, x_sample, cache_k, cache_v, state_conv, w_in, attn_sinks, conv_w,
           w_attn_out, w_conv_out, w_o, ln1_g, ln1_b, w_router, router_bias,
           w_exp_gate, w_exp_up, w_exp_down, w_sh_gate, w_sh_up, w_sh_down, ln2_g, ln2_b,
           _stop_after=None, _debug=False, _cores=None):
    f = lambda a: np.ascontiguousarray(np.asarray(a, dtype=np.float32))
    x_prompt, x_sample = f(x_prompt), f(x_sample)
    key = (_stop_after, _debug)
    if key not in _NC_CACHE:
        _NC_CACHE[key] = build_program(_stop_after, _debug)
    nc = _NC_CACHE[key]
    shared = dict(
        w_in=f(w_in[0]), sinks=f(attn_sinks), conv_w=f(conv_w[0]), w_ao=f(w_attn_out[0]), w_co=f(w_conv_out[0]),
        w_o=f(w_o[0]), ln1_g=f(ln1_g), ln1_b=f(ln1_b), w_r=f(w_router[0]), r_bias=f(router_bias),
        w_eg=f(w_exp_gate[0]), w_eu=f(w_exp_up[0]), w_ed=f(w_exp_down[0]), w_sg=f(w_sh_gate[0]),
        w_su=f(w_sh_up[0]), w_sd=f(w_sh_down[0]), ln2_g=f(ln2_g), ln2_b=f(ln2_b))
    if _stop_after is not None:
        for k_ in ("w_eg", "w_eu", "w_ed"):
            shared.pop(k_)
    in_maps = []
    cores = list(range(NCORES)) if _cores is None else list(_cores)
    for c in cores:
        n, hh = c // 2, c % 2
        xp = np.zeros((TOK + P, D), np.float32)
        if hh == 1:
            xp[0:P] = x_prompt[n, TOK - P:TOK]
        xp[P:] = x_prompt[n, hh * TOK:(hh + 1) * TOK]
        m = dict(shared)
        m.update(_consts(c))
        m.update(xp=xp, xs=f(x_sample[c * NS:(c + 1) * NS, 0, :]),
                 ck=f(cache_k[0, c * NS:(c + 1) * NS].reshape(NS, P, 256)),
                 cv=f(cache_v[0, c * NS:(c + 1) * NS].reshape(NS, P, 256)),
                 sc=f(state_conv[0, c * NS:(c + 1) * NS]))
        in_maps.append(m)
    res = run_bass_kernel_spmd(nc, in_maps, core_ids=list(range(len(cores))))
    R = res.results
    if _debug or _stop_after is not None:
        return R
    y_p = np.zeros((4, SEQ, D), np.float32)
    y_s = np.zeros((128, 1, D), np.float32)
    pk = np.zeros((1, 4, P, 4, 64), np.float32)
    pv = np.zeros((1, 4, P, 4, 64), np.float32)
    pc_ = np.zeros((1, 4, 2, 1024), np.float32)
    sk = np.zeros((1, 128, P, 4, 64), np.float32)
    sv = np.zeros((1, 128, P, 4, 64), np.float32)
    ssc_ = np.zeros((1, 128, 2, 1024), np.float32)
    for c in range(NCORES):
        n, hh = c // 2, c % 2
        y_p[n, hh * TOK:(hh + 1) * TOK] = R[c]["yp"]
        y_s[c * NS:(c + 1) * NS, 0] = R[c]["ys"]
        if hh == 1:
            pk[0, n] = R[c]["pck"].reshape(P, 4, 64)
            pv[0, n] = R[c]["pcv"].reshape(P, 4, 64)
            pc_[0, n] = R[c]["psc"]
        sk[0, c * NS:(c + 1) * NS] = R[c]["sck"].reshape(NS, P, 4, 64)
        sv[0, c * NS:(c + 1) * NS] = R[c]["scvo"].reshape(NS, P, 4, 64)
        ssc_[0, c * NS:(c + 1) * NS] = R[c]["ssc"]
    return (y_p, y_s, pk, pv, pc_, sk, sv, ssc_)
```

```python
import contextlib
import numpy as np
import concourse.bass as bass
import concourse.mybir as mybir
from concourse.bass_utils import run_bass_kernel_spmd

F32 = mybir.dt.float32
BF16 = mybir.dt.bfloat16
I32 = mybir.dt.int32
AF = mybir.ActivationFunctionType
ALU = mybir.AluOpType
AX = mybir.AxisListType

ENGS = ("pe", "act", "dve", "pool", "sp")


class _Ctr:
    def __init__(self):
        self.n = 0

    def next(self):
        self.n += 1
        return self.n


DBGC = _Ctr()

P = 128
D = 2048
NCORES = 8
SEQ = 4096
TOK = 2048
NS = 16
NT = 16
GT = 4
NG = NT // GT
NE = 64
CAP = 384
NBLK = CAP // P
NSLOT = NE * CAP
ALPHA = 2.0 ** 0.25
LN_EPS = 1e-5
PAST = 16384
OQ, OK_, OV, OB, OC, OH, OGA, OGC = 0, 1024, 1280, 1536, 2560, 3584, 4608, 6656
NEG = -30000.0


class Op:
    __slots__ = ("eng", "fn", "deps", "is_dma", "chan", "idx", "signal", "semval", "gidx")

    def __init__(self, eng, fn, is_dma, chan):
        self.eng = eng
        self.fn = fn
        self.deps = {}
        self.is_dma = is_dma
        self.chan = chan
        self.signal = False
        self.semval = None


def _slot(op):
    return ("ch", op.chan) if op.is_dma else ("eng", op.eng)


class Sched:
    def __init__(self, nc):
        self.nc = nc
        self.ops = {e: [] for e in ENGS}
        self.last_writer = {}
        self.readers = {}
        self.all_ops = []
        self.planning = False

    def _add(self, eng, fn, reads, writes, is_dma=False, chan=None):
        if self.planning:
            return None
        import os
        mx = int(os.environ.get("KMAXOPS", "0"))
        if mx and len(self.all_ops) >= mx:
            return None
        psr = [k for k in reads if isinstance(k, tuple) and k[0] == "ps"]
        if psr:
            writes = list(writes) + psr
        op = Op(eng, fn, is_dma, chan)
        deps = {}

        def add(d):
            if d is op:
                return
            s = _slot(d)
            o = deps.get(s)
            if o is None or d.gidx > o.gidx:
                deps[s] = d

        for k in reads:
            w = self.last_writer.get(k)
            if w is not None:
                add(w)
        for k in writes:
            w = self.last_writer.get(k)
            if w is not None:
                add(w)
            for r in self.readers.get(k, {}).values():
                add(r)
        op.deps = deps
        op.gidx = len(self.all_ops)
        for k in reads:
            self.readers.setdefault(k, {})[_slot(op)] = op
        for k in writes:
            self.last_writer[k] = op
            self.readers[k] = {}
        self.ops[eng].append(op)
        self.all_ops.append(op)
        return op

    def op(self, eng, fn, reads=(), writes=()):
        return self._add(eng, fn, reads, writes)

    def dma(self, eng, chan, fn, reads=(), writes=()):
        return self._add(eng, fn, reads, writes, is_dma=True, chan=chan)

    def barrier(self):
        if self.planning:
            return
        lasts = {}
        for op in self.all_ops:
            if op.fn is not None:
                lasts[_slot(op)] = op
        for e in ENGS:
            op = Op(e, None, False, None)
            op.deps = {s: d for s, d in lasts.items()}
            op.gidx = len(self.all_ops)
            self.ops[e].append(op)
            self.all_ops.append(op)

    def finalize(self, es):
        nc = self.nc
        for op in self.all_ops:
            for d in op.deps.values():
                if d.is_dma:
                    continue
                if d.eng == "pe" and op.eng == "pe" and not op.is_dma:
                    continue
                d.signal = True
        self.sem = {e: es.enter_context(nc.semaphore("sem_" + e)) for e in ENGS}
        chans = []
        for op in self.all_ops:
            if op.is_dma and op.chan not in chans:
                chans.append(op.chan)
        self.chsem = {c: es.enter_context(nc.semaphore("ch_" + str(c))) for c in chans}
        cnt = {e: 0 for e in ENGS}
        chcnt = {c: 0 for c in chans}
        for op in self.all_ops:
            if op.is_dma:
                chcnt[op.chan] += 16
                op.semval = chcnt[op.chan]
            elif op.signal:
                cnt[op.eng] += 1
                op.semval = cnt[op.eng]
        for op in self.all_ops:
            if op.is_dma and str(op.chan).startswith("cst"):
                op.semval = chcnt[op.chan]
        self.chfinal = chcnt

    def emit(self, ename, eng):
        seen = {}
        for op in self.ops[ename]:
            for s, d in op.deps.items():
                if (not d.is_dma) and d.eng == "pe" and op.eng == "pe" and not op.is_dma:
                    continue
                v = d.semval
                if v is None:
                    continue
                if seen.get(s, 0) >= v:
                    continue
                sem = self.chsem[s[1]] if s[0] == "ch" else self.sem[s[1]]
                eng.wait_ge(sem, v)
                seen[s] = v
            if op.fn is None:
                continue
            inst = op.fn(eng)
            if op.is_dma:
                inst.then_inc(self.chsem[op.chan], 16)
            elif op.signal:
                inst.then_inc(self.sem[op.eng], 1)

    def run_block(self, block):
        S = self

        def mk(ename):
            def body(eng):
                S.emit(ename, eng)
                if ename == "sp":
                    for c, v in S.chfinal.items():
                        if v > 0:
                            eng.wait_ge(S.chsem[c], v)
            return body

        block.tensor(mk("pe"))
        block.scalar(mk("act"))
        block.vector(mk("dve"))
        block.gpsimd(mk("pool"))
        block.sync(mk("sp"))


def fap(t, offset, dims, parts=None, p0=0):
    base = t if isinstance(t, bass.AP) else t[:]
    pst = base.ap[0][0]
    npart = base.ap[0][1] if parts is None else parts
    return bass.AP(tensor=base.tensor, offset=base.offset + p0 * pst + offset,
                   ap=[[pst, npart]] + [list(d) for d in dims])


class Builder:
    def __init__(self, stop_after=None, debug=False):
        self.stop_after = stop_after
        self.debug = debug
        self.nc = bass.Bass("TRN2", target_bir_lowering=False)
        self.S = Sched(self.nc)
        self.bank_rr = 0
        self.wplan = []
        self.wpos = 0
        self.tmp_rr = {}

    def bcreg(self, e):
        if getattr(self, "_bcreg", None) is None:
            self._bcreg = e.to_reg(NSLOT - 1)
        return self._bcreg

    def din(self, name, shape, dt=F32):
        return self.nc.dram_tensor(name, list(shape), dt, kind="ExternalInput").ap()

    def dout(self, name, shape, dt=F32):
        return self.nc.dram_tensor(name, list(shape), dt, kind="ExternalOutput").ap()

    def dscr(self, name, shape, dt=F32):
        return self.nc.dram_tensor(name, list(shape), dt, kind="Internal").ap()

    def bank(self, n=1):
        if n == 2 and self.bank_rr % 2 == 1:
            self.bank_rr += 1
        b = self.bank_rr % 6
        self.bank_rr += n
        return b

    def bk(self, b, n=1):
        return [("ps", b + i) for i in range(n)]

    def mm(self, out, lhsT, rhs, start, stop, reads, writes):
        self.S.op("pe", lambda e: e.matmul(out, lhsT=lhsT, rhs=rhs, start=start, stop=stop), reads, writes)

    def tr(self, out, in_, ident, reads, writes):
        self.S.op("pe", lambda e: e.transpose(out=out, in_=in_, identity=ident), reads, writes)

    def act(self, out, in_, func, reads, writes, scale=None, bias=None, accum_out=None):
        kw = {}
        if scale is not None:
            kw["scale"] = scale
        if bias is not None:
            kw["bias"] = bias
        if accum_out is not None:
            kw["accum_out"] = accum_out
        self.S.op("act", lambda e: e.activation(out=out, in_=in_, func=func, **kw), reads, writes)

    def tt(self, eng, out, in0, in1, op, reads, writes):
        self.S.op(eng, lambda e: e.tensor_tensor(out=out, in0=in0, in1=in1, op=op), reads, writes)

    def ts(self, eng, out, in0, s1, op0, reads, writes, s2=None, op1=None, accum_out=None):
        kw = {}
        if op1 is not None:
            kw["op1"] = op1
        if accum_out is not None:
            kw["accum_out"] = accum_out
        self.S.op(eng, lambda e: e.tensor_scalar(out=out, in0=in0, scalar1=s1, scalar2=s2, op0=op0, **kw), reads, writes)

    def stt(self, out, in0, scalar, in1, op0, op1, reads, writes, accum_out=None):
        kw = {}
        if accum_out is not None:
            kw["accum_out"] = accum_out
        self.S.op("dve", lambda e: e.scalar_tensor_tensor(out=out, in0=in0, scalar=scalar, in1=in1, op0=op0, op1=op1, **kw), reads, writes)

    def red(self, out, in_, op, reads, writes, axis=AX.X):
        self.S.op("dve", lambda e: e.tensor_reduce(out=out, in_=in_, axis=axis, op=op), reads, writes)

    def cp(self, eng, out, in_, reads, writes):
        if eng == "act":
            self.S.op("act", lambda e: e.activation(out=out, in_=in_, func=AF.Copy), reads, writes)
        else:
            self.S.op(eng, lambda e: e.tensor_copy(out=out, in_=in_), reads, writes)

    def dma(self, eng, chan, out, in_, reads, writes, **kw):
        return self.S.dma(eng, chan, lambda e: e.dma_start(out=out, in_=in_, **kw), reads, writes)

    def wget(self, spec):
        S = self.S
        i = self.wpos
        self.wpos += 1
        if S.planning:
            self.wplan.append(spec)
        slot = i % self.NB
        nk, ncols, parts = spec
        while self.wissued < min(len(self.wplan), i + self.NB - 2):
            self._wissue(self.wissued)
            self.wissued += 1
        ap = fap(self.wring, slot * self.WSLOT, [[ncols, nk], [1, ncols]])
        return ap, [("w", slot)]

    def _wissue(self, j):
        if self.S.planning:
            return
        nk, ncols, parts = self.wplan[j]
        slot = j % self.NB
        for (c0, n, src) in parts:
            dst = fap(self.wring, slot * self.WSLOT + c0, [[ncols, nk], [1, n]])
            self.dma("pool", "w%d" % slot, dst, src.rearrange("(k p) n -> p k n", p=P), [], [("w", slot)])

    def areset(self):
        self.apos = 0

    def aalloc(self, nbytes):
        nbytes = (nbytes + 63) // 64 * 64
        off = self.apos
        self.apos += nbytes
        assert self.apos <= self.ARENA_BYTES, ("arena overflow", self.apos)
        keys = [("ar", b) for b in range(off // 2048, (off + nbytes - 1) // 2048 + 1)]
        return off, keys

    def af32(self, n, dims=None):
        off, keys = self.aalloc(n * 4)
        ap = fap(self.arena, off // 4, dims if dims is not None else [[1, n]])
        return ap, keys, off // 4

    def abf(self, n, dims=None):
        off, keys = self.aalloc(n * 2)
        ap = fap(self.arena_bf, off // 2, dims if dims is not None else [[1, n]])
        return ap, keys, off // 2


def build_program(stop_after=None, debug=False):
    B = Builder(stop_after, debug)
    nc = B.nc
    S = B.S

    xp = B.din("xp", [TOK + P, D])
    xs = B.din("xs", [NS, D])
    ck = B.din("ck", [NS, P, 256])
    cv = B.din("cv", [NS, P, 256])
    scv = B.din("sc", [NS, 2, 1024])
    w_in = B.din("w_in", [D, 8704])
    sinks = B.din("sinks", [1, 16])
    conv_w = B.din("conv_w", [3, 1024])
    w_ao = B.din("w_ao", [1024, D])
    w_co = B.din("w_co", [1024, D])
    w_o = B.din("w_o", [D, D])
    ln1_g = B.din("ln1_g", [1, D])
    ln1_b = B.din("ln1_b", [1, D])
    w_r = B.din("w_r", [D, NE])
    r_bias = B.din("r_bias", [1, NE])
    if stop_after is None:
        w_eg = B.din("w_eg", [NE, D, 512])
        w_eu = B.din("w_eu", [NE, D, 512])
        w_ed = B.din("w_ed", [NE, 512, D])
    w_sg = B.din("w_sg", [D, 512])
    w_su = B.din("w_su", [D, 512])
    w_sd = B.din("w_sd", [512, D])
    ln2_g = B.din("ln2_g", [1, D])
    ln2_b = B.din("ln2_b", [1, D])
    cosd = B.din("cosT", [P, 2192])
    sind = B.din("sinT", [P, 2192])
    cst = B.din("cst", [P, 1024])
    mskd = B.din("msk", [P, 512])
    c16 = B.din("c16", [P, 64 + 64 + 64 + 8 + 32])

    yp = B.dout("yp", [TOK, D])
    ys = B.dout("ys", [NS, D])
    pck = B.dout("pck", [P, 256])
    pcv = B.dout("pcv", [P, 256])
    psc = B.dout("psc", [2, 1024])
    sck = B.dout("sck", [NS, P, 256])
    scvo = B.dout("scvo", [NS, P, 256])
    ssc = B.dout("ssc", [NS, 2, 1024])
    if debug:
        dbg = {k: B.dout("dbg_" + k, shp) for k, shp in [("h", [TOK + NS, D]), ("cnt", [P, NE]), ("attn", [P, 8, 640]), ("conv", [P, 8, 640]), ("d8", [P, 17, 8]), ("w8", [P, 17, 8])]}

    XG = B.dscr("XG", [NSLOT + P, D], BF16)
    Y = B.dscr("Y", [NSLOT, D], BF16)
    BASE = B.dscr("BASE", [TOK + P, D], F32)

    es = contextlib.ExitStack()
    with es:
        def sb(name, shape, dt):
            return es.enter_context(nc.sbuf_tensor(name, list(shape), dt))

        ps = es.enter_context(nc.psum_tensor("ps", [P, 8, 512], F32))
        psb = ps.bitcast(BF16)

        ident_f = sb("ident_f", [P, P], F32)
        ident = sb("ident", [P, P], BF16)
        rotT = sb("rotT", [P, P], BF16)
        triU = sb("triU", [P, P], BF16)
        ones = sb("ones", [P, P], BF16)
        msk = sb("mskt", [P, 512], F32)
        c16t = sb("c16t", [P, 232], F32)
        sink8 = sb("sink8", [P, 16], F32)
        sinkraw = sb("sinkraw", [P, 16], F32)
        rbias = sb("rbias", [P, NE], F32)
        convw = sb("convw", [P, 8, 3], F32)
        lng = sb("lng", [P, D], F32)
        lnb = sb("lnb", [P, D], F32)
        d8all = sb("d8all", [P, 17, 8], I32)
        w8all = sb("w8all", [P, 17, 8], F32)
        cbase = sb("cbase", [P, NE], F32)
        epsT = sb("epsT", [P, 1], F32)
        sinkcol = sb("sinkcol", [P, 1], F32)
        rsel_t = sb("rsel", [P, 5, NE], BF16)

        def load_consts():
            B.dma("sp", "cst", ident_f[:], cst[:, 0:128], [], ["ident_f"])
            B.dma("pool", "cstp", ident[:], cst[:, 0:128], [], ["ident"])
            B.dma("pool", "cstp", rotT[:], cst[:, 128:256], [], ["rotT"])
            B.dma("pool", "cstp", triU[:], cst[:, 256:384], [], ["triU"])
            B.dma("pool", "cstp", ones[:], cst[:, 384:512], [], ["ones"])
            B.dma("sp", "cst", msk[:], mskd[:, :], [], ["msk"])
            B.dma("sp", "cst", c16t[:], c16[:, :], [], ["c16t"])
            B.dma("sp", "cst", sinkraw[:], fap_dram_bcast(sinks, 16), [], ["sinkraw"])
            B.dma("sp", "cst", rbias[:], fap_dram_bcast(r_bias, NE), [], ["rbias"])
            for j in range(3):
                B.dma("sp", "cst", convw[:, :, j], conv_w[j, :].rearrange("(c p) -> p c", p=P), [], ["convw%d" % j], allow_slow_non_contiguous=True)
            B.ts("dve", fap(sink8, 0, [[4, 4], [2, 2], [1, 2]]), fap(sinkraw, 0, [[4, 4], [1, 2], [2, 2]]), 8.0, ALU.mult, ["sinkraw"], ["sink8"])
            S.op("dve", lambda e: e.memset(cbase[:], 0.0), [], ["cbase"])
            S.op("dve", lambda e: e.memset(w8all[:], 0.0), [], [("w8", t) for t in range(17)])
            S.op("dve", lambda e: e.memset(epsT[:], LN_EPS), [], ["epsT"])
            B.dma("sp", "cst", sinkcol[0:16, :], sinks.rearrange("a h -> h a"), [], ["sinkcol"], allow_slow_non_contiguous=True)
            B.ts("dve", sinkcol[0:16, :], sinkcol[0:16, :], 8.0, ALU.mult, ["sinkcol"], ["sinkcol"])

        def fap_dram_bcast(src, n):
            return bass.AP(tensor=src.tensor, offset=src.offset, ap=[[0, P], [1, n]])

        load_consts()

        pa = contextlib.ExitStack()
        with pa:
            def sba(name, shape, dt):
                return pa.enter_context(nc.sbuf_tensor(name, list(shape), dt))

            NCMAX = 640
            bigT = sba("bigT", [P, 16, NCMAX], BF16)
            attnT = sba("attnT", [P, 8, NCMAX], BF16)
            convT = sba("convT", [P, 8, NCMAX], BF16)
            kTl = sba("kTl", [P, 4, P + NCMAX], BF16)
            kT32 = sba("kT32", [P, 2, 144], F32)
            vl = sba("vl", [P, 6, 256], BF16)
            v32 = sba("v32", [P, 2, 256], F32)
            cosl = sba("cosl", [P, NCMAX], F32)
            sinl = sba("sinl", [P, NCMAX], F32)
            uprev = sba("uprev", [P, 8, 2], F32)
            hres = sba("hres", [P, 5, D], F32)
            B.NB = 6
            B.WSLOT = 4096
            B.wring = sba("wring", [P, B.NB * B.WSLOT], BF16)
            B.ARENA_BYTES = 38 * 1024
            B.arena = sba("arena", [P, B.ARENA_BYTES // 4], F32)
            B.arena_bf = B.arena.bitcast(BF16)

            hres_bf = hres.bitcast(BF16)
            def zero_fill_xg():
                zt = hres_bf[:, 4, 0:D]
                S.op("dve", lambda e: e.memset(zt, 0.0), [], [("hres", 4)])
                nrow_total = NSLOT + P
                r = 0
                while r < nrow_total:
                    n = min(4 * P, nrow_total - r)
                    B.dma("act", "zf", XG[r:r + n, :].rearrange("(a p) d -> p a d", p=P), fap(hres_bf, 4 * 4096, [[0, n // P], [1, D]]), [("hres", 4)], ["XG"])
                    r += n

            B.dma("sp", "cst", lng[:], fap_dram_bcast(ln1_g, D), [], ["lng"])
            B.dma("sp", "cst", lnb[:], fap_dram_bcast(ln1_b, D), [], ["lnb"])

            if debug:
                S.op("dve", lambda e: e.memset(attnT[:], 0.0), [], ["attnT"])
                S.op("dve", lambda e: e.memset(convT[:], 0.0), [], ["convT"])

            def phase_a():
                B.wpos = 0
                B.wissued = 0
                for g in range(NG):
                    group(g)

            def make_tiles(g):
                halo = (g == 0)
                samp = (g == NG - 1)
                moff = P if halo else 0
                soff = moff + GT * P
                tl = []
                if halo:
                    tl.append((0, P, xp[0:P, :], "halo", None))
                for t in range(GT):
                    tt_ = g * GT + t
                    tl.append((moff + t * P, P, xp[P + tt_ * P:P + (tt_ + 1) * P, :], "main", tt_))
                if samp:
                    tl.append((soff, NS, xs[:, :], "samp", None))
                return tl

            def xpf_loc(i, k):
                if i < 2:
                    return attnT, i * 2048 + k * P, "attnT"
                if i < 4:
                    return convT, (i - 2) * 2048 + k * P, "convT"
                if k < 8:
                    return attnT, 4096 + k * P, "attnT"
                return convT, 4096 + (k - 8) * P, "convT"

            def prefetch_x(tl):
                for i, (c0, nr, src_, kind, tt_) in enumerate(tl):
                    if i < 4:
                        base, off, key = xpf_loc(i, 0)
                        B.dma("pool", "xpf%d" % i, fap(base, off, [[1, D]], parts=nr), src_, [], [key])
                    else:
                        B.dma("pool", "xpf4", fap(attnT, 4096, [[1, 1024]], parts=nr), src_[:, 0:1024], [], ["attnT"])
                        B.dma("pool", "xpf5", fap(convT, 4096, [[1, 1024]], parts=nr), src_[:, 1024:2048], [], ["convT"])

            def group(g):
                halo = (g == 0)
                samp = (g == NG - 1)
                moff = P if halo else 0
                NC = moff + GT * P + (NS if samp else 0)
                soff = moff + GT * P
                absb = (P + GT * P * g) - moff
                own_segs = [(moff, GT * P)] + ([(soff, NS)] if samp else [])
                all_segs = ([(0, P)] if halo else []) + own_segs
                tiles = make_tiles(g)
                own_tiles = [tl for tl in tiles if tl[3] != "halo"]
                if g == 0:
                    prefetch_x(tiles)

                B.areset()
                B.dma("sp", "tabc", cosl[:, 0:NC], cosd[:, absb:absb + NC], [], ["cosl"])
                B.dma("sp", "tabs", sinl[:, 0:NC], sind[:, absb:absb + NC], [], ["sinl"])
                for i, (c0, nr, src, kind, tt_) in enumerate(tiles):
                    b2 = B.bank(2)
                    for k in range(16):
                        base, off, key = xpf_loc(i, k)
                        o = fap(psb, (b2 + k // 8) * 1024 + (k % 8) * nr, [[1, nr]])
                        B.tr(o, fap(base, off, [[1, P]], parts=nr), ident[0:nr, 0:nr], [key, "ident"], B.bk(b2 + k // 8))
                    for hh in range(2):
                        src_ap = fap(psb, (b2 + hh) * 1024, [[nr, 8], [1, nr]])
                        B.cp("dve" if hh == 0 else "act", bigT[:, hh * 8:(hh + 1) * 8, c0:c0 + nr], src_ap, B.bk(b2 + hh), ["bigT"])

                if B.stop_after == "S0":
                    return
                def proj(wt, wk, nk, col_lo, segs, rhs_t, rhs_key, consumer):
                    for (c0, n) in segs:
                        b = B.bank()
                        for k in range(nk):
                            B.mm(ps[:, b, 0:n], wt[:, k, col_lo:col_lo + P], rhs_t[:, k, c0:c0 + n], k == 0, k == nk - 1,
                                 wk + [rhs_key], B.bk(b))
                        consumer(b, c0, n)

                def win_spec(col0, ncols=256):
                    return (16, ncols, [(0, ncols, w_in[:, col0:col0 + ncols])])

                B.areset()
                qT, qTk, _ = B.abf(8 * NCMAX, [[NCMAX, 8], [1, NCMAX]])
                qmark = B.apos
                qb = [B.abf(512) for _ in range(2)]
                t1 = [B.af32(512) for _ in range(2)]
                t2 = [B.af32(512) for _ in range(2)]
                rr = [0]

                def rope(b, c0, n, out_bf, out_keys, out_f32=None, out_f32_keys=None):
                    i = rr[0] % 2
                    rr[0] += 1
                    q_b, qbk, _ = qb[i]
                    a1, a1k, _ = t1[i]
                    a2, a2k, _ = t2[i]
                    B.cp("act", q_b[:, 0:n], ps[:, b, 0:n], B.bk(b), qbk)
                    B.tt("dve", a1[:, 0:n], ps[:, b, 0:n], cosl[:, c0:c0 + n], ALU.mult, B.bk(b) + ["cosl"], a1k)
                    b2 = B.bank()
                    B.mm(ps[:, b2, 0:n], rotT[:], q_b[:, 0:n], True, True, qbk + ["rotT"], B.bk(b2))
                    B.tt("dve", a2[:, 0:n], ps[:, b2, 0:n], sinl[:, c0:c0 + n], ALU.mult, B.bk(b2) + ["sinl"], a2k)
                    B.tt("dve", out_bf, a1[:, 0:n], a2[:, 0:n], ALU.add, a1k + a2k, out_keys)
                    if out_f32 is not None:
                        B.tt("dve", out_f32, a1[:, 0:n], a2[:, 0:n], ALU.add, a1k + a2k, out_f32_keys)

                for qs in range(4):
                    wt, wk = B.wget(win_spec(OQ + qs * 256))
                    for cc in range(2):
                        c = qs * 2 + cc
                        proj(wt, wk, 16, cc * P, own_segs, bigT, "bigT",
                             lambda b, c0, n, c=c: rope(b, c0, n, qT[:, c, c0:c0 + n], qTk))
                if B.stop_after == "S1q":
                    return
                if not halo:
                    ksrc = (P if g == 1 else 0) + GT * P
                    B.cp("act", kTl[:, :, 0:P], kTl[:, :, ksrc:ksrc + P], ["kTl"], ["kTl"])
                    B.cp("act", vl[:, 0, :], vl[:, (GT + 1) if g == 1 else GT, :], ["vl"], ["vl"])
                wt, wk = B.wget(win_spec(OK_))
                krt = [B.af32(512) for _ in range(2)]
                kri = [0]
                ksegs = list(all_segs)
                if samp:
                    ksegs = [(moff, (GT - 1) * P), (moff + (GT - 1) * P, P), (soff, NS)]
                for kp in range(2):
                    def kcons(b, c0, n, kp=kp):
                        kr, krk, _ = krt[kri[0] % 2]
                        kri[0] += 1
                        rope(b, c0, n, kr[:, 0:n], krk)
                        for j in range(2):
                            kv = 2 * kp + j
                            for dup in range(2):
                                B.cp("act" if dup == 0 else "dve", kTl[64 * dup:64 * dup + 64, kv, P + c0:P + c0 + n],
                                     kr[64 * j:64 * j + 64, 0:n], krk, ["kTl"])
                        if samp and c0 == soff:
                            B.cp("act", kT32[:, kp, P:P + NS], kr[:, 0:n], krk, ["kT32"])
                        elif samp and c0 == moff + (GT - 1) * P:
                            B.cp("dve", kT32[:, kp, 0:P], kr[:, 0:n], krk, ["kT32"])
                    proj(wt, wk, 16, kp * P, ksegs, bigT, "bigT", kcons)
                if B.stop_after == "S1k":
                    return
                wt, wk = B.wget(win_spec(OV))
                for li, (c0, nr, src, kind, tt_) in enumerate(tiles):
                    b = B.bank()
                    for k in range(16):
                        B.mm(ps[0:nr, b, 0:256], bigT[:, k, c0:c0 + nr], wt[:, k, :], k == 0, k == 15, wk + ["bigT"], B.bk(b))
                    B.cp("act", vl[0:nr, 1 + li, :], ps[0:nr, b, 0:256], B.bk(b), ["vl"])
                    if samp and kind == "samp":
                        B.cp("dve", v32[0:nr, 1, :], ps[0:nr, b, 0:256], B.bk(b), ["v32"])
                    if samp and kind == "main" and tt_ == NT - 1:
                        B.cp("dve", v32[:, 0, :], ps[:, b, 0:256], B.bk(b), ["v32"])

                if B.stop_after == "S1a":
                    return
                B.apos = qmark
                Sm = [B.af32(1024, [[256, 4], [1, 256]]) for _ in range(2)]
                Pn = [B.abf(1024, [[256, 4], [1, 256]]) for _ in range(2)]
                PTt = [B.abf(1024, [[128, 8], [1, 128]]) for _ in range(2)]
                sm_ = [B.af32(32) for _ in range(2)]
                items = []
                for li, (c0, nr, src_, kind, tt_) in enumerate(tiles):
                    if kind == "main":
                        for kv in range(4):
                            items.append((li, c0, tt_, kv))
                bpv = 6
                sc_bank = {}

                def a_scores(idx):
                    li, c0, tt_, kv = items[idx]
                    b2 = B.bank(2)
                    sc_bank[idx] = b2
                    for hh in range(4):
                        c = 2 * kv + hh // 2
                        half = hh % 2
                        B.mm(ps[:, b2 + half, (hh // 2) * 256:(hh // 2) * 256 + 256],
                             qT[64 * half:64 * half + 64, c, c0:c0 + P],
                             kTl[64 * half:64 * half + 64, kv, c0:c0 + 256], True, True,
                             qTk + ["kTl"], B.bk(b2 + half))

                def a_ctx(idx):
                    li, c0, tt_, kv = items[idx]
                    i = idx % 2
                    Sx, Sk, _ = Sm[i]
                    Px, Pk, _ = Pn[i]
                    PT, PTk, _ = PTt[i]
                    sx, sk_, _ = sm_[i]
                    return li, c0, tt_, kv, Sx, Sk, Px, Pk, PT, PTk, sx, sk_

                def a_A(idx):
                    li, c0, tt_, kv, Sx, Sk, Px, Pk, PT, PTk, sx, sk_ = a_ctx(idx)
                    b2 = sc_bank[idx]
                    mk_off = 256 if tt_ == 0 else 0
                    pin = fap(ps, b2 * 512, [[256, 4], [1, 256]])
                    B.tt("dve", Sx, pin, fap(msk, mk_off, [[0, 4], [1, 256]]), ALU.add, B.bk(b2, 2) + ["msk"], Sk)
                    mx = sx[:, 0:4]
                    m8 = sx[:, 4:8]
                    sm = sx[:, 8:12]
                    tq = sx[:, 12:16]
                    es_ = sx[:, 16:20]
                    rv = sx[:, 20:24]
                    nm = sx[:, 24:28]
                    B.red(mx, Sx, ALU.max, Sk, sk_)
                    B.tt("dve", m8, mx, sink8[:, 4 * kv:4 * kv + 4], ALU.max, sk_ + ["sink8"], sk_)
                    B.ts("dve", nm, m8, -0.125, ALU.mult, sk_, sk_)
                    B.tt("dve", tq, sink8[:, 4 * kv:4 * kv + 4], m8, ALU.subtract, sk_ + ["sink8"], sk_)

                def a_E(idx):
                    li, c0, tt_, kv, Sx, Sk, Px, Pk, PT, PTk, sx, sk_ = a_ctx(idx)
                    for s in range(4):
                        B.act(Sx[:, s, :], Sx[:, s, :], AF.Exp, Sk + sk_, Sk + sk_, scale=0.125, bias=sx[:, 24 + s:25 + s], accum_out=sx[:, 8 + s:9 + s])

                def a_B(idx):
                    li, c0, tt_, kv, Sx, Sk, Px, Pk, PT, PTk, sx, sk_ = a_ctx(idx)
                    sm = sx[:, 8:12]
                    tq = sx[:, 12:16]
                    es_ = sx[:, 16:20]
                    rv = sx[:, 20:24]
                    B.act(es_, tq, AF.Exp, sk_, sk_, scale=0.125)
                    B.tt("dve", rv, sm, es_, ALU.add, sk_, sk_)
                    S.op("dve", lambda e, rv=rv: e.reciprocal(out=rv, in_=rv), sk_, sk_)
                    for s in range(4):
                        B.act(Px[:, s, :], Sx[:, s, :], AF.Copy, Sk + sk_, Pk, scale=sx[:, 20 + s:21 + s])

                def a_C(idx):
                    li, c0, tt_, kv, Sx, Sk, Px, Pk, PT, PTk, sx, sk_ = a_ctx(idx)
                    bt = B.bank()
                    for hh in range(4):
                        for kb in range(2):
                            B.tr(psb[:, bt, (hh * 2 + kb) * P:(hh * 2 + kb + 1) * P], Px[:, (hh % 2) * 2 + hh // 2, kb * P:(kb + 1) * P], ident[:],
                                 Pk + ["ident"], B.bk(bt))
                    B.cp("act", PT, fap(psb, bt * 1024, [[128, 8], [1, 128]]), B.bk(bt), PTk)
                    for hh in range(4):
                        c = 2 * kv + hh // 2
                        half = hh % 2
                        o = ps[64 * half:64 * half + 64, bpv + c // 4, (c % 4) * P:(c % 4 + 1) * P]
                        for kb in range(2):
                            B.mm(o, vl[:, li + kb, kv * 64:(kv + 1) * 64], PT[:, hh * 2 + kb, :], kb == 0, kb == 1,
                                 ["vl"] + PTk, B.bk(bpv + c // 4))
                    if kv == 3:
                        for hh in range(2):
                            B.cp("act" if hh == 0 else "dve", attnT[:, hh * 4:(hh + 1) * 4, c0:c0 + P],
                                 fap(ps, (bpv + hh) * 512, [[128, 4], [1, 128]]), B.bk(bpv + hh), ["attnT"])

                if items:
                    n_it = len(items)
                    a_scores(0)
                    if n_it > 1:
                        a_scores(1)
                    a_A(0)
                    for idx in range(n_it):
                        a_E(idx)
                        if idx + 2 < n_it:
                            a_scores(idx + 2)
                        if idx + 1 < n_it:
                            a_A(idx + 1)
                        a_B(idx)
                        a_C(idx)

                if B.stop_after == "S1b":
                    return
                if samp:
                    B.apos = qmark
                    sample_attention(qT, qTk, soff)

                if B.stop_after == "S1":
                    return

                if g == 0:
                    zero_fill_xg()
                B.areset()
                NU = 2 + NCMAX
                hs = [B.af32(512) for _ in range(2)]
                bs = [B.af32(512) for _ in range(2)]
                ub = [B.af32(NU) for _ in range(2)]
                tb = [B.af32(512) for _ in range(2)]
                if samp:
                    stT, stTk, _ = B.af32(8 * 32, [[32, 8], [1, 32]])
                    st_in, st_ink, _ = B.af32(1024)
                    us_all, us_allk, _ = B.af32(8 * NS, [[NS, 8], [1, NS]])
                    ul_all, ul_allk, _ = B.af32(8 * 2, [[2, 8], [1, 2]])
                    B.dma("sp", "st", fap(st_in, 0, [[1, 1024]], parts=32), scv.rearrange("b j c -> (b j) c"), [], st_ink)
                    bq = B.bank()
                    for cc in range(8):
                        B.tr(ps[:, bq, cc * 32:(cc + 1) * 32], fap(st_in, cc * P, [[1, P]], parts=32), ident_f[0:32, 0:32],
                             st_ink + ["ident_f"], B.bk(bq))
                    B.cp("dve", stT, fap(ps, bq * 512, [[32, 8], [1, 32]]), B.bk(bq), stTk)
                for cc in range(8):
                    if cc % 2 == 0:
                        wb_, wbk_ = B.wget(win_spec(OB + (cc // 2) * 256))
                        wc_, wck_ = B.wget(win_spec(OC + (cc // 2) * 256))
                        wh_, whk_ = B.wget(win_spec(OH + (cc // 2) * 256))
                    cj = (cc % 2) * P
                    i = cc % 2
                    u, uk, _ = ub[i]
                    if not halo:
                        B.cp("act", u[:, 0:2], uprev[:, cc, :], ["uprev"], uk)
                    for (c0, n) in all_segs:
                        hx, hk, _ = hs[i]
                        bx, bxk, _ = bs[i]
                        tx, txk, _ = tb[i]
                        is_own = (c0, n) in own_segs
                        bh = B.bank()
                        for k in range(16):
                            B.mm(ps[:, bh, 0:n], wh_[:, k, cj:cj + P], bigT[:, k, c0:c0 + n], k == 0, k == 15, whk_ + ["bigT"], B.bk(bh))
                        B.cp("act", hx[:, 0:n], ps[:, bh, 0:n], B.bk(bh), hk)
                        bc = B.bank()
                        for k in range(16):
                            B.mm(ps[:, bc, 0:n], wc_[:, k, cj:cj + P], bigT[:, k, c0:c0 + n], k == 0, k == 15, wck_ + ["bigT"], B.bk(bc))
                        B.tt("dve", u[:, 2 + c0:2 + c0 + n], ps[:, bc, 0:n], hx[:, 0:n], ALU.mult, B.bk(bc) + hk, uk)
                        if not is_own:
                            continue
                        bb = B.bank()
                        for k in range(16):
                            B.mm(ps[:, bb, 0:n], wb_[:, k, cj:cj + P], bigT[:, k, c0:c0 + n], k == 0, k == 15, wbk_ + ["bigT"], B.bk(bb))
                        B.cp("act", bx[:, 0:n], ps[:, bb, 0:n], B.bk(bb), bxk)
                        if c0 == soff and samp:
                            s0 = fap(stT, cc * 32, [[2, NS]])
                            s1 = fap(stT, cc * 32 + 1, [[2, NS]])
                            B.ts("dve", tx[:, 0:n], u[:, 2 + c0:2 + c0 + n], convw[:, cc, 2:3], ALU.mult, uk + ["convw0", "convw1", "convw2"], txk)
                            B.stt(tx[:, 0:n], s1, convw[:, cc, 1:2], tx[:, 0:n], ALU.mult, ALU.add, stTk + ["convw0", "convw1", "convw2"] + txk, txk)
                            B.stt(tx[:, 0:n], s0, convw[:, cc, 0:1], tx[:, 0:n], ALU.mult, ALU.add, stTk + ["convw0", "convw1", "convw2"] + txk, txk)
                            B.cp("act", us_all[:, cc, :], u[:, 2 + c0:2 + c0 + n], uk, us_allk)
                        else:
                            B.ts("dve", tx[:, 0:n], u[:, 2 + c0:2 + c0 + n], convw[:, cc, 2:3], ALU.mult, uk + ["convw0", "convw1", "convw2"], txk)
                            B.stt(tx[:, 0:n], u[:, 1 + c0:1 + c0 + n], convw[:, cc, 1:2], tx[:, 0:n], ALU.mult, ALU.add, uk + ["convw0", "convw1", "convw2"] + txk, txk)
                            B.stt(tx[:, 0:n], u[:, c0:c0 + n], convw[:, cc, 0:1], tx[:, 0:n], ALU.mult, ALU.add, uk + ["convw0", "convw1", "convw2"] + txk, txk)
                            B.cp("act", uprev[:, cc, :], u[:, 2 + c0 + n - 2:2 + c0 + n], uk, ["uprev"])
                            if samp:
                                B.cp("act", ul_all[:, cc, :], u[:, 2 + c0 + n - 2:2 + c0 + n], uk, ul_allk)
                        B.tt("dve", convT[:, cc, c0:c0 + n], tx[:, 0:n], bx[:, 0:n], ALU.mult, txk + bxk, ["convT"])
                if samp:
                    def fm_to_tm(src3, srck, n):
                        b2 = B.bank(2)
                        for cc in range(8):
                            B.tr(ps[0:n, b2 + cc // 4, (cc % 4) * P:(cc % 4 + 1) * P], src3[:, cc, :], ident_f[:], srck + ["ident_f"], B.bk(b2 + cc // 4))
                        o, ok_, _ = B.af32(1024)
                        B.cp("dve", fap(o, 0, [[1, 1024]], parts=n), fap(ps, b2 * 512, [[1, 1024]], parts=n), B.bk(b2, 2), ok_)
                        return o, ok_
                    uo, uok = fm_to_tm(us_all, us_allk, NS)
                    B.dma("sp", "o_ssc", ssc[:, 1, :], fap(uo, 0, [[1, 1024]], parts=NS), uok, ["ssc"])
                    B.dma("sp", "o_ssc0", ssc[:, 0, :], scv[:, 1, :], [], ["ssc0"])
                    ul, ulk = fm_to_tm(ul_all, ul_allk, 2)
                    B.dma("sp", "o_psc", psc[:, :], fap(ul, 0, [[1, 1024]], parts=2), ulk, ["psc"])
                    kc, kck, _ = B.af32(256)
                    bq5 = B.bank()
                    for kp in range(2):
                        B.tr(ps[:, bq5, kp * P:(kp + 1) * P], kT32[:, kp, 0:P], ident_f[:], ["kT32", "ident_f"], B.bk(bq5))
                    B.cp("dve", kc, ps[:, bq5, 0:256], B.bk(bq5), kck)
                    B.dma("sp", "o_pck", pck[:, :], kc, kck, ["pck"])
                    B.dma("sp", "o_pcv", pcv[:, :], v32[:, 0, :], ["v32"], ["pcv"])

                if debug and g == 0:
                    B.dma("pool", "dbgp0_" + str(DBGC.next()), dbg["attn"][:, :, :], attnT[:], ["attnT"], [])
                    B.dma("pool", "dbgp1_" + str(DBGC.next()), dbg["conv"][:, :, :], convT[:], ["convT"], [])
                if B.stop_after == "S2":
                    return

                B.areset()
                mixT, mixk, _ = B.abf(16 * NCMAX, [[NCMAX, 16], [1, NCMAX]])
                sg = [B.af32(512) for _ in range(2)]
                ta = [B.af32(512) for _ in range(2)]
                for jb in range(8):
                    wga, wgak = B.wget(win_spec(OGA + jb * 256))
                    wgc, wgck = B.wget(win_spec(OGC + jb * 256))
                    wao, waok = B.wget((8, 512, [(0, 256, w_ao[:, jb * 256:(jb + 1) * 256]), (256, 256, w_co[:, jb * 256:(jb + 1) * 256])]))
                    for jj in range(2):
                        j = jb * 2 + jj
                        for (c0, n) in own_segs:
                            i = j % 2
                            sgx, sgk, _ = sg[i]
                            tax, tak, _ = ta[i]
                            b = B.bank()
                            for k in range(16):
                                B.mm(ps[:, b, 0:n], wga[:, k, jj * P:(jj + 1) * P], bigT[:, k, c0:c0 + n], k == 0, k == 15, wgak + ["bigT"], B.bk(b))
                            B.act(sgx[:, 0:n], ps[:, b, 0:n], AF.Sigmoid, B.bk(b), sgk)
                            b = B.bank()
                            for k in range(8):
                                B.mm(ps[:, b, 0:n], wao[:, k, jj * P:(jj + 1) * P], attnT[:, k, c0:c0 + n], k == 0, k == 7, waok + ["attnT"], B.bk(b))
                            B.tt("dve", tax[:, 0:n], ps[:, b, 0:n], sgx[:, 0:n], ALU.mult, B.bk(b) + sgk, tak)
                            b = B.bank()
                            for k in range(16):
                                B.mm(ps[:, b, 0:n], wgc[:, k, jj * P:(jj + 1) * P], bigT[:, k, c0:c0 + n], k == 0, k == 15, wgck + ["bigT"], B.bk(b))
                            B.act(sgx[:, 0:n], ps[:, b, 0:n], AF.Sigmoid, B.bk(b), sgk)
                            b = B.bank()
                            for k in range(8):
                                B.mm(ps[:, b, 0:n], wao[:, k, 256 + jj * P:256 + (jj + 1) * P], convT[:, k, c0:c0 + n], k == 0, k == 7, waok + ["convT"], B.bk(b))
                            B.tt("dve", sgx[:, 0:n], ps[:, b, 0:n], sgx[:, 0:n], ALU.mult, B.bk(b) + sgk, sgk)
                            B.tt("dve", mixT[:, j, c0:c0 + n], tax[:, 0:n], sgx[:, 0:n], ALU.add, tak + sgk, mixk)

                for li, (c0, nr, src, kind, tt_) in enumerate(own_tiles):
                    B.dma("sp", "xres%d" % li, hres[0:nr, li, :], src, [], [("hres", li)])
                for n4 in range(4):
                    wlo, wlok = B.wget((8, 512, [(0, 512, w_o[0:1024, n4 * 512:(n4 + 1) * 512])]))
                    whi, whik = B.wget((8, 512, [(0, 512, w_o[1024:2048, n4 * 512:(n4 + 1) * 512])]))
                    for li, (c0, nr, src, kind, tt_) in enumerate(own_tiles):
                        b = B.bank()
                        for k in range(16):
                            w_, wk_ = (wlo, wlok) if k < 8 else (whi, whik)
                            B.mm(ps[0:nr, b, :], mixT[:, k, c0:c0 + nr], w_[:, k % 8, :], k == 0, k == 15, mixk + wk_, B.bk(b))
                        hr = hres[0:nr, li, n4 * 512:(n4 + 1) * 512]
                        B.stt(hr, hr, ALPHA, ps[0:nr, b, :], ALU.mult, ALU.add, [("hres", li)] + B.bk(b), [("hres", li)])
                if B.stop_after == "S3":
                    return

                B.areset()
                if g + 1 < NG:
                    prefetch_x(make_tiles(g + 1))
                lnt = [B.af32(32) for _ in range(2)]
                hbf = [B.abf(D) for _ in range(2)]
                rts = [B.af32(64 * 8) for _ in range(len(own_tiles))]
                actT, actk, _ = B.abf(4 * NCMAX, [[NCMAX, 4], [1, NCMAX]])
                sgs = [B.af32(512) for _ in range(2)]
                wrt, wrk = B.wget((16, 64, [(0, 64, w_r[:, :])]))
                for li, (c0, nr, src, kind, tt_) in enumerate(own_tiles):
                    layer_norm(hres[0:nr, li, :], [("hres", li)], nr, lnt[li % 2])
                    if debug:
                        r0 = TOK if kind == "samp" else tt_ * P
                        B.dma("sp", "dbg0_" + str(DBGC.next()), dbg["h"][r0:r0 + nr, :], hres[0:nr, li, :], [("hres", li)], [])
                    hb, hbk, _ = hbf[li % 2]
                    B.cp("act", fap(hb, 0, [[1, D]], parts=nr), hres[0:nr, li, :], [("hres", li)], hbk)
                    b2 = B.bank(2)
                    for k in range(16):
                        o = fap(psb, (b2 + k // 8) * 1024 + (k % 8) * nr, [[1, nr]])
                        B.tr(o, fap(hb, k * P, [[1, P]], parts=nr), ident[0:nr, 0:nr], hbk + ["ident"], B.bk(b2 + k // 8))
                    for hh in range(2):
                        src_ap = fap(psb, (b2 + hh) * 1024, [[nr, 8], [1, nr]])
                        B.cp("dve" if hh == 0 else "act", bigT[:, hh * 8:(hh + 1) * 8, c0:c0 + nr], src_ap, B.bk(b2 + hh), ["bigT"])
                    tix = NT if kind == "samp" else tt_
                    routing(1, li, c0, nr, tix, wrt, wrk, rts[li], None, None)
                r2_done = 0
                for cb in range(2):
                    wg_, wgk_ = B.wget((16, 256, [(0, 256, w_sg[:, cb * 256:(cb + 1) * 256])]))
                    wu_, wuk_ = B.wget((16, 256, [(0, 256, w_su[:, cb * 256:(cb + 1) * 256])]))
                    for jj in range(2):
                        c = cb * 2 + jj
                        for _ in range(2):
                            if r2_done < len(own_tiles) and (r2_done <= c + 1 or c == 3):
                                li = r2_done
                                (c0_, nr_, src_, kind_, tt2) = own_tiles[li]
                                routing(2, li, c0_, nr_, NT if kind_ == "samp" else tt2, wrt, wrk, rts[li], None, None)
                                r2_done += 1
                        for (c0, n) in own_segs:
                            sx_, sxk, _ = sgs[c % 2]
                            b = B.bank()
                            for k in range(16):
                                B.mm(ps[:, b, 0:n], wg_[:, k, jj * P:(jj + 1) * P], bigT[:, k, c0:c0 + n], k == 0, k == 15, wgk_ + ["bigT"], B.bk(b))
                            B.act(sx_[:, 0:n], ps[:, b, 0:n], AF.Silu, B.bk(b), sxk)
                            b = B.bank()
                            for k in range(16):
                                B.mm(ps[:, b, 0:n], wu_[:, k, jj * P:(jj + 1) * P], bigT[:, k, c0:c0 + n], k == 0, k == 15, wuk_ + ["bigT"], B.bk(b))
                            B.tt("dve", actT[:, c, c0:c0 + n], ps[:, b, 0:n], sx_[:, 0:n], ALU.mult, B.bk(b) + sxk, actk)
                while r2_done < len(own_tiles):
                    li = r2_done
                    (c0_, nr_, src_, kind_, tt2) = own_tiles[li]
                    routing(2, li, c0_, nr_, NT if kind_ == "samp" else tt2, wrt, wrk, rts[li], None, None)
                    r2_done += 1
                for li, (c0, nr, src, kind, tt_) in enumerate(own_tiles):
                    hb, hbk, _ = hbf[li % 2]
                    if kind == "samp":
                        S.op("dve", lambda e, hb=hb: e.memset(hb, 0.0), [], hbk)
                    B.cp("act", fap(hb, 0, [[1, D]], parts=nr), hres[0:nr, li, :], [("hres", li)], hbk)
                    tix = NT if kind == "samp" else tt_
                    routing(3, li, c0, nr, tix, wrt, wrk, rts[li], hb, hbk)
                for nb in range(2):
                    wd_, wdk_ = B.wget((4, 1024, [(0, 1024, w_sd[:, nb * 1024:(nb + 1) * 1024])]))
                    for n2 in range(2):
                        n4 = nb * 2 + n2
                        for li, (c0, nr, src, kind, tt_) in enumerate(own_tiles):
                            b = B.bank()
                            for k in range(4):
                                B.mm(ps[0:nr, b, :], actT[:, k, c0:c0 + nr], wd_[:, k, n2 * 512:(n2 + 1) * 512], k == 0, k == 3, actk + wdk_, B.bk(b))
                            hr = hres[0:nr, li, n4 * 512:(n4 + 1) * 512]
                            B.stt(hr, hr, ALPHA, ps[0:nr, b, :], ALU.mult, ALU.add, [("hres", li)] + B.bk(b), [("hres", li)])
                for li, (c0, nr, src, kind, tt_) in enumerate(own_tiles):
                    r0 = TOK if kind == "samp" else tt_ * P
                    B.dma("sp", "base%d" % li, BASE[r0:r0 + nr, :], hres[0:nr, li, :], [("hres", li)], ["BASE"])

            def layer_norm(xap, xkeys, nr, tmp, out=None, out_keys=None):
                st, stk, _ = tmp
                for q in range(4):
                    S.op("dve", lambda e, q=q: e.bn_stats(out=fap(st, q * 6, [[1, 6]], parts=nr), in_=fap(xap, q * 512, [[1, 512]], parts=nr)), xkeys, stk)
                mv = fap(st, 24, [[1, 2]], parts=nr)
                S.op("dve", lambda e: e.bn_aggr(out=mv, in_=fap(st, 0, [[1, 24]], parts=nr)), stk, stk)
                rs = fap(st, 26, [[1, 1]], parts=nr)
                B.act(rs, fap(st, 25, [[1, 1]], parts=nr), AF.Sqrt, stk + ["epsT"], stk, bias=epsT[0:nr, :])
                S.op("dve", lambda e: e.reciprocal(out=rs, in_=rs), stk, stk)
                o = xap if out is None else out
                ok_ = xkeys if out is None else out_keys
                B.ts("dve", o, xap, fap(st, 24, [[1, 1]], parts=nr), ALU.subtract, xkeys + stk, ok_, s2=rs, op1=ALU.mult)
                B.tt("dve", o, o, lng[0:nr, :], ALU.mult, ok_ + ["lng"], ok_)
                B.tt("dve", o, o, lnb[0:nr, :], ALU.add, ok_ + ["lnb"], ok_)

            def routing(phase, li, c0, nr, tix, wrt, wrk, rt, hb, hbk):
                r, rk, _ = rt
                def rv(i, n=NE, parts=nr):
                    return fap(r, i * 64, [[1, n]], parts=parts)
                sc_ = rv(0)
                ch = rv(1)
                tmp = rv(2)
                sel = rv(3, parts=P)
                wd = rv(4)
                key = rv(5, parts=P)
                sm = fap(r, 6 * 64, [[1, 64]], parts=nr)
                m1 = fap(r, 6 * 64, [[1, 8]], parts=nr)
                m2 = fap(r, 6 * 64 + 8, [[1, 8]], parts=nr)
                gs = fap(r, 6 * 64 + 16, [[1, 8]], parts=nr)
                g8 = fap(r, 6 * 64 + 24, [[1, 8]], parts=nr)
                pen = fap(r, 6 * 64 + 32, [[1, 8]], parts=nr)
                c8 = fap(r, 6 * 64 + 40, [[1, 8]], parts=nr)
                ssum = fap(r, 6 * 64 + 48, [[1, 1]], parts=nr)
                ch3 = fap(r, 1 * 64, [[8, 8], [1, 8]], parts=nr)
                tmp3 = fap(r, 2 * 64, [[8, 8], [1, 8]], parts=nr)
                selb, selbk = rsel_t[:, li, :], [("rsel", li)]
                pos = rv(7, parts=P)
                d8f = fap(r, 6 * 64 + 56, [[1, 8]], parts=P)
                BIGC = float(2 * NSLOT)
                if phase == 1:
                    b = B.bank()
                    for k in range(16):
                        B.mm(ps[0:nr, b, 0:NE], bigT[:, k, c0:c0 + nr], wrt[:, k, :], k == 0, k == 15, ["bigT"] + wrk, B.bk(b))

                    B.act(sc_, ps[0:nr, b, 0:NE], AF.Sigmoid, B.bk(b), rk)
                    return
                if phase == 2:
                    B.tt("dve", ch, sc_, rbias[0:nr, :], ALU.add, rk + ["rbias"], rk)
                    B.red(m1, ch3, ALU.max, rk, rk)
                    B.tt("dve", tmp3, ch3, fap(r, 6 * 64, [[1, 8], [0, 8]], parts=nr), ALU.is_equal, rk, rk)
                    B.stt(tmp, tmp, -1e9, ch, ALU.mult, ALU.add, rk, rk)
                    B.red(m2, tmp3, ALU.max, rk, rk)
                    B.tt("dve", gs, m1, m2, ALU.add, rk, rk)
                    S.op("dve", lambda e: e.max(out=g8, in_=gs), rk, rk)
                    B.ts("dve", pen, gs, fap(r, 6 * 64 + 24 + 3, [[1, 1]], parts=nr), ALU.is_ge, rk, rk, s2=-1.0, op1=ALU.add)
                    B.ts("dve", pen, pen, 1e9, ALU.mult, rk, rk)
                    B.tt("dve", tmp3, ch3, fap(r, 6 * 64 + 32, [[1, 8], [0, 8]], parts=nr), ALU.add, rk, rk)
                    S.op("dve", lambda e: e.max(out=c8, in_=tmp), rk, rk)
                    if nr < P:
                        S.op("dve", lambda e: e.memset(sel, 0.0), rk, rk)
                    B.ts("dve", rv(3), tmp, fap(r, 6 * 64 + 40 + 7, [[1, 1]], parts=nr), ALU.is_ge, rk, rk)
                    B.stt(wd, rv(3), 1.0, sc_, ALU.mult, ALU.mult, rk, rk, accum_out=ssum)
                    S.op("dve", lambda e: e.reciprocal(out=ssum, in_=ssum), rk, rk)
                    B.ts("dve", wd, wd, ssum, ALU.mult, rk, rk, s2=2.5, op1=ALU.mult)
                    B.cp("dve", selb, sel, rk, selbk)
                    return
                bp = B.bank()
                B.mm(ps[:, bp, 0:NE], triU[:], selb, True, True, ["triU"] + selbk, B.bk(bp))
                B.tt("dve", pos, ps[:, bp, 0:NE], cbase[:], ALU.add, B.bk(bp) + ["cbase"], rk)
                bp2 = B.bank()
                B.mm(ps[:, bp2, 0:NE], ones[:], selb, True, True, ["ones"] + selbk, B.bk(bp2))
                B.tt("dve", cbase[:], cbase[:], ps[:, bp2, 0:NE], ALU.add, B.bk(bp2) + ["cbase"], ["cbase"])
                B.ts("dve", key, pos, float(CAP), ALU.is_lt, rk, rk)
                B.tt("dve", key, key, sel, ALU.mult, rk, rk)
                B.tt("dve", pos, pos, c16t[:, 64:128], ALU.add, rk + ["c16t"], rk)
                B.ts("dve", pos, pos, -1.0, ALU.mult, rk, rk, s2=BIGC, op1=ALU.add)
                B.tt("dve", key, key, pos, ALU.mult, rk, rk)
                S.op("dve", lambda e: e.max(out=d8f, in_=key), rk, rk)
                for j in range(8):
                    B.stt(rv(2, parts=nr), fap(r, 5 * 64, [[1, NE]], parts=nr), fap(r, 6 * 64 + 56 + j, [[1, 1]], parts=nr), wd,
                          ALU.is_equal, ALU.mult, rk, rk + [("w8", tix)], accum_out=w8all[0:nr, tix, j:j + 1])
                B.ts("dve", fap(r, 2 * 64, [[1, 8]], parts=nr), fap(r, 6 * 64 + 56, [[1, 8]], parts=nr), 0.0, ALU.is_gt, rk, rk)
                B.tt("dve", w8all[0:nr, tix, :], w8all[0:nr, tix, :], fap(r, 2 * 64, [[1, 8]], parts=nr), ALU.mult, rk + [("w8", tix)], [("w8", tix)])
                B.ts("dve", d8f, d8f, -1.0, ALU.mult, rk, rk, s2=BIGC, op1=ALU.add)
                B.cp("dve", d8all[:, tix, :], d8f, rk, [("d8", tix)])
                for j in range(8):
                    S.dma("pool", "scat%d" % (li % 2), lambda e, j=j: e.indirect_dma_start(
                        out=XG[:, :], out_offset=bass.IndirectOffsetOnAxis(ap=d8all[:, tix, j:j + 1], axis=0),
                        in_=hb, in_offset=None, bounds_check=B.bcreg(e), oob_is_err=False),
                        hbk + [("d8", tix), "XG"], [("XGs", tix, j)])


            def sample_attention(qT, qTk, soff):
                kn, knk, _ = B.af32(256)
                bq = B.bank()
                for kp in range(2):
                    B.tr(ps[0:NS, bq, kp * P:(kp + 1) * P], kT32[:, kp, P:P + NS], ident_f[:], ["kT32", "ident_f"], B.bk(bq))
                B.cp("dve", fap(kn, 0, [[1, 256]], parts=NS), ps[0:NS, bq, 0:256], B.bk(bq), knk)
                B.dma("sp", "sck", sck[:, 0:P - 1, :], ck[:, 1:P, :], [], ["sck"])
                B.dma("sp", "sck", sck[:, P - 1, :], fap(kn, 0, [[1, 256]], parts=NS), knk, ["sck"])
                B.dma("sp", "scv", scvo[:, 0:P - 1, :], cv[:, 1:P, :], [], ["scvo"])
                B.dma("sp", "scv", scvo[:, P - 1, :], v32[0:NS, 1, :], ["v32"], ["scvo"])
                Ke, Kek = fap(hres_bf, 0 * 4096, [[256, NS], [1, 256]]), [("hres", 0)]
                Ve, Vek = fap(hres_bf, 1 * 4096, [[256, NS], [1, 256]]), [("hres", 1)]
                stg = fap(hres, 2 * D, [[256, NS], [1, 256]])
                stgk = [("hres", 2), ("hres", 3)]
                B.dma("sp", "ske", stg, sck.rearrange("b k d -> k b d"), ["sck"], stgk)
                B.cp("act", Ke, stg, stgk, Kek)
                B.dma("sp", "ske", stg, scvo.rearrange("b k d -> k b d"), ["scvo"], stgk)
                B.cp("dve", Ve, stg, stgk, Vek)
                QM, QMk, _ = B.abf(64, [[16, 4], [1, 16]])
                KT2, KT2k, _ = B.abf(4 * P, [[P, 4], [1, P]])
                Ssb, Ssk = fap(hres, 2 * D, [[P, NS], [1, P]]), [("hres", 2)]
                for b_ in range(NS):
                    qin = fap(qT, soff + b_, [[0, 4], [640, 8], [0, 2]])
                    B.tt("dve", fap(QM, 0, [[16, 4], [2, 8], [1, 2]]), qin, fap(c16t, 0, [[16, 4], [2, 8], [1, 2]]), ALU.mult, qTk + ["c16t"], QMk)
                    bt = B.bank()
                    for kv in range(4):
                        for dup in range(2):
                            B.tr(psb[64 * dup:64 * dup + 64, bt, kv * P:(kv + 1) * P], Ke[:, b_, kv * 64:(kv + 1) * 64], ident[:], Kek + ["ident"], B.bk(bt))
                    B.cp("act", KT2, fap(psb, bt * 1024, [[P, 4], [1, P]]), B.bk(bt), KT2k)
                    bsc = B.bank()
                    for kv in range(4):
                        B.mm(ps[0:16, bsc, 0:P], QM[:, kv, :], KT2[:, kv, :], kv == 0, kv == 3, QMk + KT2k, B.bk(bsc))
                    B.cp("dve", fap(Ssb, b_ * P, [[1, P]], parts=16), ps[0:16, bsc, 0:P], B.bk(bsc), Ssk)
                sx, sxk, _ = B.af32(5 * NS)
                S16 = fap(Ssb, 0, [[P, NS], [1, P]], parts=16)
                mx = fap(sx, 0, [[1, NS]], parts=16)
                sm = fap(sx, NS, [[1, NS]], parts=16)
                tq = fap(sx, 2 * NS, [[1, NS]], parts=16)
                rv_ = fap(sx, 3 * NS, [[1, NS]], parts=16)
                B.red(mx, S16, ALU.max, Ssk, sxk)
                B.ts("dve", mx, mx, sinkcol[0:16, 0:1], ALU.max, sxk + ["sinkcol"], sxk)
                B.tt("dve", S16, S16, fap(sx, 0, [[1, NS], [0, P]], parts=16), ALU.subtract, Ssk + sxk, Ssk)
                B.act(S16, S16, AF.Exp, Ssk, Ssk, scale=0.125)
                B.red(sm, S16, ALU.add, Ssk, sxk)
                B.ts("dve", tq, mx, -1.0, ALU.mult, sxk, sxk, s2=sinkcol[0:16, 0:1], op1=ALU.add)
                B.act(tq, tq, AF.Exp, sxk, sxk, scale=0.125)
                B.tt("dve", rv_, sm, tq, ALU.add, sxk, sxk)
                S.op("dve", lambda e: e.reciprocal(out=rv_, in_=rv_), sxk, sxk)
                Pb, Pbk = fap(hres_bf, 4 * 4096, [[P, NS], [1, P]]), [("hres", 4)]
                B.tt("dve", fap(Pb, 0, [[P, NS], [1, P]], parts=16), S16, fap(sx, 3 * NS, [[1, NS], [0, P]], parts=16), ALU.mult, Ssk + sxk, Pbk)
                bt = B.bank()
                for b_ in range(NS):
                    B.tr(psb[:, bt, b_ * 16:(b_ + 1) * 16], fap(Pb, b_ * P, [[1, P]], parts=16), ident[0:16, 0:16], Pbk + ["ident"], B.bk(bt))
                PTs, PTsk, _ = B.abf(NS * 16)
                B.cp("act", PTs, psb[:, bt, 0:NS * 16], B.bk(bt), PTsk)
                As, Ask = fap(hres, 4 * D + 1024, [[64, NS], [1, 64]]), [("hres", 4)]
                tmpv, tmpvk, _ = B.af32(256)
                for b_ in range(NS):
                    bo = B.bank()
                    B.mm(ps[0:16, bo, 0:256], PTs[:, b_ * 16:(b_ + 1) * 16], Ve[:, b_, :], True, True, PTsk + Vek, B.bk(bo))
                    B.tt("dve", fap(tmpv, 0, [[64, 4], [1, 64]], parts=16), fap(ps, bo * 512, [[64, 4], [1, 64]], parts=16),
                         fap(c16t, 192, [[1, 4], [0, 64]], parts=16), ALU.mult, B.bk(bo) + ["c16t"], tmpvk)
                    B.red(fap(As, b_ * 64, [[1, 64]], parts=16), fap(tmpv, 0, [[1, 64], [64, 4]], parts=16), ALU.add, tmpvk, Ask)
                A2, A2k = fap(hres, 3 * D, [[P, NS], [1, P]]), [("hres", 3)]
                B.tt("dve", fap(A2, 0, [[P, NS], [64, 2], [1, 64]], parts=16), fap(As, 0, [[64, NS], [0, 2], [1, 64]], parts=16),
                     fap(c16t, 200, [[0, NS], [1, 2], [0, 64]], parts=16), ALU.mult, Ask + ["c16t"], A2k)
                bt2 = B.bank()
                for b_ in range(NS):
                    B.tr(ps[:, bt2, b_ * 16:(b_ + 1) * 16], fap(A2, b_ * P, [[1, P]], parts=16), ident_f[0:16, 0:16], A2k + ["ident_f"], B.bk(bt2))
                af_, afk, _ = B.af32(8 * NS, [[NS, 8], [1, NS]])
                B.red(af_, fap(ps, bt2 * 512, [[2, 8], [16, NS], [1, 2]]), ALU.add, B.bk(bt2), afk)
                B.cp("dve", fap(attnT, soff, [[640, 8], [1, NS]]), af_, afk, ["attnT"])


            S.planning = True
            phase_a()
            S.planning = False
            B.bank_rr = 0
            phase_a()
        S.barrier()

        if stop_after is None:
            pb = contextlib.ExitStack()
            with pb:
                def sbb(name, shape, dt):
                    return pb.enter_context(nc.sbuf_tensor(name, list(shape), dt))
                wg = [sbb("wg%d" % i, [P, 16, 512], BF16) for i in range(2)]
                wu = [sbb("wu%d" % i, [P, 16, 512], BF16) for i in range(2)]
                wdn = [sbb("wd%d" % i, [P, 4, D], BF16) for i in range(2)]
                xb_ = [sbb("xb%d" % i, [P, NBLK, D], BF16) for i in range(2)]
                xbT = [sbb("xbT%d" % i, [P, 16, CAP], BF16) for i in range(2)]
                acT = [sbb("acT%d" % i, [P, 4, CAP], BF16) for i in range(2)]
                sgt = [sbb("sgt%d" % i, [P, CAP], F32) for i in range(2)]
                yo = [sbb("yo%d" % i, [P, D], BF16) for i in range(2)]

                def load_expert(e):
                    i = e % 2
                    for k0 in range(0, 16, 8):
                        B.dma("pool", "eg%d" % i, wg[i][:, k0:k0 + 8, :], w_eg[e, k0 * P:(k0 + 8) * P, :].rearrange("(k p) n -> p k n", p=P), [], [("wg", i)])
                        B.dma("pool", "eu%d" % i, wu[i][:, k0:k0 + 8, :], w_eu[e, k0 * P:(k0 + 8) * P, :].rearrange("(k p) n -> p k n", p=P), [], [("wu", i)])
                    for k in range(0, 4, 2):
                        B.dma("pool", "ed%d" % i, wdn[i][:, k:k + 2, :], w_ed[e, k * P:(k + 2) * P, :].rearrange("(k p) n -> p k n", p=P), [], [("wdn", i)])
                    B.dma("sp", "xb%d" % i, xb_[i][:], XG[e * CAP:(e + 1) * CAP, :].rearrange("(b p) d -> p b d", p=P), ["XG"] + [("XGs", t, j) for t in range(17) for j in range(8)], [("xb", i)])

                load_expert(0)
                yi = 0
                for e in range(NE):
                    i = e % 2
                    if e + 1 < NE:
                        load_expert(e + 1)
                    for blk in range(NBLK):
                        for h2 in range(2):
                            bt = B.bank()
                            for k8 in range(8):
                                k = h2 * 8 + k8
                                B.tr(psb[:, bt, k8 * P:(k8 + 1) * P], xb_[i][:, blk, k * P:(k + 1) * P], ident[:], [("xb", i), "ident"], B.bk(bt))
                            B.cp("act" if (blk * 2 + h2) % 2 == 0 else "dve", xbT[i][:, h2 * 8:(h2 + 1) * 8, blk * P:(blk + 1) * P],
                                 fap(psb, bt * 1024, [[P, 8], [1, P]]), B.bk(bt), [("xbT", i)])
                    for c in range(4):
                        bg = B.bank()
                        for k in range(16):
                            B.mm(ps[:, bg, 0:CAP], wg[i][:, k, c * P:(c + 1) * P], xbT[i][:, k, :], k == 0, k == 15, [("wg", i), ("xbT", i)], B.bk(bg))
                        B.act(sgt[c % 2][:], ps[:, bg, 0:CAP], AF.Silu, B.bk(bg), [("sgt", c % 2)])
                        bu = B.bank()
                        for k in range(16):
                            B.mm(ps[:, bu, 0:CAP], wu[i][:, k, c * P:(c + 1) * P], xbT[i][:, k, :], k == 0, k == 15, [("wu", i), ("xbT", i)], B.bk(bu))
                        B.tt("dve", acT[i][:, c, :], ps[:, bu, 0:CAP], sgt[c % 2][:], ALU.mult, B.bk(bu) + [("sgt", c % 2)], [("acT", i)])
                    for blk in range(NBLK):
                        y_ = yo[yi % 2]
                        yk = [("yo", yi % 2)]
                        for n4 in range(4):
                            b = B.bank()
                            for k in range(4):
                                B.mm(ps[:, b, :], acT[i][:, k, blk * P:(blk + 1) * P], wdn[i][:, k, n4 * 512:(n4 + 1) * 512], k == 0, k == 3, [("acT", i), ("wdn", i)], B.bk(b))
                            B.cp("act" if n4 % 2 == 0 else "dve", y_[:, n4 * 512:(n4 + 1) * 512], ps[:, b, :], B.bk(b), yk)
                        r0 = e * CAP + blk * P
                        B.dma("sp", "yst%d" % (yi % 2), Y[r0:r0 + P, :], y_[:], yk, ["Y"])
                        yi += 1
            S.barrier()

            pc = contextlib.ExitStack()
            with pc:
                def sbc(name, shape, dt):
                    return pc.enter_context(nc.sbuf_tensor(name, list(shape), dt))
                acc = [sbc("acc%d" % i, [P, D], F32) for i in range(2)]
                yg = [sbc("yg%d" % i, [P, D], BF16) for i in range(6)]
                lnt2 = [sbc("lnt2%d" % i, [P, 32], F32) for i in range(2)]
                B.dma("sp", "cst2", lng[:], fap_dram_bcast(ln2_g, D), ["lng"], ["lng"])
                B.dma("sp", "cst2", lnb[:], fap_dram_bcast(ln2_b, D), ["lnb"], ["lnb"])
                dgt = [sbc("dg%d" % i, [P, 16, P], BF16) for i in range(2)]
                wsm = [sbc("wsm%d" % i, [P, 32], F32) for i in range(2)]
                wbf = [sbc("wbf%d" % i, [P, 8], BF16) for i in range(2)]
                gi = [0]

                def f_ctx(tix):
                    nr = P if tix < NT else NS
                    r0 = tix * P if tix < NT else TOK
                    par = tix % 2
                    bks = [4 * par + n4 for n4 in range(4)]
                    return nr, r0, acc[par], [("acc", par)], dgt[par], [("dg", par)], wsm[par], [("wsm", par)], wbf[par], bks

                def f_prep(tix):
                    nr, r0, a, ak, dg, dgk, ws, wsk, wb16, bks = f_ctx(tix)
                    B.dma("sp", "bl%d" % (tix % 2), a[0:nr, :], BASE[r0:r0 + nr, :], ["BASE"], ak)
                    B.cp("dve", wb16[0:nr, :], w8all[0:nr, tix, :], [("w8", tix)], wsk)
                    B.cp("dve", ws[0:nr, 0:8], wb16[0:nr, :], wsk, wsk)
                    B.tt("dve", ws[0:nr, 8:16], w8all[0:nr, tix, :], ws[0:nr, 0:8], ALU.subtract, wsk + [("w8", tix)], wsk)
                    for j in range(8):
                        B.act(dg[0:nr, 2 * j, 0:nr], ident_f[0:nr, 0:nr], AF.Copy, wsk + ["ident_f"], dgk, scale=ws[0:nr, j:j + 1])
                        B.act(dg[0:nr, 2 * j + 1, 0:nr], ident_f[0:nr, 0:nr], AF.Copy, wsk + ["ident_f"], dgk, scale=ws[0:nr, 8 + j:9 + j])
                    for j in range(8):
                        g_ = yg[gi[0] % 6]
                        gk = [("yg", gi[0] % 6)]
                        S.dma("pool", "yg%d" % (gi[0] % 6), lambda e, g_=g_, tix=tix, j=j: e.indirect_dma_start(
                            out=g_[:], out_offset=None, in_=Y[:, :],
                            in_offset=bass.IndirectOffsetOnAxis(ap=d8all[:, tix, j:j + 1], axis=0),
                            bounds_check=B.bcreg(e), oob_is_err=False), ["Y", ("d8", tix)], gk)
                        for part in range(2):
                            for n4 in range(4):
                                B.mm(ps[0:nr, bks[n4], :], dg[0:nr, 2 * j + part, 0:nr], g_[0:nr, n4 * 512:(n4 + 1) * 512],
                                     j == 0 and part == 0, j == 7 and part == 1, gk + dgk, B.bk(bks[n4]))
                        gi[0] += 1

                def f_finish(tix):
                    nr, r0, a, ak, dg, dgk, ws, wsk, wb16, bks = f_ctx(tix)
                    for n4 in range(4):
                        B.tt("dve", a[0:nr, n4 * 512:(n4 + 1) * 512], a[0:nr, n4 * 512:(n4 + 1) * 512], ps[0:nr, bks[n4], :], ALU.add,
                             ak + B.bk(bks[n4]), ak)
                    layer_norm(a[0:nr, :], ak, nr, (lnt2[tix % 2][:], [("lnt2", tix % 2)], None))
                    if tix < NT:
                        B.dma("sp", "yout%d" % (tix % 2), yp[r0:r0 + nr, :], a[0:nr, :], ak, ["yp"])
                    else:
                        B.dma("sp", "yout%d" % (tix % 2), ys[:, :], a[0:nr, :], ak, ["ys"])

                f_prep(0)
                for tix in range(17):
                    if tix + 1 < 17:
                        f_prep(tix + 1)
                    f_finish(tix)
                if debug:
                    B.dma("sp", "dbg1_" + str(DBGC.next()), dbg["cnt"][:, :], cbase[:], ["cbase"], [])
                    dd = sbc("ddbg", [P, 17, 8], F32)
                    B.cp("dve", dd[:], d8all[:], [("d8", t) for t in range(17)], ["ddbg"])
                    B.dma("sp", "dbg2_" + str(DBGC.next()), dbg["d8"][:, :, :], dd[:], ["ddbg"], [])
                    B.dma("sp", "dbg3_" + str(DBGC.next()), dbg["w8"][:, :, :], w8all[:], [("w8", t) for t in range(17)], [])

        S.finalize(es)
        with nc.Block() as block:
            S.run_block(block)
    return nc


def _consts(core):
    hh = core % 2
    half = 8
    inv_freq = np.power(np.float32(500000.0), -np.arange(half, dtype=np.float32) * np.float32(2.0 / 16)).astype(np.float32)
    pos = np.zeros(2192, np.float32)
    pos[0:P] = hh * TOK - P + np.arange(P)
    pos[P:P + TOK] = hh * TOK + np.arange(TOK)
    pos[P + TOK:] = PAST
    pos = np.maximum(pos, 0).astype(np.float32)
    ang = (pos[:, None] * inv_freq[None, :]).astype(np.float32)
    cosv = np.cos(ang).astype(np.float32)
    sinv = np.sin(ang).astype(np.float32)
    cosT = np.ones((P, 2192), np.float32)
    sinT = np.zeros((P, 2192), np.float32)
    for p in range(P):
        d = p % 64
        if d < 16:
            cosT[p] = cosv[:, d % 8]
            sinT[p] = sinv[:, d % 8]
    ident = np.eye(P, dtype=np.float32)
    R = np.zeros((P, P), np.float32)
    for m in range(P):
        d = m % 64
        if d < 8:
            R[m, m + 8] = -1.0
        elif d < 16:
            R[m, m - 8] = 1.0
    rotT = R.T.copy()
    triU = np.triu(np.ones((P, P), np.float32), 1)
    ones = np.ones((P, P), np.float32)
    cst = np.concatenate([ident, rotT, triU, ones, np.zeros((P, 512), np.float32)], axis=1)
    a = np.arange(P)[:, None]
    c = np.arange(2 * P)[None, :]
    valid = (c > a) & (c <= a + P)
    mask_gen = np.where(valid, 0.0, NEG).astype(np.float32)
    if hh == 0:
        mask_first = np.where(valid & (c >= P), 0.0, NEG).astype(np.float32)
    else:
        mask_first = mask_gen
    msk = np.concatenate([mask_gen, mask_first], axis=1)
    c16 = np.zeros((P, 232), np.float32)
    for p in range(P):
        for kv in range(4):
            for h in range(16):
                c16[p, kv * 16 + h] = 1.0 if ((p // 64) == (h % 2) and (h // 4) == kv) else 0.0
    c16[:, 64:128] = (np.arange(NE) * CAP)[None, :]
    c16[:, 128:192] = np.arange(NE)[None, :]
    for h in range(16):
        for kv in range(4):
            c16[h, 192 + kv] = 1.0 if h // 4 == kv else 0.0
        for par in range(2):
            c16[h, 200 + par] = 1.0 if h % 2 == par else 0.0
    return dict(cosT=cosT, sinT=sinT, cst=cst, msk=msk, c16=c16)


_NC_CACHE = {}


def kernel(x_prompt, x_sample, cache_k, cache_v, state_conv, w_in, attn_sinks, conv_w,
           w_attn_out, w_conv_out, w_o, ln1_g, ln1_b, w_router, router_bias,
           w_exp_gate, w_exp_up, w_exp_down, w_sh_gate, w_sh_up, w_sh_down, ln2_g, ln2_b,
           _stop_after=None, _debug=False, _cores=None):
    f = lambda a: np.ascontiguousarray(np.asarray(a, dtype=np.float32))
    x_prompt, x_sample = f(x_prompt), f(x_sample)
    key = (_stop_after, _debug)
    if key not in _NC_CACHE:
        _NC_CACHE[key] = build_program(_stop_after, _debug)
    nc = _NC_CACHE[key]
    shared = dict(
        w_in=f(w_in[0]), sinks=f(attn_sinks), conv_w=f(conv_w[0]), w_ao=f(w_attn_out[0]), w_co=f(w_conv_out[0]),
        w_o=f(w_o[0]), ln1_g=f(ln1_g), ln1_b=f(ln1_b), w_r=f(w_router[0]), r_bias=f(router_bias),
        w_eg=f(w_exp_gate[0]), w_eu=f(w_exp_up[0]), w_ed=f(w_exp_down[0]), w_sg=f(w_sh_gate[0]),
        w_su=f(w_sh_up[0]), w_sd=f(w_sh_down[0]), ln2_g=f(ln2_g), ln2_b=f(ln2_b))
    if _stop_after is not None:
        for k_ in ("w_eg", "w_eu", "w_ed"):
            shared.pop(k_)
    in_maps = []
    cores = list(range(NCORES)) if _cores is None else list(_cores)
    for c in cores:
        n, hh = c // 2, c % 2
        xp = np.zeros((TOK + P, D), np.float32)
        if hh == 1:
            xp[0:P] = x_prompt[n, TOK - P:TOK]
        xp[P:] = x_prompt[n, hh * TOK:(hh + 1) * TOK]
        m = dict(shared)
        m.update(_consts(c))
        m.update(xp=xp, xs=f(x_sample[c * NS:(c + 1) * NS, 0, :]),
                 ck=f(cache_k[0, c * NS:(c + 1) * NS].reshape(NS, P, 256)),
                 cv=f(cache_v[0, c * NS:(c + 1) * NS].reshape(NS, P, 256)),
                 sc=f(state_conv[0, c * NS:(c + 1) * NS]))
        in_maps.append(m)
    res = run_bass_kernel_spmd(nc, in_maps, core_ids=list(range(len(cores))))
    R = res.results
    if _debug or _stop_after is not None:
        return R
    y_p = np.zeros((4, SEQ, D), np.float32)
    y_s = np.zeros((128, 1, D), np.float32)
    pk = np.zeros((1, 4, P, 4, 64), np.float32)
    pv = np.zeros((1, 4, P, 4, 64), np.float32)
    pc_ = np.zeros((1, 4, 2, 1024), np.float32)
    sk = np.zeros((1, 128, P, 4, 64), np.float32)
    sv = np.zeros((1, 128, P, 4, 64), np.float32)
    ssc_ = np.zeros((1, 128, 2, 1024), np.float32)
    for c in range(NCORES):
        n, hh = c // 2, c % 2
        y_p[n, hh * TOK:(hh + 1) * TOK] = R[c]["yp"]
        y_s[c * NS:(c + 1) * NS, 0] = R[c]["ys"]
        if hh == 1:
            pk[0, n] = R[c]["pck"].reshape(P, 4, 64)
            pv[0, n] = R[c]["pcv"].reshape(P, 4, 64)
            pc_[0, n] = R[c]["psc"]
        sk[0, c * NS:(c + 1) * NS] = R[c]["sck"].reshape(NS, P, 4, 64)
        sv[0, c * NS:(c + 1) * NS] = R[c]["scvo"].reshape(NS, P, 4, 64)
        ssc_[0, c * NS:(c + 1) * NS] = R[c]["ssc"]
    return (y_p, y_s, pk, pv, pc_, sk, sv, ssc_)
```

```python
import contextlib
import numpy as np
import concourse.bass as bass
import concourse.mybir as mybir
from concourse.bass_utils import run_bass_kernel_spmd

F32 = mybir.dt.float32
BF16 = mybir.dt.bfloat16
I32 = mybir.dt.int32
AF = mybir.ActivationFunctionType
ALU = mybir.AluOpType
AX = mybir.AxisListType

ENGS = ("pe", "act", "dve", "pool", "sp")


class _Ctr:
    def __init__(self):
        self.n = 0

    def next(self):
        self.n += 1
        return self.n


DBGC = _Ctr()

P = 128
D = 2048
NCORES = 8
SEQ = 4096
TOK = 2048
NS = 16
NT = 16
GT = 4
NG = NT // GT
NE = 64
CAP = 384
NBLK = CAP // P
NSLOT = NE * CAP
ALPHA = 2.0 ** 0.25
LN_EPS = 1e-5
PAST = 16384
OQ, OK_, OV, OB, OC, OH, OGA, OGC = 0, 1024, 1280, 1536, 2560, 3584, 4608, 6656
NEG = -30000.0


class Op:
    __slots__ = ("eng", "fn", "deps", "is_dma", "chan", "idx", "signal", "semval", "gidx")

    def __init__(self, eng, fn, is_dma, chan):
        self.eng = eng
        self.fn = fn
        self.deps = {}
        self.is_dma = is_dma
        self.chan = chan
        self.signal = False
        self.semval = None


def _slot(op):
    return ("ch", op.chan) if op.is_dma else ("eng", op.eng)


class Sched:
    def __init__(self, nc):
        self.nc = nc
        self.ops = {e: [] for e in ENGS}
        self.last_writer = {}
        self.readers = {}
        self.all_ops = []
        self.planning = False

    def _add(self, eng, fn, reads, writes, is_dma=False, chan=None):
        if self.planning:
            return None
        import os
        mx = int(os.environ.get("KMAXOPS", "0"))
        if mx and len(self.all_ops) >= mx:
            return None
        psr = [k for k in reads if isinstance(k, tuple) and k[0] == "ps"]
        if psr:
            writes = list(writes) + psr
        op = Op(eng, fn, is_dma, chan)
        deps = {}

        def add(d):
            if d is op:
                return
            s = _slot(d)
            o = deps.get(s)
            if o is None or d.gidx > o.gidx:
                deps[s] = d

        for k in reads:
            w = self.last_writer.get(k)
            if w is not None:
                add(w)
        for k in writes:
            w = self.last_writer.get(k)
            if w is not None:
                add(w)
            for r in self.readers.get(k, {}).values():
                add(r)
        op.deps = deps
        op.gidx = len(self.all_ops)
        for k in reads:
            self.readers.setdefault(k, {})[_slot(op)] = op
        for k in writes:
            self.last_writer[k] = op
            self.readers[k] = {}
        self.ops[eng].append(op)
        self.all_ops.append(op)
        return op

    def op(self, eng, fn, reads=(), writes=()):
        return self._add(eng, fn, reads, writes)

    def dma(self, eng, chan, fn, reads=(), writes=()):
        return self._add(eng, fn, reads, writes, is_dma=True, chan=chan)

    def barrier(self):
        if self.planning:
            return
        lasts = {}
        for op in self.all_ops:
            if op.fn is not None:
                lasts[_slot(op)] = op
        for e in ENGS:
            op = Op(e, None, False, None)
            op.deps = {s: d for s, d in lasts.items()}
            op.gidx = len(self.all_ops)
            self.ops[e].append(op)
            self.all_ops.append(op)

    def finalize(self, es):
        nc = self.nc
        for op in self.all_ops:
            for d in op.deps.values():
                if d.is_dma:
                    continue
                if d.eng == "pe" and op.eng == "pe" and not op.is_dma:
                    continue
                d.signal = True
        self.sem = {e: es.enter_context(nc.semaphore("sem_" + e)) for e in ENGS}
        chans = []
        for op in self.all_ops:
            if op.is_dma and op.chan not in chans:
                chans.append(op.chan)
        self.chsem = {c: es.enter_context(nc.semaphore("ch_" + str(c))) for c in chans}
        cnt = {e: 0 for e in ENGS}
        chcnt = {c: 0 for c in chans}
        for op in self.all_ops:
            if op.is_dma:
                chcnt[op.chan] += 16
                op.semval = chcnt[op.chan]
            elif op.signal:
                cnt[op.eng] += 1
                op.semval = cnt[op.eng]
        for op in self.all_ops:
            if op.is_dma and str(op.chan).startswith("cst"):
                op.semval = chcnt[op.chan]
        self.chfinal = chcnt

    def emit(self, ename, eng):
        seen = {}
        for op in self.ops[ename]:
            for s, d in op.deps.items():
                if (not d.is_dma) and d.eng == "pe" and op.eng == "pe" and not op.is_dma:
                    continue
                v = d.semval
                if v is None:
                    continue
                if seen.get(s, 0) >= v:
                    continue
                sem = self.chsem[s[1]] if s[0] == "ch" else self.sem[s[1]]
                eng.wait_ge(sem, v)
                seen[s] = v
            if op.fn is None:
                continue
            inst = op.fn(eng)
            if op.is_dma:
                inst.then_inc(self.chsem[op.chan], 16)
            elif op.signal:
                inst.then_inc(self.sem[op.eng], 1)

    def run_block(self, block):
        S = self

        def mk(ename):
            def body(eng):
                S.emit(ename, eng)
                if ename == "sp":
                    for c, v in S.chfinal.items():
                        if v > 0:
                            eng.wait_ge(S.chsem[c], v)
            return body

        block.tensor(mk("pe"))
        block.scalar(mk("act"))
        block.vector(mk("dve"))
        block.gpsimd(mk("pool"))
        block.sync(mk("sp"))


def fap(t, offset, dims, parts=None, p0=0):
    base = t if isinstance(t, bass.AP) else t[:]
    pst = base.ap[0][0]
    npart = base.ap[0][1] if parts is None else parts
    return bass.AP(tensor=base.tensor, offset=base.offset + p0 * pst + offset,
                   ap=[[pst, npart]] + [list(d) for d in dims])


class Builder:
    def __init__(self, stop_after=None, debug=False):
        self.stop_after = stop_after
        self.debug = debug
        self.nc = bass.Bass("TRN2", target_bir_lowering=False)
        self.S = Sched(self.nc)
        self.bank_rr = 0
        self.wplan = []
        self.wpos = 0
        self.tmp_rr = {}

    def bcreg(self, e):
        if getattr(self, "_bcreg", None) is None:
            self._bcreg = e.to_reg(NSLOT - 1)
        return self._bcreg

    def din(self, name, shape, dt=F32):
        return self.nc.dram_tensor(name, list(shape), dt, kind="ExternalInput").ap()

    def dout(self, name, shape, dt=F32):
        return self.nc.dram_tensor(name, list(shape), dt, kind="ExternalOutput").ap()

    def dscr(self, name, shape, dt=F32):
        return self.nc.dram_tensor(name, list(shape), dt, kind="Internal").ap()

    def bank(self, n=1):
        if n == 2 and self.bank_rr % 2 == 1:
            self.bank_rr += 1
        b = self.bank_rr % 6
        self.bank_rr += n
        return b

    def bk(self, b, n=1):
        return [("ps", b + i) for i in range(n)]

    def mm(self, out, lhsT, rhs, start, stop, reads, writes):
        self.S.op("pe", lambda e: e.matmul(out, lhsT=lhsT, rhs=rhs, start=start, stop=stop), reads, writes)

    def tr(self, out, in_, ident, reads, writes):
        self.S.op("pe", lambda e: e.transpose(out=out, in_=in_, identity=ident), reads, writes)

    def act(self, out, in_, func, reads, writes, scale=None, bias=None, accum_out=None):
        kw = {}
        if scale is not None:
            kw["scale"] = scale
        if bias is not None:
            kw["bias"] = bias
        if accum_out is not None:
            kw["accum_out"] = accum_out
        self.S.op("act", lambda e: e.activation(out=out, in_=in_, func=func, **kw), reads, writes)

    def tt(self, eng, out, in0, in1, op, reads, writes):
        self.S.op(eng, lambda e: e.tensor_tensor(out=out, in0=in0, in1=in1, op=op), reads, writes)

    def ts(self, eng, out, in0, s1, op0, reads, writes, s2=None, op1=None, accum_out=None):
        kw = {}
        if op1 is not None:
            kw["op1"] = op1
        if accum_out is not None:
            kw["accum_out"] = accum_out
        self.S.op(eng, lambda e: e.tensor_scalar(out=out, in0=in0, scalar1=s1, scalar2=s2, op0=op0, **kw), reads, writes)

    def stt(self, out, in0, scalar, in1, op0, op1, reads, writes, accum_out=None):
        kw = {}
        if accum_out is not None:
            kw["accum_out"] = accum_out
        self.S.op("dve", lambda e: e.scalar_tensor_tensor(out=out, in0=in0, scalar=scalar, in1=in1, op0=op0, op1=op1, **kw), reads, writes)

    def red(self, out, in_, op, reads, writes, axis=AX.X):
        self.S.op("dve", lambda e: e.tensor_reduce(out=out, in_=in_, axis=axis, op=op), reads, writes)

    def cp(self, eng, out, in_, reads, writes):
        if eng == "act":
            self.S.op("act", lambda e: e.activation(out=out, in_=in_, func=AF.Copy), reads, writes)
        else:
            self.S.op(eng, lambda e: e.tensor_copy(out=out, in_=in_), reads, writes)

    def dma(self, eng, chan, out, in_, reads, writes, **kw):
        return self.S.dma(eng, chan, lambda e: e.dma_start(out=out, in_=in_, **kw), reads, writes)

    def wget(self, spec):
        S = self.S
        i = self.wpos
        self.wpos += 1
        if S.planning:
            self.wplan.append(spec)
        slot = i % self.NB
        nk, ncols, parts = spec
        while self.wissued < min(len(self.wplan), i + self.NB - 2):
            self._wissue(self.wissued)
            self.wissued += 1
        ap = fap(self.wring, slot * self.WSLOT, [[ncols, nk], [1, ncols]])
        return ap, [("w", slot)]

    def _wissue(self, j):
        if self.S.planning:
            return
        nk, ncols, parts = self.wplan[j]
        slot = j % self.NB
        for (c0, n, src) in parts:
            dst = fap(self.wring, slot * self.WSLOT + c0, [[ncols, nk], [1, n]])
            self.dma("pool", "w%d" % slot, dst, src.rearrange("(k p) n -> p k n", p=P), [], [("w", slot)])

    def areset(self):
        self.apos = 0

    def aalloc(self, nbytes):
        nbytes = (nbytes + 63) // 64 * 64
        off = self.apos
        self.apos += nbytes
        assert self.apos <= self.ARENA_BYTES, ("arena overflow", self.apos)
        keys = [("ar", b) for b in range(off // 2048, (off + nbytes - 1) // 2048 + 1)]
        return off, keys

    def af32(self, n, dims=None):
        off, keys = self.aalloc(n * 4)
        ap = fap(self.arena, off // 4, dims if dims is not None else [[1, n]])
        return ap, keys, off // 4

    def abf(self, n, dims=None):
        off, keys = self.aalloc(n * 2)
        ap = fap(self.arena_bf, off // 2, dims if dims is not None else [[1, n]])
        return ap, keys, off // 2


def build_program(stop_after=None, debug=False):
    B = Builder(stop_after, debug)
    nc = B.nc
    S = B.S

    xp = B.din("xp", [TOK + P, D])
    xs = B.din("xs", [NS, D])
    ck = B.din("ck", [NS, P, 256])
    cv = B.din("cv", [NS, P, 256])
    scv = B.din("sc", [NS, 2, 1024])
    w_in = B.din("w_in", [D, 8704])
    sinks = B.din("sinks", [1, 16])
    conv_w = B.din("conv_w", [3, 1024])
    w_ao = B.din("w_ao", [1024, D])
    w_co = B.din("w_co", [1024, D])
    w_o = B.din("w_o", [D, D])
    ln1_g = B.din("ln1_g", [1, D])
    ln1_b = B.din("ln1_b", [1, D])
    w_r = B.din("w_r", [D, NE])
    r_bias = B.din("r_bias", [1, NE])
    if stop_after is None:
        w_eg = B.din("w_eg", [NE, D, 512])
        w_eu = B.din("w_eu", [NE, D, 512])
        w_ed = B.din("w_ed", [NE, 512, D])
    w_sg = B.din("w_sg", [D, 512])
    w_su = B.din("w_su", [D, 512])
    w_sd = B.din("w_sd", [512, D])
    ln2_g = B.din("ln2_g", [1, D])
    ln2_b = B.din("ln2_b", [1, D])
    cosd = B.din("cosT", [P, 2192])
    sind = B.din("sinT", [P, 2192])
    cst = B.din("cst", [P, 1024])
    mskd = B.din("msk", [P, 512])
    c16 = B.din("c16", [P, 64 + 64 + 64 + 8 + 32])

    yp = B.dout("yp", [TOK, D])
    ys = B.dout("ys", [NS, D])
    pck = B.dout("pck", [P, 256])
    pcv = B.dout("pcv", [P, 256])
    psc = B.dout("psc", [2, 1024])
    sck = B.dout("sck", [NS, P, 256])
    scvo = B.dout("scvo", [NS, P, 256])
    ssc = B.dout("ssc", [NS, 2, 1024])
    if debug:
        dbg = {k: B.dout("dbg_" + k, shp) for k, shp in [("h", [TOK + NS, D]), ("cnt", [P, NE]), ("attn", [P, 8, 640]), ("conv", [P, 8, 640]), ("d8", [P, 17, 8]), ("w8", [P, 17, 8])]}

    XG = B.dscr("XG", [NSLOT + P, D], BF16)
    Y = B.dscr("Y", [NSLOT, D], BF16)
    BASE = B.dscr("BASE", [TOK + P, D], F32)

    es = contextlib.ExitStack()
    with es:
        def sb(name, shape, dt):
            return es.enter_context(nc.sbuf_tensor(name, list(shape), dt))

        ps = es.enter_context(nc.psum_tensor("ps", [P, 8, 512], F32))
        psb = ps.bitcast(BF16)

        ident_f = sb("ident_f", [P, P], F32)
        ident = sb("ident", [P, P], BF16)
        rotT = sb("rotT", [P, P], BF16)
        triU = sb("triU", [P, P], BF16)
        ones = sb("ones", [P, P], BF16)
        msk = sb("mskt", [P, 512], F32)
        c16t = sb("c16t", [P, 232], F32)
        sink8 = sb("sink8", [P, 16], F32)
        sinkraw = sb("sinkraw", [P, 16], F32)
        rbias = sb("rbias", [P, NE], F32)
        convw = sb("convw", [P, 8, 3], F32)
        lng = sb("lng", [P, D], F32)
        lnb = sb("lnb", [P, D], F32)
        d8all = sb("d8all", [P, 17, 8], I32)
        w8all = sb("w8all", [P, 17, 8], F32)
        cbase = sb("cbase", [P, NE], F32)
        epsT = sb("epsT", [P, 1], F32)
        sinkcol = sb("sinkcol", [P, 1], F32)
        rsel_t = sb("rsel", [P, 5, NE], BF16)

        def load_consts():
            B.dma("sp", "cst", ident_f[:], cst[:, 0:128], [], ["ident_f"])
            B.dma("pool", "cstp", ident[:], cst[:, 0:128], [], ["ident"])
            B.dma("pool", "cstp", rotT[:], cst[:, 128:256], [], ["rotT"])
            B.dma("pool", "cstp", triU[:], cst[:, 256:384], [], ["triU"])
            B.dma("pool", "cstp", ones[:], cst[:, 384:512], [], ["ones"])
            B.dma("sp", "cst", msk[:], mskd[:, :], [], ["msk"])
            B.dma("sp", "cst", c16t[:], c16[:, :], [], ["c16t"])
            B.dma("sp", "cst", sinkraw[:], fap_dram_bcast(sinks, 16), [], ["sinkraw"])
            B.dma("sp", "cst", rbias[:], fap_dram_bcast(r_bias, NE), [], ["rbias"])
            for j in range(3):
                B.dma("sp", "cst", convw[:, :, j], conv_w[j, :].rearrange("(c p) -> p c", p=P), [], ["convw%d" % j], allow_slow_non_contiguous=True)
            B.ts("dve", fap(sink8, 0, [[4, 4], [2, 2], [1, 2]]), fap(sinkraw, 0, [[4, 4], [1, 2], [2, 2]]), 8.0, ALU.mult, ["sinkraw"], ["sink8"])
            S.op("dve", lambda e: e.memset(cbase[:], 0.0), [], ["cbase"])
            S.op("dve", lambda e: e.memset(w8all[:], 0.0), [], [("w8", t) for t in range(17)])
            S.op("dve", lambda e: e.memset(epsT[:], LN_EPS), [], ["epsT"])
            B.dma("sp", "cst", sinkcol[0:16, :], sinks.rearrange("a h -> h a"), [], ["sinkcol"], allow_slow_non_contiguous=True)
            B.ts("dve", sinkcol[0:16, :], sinkcol[0:16, :], 8.0, ALU.mult, ["sinkcol"], ["sinkcol"])

        def fap_dram_bcast(src, n):
            return bass.AP(tensor=src.tensor, offset=src.offset, ap=[[0, P], [1, n]])

        load_consts()

        pa = contextlib.ExitStack()
        with pa:
            def sba(name, shape, dt):
                return pa.enter_context(nc.sbuf_tensor(name, list(shape), dt))

            NCMAX = 640
            bigT = sba("bigT", [P, 16, NCMAX], BF16)
            attnT = sba("attnT", [P, 8, NCMAX], BF16)
            convT = sba("convT", [P, 8, NCMAX], BF16)
            kTl = sba("kTl", [P, 4, P + NCMAX], BF16)
            kT32 = sba("kT32", [P, 2, 144], F32)
            vl = sba("vl", [P, 6, 256], BF16)
            v32 = sba("v32", [P, 2, 256], F32)
            cosl = sba("cosl", [P, NCMAX], F32)
            sinl = sba("sinl", [P, NCMAX], F32)
            uprev = sba("uprev", [P, 8, 2], F32)
            hres = sba("hres", [P, 5, D], F32)
            B.NB = 6
            B.WSLOT = 4096
            B.wring = sba("wring", [P, B.NB * B.WSLOT], BF16)
            B.ARENA_BYTES = 38 * 1024
            B.arena = sba("arena", [P, B.ARENA_BYTES // 4], F32)
            B.arena_bf = B.arena.bitcast(BF16)

            hres_bf = hres.bitcast(BF16)
            def zero_fill_xg():
                zt = hres_bf[:, 4, 0:D]
                S.op("dve", lambda e: e.memset(zt, 0.0), [], [("hres", 4)])
                nrow_total = NSLOT + P
                r = 0
                while r < nrow_total:
                    n = min(4 * P, nrow_total - r)
                    B.dma("act", "zf", XG[r:r + n, :].rearrange("(a p) d -> p a d", p=P), fap(hres_bf, 4 * 4096, [[0, n // P], [1, D]]), [("hres", 4)], ["XG"])
                    r += n

            B.dma("sp", "cst", lng[:], fap_dram_bcast(ln1_g, D), [], ["lng"])
            B.dma("sp", "cst", lnb[:], fap_dram_bcast(ln1_b, D), [], ["lnb"])

            if debug:
                S.op("dve", lambda e: e.memset(attnT[:], 0.0), [], ["attnT"])
                S.op("dve", lambda e: e.memset(convT[:], 0.0), [], ["convT"])

            def phase_a():
                B.wpos = 0
                B.wissued = 0
                for g in range(NG):
                    group(g)

            def make_tiles(g):
                halo = (g == 0)
                samp = (g == NG - 1)
                moff = P if halo else 0
                soff = moff + GT * P
                tl = []
                if halo:
                    tl.append((0, P, xp[0:P, :], "halo", None))
                for t in range(GT):
                    tt_ = g * GT + t
                    tl.append((moff + t * P, P, xp[P + tt_ * P:P + (tt_ + 1) * P, :], "main", tt_))
                if samp:
                    tl.append((soff, NS, xs[:, :], "samp", None))
                return tl

            def xpf_loc(i, k):
                if i < 2:
                    return attnT, i * 2048 + k * P, "attnT"
                if i < 4:
                    return convT, (i - 2) * 2048 + k * P, "convT"
                if k < 8:
                    return attnT, 4096 + k * P, "attnT"
                return convT, 4096 + (k - 8) * P, "convT"

            def prefetch_x(tl):
                for i, (c0, nr, src_, kind, tt_) in enumerate(tl):
                    if i < 4:
                        base, off, key = xpf_loc(i, 0)
                        B.dma("pool", "xpf%d" % i, fap(base, off, [[1, D]], parts=nr), src_, [], [key])
                    else:
                        B.dma("pool", "xpf4", fap(attnT, 4096, [[1, 1024]], parts=nr), src_[:, 0:1024], [], ["attnT"])
                        B.dma("pool", "xpf5", fap(convT, 4096, [[1, 1024]], parts=nr), src_[:, 1024:2048], [], ["convT"])

            def group(g):
                halo = (g == 0)
                samp = (g == NG - 1)
                moff = P if halo else 0
                NC = moff + GT * P + (NS if samp else 0)
                soff = moff + GT * P
                absb = (P + GT * P * g) - moff
                own_segs = [(moff, GT * P)] + ([(soff, NS)] if samp else [])
                all_segs = ([(0, P)] if halo else []) + own_segs
                tiles = make_tiles(g)
                own_tiles = [tl for tl in tiles if tl[3] != "halo"]
                if g == 0:
                    prefetch_x(tiles)

                B.areset()
                B.dma("sp", "tabc", cosl[:, 0:NC], cosd[:, absb:absb + NC], [], ["cosl"])
                B.dma("sp", "tabs", sinl[:, 0:NC], sind[:, absb:absb + NC], [], ["sinl"])
                for i, (c0, nr, src, kind, tt_) in enumerate(tiles):
                    b2 = B.bank(2)
                    for k in range(16):
                        base, off, key = xpf_loc(i, k)
                        o = fap(psb, (b2 + k // 8) * 1024 + (k % 8) * nr, [[1, nr]])
                        B.tr(o, fap(base, off, [[1, P]], parts=nr), ident[0:nr, 0:nr], [key, "ident"], B.bk(b2 + k // 8))
                    for hh in range(2):
                        src_ap = fap(psb, (b2 + hh) * 1024, [[nr, 8], [1, nr]])
                        B.cp("dve" if hh == 0 else "act", bigT[:, hh * 8:(hh + 1) * 8, c0:c0 + nr], src_ap, B.bk(b2 + hh), ["bigT"])

                if B.stop_after == "S0":
                    return
                def proj(wt, wk, nk, col_lo, segs, rhs_t, rhs_key, consumer):
                    for (c0, n) in segs:
                        b = B.bank()
                        for k in range(nk):
                            B.mm(ps[:, b, 0:n], wt[:, k, col_lo:col_lo + P], rhs_t[:, k, c0:c0 + n], k == 0, k == nk - 1,
                                 wk + [rhs_key], B.bk(b))
                        consumer(b, c0, n)

                def win_spec(col0, ncols=256):
                    return (16, ncols, [(0, ncols, w_in[:, col0:col0 + ncols])])

                B.areset()
                qT, qTk, _ = B.abf(8 * NCMAX, [[NCMAX, 8], [1, NCMAX]])
                qmark = B.apos
                qb = [B.abf(512) for _ in range(2)]
                t1 = [B.af32(512) for _ in range(2)]
                t2 = [B.af32(512) for _ in range(2)]
                rr = [0]

                def rope(b, c0, n, out_bf, out_keys, out_f32=None, out_f32_keys=None):
                    i = rr[0] % 2
                    rr[0] += 1
                    q_b, qbk, _ = qb[i]
                    a1, a1k, _ = t1[i]
                    a2, a2k, _ = t2[i]
                    B.cp("act", q_b[:, 0:n], ps[:, b, 0:n], B.bk(b), qbk)
                    B.tt("dve", a1[:, 0:n], ps[:, b, 0:n], cosl[:, c0:c0 + n], ALU.mult, B.bk(b) + ["cosl"], a1k)
                    b2 = B.bank()
                    B.mm(ps[:, b2, 0:n], rotT[:], q_b[:, 0:n], True, True, qbk + ["rotT"], B.bk(b2))
                    B.tt("dve", a2[:, 0:n], ps[:, b2, 0:n], sinl[:, c0:c0 + n], ALU.mult, B.bk(b2) + ["sinl"], a2k)
                    B.tt("dve", out_bf, a1[:, 0:n], a2[:, 0:n], ALU.add, a1k + a2k, out_keys)
                    if out_f32 is not None:
                        B.tt("dve", out_f32, a1[:, 0:n], a2[:, 0:n], ALU.add, a1k + a2k, out_f32_keys)

                for qs in range(4):
                    wt, wk = B.wget(win_spec(OQ + qs * 256))
                    for cc in range(2):
                        c = qs * 2 + cc
                        proj(wt, wk, 16, cc * P, own_segs, bigT, "bigT",
                             lambda b, c0, n, c=c: rope(b, c0, n, qT[:, c, c0:c0 + n], qTk))
                if B.stop_after == "S1q":
                    return
                if not halo:
                    ksrc = (P if g == 1 else 0) + GT * P
                    B.cp("act", kTl[:, :, 0:P], kTl[:, :, ksrc:ksrc + P], ["kTl"], ["kTl"])
                    B.cp("act", vl[:, 0, :], vl[:, (GT + 1) if g == 1 else GT, :], ["vl"], ["vl"])
                wt, wk = B.wget(win_spec(OK_))
                krt = [B.af32(512) for _ in range(2)]
                kri = [0]
                ksegs = list(all_segs)
                if samp:
                    ksegs = [(moff, (GT - 1) * P), (moff + (GT - 1) * P, P), (soff, NS)]
                for kp in range(2):
                    def kcons(b, c0, n, kp=kp):
                        kr, krk, _ = krt[kri[0] % 2]
                        kri[0] += 1
                        rope(b, c0, n, kr[:, 0:n], krk)
                        for j in range(2):
                            kv = 2 * kp + j
                            for dup in range(2):
                                B.cp("act" if dup == 0 else "dve", kTl[64 * dup:64 * dup + 64, kv, P + c0:P + c0 + n],
                                     kr[64 * j:64 * j + 64, 0:n], krk, ["kTl"])
                        if samp and c0 == soff:
                            B.cp("act", kT32[:, kp, P:P + NS], kr[:, 0:n], krk, ["kT32"])
                        elif samp and c0 == moff + (GT - 1) * P:
                            B.cp("dve", kT32[:, kp, 0:P], kr[:, 0:n], krk, ["kT32"])
                    proj(wt, wk, 16, kp * P, ksegs, bigT, "bigT", kcons)
                if B.stop_after == "S1k":
                    return
                wt, wk = B.wget(win_spec(OV))
                for li, (c0, nr, src, kind, tt_) in enumerate(tiles):
                    b = B.bank()
                    for k in range(16):
                        B.mm(ps[0:nr, b, 0:256], bigT[:, k, c0:c0 + nr], wt[:, k, :], k == 0, k == 15, wk + ["bigT"], B.bk(b))
                    B.cp("act", vl[0:nr, 1 + li, :], ps[0:nr, b, 0:256], B.bk(b), ["vl"])
                    if samp and kind == "samp":
                        B.cp("dve", v32[0:nr, 1, :], ps[0:nr, b, 0:256], B.bk(b), ["v32"])
                    if samp and kind == "main" and tt_ == NT - 1:
                        B.cp("dve", v32[:, 0, :], ps[:, b, 0:256], B.bk(b), ["v32"])

                if B.stop_after == "S1a":
                    return
                B.apos = qmark
                Sm = [B.af32(1024, [[256, 4], [1, 256]]) for _ in range(2)]
                Pn = [B.abf(1024, [[256, 4], [1, 256]]) for _ in range(2)]
                PTt = [B.abf(1024, [[128, 8], [1, 128]]) for _ in range(2)]
                sm_ = [B.af32(32) for _ in range(2)]
                items = []
                for li, (c0, nr, src_, kind, tt_) in enumerate(tiles):
                    if kind == "main":
                        for kv in range(4):
                            items.append((li, c0, tt_, kv))
                bpv = 6
                sc_bank = {}

                def a_scores(idx):
                    li, c0, tt_, kv = items[idx]
                    b2 = B.bank(2)
                    sc_bank[idx] = b2
                    for hh in range(4):
                        c = 2 * kv + hh // 2
                        half = hh % 2
                        B.mm(ps[:, b2 + half, (hh // 2) * 256:(hh // 2) * 256 + 256],
                             qT[64 * half:64 * half + 64, c, c0:c0 + P],
                             kTl[64 * half:64 * half + 64, kv, c0:c0 + 256], True, True,
                             qTk + ["kTl"], B.bk(b2 + half))

                def a_ctx(idx):
                    li, c0, tt_, kv = items[idx]
                    i = idx % 2
                    Sx, Sk, _ = Sm[i]
                    Px, Pk, _ = Pn[i]
                    PT, PTk, _ = PTt[i]
                    sx, sk_, _ = sm_[i]
                    return li, c0, tt_, kv, Sx, Sk, Px, Pk, PT, PTk, sx, sk_

                def a_A(idx):
                    li, c0, tt_, kv, Sx, Sk, Px, Pk, PT, PTk, sx, sk_ = a_ctx(idx)
                    b2 = sc_bank[idx]
                    mk_off = 256 if tt_ == 0 else 0
                    pin = fap(ps, b2 * 512, [[256, 4], [1, 256]])
                    B.tt("dve", Sx, pin, fap(msk, mk_off, [[0, 4], [1, 256]]), ALU.add, B.bk(b2, 2) + ["msk"], Sk)
                    mx = sx[:, 0:4]
                    m8 = sx[:, 4:8]
                    sm = sx[:, 8:12]
                    tq = sx[:, 12:16]
                    es_ = sx[:, 16:20]
                    rv = sx[:, 20:24]
                    nm = sx[:, 24:28]
                    B.red(mx, Sx, ALU.max, Sk, sk_)
                    B.tt("dve", m8, mx, sink8[:, 4 * kv:4 * kv + 4], ALU.max, sk_ + ["sink8"], sk_)
                    B.ts("dve", nm, m8, -0.125, ALU.mult, sk_, sk_)
                    B.tt("dve", tq, sink8[:, 4 * kv:4 * kv + 4], m8, ALU.subtract, sk_ + ["sink8"], sk_)

                def a_E(idx):
                    li, c0, tt_, kv, Sx, Sk, Px, Pk, PT, PTk, sx, sk_ = a_ctx(idx)
                    for s in range(4):
                        B.act(Sx[:, s, :], Sx[:, s, :], AF.Exp, Sk + sk_, Sk + sk_, scale=0.125, bias=sx[:, 24 + s:25 + s], accum_out=sx[:, 8 + s:9 + s])

                def a_B(idx):
                    li, c0, tt_, kv, Sx, Sk, Px, Pk, PT, PTk, sx, sk_ = a_ctx(idx)
                    sm = sx[:, 8:12]
                    tq = sx[:, 12:16]
                    es_ = sx[:, 16:20]
                    rv = sx[:, 20:24]
                    B.act(es_, tq, AF.Exp, sk_, sk_, scale=0.125)
                    B.tt("dve", rv, sm, es_, ALU.add, sk_, sk_)
                    S.op("dve", lambda e, rv=rv: e.reciprocal(out=rv, in_=rv), sk_, sk_)
                    for s in range(4):
                        B.act(Px[:, s, :], Sx[:, s, :], AF.Copy, Sk + sk_, Pk, scale=sx[:, 20 + s:21 + s])

                def a_C(idx):
                    li, c0, tt_, kv, Sx, Sk, Px, Pk, PT, PTk, sx, sk_ = a_ctx(idx)
                    bt = B.bank()
                    for hh in range(4):
                        for kb in range(2):
                            B.tr(psb[:, bt, (hh * 2 + kb) * P:(hh * 2 + kb + 1) * P], Px[:, (hh % 2) * 2 + hh // 2, kb * P:(kb + 1) * P], ident[:],
                                 Pk + ["ident"], B.bk(bt))
                    B.cp("act", PT, fap(psb, bt * 1024, [[128, 8], [1, 128]]), B.bk(bt), PTk)
                    for hh in range(4):
                        c = 2 * kv + hh // 2
                        half = hh % 2
                        o = ps[64 * half:64 * half + 64, bpv + c // 4, (c % 4) * P:(c % 4 + 1) * P]
                        for kb in range(2):
                            B.mm(o, vl[:, li + kb, kv * 64:(kv + 1) * 64], PT[:, hh * 2 + kb, :], kb == 0, kb == 1,
                                 ["vl"] + PTk, B.bk(bpv + c // 4))
                    if kv == 3:
                        for hh in range(2):
                            B.cp("act" if hh == 0 else "dve", attnT[:, hh * 4:(hh + 1) * 4, c0:c0 + P],
                                 fap(ps, (bpv + hh) * 512, [[128, 4], [1, 128]]), B.bk(bpv + hh), ["attnT"])

                if items:
                    n_it = len(items)
                    a_scores(0)
                    if n_it > 1:
                        a_scores(1)
                    a_A(0)
                    for idx in range(n_it):
                        a_E(idx)
                        if idx + 2 < n_it:
                            a_scores(idx + 2)
                        if idx + 1 < n_it:
                            a_A(idx + 1)
                        a_B(idx)
                        a_C(idx)

                if B.stop_after == "S1b":
                    return
                if samp:
                    B.apos = qmark
                    sample_attention(qT, qTk, soff)

                if B.stop_after == "S1":
                    return

                if g == 0:
                    zero_fill_xg()
                B.areset()
                NU = 2 + NCMAX
                hs = [B.af32(512) for _ in range(2)]
                bs = [B.af32(512) for _ in range(2)]
                ub = [B.af32(NU) for _ in range(2)]
                tb = [B.af32(512) for _ in range(2)]
                if samp:
                    stT, stTk, _ = B.af32(8 * 32, [[32, 8], [1, 32]])
                    st_in, st_ink, _ = B.af32(1024)
                    us_all, us_allk, _ = B.af32(8 * NS, [[NS, 8], [1, NS]])
                    ul_all, ul_allk, _ = B.af32(8 * 2, [[2, 8], [1, 2]])
                    B.dma("sp", "st", fap(st_in, 0, [[1, 1024]], parts=32), scv.rearrange("b j c -> (b j) c"), [], st_ink)
                    bq = B.bank()
                    for cc in range(8):
                        B.tr(ps[:, bq, cc * 32:(cc + 1) * 32], fap(st_in, cc * P, [[1, P]], parts=32), ident_f[0:32, 0:32],
                             st_ink + ["ident_f"], B.bk(bq))
                    B.cp("dve", stT, fap(ps, bq * 512, [[32, 8], [1, 32]]), B.bk(bq), stTk)
                for cc in range(8):
                    if cc % 2 == 0:
                        wb_, wbk_ = B.wget(win_spec(OB + (cc // 2) * 256))
                        wc_, wck_ = B.wget(win_spec(OC + (cc // 2) * 256))
                        wh_, whk_ = B.wget(win_spec(OH + (cc // 2) * 256))
                    cj = (cc % 2) * P
                    i = cc % 2
                    u, uk, _ = ub[i]
                    if not halo:
                        B.cp("act", u[:, 0:2], uprev[:, cc, :], ["uprev"], uk)
                    for (c0, n) in all_segs:
                        hx, hk, _ = hs[i]
                        bx, bxk, _ = bs[i]
                        tx, txk, _ = tb[i]
                        is_own = (c0, n) in own_segs
                        bh = B.bank()
                        for k in range(16):
                            B.mm(ps[:, bh, 0:n], wh_[:, k, cj:cj + P], bigT[:, k, c0:c0 + n], k == 0, k == 15, whk_ + ["bigT"], B.bk(bh))
                        B.cp("act", hx[:, 0:n], ps[:, bh, 0:n], B.bk(bh), hk)
                        bc = B.bank()
                        for k in range(16):
                            B.mm(ps[:, bc, 0:n], wc_[:, k, cj:cj + P], bigT[:, k, c0:c0 + n], k == 0, k == 15, wck_ + ["bigT"], B.bk(bc))
                        B.tt("dve", u[:, 2 + c0:2 + c0 + n], ps[:, bc, 0:n], hx[:, 0:n], ALU.mult, B.bk(bc) + hk, uk)
                        if not is_own:
                            continue
                        bb = B.bank()
                        for k in range(16):
                            B.mm(ps[:, bb, 0:n], wb_[:, k, cj:cj + P], bigT[:, k, c0:c0 + n], k == 0, k == 15, wbk_ + ["bigT"], B.bk(bb))
                        B.cp("act", bx[:, 0:n], ps[:, bb, 0:n], B.bk(bb), bxk)
                        if c0 == soff and samp:
                            s0 = fap(stT, cc * 32, [[2, NS]])
                            s1 = fap(stT, cc * 32 + 1, [[2, NS]])
                            B.ts("dve", tx[:, 0:n], u[:, 2 + c0:2 + c0 + n], convw[:, cc, 2:3], ALU.mult, uk + ["convw0", "convw1", "convw2"], txk)
                            B.stt(tx[:, 0:n], s1, convw[:, cc, 1:2], tx[:, 0:n], ALU.mult, ALU.add, stTk + ["convw0", "convw1", "convw2"] + txk, txk)
                            B.stt(tx[:, 0:n], s0, convw[:, cc, 0:1], tx[:, 0:n], ALU.mult, ALU.add, stTk + ["convw0", "convw1", "convw2"] + txk, txk)
                            B.cp("act", us_all[:, cc, :], u[:, 2 + c0:2 + c0 + n], uk, us_allk)
                        else:
                            B.ts("dve", tx[:, 0:n], u[:, 2 + c0:2 + c0 + n], convw[:, cc, 2:3], ALU.mult, uk + ["convw0", "convw1", "convw2"], txk)
                            B.stt(tx[:, 0:n], u[:, 1 + c0:1 + c0 + n], convw[:, cc, 1:2], tx[:, 0:n], ALU.mult, ALU.add, uk + ["convw0", "convw1", "convw2"] + txk, txk)
                            B.stt(tx[:, 0:n], u[:, c0:c0 + n], convw[:, cc, 0:1], tx[:, 0:n], ALU.mult, ALU.add, uk + ["convw0", "convw1", "convw2"] + txk, txk)
                            B.cp("act", uprev[:, cc, :], u[:, 2 + c0 + n - 2:2 + c0 + n], uk, ["uprev"])
                            if samp:
                                B.cp("act", ul_all[:, cc, :], u[:, 2 + c0 + n - 2:2 + c0 + n], uk, ul_allk)
                        B.tt("dve", convT[:, cc, c0:c0 + n], tx[:, 0:n], bx[:, 0:n], ALU.mult, txk + bxk, ["convT"])
                if samp:
                    def fm_to_tm(src3, srck, n):
                        b2 = B.bank(2)
                        for cc in range(8):
                            B.tr(ps[0:n, b2 + cc // 4, (cc % 4) * P:(cc % 4 + 1) * P], src3[:, cc, :], ident_f[:], srck + ["ident_f"], B.bk(b2 + cc // 4))
                        o, ok_, _ = B.af32(1024)
                        B.cp("dve", fap(o, 0, [[1, 1024]], parts=n), fap(ps, b2 * 512, [[1, 1024]], parts=n), B.bk(b2, 2), ok_)
                        return o, ok_
                    uo, uok = fm_to_tm(us_all, us_allk, NS)
                    B.dma("sp", "o_ssc", ssc[:, 1, :], fap(uo, 0, [[1, 1024]], parts=NS), uok, ["ssc"])
                    B.dma("sp", "o_ssc0", ssc[:, 0, :], scv[:, 1, :], [], ["ssc0"])
                    ul, ulk = fm_to_tm(ul_all, ul_allk, 2)
                    B.dma("sp", "o_psc", psc[:, :], fap(ul, 0, [[1, 1024]], parts=2), ulk, ["psc"])
                    kc, kck, _ = B.af32(256)
                    bq5 = B.bank()
                    for kp in range(2):
                        B.tr(ps[:, bq5, kp * P:(kp + 1) * P], kT32[:, kp, 0:P], ident_f[:], ["kT32", "ident_f"], B.bk(bq5))
                    B.cp("dve", kc, ps[:, bq5, 0:256], B.bk(bq5), kck)
                    B.dma("sp", "o_pck", pck[:, :], kc, kck, ["pck"])
                    B.dma("sp", "o_pcv", pcv[:, :], v32[:, 0, :], ["v32"], ["pcv"])

                if debug and g == 0:
                    B.dma("pool", "dbgp0_" + str(DBGC.next()), dbg["attn"][:, :, :], attnT[:], ["attnT"], [])
                    B.dma("pool", "dbgp1_" + str(DBGC.next()), dbg["conv"][:, :, :], convT[:], ["convT"], [])
                if B.stop_after == "S2":
                    return

                B.areset()
                mixT, mixk, _ = B.abf(16 * NCMAX, [[NCMAX, 16], [1, NCMAX]])
                sg = [B.af32(512) for _ in range(2)]
                ta = [B.af32(512) for _ in range(2)]
                for jb in range(8):
                    wga, wgak = B.wget(win_spec(OGA + jb * 256))
                    wgc, wgck = B.wget(win_spec(OGC + jb * 256))
                    wao, waok = B.wget((8, 512, [(0, 256, w_ao[:, jb * 256:(jb + 1) * 256]), (256, 256, w_co[:, jb * 256:(jb + 1) * 256])]))
                    for jj in range(2):
                        j = jb * 2 + jj
                        for (c0, n) in own_segs:
                            i = j % 2
                            sgx, sgk, _ = sg[i]
                            tax, tak, _ = ta[i]
                            b = B.bank()
                            for k in range(16):
                                B.mm(ps[:, b, 0:n], wga[:, k, jj * P:(jj + 1) * P], bigT[:, k, c0:c0 + n], k == 0, k == 15, wgak + ["bigT"], B.bk(b))
                            B.act(sgx[:, 0:n], ps[:, b, 0:n], AF.Sigmoid, B.bk(b), sgk)
                            b = B.bank()
                            for k in range(8):
                                B.mm(ps[:, b, 0:n], wao[:, k, jj * P:(jj + 1) * P], attnT[:, k, c0:c0 + n], k == 0, k == 7, waok + ["attnT"], B.bk(b))
                            B.tt("dve", tax[:, 0:n], ps[:, b, 0:n], sgx[:, 0:n], ALU.mult, B.bk(b) + sgk, tak)
                            b = B.bank()
                            for k in range(16):
                                B.mm(ps[:, b, 0:n], wgc[:, k, jj * P:(jj + 1) * P], bigT[:, k, c0:c0 + n], k == 0, k == 15, wgck + ["bigT"], B.bk(b))
                            B.act(sgx[:, 0:n], ps[:, b, 0:n], AF.Sigmoid, B.bk(b), sgk)
                            b = B.bank()
                            for k in range(8):
                                B.mm(ps[:, b, 0:n], wao[:, k, 256 + jj * P:256 + (jj + 1) * P], convT[:, k, c0:c0 + n], k == 0, k == 7, waok + ["convT"], B.bk(b))
                            B.tt("dve", sgx[:, 0:n], ps[:, b, 0:n], sgx[:, 0:n], ALU.mult, B.bk(b) + sgk, sgk)
                            B.tt("dve", mixT[:, j, c0:c0 + n], tax[:, 0:n], sgx[:, 0:n], ALU.add, tak + sgk, mixk)

                for li, (c0, nr, src, kind, tt_) in enumerate(own_tiles):
                    B.dma("sp", "xres%d" % li, hres[0:nr, li, :], src, [], [("hres", li)])
                for n4 in range(4):
                    wlo, wlok = B.wget((8, 512, [(0, 512, w_o[0:1024, n4 * 512:(n4 + 1) * 512])]))
                    whi, whik = B.wget((8, 512, [(0, 512, w_o[1024:2048, n4 * 512:(n4 + 1) * 512])]))
                    for li, (c0, nr, src, kind, tt_) in enumerate(own_tiles):
                        b = B.bank()
                        for k in range(16):
                            w_, wk_ = (wlo, wlok) if k < 8 else (whi, whik)
                            B.mm(ps[0:nr, b, :], mixT[:, k, c0:c0 + nr], w_[:, k % 8, :], k == 0, k == 15, mixk + wk_, B.bk(b))
                        hr = hres[0:nr, li, n4 * 512:(n4 + 1) * 512]
                        B.stt(hr, hr, ALPHA, ps[0:nr, b, :], ALU.mult, ALU.add, [("hres", li)] + B.bk(b), [("hres", li)])
                if B.stop_after == "S3":
                    return

                B.areset()
                if g + 1 < NG:
                    prefetch_x(make_tiles(g + 1))
                lnt = [B.af32(32) for _ in range(2)]
                hbf = [B.abf(D) for _ in range(2)]
                rts = [B.af32(64 * 8) for _ in range(len(own_tiles))]
                actT, actk, _ = B.abf(4 * NCMAX, [[NCMAX, 4], [1, NCMAX]])
                sgs = [B.af32(512) for _ in range(2)]
                wrt, wrk = B.wget((16, 64, [(0, 64, w_r[:, :])]))
                for li, (c0, nr, src, kind, tt_) in enumerate(own_tiles):
                    layer_norm(hres[0:nr, li, :], [("hres", li)], nr, lnt[li % 2])
                    if debug:
                        r0 = TOK if kind == "samp" else tt_ * P
                        B.dma("sp", "dbg0_" + str(DBGC.next()), dbg["h"][r0:r0 + nr, :], hres[0:nr, li, :], [("hres", li)], [])
                    hb, hbk, _ = hbf[li % 2]
                    B.cp("act", fap(hb, 0, [[1, D]], parts=nr), hres[0:nr, li, :], [("hres", li)], hbk)
                    b2 = B.bank(2)
                    for k in range(16):
                        o = fap(psb, (b2 + k // 8) * 1024 + (k % 8) * nr, [[1, nr]])
                        B.tr(o, fap(hb, k * P, [[1, P]], parts=nr), ident[0:nr, 0:nr], hbk + ["ident"], B.bk(b2 + k // 8))
                    for hh in range(2):
                        src_ap = fap(psb, (b2 + hh) * 1024, [[nr, 8], [1, nr]])
                        B.cp("dve" if hh == 0 else "act", bigT[:, hh * 8:(hh + 1) * 8, c0:c0 + nr], src_ap, B.bk(b2 + hh), ["bigT"])
                    tix = NT if kind == "samp" else tt_
                    routing(1, li, c0, nr, tix, wrt, wrk, rts[li], None, None)
                r2_done = 0
                for cb in range(2):
                    wg_, wgk_ = B.wget((16, 256, [(0, 256, w_sg[:, cb * 256:(cb + 1) * 256])]))
                    wu_, wuk_ = B.wget((16, 256, [(0, 256, w_su[:, cb * 256:(cb + 1) * 256])]))
                    for jj in range(2):
                        c = cb * 2 + jj
                        for _ in range(2):
                            if r2_done < len(own_tiles) and (r2_done <= c + 1 or c == 3):
                                li = r2_done
                                (c0_, nr_, src_, kind_, tt2) = own_tiles[li]
                                routing(2, li, c0_, nr_, NT if kind_ == "samp" else tt2, wrt, wrk, rts[li], None, None)
                                r2_done += 1
                        for (c0, n) in own_segs:
                            sx_, sxk, _ = sgs[c % 2]
                            b = B.bank()
                            for k in range(16):
                                B.mm(ps[:, b, 0:n], wg_[:, k, jj * P:(jj + 1) * P], bigT[:, k, c0:c0 + n], k == 0, k == 15, wgk_ + ["bigT"], B.bk(b))
                            B.act(sx_[:, 0:n], ps[:, b, 0:n], AF.Silu, B.bk(b), sxk)
                            b = B.bank()
                            for k in range(16):
                                B.mm(ps[:, b, 0:n], wu_[:, k, jj * P:(jj + 1) * P], bigT[:, k, c0:c0 + n], k == 0, k == 15, wuk_ + ["bigT"], B.bk(b))
                            B.tt("dve", actT[:, c, c0:c0 + n], ps[:, b, 0:n], sx_[:, 0:n], ALU.mult, B.bk(b) + sxk, actk)
                while r2_done < len(own_tiles):
                    li = r2_done
                    (c0_, nr_, src_, kind_, tt2) = own_tiles[li]
                    routing(2, li, c0_, nr_, NT if kind_ == "samp" else tt2, wrt, wrk, rts[li], None, None)
                    r2_done += 1
                for li, (c0, nr, src, kind, tt_) in enumerate(own_tiles):
                    tix = NT if kind == "samp" else tt_
                    routing(3, li, c0, nr, tix, wrt, wrk, rts[li], None, None)
                for li, (c0, nr, src, kind, tt_) in enumerate(own_tiles):
                    hb, hbk, _ = hbf[li % 2]
                    if kind == "samp":
                        S.op("dve", lambda e, hb=hb: e.memset(hb, 0.0), [], hbk)
                    B.cp("act", fap(hb, 0, [[1, D]], parts=nr), hres[0:nr, li, :], [("hres", li)], hbk)
                    tix = NT if kind == "samp" else tt_
                    routing4(li, c0, nr, tix, rts[li], hb, hbk)
                for nb in range(2):
                    wd_, wdk_ = B.wget((4, 1024, [(0, 1024, w_sd[:, nb * 1024:(nb + 1) * 1024])]))
                    for n2 in range(2):
                        n4 = nb * 2 + n2
                        for li, (c0, nr, src, kind, tt_) in enumerate(own_tiles):
                            b = B.bank()
                            for k in range(4):
                                B.mm(ps[0:nr, b, :], actT[:, k, c0:c0 + nr], wd_[:, k, n2 * 512:(n2 + 1) * 512], k == 0, k == 3, actk + wdk_, B.bk(b))
                            hr = hres[0:nr, li, n4 * 512:(n4 + 1) * 512]
                            B.stt(hr, hr, ALPHA, ps[0:nr, b, :], ALU.mult, ALU.add, [("hres", li)] + B.bk(b), [("hres", li)])
                for li, (c0, nr, src, kind, tt_) in enumerate(own_tiles):
                    r0 = TOK if kind == "samp" else tt_ * P
                    B.dma("sp", "base%d" % li, BASE[r0:r0 + nr, :], hres[0:nr, li, :], [("hres", li)], ["BASE"])

            def layer_norm(xap, xkeys, nr, tmp, out=None, out_keys=None):
                st, stk, _ = tmp
                for q in range(4):
                    S.op("dve", lambda e, q=q: e.bn_stats(out=fap(st, q * 6, [[1, 6]], parts=nr), in_=fap(xap, q * 512, [[1, 512]], parts=nr)), xkeys, stk)
                mv = fap(st, 24, [[1, 2]], parts=nr)
                S.op("dve", lambda e: e.bn_aggr(out=mv, in_=fap(st, 0, [[1, 24]], parts=nr)), stk, stk)
                rs = fap(st, 26, [[1, 1]], parts=nr)
                B.act(rs, fap(st, 25, [[1, 1]], parts=nr), AF.Sqrt, stk + ["epsT"], stk, bias=epsT[0:nr, :])
                S.op("dve", lambda e: e.reciprocal(out=rs, in_=rs), stk, stk)
                o = xap if out is None else out
                ok_ = xkeys if out is None else out_keys
                B.ts("dve", o, xap, fap(st, 24, [[1, 1]], parts=nr), ALU.subtract, xkeys + stk, ok_, s2=rs, op1=ALU.mult)
                B.tt("dve", o, o, lng[0:nr, :], ALU.mult, ok_ + ["lng"], ok_)
                B.tt("dve", o, o, lnb[0:nr, :], ALU.add, ok_ + ["lnb"], ok_)

            def routing(phase, li, c0, nr, tix, wrt, wrk, rt, hb, hbk):
                r, rk, _ = rt
                def rv(i, n=NE, parts=nr):
                    return fap(r, i * 64, [[1, n]], parts=parts)
                sc_ = rv(0)
                ch = rv(1)
                tmp = rv(2)
                sel = rv(3, parts=P)
                wd = rv(4)
                key = rv(5, parts=P)
                sm = fap(r, 6 * 64, [[1, 64]], parts=nr)
                m1 = fap(r, 6 * 64, [[1, 8]], parts=nr)
                m2 = fap(r, 6 * 64 + 8, [[1, 8]], parts=nr)
                gs = fap(r, 6 * 64 + 16, [[1, 8]], parts=nr)
                g8 = fap(r, 6 * 64 + 24, [[1, 8]], parts=nr)
                pen = fap(r, 6 * 64 + 32, [[1, 8]], parts=nr)
                c8 = fap(r, 6 * 64 + 40, [[1, 8]], parts=nr)
                ssum = fap(r, 6 * 64 + 48, [[1, 1]], parts=nr)
                ch3 = fap(r, 1 * 64, [[8, 8], [1, 8]], parts=nr)
                tmp3 = fap(r, 2 * 64, [[8, 8], [1, 8]], parts=nr)
                selb, selbk = rsel_t[:, li, :], [("rsel", li)]
                pos = rv(7, parts=P)
                d8f = fap(r, 6 * 64 + 56, [[1, 8]], parts=P)
                BIGC = float(2 * NSLOT)
                if phase == 1:
                    b = B.bank()
                    for k in range(16):
                        B.mm(ps[0:nr, b, 0:NE], bigT[:, k, c0:c0 + nr], wrt[:, k, :], k == 0, k == 15, ["bigT"] + wrk, B.bk(b))

                    B.act(sc_, ps[0:nr, b, 0:NE], AF.Sigmoid, B.bk(b), rk)
                    return
                if phase == 2:
                    B.tt("dve", ch, sc_, rbias[0:nr, :], ALU.add, rk + ["rbias"], rk)
                    B.red(m1, ch3, ALU.max, rk, rk)
                    B.tt("dve", tmp3, ch3, fap(r, 6 * 64, [[1, 8], [0, 8]], parts=nr), ALU.is_equal, rk, rk)
                    B.stt(tmp, tmp, -1e9, ch, ALU.mult, ALU.add, rk, rk)
                    B.red(m2, tmp3, ALU.max, rk, rk)
                    B.tt("dve", gs, m1, m2, ALU.add, rk, rk)
                    S.op("dve", lambda e: e.max(out=g8, in_=gs), rk, rk)
                    B.ts("dve", pen, gs, fap(r, 6 * 64 + 24 + 3, [[1, 1]], parts=nr), ALU.is_ge, rk, rk, s2=-1.0, op1=ALU.add)
                    B.ts("dve", pen, pen, 1e9, ALU.mult, rk, rk)
                    B.tt("dve", tmp3, ch3, fap(r, 6 * 64 + 32, [[1, 8], [0, 8]], parts=nr), ALU.add, rk, rk)
                    S.op("dve", lambda e: e.max(out=c8, in_=tmp), rk, rk)
                    if nr < P:
                        S.op("dve", lambda e: e.memset(sel, 0.0), rk, rk)
                    B.ts("dve", rv(3), tmp, fap(r, 6 * 64 + 40 + 7, [[1, 1]], parts=nr), ALU.is_ge, rk, rk)
                    B.stt(wd, rv(3), 1.0, sc_, ALU.mult, ALU.mult, rk, rk, accum_out=ssum)
                    S.op("dve", lambda e: e.reciprocal(out=ssum, in_=ssum), rk, rk)
                    B.ts("dve", wd, wd, ssum, ALU.mult, rk, rk, s2=2.5, op1=ALU.mult)
                    B.cp("dve", selb, sel, rk, selbk)
                    return
                bp = B.bank()
                B.mm(ps[:, bp, 0:NE], triU[:], selb, True, True, ["triU"] + selbk, B.bk(bp))
                B.tt("dve", pos, ps[:, bp, 0:NE], cbase[:], ALU.add, B.bk(bp) + ["cbase"], rk)
                bp2 = B.bank()
                B.mm(ps[:, bp2, 0:NE], ones[:], selb, True, True, ["ones"] + selbk, B.bk(bp2))
                B.tt("dve", cbase[:], cbase[:], ps[:, bp2, 0:NE], ALU.add, B.bk(bp2) + ["cbase"], ["cbase"])
                return

            def routing4(li, c0, nr, tix, rt, hb, hbk):
                r, rk, _ = rt

                def rv(i, n=NE, parts=nr):
                    return fap(r, i * 64, [[1, n]], parts=parts)
                sel = rv(3, parts=P)
                wd = rv(4)
                key = rv(5, parts=P)
                pos = rv(7, parts=P)
                d8f = fap(r, 6 * 64 + 56, [[1, 8]], parts=P)
                BIGC = float(2 * NSLOT)
                B.ts("dve", key, pos, float(CAP), ALU.is_lt, rk, rk)
                B.tt("dve", key, key, sel, ALU.mult, rk, rk)
                B.tt("dve", pos, pos, c16t[:, 64:128], ALU.add, rk + ["c16t"], rk)
                B.ts("dve", pos, pos, -1.0, ALU.mult, rk, rk, s2=BIGC, op1=ALU.add)
                B.tt("dve", key, key, pos, ALU.mult, rk, rk)
                S.op("dve", lambda e: e.max(out=d8f, in_=key), rk, rk)
                for j in range(8):
                    B.stt(rv(2, parts=nr), fap(r, 5 * 64, [[1, NE]], parts=nr), fap(r, 6 * 64 + 56 + j, [[1, 1]], parts=nr), wd,
                          ALU.is_equal, ALU.mult, rk, rk + [("w8", tix)], accum_out=w8all[0:nr, tix, j:j + 1])
                B.ts("dve", fap(r, 2 * 64, [[1, 8]], parts=nr), fap(r, 6 * 64 + 56, [[1, 8]], parts=nr), 0.0, ALU.is_gt, rk, rk)
                B.tt("dve", w8all[0:nr, tix, :], w8all[0:nr, tix, :], fap(r, 2 * 64, [[1, 8]], parts=nr), ALU.mult, rk + [("w8", tix)], [("w8", tix)])
                B.ts("dve", d8f, d8f, -1.0, ALU.mult, rk, rk, s2=BIGC, op1=ALU.add)
                B.cp("dve", d8all[:, tix, :], d8f, rk, [("d8", tix)])
                for j in range(8):
                    S.dma("pool", "scat%d" % (li % 2), lambda e, j=j: e.indirect_dma_start(
                        out=XG[:, :], out_offset=bass.IndirectOffsetOnAxis(ap=d8all[:, tix, j:j + 1], axis=0),
                        in_=hb, in_offset=None, bounds_check=B.bcreg(e), oob_is_err=False),
                        hbk + [("d8", tix), "XG"], [("XGs", tix, j)])


            def sample_attention(qT, qTk, soff):
                kn, knk, _ = B.af32(256)
                bq = B.bank()
                for kp in range(2):
                    B.tr(ps[0:NS, bq, kp * P:(kp + 1) * P], kT32[:, kp, P:P + NS], ident_f[:], ["kT32", "ident_f"], B.bk(bq))
                B.cp("dve", fap(kn, 0, [[1, 256]], parts=NS), ps[0:NS, bq, 0:256], B.bk(bq), knk)
                B.dma("sp", "sck", sck[:, 0:P - 1, :], ck[:, 1:P, :], [], ["sck"])
                B.dma("sp", "sck", sck[:, P - 1, :], fap(kn, 0, [[1, 256]], parts=NS), knk, ["sck"])
                B.dma("sp", "scv", scvo[:, 0:P - 1, :], cv[:, 1:P, :], [], ["scvo"])
                B.dma("sp", "scv", scvo[:, P - 1, :], v32[0:NS, 1, :], ["v32"], ["scvo"])
                Ke, Kek = fap(hres_bf, 0 * 4096, [[256, NS], [1, 256]]), [("hres", 0)]
                Ve, Vek = fap(hres_bf, 1 * 4096, [[256, NS], [1, 256]]), [("hres", 1)]
                stg = fap(hres, 2 * D, [[256, NS], [1, 256]])
                stgk = [("hres", 2), ("hres", 3)]
                B.dma("sp", "ske", stg, sck.rearrange("b k d -> k b d"), ["sck"], stgk)
                B.cp("act", Ke, stg, stgk, Kek)
                B.dma("sp", "ske", stg, scvo.rearrange("b k d -> k b d"), ["scvo"], stgk)
                B.cp("dve", Ve, stg, stgk, Vek)
                QM, QMk, _ = B.abf(64, [[16, 4], [1, 16]])
                KT2, KT2k, _ = B.abf(4 * P, [[P, 4], [1, P]])
                Ssb, Ssk = fap(hres, 2 * D, [[P, NS], [1, P]]), [("hres", 2)]
                for b_ in range(NS):
                    qin = fap(qT, soff + b_, [[0, 4], [640, 8], [0, 2]])
                    B.tt("dve", fap(QM, 0, [[16, 4], [2, 8], [1, 2]]), qin, fap(c16t, 0, [[16, 4], [2, 8], [1, 2]]), ALU.mult, qTk + ["c16t"], QMk)
                    bt = B.bank()
                    for kv in range(4):
                        for dup in range(2):
                            B.tr(psb[64 * dup:64 * dup + 64, bt, kv * P:(kv + 1) * P], Ke[:, b_, kv * 64:(kv + 1) * 64], ident[:], Kek + ["ident"], B.bk(bt))
                    B.cp("act", KT2, fap(psb, bt * 1024, [[P, 4], [1, P]]), B.bk(bt), KT2k)
                    bsc = B.bank()
                    for kv in range(4):
                        B.mm(ps[0:16, bsc, 0:P], QM[:, kv, :], KT2[:, kv, :], kv == 0, kv == 3, QMk + KT2k, B.bk(bsc))
                    B.cp("dve", fap(Ssb, b_ * P, [[1, P]], parts=16), ps[0:16, bsc, 0:P], B.bk(bsc), Ssk)
                sx, sxk, _ = B.af32(5 * NS)
                S16 = fap(Ssb, 0, [[P, NS], [1, P]], parts=16)
                mx = fap(sx, 0, [[1, NS]], parts=16)
                sm = fap(sx, NS, [[1, NS]], parts=16)
                tq = fap(sx, 2 * NS, [[1, NS]], parts=16)
                rv_ = fap(sx, 3 * NS, [[1, NS]], parts=16)
                B.red(mx, S16, ALU.max, Ssk, sxk)
                B.ts("dve", mx, mx, sinkcol[0:16, 0:1], ALU.max, sxk + ["sinkcol"], sxk)
                B.tt("dve", S16, S16, fap(sx, 0, [[1, NS], [0, P]], parts=16), ALU.subtract, Ssk + sxk, Ssk)
                B.act(S16, S16, AF.Exp, Ssk, Ssk, scale=0.125)
                B.red(sm, S16, ALU.add, Ssk, sxk)
                B.ts("dve", tq, mx, -1.0, ALU.mult, sxk, sxk, s2=sinkcol[0:16, 0:1], op1=ALU.add)
                B.act(tq, tq, AF.Exp, sxk, sxk, scale=0.125)
                B.tt("dve", rv_, sm, tq, ALU.add, sxk, sxk)
                S.op("dve", lambda e: e.reciprocal(out=rv_, in_=rv_), sxk, sxk)
                Pb, Pbk = fap(hres_bf, 4 * 4096, [[P, NS], [1, P]]), [("hres", 4)]
                B.tt("dve", fap(Pb, 0, [[P, NS], [1, P]], parts=16), S16, fap(sx, 3 * NS, [[1, NS], [0, P]], parts=16), ALU.mult, Ssk + sxk, Pbk)
                bt = B.bank()
                for b_ in range(NS):
                    B.tr(psb[:, bt, b_ * 16:(b_ + 1) * 16], fap(Pb, b_ * P, [[1, P]], parts=16), ident[0:16, 0:16], Pbk + ["ident"], B.bk(bt))
                PTs, PTsk, _ = B.abf(NS * 16)
                B.cp("act", PTs, psb[:, bt, 0:NS * 16], B.bk(bt), PTsk)
                As, Ask = fap(hres, 4 * D + 1024, [[64, NS], [1, 64]]), [("hres", 4)]
                tmpv, tmpvk, _ = B.af32(256)
                for b_ in range(NS):
                    bo = B.bank()
                    B.mm(ps[0:16, bo, 0:256], PTs[:, b_ * 16:(b_ + 1) * 16], Ve[:, b_, :], True, True, PTsk + Vek, B.bk(bo))
                    B.tt("dve", fap(tmpv, 0, [[64, 4], [1, 64]], parts=16), fap(ps, bo * 512, [[64, 4], [1, 64]], parts=16),
                         fap(c16t, 192, [[1, 4], [0, 64]], parts=16), ALU.mult, B.bk(bo) + ["c16t"], tmpvk)
                    B.red(fap(As, b_ * 64, [[1, 64]], parts=16), fap(tmpv, 0, [[1, 64], [64, 4]], parts=16), ALU.add, tmpvk, Ask)
                A2, A2k = fap(hres, 3 * D, [[P, NS], [1, P]]), [("hres", 3)]
                B.tt("dve", fap(A2, 0, [[P, NS], [64, 2], [1, 64]], parts=16), fap(As, 0, [[64, NS], [0, 2], [1, 64]], parts=16),
                     fap(c16t, 200, [[0, NS], [1, 2], [0, 64]], parts=16), ALU.mult, Ask + ["c16t"], A2k)
                bt2 = B.bank()
                for b_ in range(NS):
                    B.tr(ps[:, bt2, b_ * 16:(b_ + 1) * 16], fap(A2, b_ * P, [[1, P]], parts=16), ident_f[0:16, 0:16], A2k + ["ident_f"], B.bk(bt2))
                af_, afk, _ = B.af32(8 * NS, [[NS, 8], [1, NS]])
                B.red(af_, fap(ps, bt2 * 512, [[2, 8], [16, NS], [1, 2]]), ALU.add, B.bk(bt2), afk)
                B.cp("dve", fap(attnT, soff, [[640, 8], [1, NS]]), af_, afk, ["attnT"])


            S.planning = True
            phase_a()
            S.planning = False
            B.bank_rr = 0
            phase_a()
        S.barrier()

        if stop_after is None:
            pb = contextlib.ExitStack()
            with pb:
                def sbb(name, shape, dt):
                    return pb.enter_context(nc.sbuf_tensor(name, list(shape), dt))
                wg = [sbb("wg%d" % i, [P, 16, 512], BF16) for i in range(2)]
                wu = [sbb("wu%d" % i, [P, 16, 512], BF16) for i in range(2)]
                wdn = [sbb("wd%d" % i, [P, 4, D], BF16) for i in range(2)]
                xb_ = [sbb("xb%d" % i, [P, NBLK, D], BF16) for i in range(2)]
                xbT = [sbb("xbT%d" % i, [P, 16, CAP], BF16) for i in range(2)]
                acT = [sbb("acT%d" % i, [P, 4, CAP], BF16) for i in range(2)]
                sgt = [sbb("sgt%d" % i, [P, CAP], F32) for i in range(2)]
                yo = [sbb("yo%d" % i, [P, D], BF16) for i in range(2)]

                def load_expert(e):
                    i = e % 2
                    for k0 in range(0, 16, 8):
                        B.dma("pool", "eg%d" % i, wg[i][:, k0:k0 + 8, :], w_eg[e, k0 * P:(k0 + 8) * P, :].rearrange("(k p) n -> p k n", p=P), [], [("wg", i)])
                        B.dma("pool", "eu%d" % i, wu[i][:, k0:k0 + 8, :], w_eu[e, k0 * P:(k0 + 8) * P, :].rearrange("(k p) n -> p k n", p=P), [], [("wu", i)])
                    for k in range(0, 4, 2):
                        B.dma("pool", "ed%d" % i, wdn[i][:, k:k + 2, :], w_ed[e, k * P:(k + 2) * P, :].rearrange("(k p) n -> p k n", p=P), [], [("wdn", i)])
                    B.dma("sp", "xb%d" % i, xb_[i][:], XG[e * CAP:(e + 1) * CAP, :].rearrange("(b p) d -> p b d", p=P), ["XG"] + [("XGs", t, j) for t in range(17) for j in range(8)], [("xb", i)])

                load_expert(0)
                yi = 0
                for e in range(NE):
                    i = e % 2
                    if e + 1 < NE:
                        load_expert(e + 1)
                    for blk in range(NBLK):
                        for h2 in range(2):
                            bt = B.bank()
                            for k8 in range(8):
                                k = h2 * 8 + k8
                                B.tr(psb[:, bt, k8 * P:(k8 + 1) * P], xb_[i][:, blk, k * P:(k + 1) * P], ident[:], [("xb", i), "ident"], B.bk(bt))
                            B.cp("act" if (blk * 2 + h2) % 2 == 0 else "dve", xbT[i][:, h2 * 8:(h2 + 1) * 8, blk * P:(blk + 1) * P],
                                 fap(psb, bt * 1024, [[P, 8], [1, P]]), B.bk(bt), [("xbT", i)])
                    for c in range(4):
                        bg = B.bank()
                        for k in range(16):
                            B.mm(ps[:, bg, 0:CAP], wg[i][:, k, c * P:(c + 1) * P], xbT[i][:, k, :], k == 0, k == 15, [("wg", i), ("xbT", i)], B.bk(bg))
                        B.act(sgt[c % 2][:], ps[:, bg, 0:CAP], AF.Silu, B.bk(bg), [("sgt", c % 2)])
                        bu = B.bank()
                        for k in range(16):
                            B.mm(ps[:, bu, 0:CAP], wu[i][:, k, c * P:(c + 1) * P], xbT[i][:, k, :], k == 0, k == 15, [("wu", i), ("xbT", i)], B.bk(bu))
                        B.tt("dve", acT[i][:, c, :], ps[:, bu, 0:CAP], sgt[c % 2][:], ALU.mult, B.bk(bu) + [("sgt", c % 2)], [("acT", i)])
                    for blk in range(NBLK):
                        y_ = yo[yi % 2]
                        yk = [("yo", yi % 2)]
                        for n4 in range(4):
                            b = B.bank()
                            for k in range(4):
                                B.mm(ps[:, b, :], acT[i][:, k, blk * P:(blk + 1) * P], wdn[i][:, k, n4 * 512:(n4 + 1) * 512], k == 0, k == 3, [("acT", i), ("wdn", i)], B.bk(b))
                            B.cp("act" if n4 % 2 == 0 else "dve", y_[:, n4 * 512:(n4 + 1) * 512], ps[:, b, :], B.bk(b), yk)
                        r0 = e * CAP + blk * P
                        B.dma("sp", "yst%d" % (yi % 2), Y[r0:r0 + P, :], y_[:], yk, ["Y"])
                        yi += 1
            S.barrier()

            pc = contextlib.ExitStack()
            with pc:
                def sbc(name, shape, dt):
                    return pc.enter_context(nc.sbuf_tensor(name, list(shape), dt))
                acc = [sbc("acc%d" % i, [P, D], F32) for i in range(2)]
                yg = [sbc("yg%d" % i, [P, D], BF16) for i in range(6)]
                lnt2 = [sbc("lnt2%d" % i, [P, 32], F32) for i in range(2)]
                B.dma("sp", "cst2", lng[:], fap_dram_bcast(ln2_g, D), ["lng"], ["lng"])
                B.dma("sp", "cst2", lnb[:], fap_dram_bcast(ln2_b, D), ["lnb"], ["lnb"])
                dgt = [sbc("dg%d" % i, [P, 16, P], BF16) for i in range(2)]
                wsm = [sbc("wsm%d" % i, [P, 32], F32) for i in range(2)]
                wbf = [sbc("wbf%d" % i, [P, 8], BF16) for i in range(2)]
                gi = [0]

                def f_ctx(tix):
                    nr = P if tix < NT else NS
                    r0 = tix * P if tix < NT else TOK
                    par = tix % 2
                    bks = [4 * par + n4 for n4 in range(4)]
                    return nr, r0, acc[par], [("acc", par)], dgt[par], [("dg", par)], wsm[par], [("wsm", par)], wbf[par], bks

                def f_prep(tix):
                    nr, r0, a, ak, dg, dgk, ws, wsk, wb16, bks = f_ctx(tix)
                    B.dma("sp", "bl%d" % (tix % 2), a[0:nr, :], BASE[r0:r0 + nr, :], ["BASE"], ak)
                    B.cp("dve", wb16[0:nr, :], w8all[0:nr, tix, :], [("w8", tix)], wsk)
                    B.cp("dve", ws[0:nr, 0:8], wb16[0:nr, :], wsk, wsk)
                    B.tt("dve", ws[0:nr, 8:16], w8all[0:nr, tix, :], ws[0:nr, 0:8], ALU.subtract, wsk + [("w8", tix)], wsk)
                    for j in range(8):
                        B.act(dg[0:nr, 2 * j, 0:nr], ident_f[0:nr, 0:nr], AF.Copy, wsk + ["ident_f"], dgk, scale=ws[0:nr, j:j + 1])
                        B.act(dg[0:nr, 2 * j + 1, 0:nr], ident_f[0:nr, 0:nr], AF.Copy, wsk + ["ident_f"], dgk, scale=ws[0:nr, 8 + j:9 + j])
                    for j in range(8):
                        g_ = yg[gi[0] % 6]
                        gk = [("yg", gi[0] % 6)]
                        S.dma("pool", "yg%d" % (gi[0] % 6), lambda e, g_=g_, tix=tix, j=j: e.indirect_dma_start(
                            out=g_[:], out_offset=None, in_=Y[:, :],
                            in_offset=bass.IndirectOffsetOnAxis(ap=d8all[:, tix, j:j + 1], axis=0),
                            bounds_check=B.bcreg(e), oob_is_err=False), ["Y", ("d8", tix)], gk)
                        for part in range(2):
                            for n4 in range(4):
                                B.mm(ps[0:nr, bks[n4], :], dg[0:nr, 2 * j + part, 0:nr], g_[0:nr, n4 * 512:(n4 + 1) * 512],
                                     j == 0 and part == 0, j == 7 and part == 1, gk + dgk, B.bk(bks[n4]))
                        gi[0] += 1

                def f_finish(tix):
                    nr, r0, a, ak, dg, dgk, ws, wsk, wb16, bks = f_ctx(tix)
                    for n4 in range(4):
                        B.tt("dve", a[0:nr, n4 * 512:(n4 + 1) * 512], a[0:nr, n4 * 512:(n4 + 1) * 512], ps[0:nr, bks[n4], :], ALU.add,
                             ak + B.bk(bks[n4]), ak)
                    layer_norm(a[0:nr, :], ak, nr, (lnt2[tix % 2][:], [("lnt2", tix % 2)], None))
                    if tix < NT:
                        B.dma("sp", "yout%d" % (tix % 2), yp[r0:r0 + nr, :], a[0:nr, :], ak, ["yp"])
                    else:
                        B.dma("sp", "yout%d" % (tix % 2), ys[:, :], a[0:nr, :], ak, ["ys"])

                f_prep(0)
                for tix in range(17):
                    if tix + 1 < 17:
                        f_prep(tix + 1)
                    f_finish(tix)
                if debug:
                    B.dma("sp", "dbg1_" + str(DBGC.next()), dbg["cnt"][:, :], cbase[:], ["cbase"], [])
                    dd = sbc("ddbg", [P, 17, 8], F32)
                    B.cp("dve", dd[:], d8all[:], [("d8", t) for t in range(17)], ["ddbg"])
                    B.dma("sp", "dbg2_" + str(DBGC.next()), dbg["d8"][:, :, :], dd[:], ["ddbg"], [])
                    B.dma("sp", "dbg3_" + str(DBGC.next()), dbg["w8"][:, :, :], w8all[:], [("w8", t) for t in range(17)], [])

        S.finalize(es)
        with nc.Block() as block:
            S.run_block(block)
    return nc


def _consts(core):
    hh = core % 2
    half = 8
    inv_freq = np.power(np.float32(500000.0), -np.arange(half, dtype=np.float32) * np.float32(2.0 / 16)).astype(np.float32)
    pos = np.zeros(2192, np.float32)
    pos[0:P] = hh * TOK - P + np.arange(P)
    pos[P:P + TOK] = hh * TOK + np.arange(TOK)
    pos[P + TOK:] = PAST
    pos = np.maximum(pos, 0).astype(np.float32)
    ang = (pos[:, None] * inv_freq[None, :]).astype(np.float32)
    cosv = np.cos(ang).astype(np.float32)
    sinv = np.sin(ang).astype(np.float32)
    cosT = np.ones((P, 2192), np.float32)
    sinT = np.zeros((P, 2192), np.float32)
    for p in range(P):
        d = p % 64
        if d < 16:
            cosT[p] = cosv[:, d % 8]
            sinT[p] = sinv[:, d % 8]
    ident = np.eye(P, dtype=np.float32)
    R = np.zeros((P, P), np.float32)
    for m in range(P):
        d = m % 64
        if d < 8:
            R[m, m + 8] = -1.0
        elif d < 16:
            R[m, m - 8] = 1.0
    rotT = R.T.copy()
    triU = np.triu(np.ones((P, P), np.float32), 1)
    ones = np.ones((P, P), np.float32)
    cst = np.concatenate([ident, rotT, triU, ones, np.zeros((P, 512), np.float32)], axis=1)
    a = np.arange(P)[:, None]
    c = np.arange(2 * P)[None, :]
    valid = (c > a) & (c <= a + P)
    mask_gen = np.where(valid, 0.0, NEG).astype(np.float32)
    if hh == 0:
        mask_first = np.where(valid & (c >= P), 0.0, NEG).astype(np.float32)
    else:
        mask_first = mask_gen
    msk = np.concatenate([mask_gen, mask_first], axis=1)
    c16 = np.zeros((P, 232), np.float32)
    for p in range(P):
        for kv in range(4):
            for h in range(16):
                c16[p, kv * 16 + h] = 1.0 if ((p // 64) == (h % 2) and (h // 4) == kv) else 0.0
    c16[:, 64:128] = (np.arange(NE) * CAP)[None, :]
    c16[:, 128:192] = np.arange(NE)[None, :]
    for h in range(16):
        for kv in range(4):
            c16[h, 192 + kv] = 1.0 if h // 4 == kv else 0.0
        for par in range(2):
            c16[h, 200 + par] = 1.0 if h % 2 == par else 0.0
    return dict(cosT=cosT, sinT=sinT, cst=cst, msk=msk, c16=c16)


_NC_CACHE = {}


def kernel(x_prompt, x_sample, cache_k, cache_v, state_conv, w_in, attn_sinks, conv_w,
           w_attn_out, w_conv_out, w_o, ln1_g, ln1_b, w_router, router_bias,
           w_exp_gate, w_exp_up, w_exp_down, w_sh_gate, w_sh_up, w_sh_down, ln2_g, ln2_b,
           _stop_after=None, _debug=False, _cores=None):
    f = lambda a: np.ascontiguousarray(np.asarray(a, dtype=np.float32))
    x_prompt, x_sample = f(x_prompt), f(x_sample)
    key = (_stop_after, _debug)
    if key not in _NC_CACHE:
        _NC_CACHE[key] = build_program(_stop_after, _debug)
    nc = _NC_CACHE[key]
    shared = dict(
        w_in=f(w_in[0]), sinks=f(attn_sinks), conv_w=f(conv_w[0]), w_ao=f(w_attn_out[0]), w_co=f(w_conv_out[0]),
        w_o=f(w_o[0]), ln1_g=f(ln1_g), ln1_b=f(ln1_b), w_r=f(w_router[0]), r_bias=f(router_bias),
        w_eg=f(w_exp_gate[0]), w_eu=f(w_exp_up[0]), w_ed=f(w_exp_down[0]), w_sg=f(w_sh_gate[0]),
        w_su=f(w_sh_up[0]), w_sd=f(w_sh_down[0]), ln2_g=f(ln2_g), ln2_b=f(ln2_b))
    if _stop_after is not None:
        for k_ in ("w_eg", "w_eu", "w_ed"):
            shared.pop(k_)
    in_maps = []
    cores = list(range(NCORES)) if _cores is None else list(_cores)
    for c in cores:
        n, hh = c // 2, c % 2
        xp = np.zeros((TOK + P, D), np.float32)
        if hh == 1:
            xp[0:P] = x_prompt[n, TOK - P:TOK]
        xp[P:] = x_prompt[n, hh * TOK:(hh + 1) * TOK]
        m = dict(shared)
        m.update(_consts(c))
        m.update(xp=xp, xs=f(x_sample[c * NS:(c + 1) * NS, 0, :]),
                 ck=f(cache_k[0, c * NS:(c + 1) * NS].reshape(NS, P, 256)),
                 cv=f(cache_v[0, c * NS:(c + 1) * NS].reshape(NS, P, 256)),
                 sc=f(state_conv[0, c * NS:(c + 1) * NS]))
        in_maps.append(m)
    res = run_bass_kernel_spmd(nc, in_maps, core_ids=list(range(len(cores))))
    R = res.results
    if _debug or _stop_after is not None:
        return R
    y_p = np.zeros((4, SEQ, D), np.float32)
    y_s = np.zeros((128, 1, D), np.float32)
    pk = np.zeros((1, 4, P, 4, 64), np.float32)
    pv = np.zeros((1, 4, P, 4, 64), np.float32)
    pc_ = np.zeros((1, 4, 2, 1024), np.float32)
    sk = np.zeros((1, 128, P, 4, 64), np.float32)
    sv = np.zeros((1, 128, P, 4, 64), np.float32)
    ssc_ = np.zeros((1, 128, 2, 1024), np.float32)
    for c in range(NCORES):
        n, hh = c // 2, c % 2
        y_p[n, hh * TOK:(hh + 1) * TOK] = R[c]["yp"]
        y_s[c * NS:(c + 1) * NS, 0] = R[c]["ys"]
        if hh == 1:
            pk[0, n] = R[c]["pck"].reshape(P, 4, 64)
            pv[0, n] = R[c]["pcv"].reshape(P, 4, 64)
            pc_[0, n] = R[c]["psc"]
        sk[0, c * NS:(c + 1) * NS] = R[c]["sck"].reshape(NS, P, 4, 64)
        sv[0, c * NS:(c + 1) * NS] = R[c]["scvo"].reshape(NS, P, 4, 64)
        ssc_[0, c * NS:(c + 1) * NS] = R[c]["ssc"]
    return (y_p, y_s, pk, pv, pc_, sk, sv, ssc_)
```

```python
import contextlib
import numpy as np
import concourse.bass as bass
import concourse.mybir as mybir
from concourse.bass_utils import run_bass_kernel_spmd

F32 = mybir.dt.float32
BF16 = mybir.dt.bfloat16
I32 = mybir.dt.int32
AF = mybir.ActivationFunctionType
ALU = mybir.AluOpType
AX = mybir.AxisListType

ENGS = ("pe", "act", "dve", "pool", "sp")


class _Ctr:
    def __init__(self):
        self.n = 0

    def next(self):
        self.n += 1
        return self.n


DBGC = _Ctr()

P = 128
D = 2048
NCORES = 8
SEQ = 4096
TOK = 2048
NS = 16
NT = 16
GT = 4
NG = NT // GT
NE = 64
CAP = 384
NBLK = CAP // P
NSLOT = NE * CAP
ALPHA = 2.0 ** 0.25
LN_EPS = 1e-5
PAST = 16384
OQ, OK_, OV, OB, OC, OH, OGA, OGC = 0, 1024, 1280, 1536, 2560, 3584, 4608, 6656
NEG = -30000.0


class Op:
    __slots__ = ("eng", "fn", "deps", "is_dma", "chan", "idx", "signal", "semval", "gidx")

    def __init__(self, eng, fn, is_dma, chan):
        self.eng = eng
        self.fn = fn
        self.deps = {}
        self.is_dma = is_dma
        self.chan = chan
        self.signal = False
        self.semval = None


def _slot(op):
    return ("ch", op.chan) if op.is_dma else ("eng", op.eng)


class Sched:
    def __init__(self, nc):
        self.nc = nc
        self.ops = {e: [] for e in ENGS}
        self.last_writer = {}
        self.readers = {}
        self.all_ops = []
        self.planning = False

    def _add(self, eng, fn, reads, writes, is_dma=False, chan=None):
        if self.planning:
            return None
        import os
        mx = int(os.environ.get("KMAXOPS", "0"))
        if mx and len(self.all_ops) >= mx:
            return None
        psr = [k for k in reads if isinstance(k, tuple) and k[0] == "ps"]
        if psr:
            writes = list(writes) + psr
        op = Op(eng, fn, is_dma, chan)
        deps = {}

        def add(d):
            if d is op:
                return
            s = _slot(d)
            o = deps.get(s)
            if o is None or d.gidx > o.gidx:
                deps[s] = d

        for k in reads:
            w = self.last_writer.get(k)
            if w is not None:
                add(w)
        for k in writes:
            w = self.last_writer.get(k)
            if w is not None:
                add(w)
            for r in self.readers.get(k, {}).values():
                add(r)
        op.deps = deps
        op.gidx = len(self.all_ops)
        for k in reads:
            self.readers.setdefault(k, {})[_slot(op)] = op
        for k in writes:
            self.last_writer[k] = op
            self.readers[k] = {}
        self.ops[eng].append(op)
        self.all_ops.append(op)
        return op

    def op(self, eng, fn, reads=(), writes=()):
        return self._add(eng, fn, reads, writes)

    def dma(self, eng, chan, fn, reads=(), writes=()):
        return self._add(eng, fn, reads, writes, is_dma=True, chan=chan)

    def barrier(self):
        if self.planning:
            return
        lasts = {}
        for op in self.all_ops:
            if op.fn is not None:
                lasts[_slot(op)] = op
        for e in ENGS:
            op = Op(e, None, False, None)
            op.deps = {s: d for s, d in lasts.items()}
            op.gidx = len(self.all_ops)
            self.ops[e].append(op)
            self.all_ops.append(op)

    def finalize(self, es):
        nc = self.nc
        for op in self.all_ops:
            for d in op.deps.values():
                if d.is_dma:
                    continue
                if d.eng == "pe" and op.eng == "pe" and not op.is_dma:
                    continue
                d.signal = True
        self.sem = {e: es.enter_context(nc.semaphore("sem_" + e)) for e in ENGS}
        chans = []
        for op in self.all_ops:
            if op.is_dma and op.chan not in chans:
                chans.append(op.chan)
        self.chsem = {c: es.enter_context(nc.semaphore("ch_" + str(c))) for c in chans}
        cnt = {e: 0 for e in ENGS}
        chcnt = {c: 0 for c in chans}
        for op in self.all_ops:
            if op.is_dma:
                chcnt[op.chan] += 16
                op.semval = chcnt[op.chan]
            elif op.signal:
                cnt[op.eng] += 1
                op.semval = cnt[op.eng]
        for op in self.all_ops:
            if op.is_dma and str(op.chan).startswith("cst"):
                op.semval = chcnt[op.chan]
        self.chfinal = chcnt

    def emit(self, ename, eng):
        seen = {}
        for op in self.ops[ename]:
            for s, d in op.deps.items():
                if (not d.is_dma) and d.eng == "pe" and op.eng == "pe" and not op.is_dma:
                    continue
                v = d.semval
                if v is None:
                    continue
                if seen.get(s, 0) >= v:
                    continue
                sem = self.chsem[s[1]] if s[0] == "ch" else self.sem[s[1]]
                eng.wait_ge(sem, v)
                seen[s] = v
            if op.fn is None:
                continue
            inst = op.fn(eng)
            if op.is_dma:
                inst.then_inc(self.chsem[op.chan], 16)
            elif op.signal:
                inst.then_inc(self.sem[op.eng], 1)

    def run_block(self, block):
        S = self

        def mk(ename):
            def body(eng):
                S.emit(ename, eng)
                if ename == "sp":
                    for c, v in S.chfinal.items():
                        if v > 0:
                            eng.wait_ge(S.chsem[c], v)
            return body

        block.tensor(mk("pe"))
        block.scalar(mk("act"))
        block.vector(mk("dve"))
        block.gpsimd(mk("pool"))
        block.sync(mk("sp"))


def fap(t, offset, dims, parts=None, p0=0):
    base = t if isinstance(t, bass.AP) else t[:]
    pst = base.ap[0][0]
    npart = base.ap[0][1] if parts is None else parts
    return bass.AP(tensor=base.tensor, offset=base.offset + p0 * pst + offset,
                   ap=[[pst, npart]] + [list(d) for d in dims])


class Builder:
    def __init__(self, stop_after=None, debug=False):
        self.stop_after = stop_after
        self.debug = debug
        self.nc = bass.Bass("TRN2", target_bir_lowering=False)
        self.S = Sched(self.nc)
        self.bank_rr = 0
        self.wplan = []
        self.wpos = 0
        self.tmp_rr = {}

    def bcreg(self, e):
        if getattr(self, "_bcreg", None) is None:
            self._bcreg = e.to_reg(NSLOT - 1)
        return self._bcreg

    def din(self, name, shape, dt=F32):
        return self.nc.dram_tensor(name, list(shape), dt, kind="ExternalInput").ap()

    def dout(self, name, shape, dt=F32):
        return self.nc.dram_tensor(name, list(shape), dt, kind="ExternalOutput").ap()

    def dscr(self, name, shape, dt=F32):
        return self.nc.dram_tensor(name, list(shape), dt, kind="Internal").ap()

    def bank(self, n=1):
        if n == 2 and self.bank_rr % 2 == 1:
            self.bank_rr += 1
        b = self.bank_rr % 6
        self.bank_rr += n
        return b

    def bk(self, b, n=1):
        return [("ps", b + i) for i in range(n)]

    def mm(self, out, lhsT, rhs, start, stop, reads, writes):
        self.S.op("pe", lambda e: e.matmul(out, lhsT=lhsT, rhs=rhs, start=start, stop=stop), reads, writes)

    def tr(self, out, in_, ident, reads, writes):
        self.S.op("pe", lambda e: e.transpose(out=out, in_=in_, identity=ident), reads, writes)

    def act(self, out, in_, func, reads, writes, scale=None, bias=None, accum_out=None):
        kw = {}
        if scale is not None:
            kw["scale"] = scale
        if bias is not None:
            kw["bias"] = bias
        if accum_out is not None:
            kw["accum_out"] = accum_out
        self.S.op("act", lambda e: e.activation(out=out, in_=in_, func=func, **kw), reads, writes)

    def tt(self, eng, out, in0, in1, op, reads, writes):
        self.S.op(eng, lambda e: e.tensor_tensor(out=out, in0=in0, in1=in1, op=op), reads, writes)

    def ts(self, eng, out, in0, s1, op0, reads, writes, s2=None, op1=None, accum_out=None):
        kw = {}
        if op1 is not None:
            kw["op1"] = op1
        if accum_out is not None:
            kw["accum_out"] = accum_out
        self.S.op(eng, lambda e: e.tensor_scalar(out=out, in0=in0, scalar1=s1, scalar2=s2, op0=op0, **kw), reads, writes)

    def stt(self, out, in0, scalar, in1, op0, op1, reads, writes, accum_out=None):
        kw = {}
        if accum_out is not None:
            kw["accum_out"] = accum_out
        self.S.op("dve", lambda e: e.scalar_tensor_tensor(out=out, in0=in0, scalar=scalar, in1=in1, op0=op0, op1=op1, **kw), reads, writes)

    def red(self, out, in_, op, reads, writes, axis=AX.X):
        self.S.op("dve", lambda e: e.tensor_reduce(out=out, in_=in_, axis=axis, op=op), reads, writes)

    def cp(self, eng, out, in_, reads, writes):
        if eng == "act":
            self.S.op("act", lambda e: e.activation(out=out, in_=in_, func=AF.Copy), reads, writes)
        else:
            self.S.op(eng, lambda e: e.tensor_copy(out=out, in_=in_), reads, writes)

    def dma(self, eng, chan, out, in_, reads, writes, **kw):
        return self.S.dma(eng, chan, lambda e: e.dma_start(out=out, in_=in_, **kw), reads, writes)

    def wget(self, spec):
        S = self.S
        i = self.wpos
        self.wpos += 1
        if S.planning:
            self.wplan.append(spec)
        slot = i % self.NB
        nk, ncols, parts = spec
        while self.wissued < min(len(self.wplan), i + self.NB - 2):
            self._wissue(self.wissued)
            self.wissued += 1
        ap = fap(self.wring, slot * self.WSLOT, [[ncols, nk], [1, ncols]])
        return ap, [("w", slot)]

    def _wissue(self, j):
        if self.S.planning:
            return
        nk, ncols, parts = self.wplan[j]
        slot = j % self.NB
        for (c0, n, src) in parts:
            dst = fap(self.wring, slot * self.WSLOT + c0, [[ncols, nk], [1, n]])
            self.dma("pool", "w%d" % slot, dst, src.rearrange("(k p) n -> p k n", p=P), [], [("w", slot)])

    def areset(self):
        self.apos = 0

    def aalloc(self, nbytes):
        nbytes = (nbytes + 63) // 64 * 64
        off = self.apos
        self.apos += nbytes
        assert self.apos <= self.ARENA_BYTES, ("arena overflow", self.apos)
        keys = [("ar", b) for b in range(off // 2048, (off + nbytes - 1) // 2048 + 1)]
        return off, keys

    def af32(self, n, dims=None):
        off, keys = self.aalloc(n * 4)
        ap = fap(self.arena, off // 4, dims if dims is not None else [[1, n]])
        return ap, keys, off // 4

    def abf(self, n, dims=None):
        off, keys = self.aalloc(n * 2)
        ap = fap(self.arena_bf, off // 2, dims if dims is not None else [[1, n]])
        return ap, keys, off // 2


def build_program(stop_after=None, debug=False):
    B = Builder(stop_after, debug)
    nc = B.nc
    S = B.S

    xp = B.din("xp", [TOK + P, D])
    xs = B.din("xs", [NS, D])
    ck = B.din("ck", [NS, P, 256])
    cv = B.din("cv", [NS, P, 256])
    scv = B.din("sc", [NS, 2, 1024])
    w_in = B.din("w_in", [D, 8704])
    sinks = B.din("sinks", [1, 16])
    conv_w = B.din("conv_w", [3, 1024])
    w_ao = B.din("w_ao", [1024, D])
    w_co = B.din("w_co", [1024, D])
    w_o = B.din("w_o", [D, D])
    ln1_g = B.din("ln1_g", [1, D])
    ln1_b = B.din("ln1_b", [1, D])
    w_r = B.din("w_r", [D, NE])
    r_bias = B.din("r_bias", [1, NE])
    if stop_after is None:
        w_eg = B.din("w_eg", [NE, D, 512])
        w_eu = B.din("w_eu", [NE, D, 512])
        w_ed = B.din("w_ed", [NE, 512, D])
    w_sg = B.din("w_sg", [D, 512])
    w_su = B.din("w_su", [D, 512])
    w_sd = B.din("w_sd", [512, D])
    ln2_g = B.din("ln2_g", [1, D])
    ln2_b = B.din("ln2_b", [1, D])
    cosd = B.din("cosT", [P, 2192])
    sind = B.din("sinT", [P, 2192])
    cst = B.din("cst", [P, 1024])
    mskd = B.din("msk", [P, 512])
    c16 = B.din("c16", [P, 64 + 64 + 64 + 8 + 32])

    yp = B.dout("yp", [TOK, D])
    ys = B.dout("ys", [NS, D])
    pck = B.dout("pck", [P, 256])
    pcv = B.dout("pcv", [P, 256])
    psc = B.dout("psc", [2, 1024])
    sck = B.dout("sck", [NS, P, 256])
    scvo = B.dout("scvo", [NS, P, 256])
    ssc = B.dout("ssc", [NS, 2, 1024])
    if debug:
        dbg = {k: B.dout("dbg_" + k, shp) for k, shp in [("h", [TOK + NS, D]), ("cnt", [P, NE]), ("attn", [P, 8, 640]), ("conv", [P, 8, 640]), ("d8", [P, 17, 8]), ("w8", [P, 17, 8])]}

    XG = B.dscr("XG", [NSLOT + P, D], BF16)
    Y = B.dscr("Y", [NSLOT, D], BF16)
    BASE = B.dscr("BASE", [TOK + P, D], F32)

    es = contextlib.ExitStack()
    with es:
        def sb(name, shape, dt):
            return es.enter_context(nc.sbuf_tensor(name, list(shape), dt))

        ps = es.enter_context(nc.psum_tensor("ps", [P, 8, 512], F32))
        psb = ps.bitcast(BF16)

        ident_f = sb("ident_f", [P, P], F32)
        ident = sb("ident", [P, P], BF16)
        rotT = sb("rotT", [P, P], BF16)
        triU = sb("triU", [P, P], BF16)
        ones = sb("ones", [P, P], BF16)
        msk = sb("mskt", [P, 512], F32)
        c16t = sb("c16t", [P, 232], F32)
        sink8 = sb("sink8", [P, 16], F32)
        sinkraw = sb("sinkraw", [P, 16], F32)
        rbias = sb("rbias", [P, NE], F32)
        convw = sb("convw", [P, 8, 3], F32)
        lng = sb("lng", [P, D], F32)
        lnb = sb("lnb", [P, D], F32)
        d8all = sb("d8all", [P, 17, 8], I32)
        w8all = sb("w8all", [P, 17, 8], F32)
        cbase = sb("cbase", [P, NE], F32)
        epsT = sb("epsT", [P, 1], F32)
        sinkcol = sb("sinkcol", [P, 1], F32)
        rsel_t = sb("rsel", [P, 5, NE], BF16)

        def load_consts():
            B.dma("sp", "cst", ident_f[:], cst[:, 0:128], [], ["ident_f"])
            B.dma("pool", "cstp", ident[:], cst[:, 0:128], [], ["ident"])
            B.dma("pool", "cstp", rotT[:], cst[:, 128:256], [], ["rotT"])
            B.dma("pool", "cstp", triU[:], cst[:, 256:384], [], ["triU"])
            B.dma("pool", "cstp", ones[:], cst[:, 384:512], [], ["ones"])
            B.dma("sp", "cst", msk[:], mskd[:, :], [], ["msk"])
            B.dma("sp", "cst", c16t[:], c16[:, :], [], ["c16t"])
            B.dma("sp", "cst", sinkraw[:], fap_dram_bcast(sinks, 16), [], ["sinkraw"])
            B.dma("sp", "cst", rbias[:], fap_dram_bcast(r_bias, NE), [], ["rbias"])
            for j in range(3):
                B.dma("sp", "cst", convw[:, :, j], conv_w[j, :].rearrange("(c p) -> p c", p=P), [], ["convw%d" % j], allow_slow_non_contiguous=True)
            B.ts("dve", fap(sink8, 0, [[4, 4], [2, 2], [1, 2]]), fap(sinkraw, 0, [[4, 4], [1, 2], [2, 2]]), 8.0, ALU.mult, ["sinkraw"], ["sink8"])
            S.op("dve", lambda e: e.memset(cbase[:], 0.0), [], ["cbase"])
            S.op("dve", lambda e: e.memset(w8all[:], 0.0), [], [("w8", t) for t in range(17)])
            S.op("dve", lambda e: e.memset(epsT[:], LN_EPS), [], ["epsT"])
            B.dma("sp", "cst", sinkcol[0:16, :], sinks.rearrange("a h -> h a"), [], ["sinkcol"], allow_slow_non_contiguous=True)
            B.ts("dve", sinkcol[0:16, :], sinkcol[0:16, :], 8.0, ALU.mult, ["sinkcol"], ["sinkcol"])

        def fap_dram_bcast(src, n):
            return bass.AP(tensor=src.tensor, offset=src.offset, ap=[[0, P], [1, n]])

        load_consts()

        pa = contextlib.ExitStack()
        with pa:
            def sba(name, shape, dt):
                return pa.enter_context(nc.sbuf_tensor(name, list(shape), dt))

            NCMAX = 640
            bigT = sba("bigT", [P, 16, NCMAX], BF16)
            attnT = sba("attnT", [P, 8, NCMAX], BF16)
            convT = sba("convT", [P, 8, NCMAX], BF16)
            kTl = sba("kTl", [P, 4, P + NCMAX], BF16)
            kT32 = sba("kT32", [P, 2, 144], F32)
            vl = sba("vl", [P, 6, 256], BF16)
            v32 = sba("v32", [P, 2, 256], F32)
            cosl = sba("cosl", [P, NCMAX], F32)
            sinl = sba("sinl", [P, NCMAX], F32)
            uprev = sba("uprev", [P, 8, 2], F32)
            hres = sba("hres", [P, 5, D], F32)
            B.NB = 6
            B.WSLOT = 4096
            B.wring = sba("wring", [P, B.NB * B.WSLOT], BF16)
            B.ARENA_BYTES = 38 * 1024
            B.arena = sba("arena", [P, B.ARENA_BYTES // 4], F32)
            B.arena_bf = B.arena.bitcast(BF16)

            hres_bf = hres.bitcast(BF16)
            def zero_fill_xg():
                zt = hres_bf[:, 4, 0:D]
                S.op("dve", lambda e: e.memset(zt, 0.0), [], [("hres", 4)])
                nrow_total = NSLOT + P
                r = 0
                while r < nrow_total:
                    n = min(4 * P, nrow_total - r)
                    B.dma("act", "zf", XG[r:r + n, :].rearrange("(a p) d -> p a d", p=P), fap(hres_bf, 4 * 4096, [[0, n // P], [1, D]]), [("hres", 4)], ["XG"])
                    r += n

            B.dma("sp", "cst", lng[:], fap_dram_bcast(ln1_g, D), [], ["lng"])
            B.dma("sp", "cst", lnb[:], fap_dram_bcast(ln1_b, D), [], ["lnb"])

            if debug:
                S.op("dve", lambda e: e.memset(attnT[:], 0.0), [], ["attnT"])
                S.op("dve", lambda e: e.memset(convT[:], 0.0), [], ["convT"])

            def phase_a():
                B.wpos = 0
                B.wissued = 0
                for g in range(NG):
                    group(g)

            def make_tiles(g):
                halo = (g == 0)
                samp = (g == NG - 1)
                moff = P if halo else 0
                soff = moff + GT * P
                tl = []
                if halo:
                    tl.append((0, P, xp[0:P, :], "halo", None))
                for t in range(GT):
                    tt_ = g * GT + t
                    tl.append((moff + t * P, P, xp[P + tt_ * P:P + (tt_ + 1) * P, :], "main", tt_))
                if samp:
                    tl.append((soff, NS, xs[:, :], "samp", None))
                return tl

            def xpf_loc(i, k):
                if i < 2:
                    return attnT, i * 2048 + k * P, "attnT"
                if i < 4:
                    return convT, (i - 2) * 2048 + k * P, "convT"
                if k < 8:
                    return attnT, 4096 + k * P, "attnT"
                return convT, 4096 + (k - 8) * P, "convT"

            def prefetch_x(tl):
                for i, (c0, nr, src_, kind, tt_) in enumerate(tl):
                    if i < 4:
                        base, off, key = xpf_loc(i, 0)
                        B.dma("pool", "xpf%d" % i, fap(base, off, [[1, D]], parts=nr), src_, [], [key])
                    else:
                        B.dma("pool", "xpf4", fap(attnT, 4096, [[1, 1024]], parts=nr), src_[:, 0:1024], [], ["attnT"])
                        B.dma("pool", "xpf5", fap(convT, 4096, [[1, 1024]], parts=nr), src_[:, 1024:2048], [], ["convT"])

            def group(g):
                halo = (g == 0)
                samp = (g == NG - 1)
                moff = P if halo else 0
                NC = moff + GT * P + (NS if samp else 0)
                soff = moff + GT * P
                absb = (P + GT * P * g) - moff
                own_segs = [(moff, GT * P)] + ([(soff, NS)] if samp else [])
                all_segs = ([(0, P)] if halo else []) + own_segs
                tiles = make_tiles(g)
                own_tiles = [tl for tl in tiles if tl[3] != "halo"]
                if g == 0:
                    prefetch_x(tiles)

                B.areset()
                B.dma("sp", "tabc", cosl[:, 0:NC], cosd[:, absb:absb + NC], [], ["cosl"])
                B.dma("sp", "tabs", sinl[:, 0:NC], sind[:, absb:absb + NC], [], ["sinl"])
                for i, (c0, nr, src, kind, tt_) in enumerate(tiles):
                    b2 = B.bank(2)
                    for k in range(16):
                        base, off, key = xpf_loc(i, k)
                        o = fap(psb, (b2 + k // 8) * 1024 + (k % 8) * nr, [[1, nr]])
                        B.tr(o, fap(base, off, [[1, P]], parts=nr), ident[0:nr, 0:nr], [key, "ident"], B.bk(b2 + k // 8))
                    for hh in range(2):
                        src_ap = fap(psb, (b2 + hh) * 1024, [[nr, 8], [1, nr]])
                        B.cp("dve" if hh == 0 else "act", bigT[:, hh * 8:(hh + 1) * 8, c0:c0 + nr], src_ap, B.bk(b2 + hh), ["bigT"])

                if B.stop_after == "S0":
                    return
                def proj(wt, wk, nk, col_lo, segs, rhs_t, rhs_key, consumer):
                    for (c0, n) in segs:
                        b = B.bank()
                        for k in range(nk):
                            B.mm(ps[:, b, 0:n], wt[:, k, col_lo:col_lo + P], rhs_t[:, k, c0:c0 + n], k == 0, k == nk - 1,
                                 wk + [rhs_key], B.bk(b))
                        consumer(b, c0, n)

                def win_spec(col0, ncols=256):
                    return (16, ncols, [(0, ncols, w_in[:, col0:col0 + ncols])])

                B.areset()
                qT, qTk, _ = B.abf(8 * NCMAX, [[NCMAX, 8], [1, NCMAX]])
                qmark = B.apos
                qb = [B.abf(512) for _ in range(2)]
                t1 = [B.af32(512) for _ in range(2)]
                t2 = [B.af32(512) for _ in range(2)]
                rr = [0]

                def rope(b, c0, n, out_bf, out_keys, out_f32=None, out_f32_keys=None):
                    i = rr[0] % 2
                    rr[0] += 1
                    q_b, qbk, _ = qb[i]
                    a1, a1k, _ = t1[i]
                    a2, a2k, _ = t2[i]
                    B.cp("act", q_b[:, 0:n], ps[:, b, 0:n], B.bk(b), qbk)
                    B.tt("dve", a1[:, 0:n], ps[:, b, 0:n], cosl[:, c0:c0 + n], ALU.mult, B.bk(b) + ["cosl"], a1k)
                    b2 = B.bank()
                    B.mm(ps[:, b2, 0:n], rotT[:], q_b[:, 0:n], True, True, qbk + ["rotT"], B.bk(b2))
                    B.tt("dve", a2[:, 0:n], ps[:, b2, 0:n], sinl[:, c0:c0 + n], ALU.mult, B.bk(b2) + ["sinl"], a2k)
                    B.tt("dve", out_bf, a1[:, 0:n], a2[:, 0:n], ALU.add, a1k + a2k, out_keys)
                    if out_f32 is not None:
                        B.tt("dve", out_f32, a1[:, 0:n], a2[:, 0:n], ALU.add, a1k + a2k, out_f32_keys)

                for qs in range(4):
                    wt, wk = B.wget(win_spec(OQ + qs * 256))
                    for cc in range(2):
                        c = qs * 2 + cc
                        proj(wt, wk, 16, cc * P, own_segs, bigT, "bigT",
                             lambda b, c0, n, c=c: rope(b, c0, n, qT[:, c, c0:c0 + n], qTk))
                if B.stop_after == "S1q":
                    return
                if not halo:
                    ksrc = (P if g == 1 else 0) + GT * P
                    B.cp("act", kTl[:, :, 0:P], kTl[:, :, ksrc:ksrc + P], ["kTl"], ["kTl"])
                    B.cp("act", vl[:, 0, :], vl[:, (GT + 1) if g == 1 else GT, :], ["vl"], ["vl"])
                wt, wk = B.wget(win_spec(OK_))
                krt = [B.af32(512) for _ in range(2)]
                kri = [0]
                ksegs = list(all_segs)
                if samp:
                    ksegs = [(moff, (GT - 1) * P), (moff + (GT - 1) * P, P), (soff, NS)]
                for kp in range(2):
                    def kcons(b, c0, n, kp=kp):
                        kr, krk, _ = krt[kri[0] % 2]
                        kri[0] += 1
                        rope(b, c0, n, kr[:, 0:n], krk)
                        for j in range(2):
                            kv = 2 * kp + j
                            for dup in range(2):
                                B.cp("act" if dup == 0 else "dve", kTl[64 * dup:64 * dup + 64, kv, P + c0:P + c0 + n],
                                     kr[64 * j:64 * j + 64, 0:n], krk, ["kTl"])
                        if samp and c0 == soff:
                            B.cp("act", kT32[:, kp, P:P + NS], kr[:, 0:n], krk, ["kT32"])
                        elif samp and c0 == moff + (GT - 1) * P:
                            B.cp("dve", kT32[:, kp, 0:P], kr[:, 0:n], krk, ["kT32"])
                    proj(wt, wk, 16, kp * P, ksegs, bigT, "bigT", kcons)
                if B.stop_after == "S1k":
                    return
                wt, wk = B.wget(win_spec(OV))
                for li, (c0, nr, src, kind, tt_) in enumerate(tiles):
                    b = B.bank()
                    for k in range(16):
                        B.mm(ps[0:nr, b, 0:256], bigT[:, k, c0:c0 + nr], wt[:, k, :], k == 0, k == 15, wk + ["bigT"], B.bk(b))
                    B.cp("act", vl[0:nr, 1 + li, :], ps[0:nr, b, 0:256], B.bk(b), ["vl"])
                    if samp and kind == "samp":
                        B.cp("dve", v32[0:nr, 1, :], ps[0:nr, b, 0:256], B.bk(b), ["v32"])
                    if samp and kind == "main" and tt_ == NT - 1:
                        B.cp("dve", v32[:, 0, :], ps[:, b, 0:256], B.bk(b), ["v32"])

                if B.stop_after == "S1a":
                    return
                B.apos = qmark
                Sm = [B.af32(1024, [[256, 4], [1, 256]]) for _ in range(2)]
                Pn = [B.abf(1024, [[256, 4], [1, 256]]) for _ in range(2)]
                PTt = [B.abf(1024, [[128, 8], [1, 128]]) for _ in range(2)]
                sm_ = [B.af32(32) for _ in range(2)]
                items = []
                for li, (c0, nr, src_, kind, tt_) in enumerate(tiles):
                    if kind == "main":
                        for kv in range(4):
                            items.append((li, c0, tt_, kv))
                bpv = 6
                sc_bank = {}

                def a_scores(idx):
                    li, c0, tt_, kv = items[idx]
                    b2 = B.bank(2)
                    sc_bank[idx] = b2
                    for hh in range(4):
                        c = 2 * kv + hh // 2
                        half = hh % 2
                        B.mm(ps[:, b2 + half, (hh // 2) * 256:(hh // 2) * 256 + 256],
                             qT[64 * half:64 * half + 64, c, c0:c0 + P],
                             kTl[64 * half:64 * half + 64, kv, c0:c0 + 256], True, True,
                             qTk + ["kTl"], B.bk(b2 + half))

                def a_ctx(idx):
                    li, c0, tt_, kv = items[idx]
                    i = idx % 2
                    Sx, Sk, _ = Sm[i]
                    Px, Pk, _ = Pn[i]
                    PT, PTk, _ = PTt[i]
                    sx, sk_, _ = sm_[i]
                    return li, c0, tt_, kv, Sx, Sk, Px, Pk, PT, PTk, sx, sk_

                def a_A(idx):
                    li, c0, tt_, kv, Sx, Sk, Px, Pk, PT, PTk, sx, sk_ = a_ctx(idx)
                    b2 = sc_bank[idx]
                    mk_off = 256 if tt_ == 0 else 0
                    pin = fap(ps, b2 * 512, [[256, 4], [1, 256]])
                    B.tt("dve", Sx, pin, fap(msk, mk_off, [[0, 4], [1, 256]]), ALU.add, B.bk(b2, 2) + ["msk"], Sk)
                    mx = sx[:, 0:4]
                    m8 = sx[:, 4:8]
                    sm = sx[:, 8:12]
                    tq = sx[:, 12:16]
                    es_ = sx[:, 16:20]
                    rv = sx[:, 20:24]
                    nm = sx[:, 24:28]
                    B.red(mx, Sx, ALU.max, Sk, sk_)
                    B.tt("dve", m8, mx, sink8[:, 4 * kv:4 * kv + 4], ALU.max, sk_ + ["sink8"], sk_)
                    B.ts("dve", nm, m8, -0.125, ALU.mult, sk_, sk_)
                    B.tt("dve", tq, sink8[:, 4 * kv:4 * kv + 4], m8, ALU.subtract, sk_ + ["sink8"], sk_)

                def a_E(idx):
                    li, c0, tt_, kv, Sx, Sk, Px, Pk, PT, PTk, sx, sk_ = a_ctx(idx)
                    for s in range(4):
                        B.act(Sx[:, s, :], Sx[:, s, :], AF.Exp, Sk + sk_, Sk + sk_, scale=0.125, bias=sx[:, 24 + s:25 + s], accum_out=sx[:, 8 + s:9 + s])

                def a_B(idx):
                    li, c0, tt_, kv, Sx, Sk, Px, Pk, PT, PTk, sx, sk_ = a_ctx(idx)
                    sm = sx[:, 8:12]
                    tq = sx[:, 12:16]
                    es_ = sx[:, 16:20]
                    rv = sx[:, 20:24]
                    B.act(es_, tq, AF.Exp, sk_, sk_, scale=0.125)
                    B.tt("dve", rv, sm, es_, ALU.add, sk_, sk_)
                    S.op("dve", lambda e, rv=rv: e.reciprocal(out=rv, in_=rv), sk_, sk_)
                    for s in range(4):
                        B.act(Px[:, s, :], Sx[:, s, :], AF.Copy, Sk + sk_, Pk, scale=sx[:, 20 + s:21 + s])

                def a_C(idx):
                    li, c0, tt_, kv, Sx, Sk, Px, Pk, PT, PTk, sx, sk_ = a_ctx(idx)
                    bt = B.bank()
                    for hh in range(4):
                        for kb in range(2):
                            B.tr(psb[:, bt, (hh * 2 + kb) * P:(hh * 2 + kb + 1) * P], Px[:, (hh % 2) * 2 + hh // 2, kb * P:(kb + 1) * P], ident[:],
                                 Pk + ["ident"], B.bk(bt))
                    B.cp("dve", PT, fap(psb, bt * 1024, [[128, 8], [1, 128]]), B.bk(bt), PTk)
                    for hh in range(4):
                        c = 2 * kv + hh // 2
                        half = hh % 2
                        o = ps[64 * half:64 * half + 64, bpv + c // 4, (c % 4) * P:(c % 4 + 1) * P]
                        for kb in range(2):
                            B.mm(o, vl[:, li + kb, kv * 64:(kv + 1) * 64], PT[:, hh * 2 + kb, :], kb == 0, kb == 1,
                                 ["vl"] + PTk, B.bk(bpv + c // 4))
                    if kv == 3:
                        for hh in range(2):
                            B.cp("act" if hh == 0 else "dve", attnT[:, hh * 4:(hh + 1) * 4, c0:c0 + P],
                                 fap(ps, (bpv + hh) * 512, [[128, 4], [1, 128]]), B.bk(bpv + hh), ["attnT"])

                if items:
                    n_it = len(items)
                    assert n_it % 2 == 0
                    a_scores(0)
                    a_scores(1)
                    a_A(0)
                    a_A(1)
                    for i0_ in range(0, n_it, 2):
                        i1_ = i0_ + 1
                        a_E(i0_)
                        a_E(i1_)
                        if i0_ + 2 < n_it:
                            a_scores(i0_ + 2)
                            a_scores(i1_ + 2)
                        a_B(i0_)
                        a_B(i1_)
                        if i0_ + 2 < n_it:
                            a_A(i0_ + 2)
                            a_A(i1_ + 2)
                        a_C(i0_)
                        a_C(i1_)

                if B.stop_after == "S1b":
                    return
                if samp:
                    B.apos = qmark
                    sample_attention(qT, qTk, soff)

                if B.stop_after == "S1":
                    return

                if g == 0:
                    zero_fill_xg()
                B.areset()
                NU = 2 + NCMAX
                hs = [B.af32(512) for _ in range(2)]
                bs = [B.af32(512) for _ in range(2)]
                ub = [B.af32(NU) for _ in range(2)]
                tb = [B.af32(512) for _ in range(2)]
                if samp:
                    stT, stTk, _ = B.af32(8 * 32, [[32, 8], [1, 32]])
                    st_in, st_ink, _ = B.af32(1024)
                    us_all, us_allk, _ = B.af32(8 * NS, [[NS, 8], [1, NS]])
                    ul_all, ul_allk, _ = B.af32(8 * 2, [[2, 8], [1, 2]])
                    B.dma("sp", "st", fap(st_in, 0, [[1, 1024]], parts=32), scv.rearrange("b j c -> (b j) c"), [], st_ink)
                    bq = B.bank()
                    for cc in range(8):
                        B.tr(ps[:, bq, cc * 32:(cc + 1) * 32], fap(st_in, cc * P, [[1, P]], parts=32), ident_f[0:32, 0:32],
                             st_ink + ["ident_f"], B.bk(bq))
                    B.cp("dve", stT, fap(ps, bq * 512, [[32, 8], [1, 32]]), B.bk(bq), stTk)
                for cc in range(8):
                    if cc % 2 == 0:
                        wb_, wbk_ = B.wget(win_spec(OB + (cc // 2) * 256))
                        wc_, wck_ = B.wget(win_spec(OC + (cc // 2) * 256))
                        wh_, whk_ = B.wget(win_spec(OH + (cc // 2) * 256))
                    cj = (cc % 2) * P
                    i = cc % 2
                    u, uk, _ = ub[i]
                    if not halo:
                        B.cp("act", u[:, 0:2], uprev[:, cc, :], ["uprev"], uk)
                    for (c0, n) in all_segs:
                        hx, hk, _ = hs[i]
                        bx, bxk, _ = bs[i]
                        tx, txk, _ = tb[i]
                        is_own = (c0, n) in own_segs
                        bh = B.bank()
                        for k in range(16):
                            B.mm(ps[:, bh, 0:n], wh_[:, k, cj:cj + P], bigT[:, k, c0:c0 + n], k == 0, k == 15, whk_ + ["bigT"], B.bk(bh))
                        B.cp("act", hx[:, 0:n], ps[:, bh, 0:n], B.bk(bh), hk)
                        bc = B.bank()
                        for k in range(16):
                            B.mm(ps[:, bc, 0:n], wc_[:, k, cj:cj + P], bigT[:, k, c0:c0 + n], k == 0, k == 15, wck_ + ["bigT"], B.bk(bc))
                        B.tt("dve", u[:, 2 + c0:2 + c0 + n], ps[:, bc, 0:n], hx[:, 0:n], ALU.mult, B.bk(bc) + hk, uk)
                        if not is_own:
                            continue
                        bb = B.bank()
                        for k in range(16):
                            B.mm(ps[:, bb, 0:n], wb_[:, k, cj:cj + P], bigT[:, k, c0:c0 + n], k == 0, k == 15, wbk_ + ["bigT"], B.bk(bb))
                        B.cp("act", bx[:, 0:n], ps[:, bb, 0:n], B.bk(bb), bxk)
                        if c0 == soff and samp:
                            s0 = fap(stT, cc * 32, [[2, NS]])
                            s1 = fap(stT, cc * 32 + 1, [[2, NS]])
                            B.ts("dve", tx[:, 0:n], u[:, 2 + c0:2 + c0 + n], convw[:, cc, 2:3], ALU.mult, uk + ["convw0", "convw1", "convw2"], txk)
                            B.stt(tx[:, 0:n], s1, convw[:, cc, 1:2], tx[:, 0:n], ALU.mult, ALU.add, stTk + ["convw0", "convw1", "convw2"] + txk, txk)
                            B.stt(tx[:, 0:n], s0, convw[:, cc, 0:1], tx[:, 0:n], ALU.mult, ALU.add, stTk + ["convw0", "convw1", "convw2"] + txk, txk)
                            B.cp("act", us_all[:, cc, :], u[:, 2 + c0:2 + c0 + n], uk, us_allk)
                        else:
                            B.ts("dve", tx[:, 0:n], u[:, 2 + c0:2 + c0 + n], convw[:, cc, 2:3], ALU.mult, uk + ["convw0", "convw1", "convw2"], txk)
                            B.stt(tx[:, 0:n], u[:, 1 + c0:1 + c0 + n], convw[:, cc, 1:2], tx[:, 0:n], ALU.mult, ALU.add, uk + ["convw0", "convw1", "convw2"] + txk, txk)
                            B.stt(tx[:, 0:n], u[:, c0:c0 + n], convw[:, cc, 0:1], tx[:, 0:n], ALU.mult, ALU.add, uk + ["convw0", "convw1", "convw2"] + txk, txk)
                            B.cp("act", uprev[:, cc, :], u[:, 2 + c0 + n - 2:2 + c0 + n], uk, ["uprev"])
                            if samp:
                                B.cp("act", ul_all[:, cc, :], u[:, 2 + c0 + n - 2:2 + c0 + n], uk, ul_allk)
                        B.tt("dve", convT[:, cc, c0:c0 + n], tx[:, 0:n], bx[:, 0:n], ALU.mult, txk + bxk, ["convT"])
                if samp:
                    def fm_to_tm(src3, srck, n):
                        b2 = B.bank(2)
                        for cc in range(8):
                            B.tr(ps[0:n, b2 + cc // 4, (cc % 4) * P:(cc % 4 + 1) * P], src3[:, cc, :], ident_f[:], srck + ["ident_f"], B.bk(b2 + cc // 4))
                        o, ok_, _ = B.af32(1024)
                        B.cp("dve", fap(o, 0, [[1, 1024]], parts=n), fap(ps, b2 * 512, [[1, 1024]], parts=n), B.bk(b2, 2), ok_)
                        return o, ok_
                    uo, uok = fm_to_tm(us_all, us_allk, NS)
                    B.dma("sp", "o_ssc", ssc[:, 1, :], fap(uo, 0, [[1, 1024]], parts=NS), uok, ["ssc"])
                    B.dma("sp", "o_ssc0", ssc[:, 0, :], scv[:, 1, :], [], ["ssc0"])
                    ul, ulk = fm_to_tm(ul_all, ul_allk, 2)
                    B.dma("sp", "o_psc", psc[:, :], fap(ul, 0, [[1, 1024]], parts=2), ulk, ["psc"])
                    kc, kck, _ = B.af32(256)
                    bq5 = B.bank()
                    for kp in range(2):
                        B.tr(ps[:, bq5, kp * P:(kp + 1) * P], kT32[:, kp, 0:P], ident_f[:], ["kT32", "ident_f"], B.bk(bq5))
                    B.cp("dve", kc, ps[:, bq5, 0:256], B.bk(bq5), kck)
                    B.dma("sp", "o_pck", pck[:, :], kc, kck, ["pck"])
                    B.dma("sp", "o_pcv", pcv[:, :], v32[:, 0, :], ["v32"], ["pcv"])

                if debug and g == 0:
                    B.dma("pool", "dbgp0_" + str(DBGC.next()), dbg["attn"][:, :, :], attnT[:], ["attnT"], [])
                    B.dma("pool", "dbgp1_" + str(DBGC.next()), dbg["conv"][:, :, :], convT[:], ["convT"], [])
                if B.stop_after == "S2":
                    return

                B.areset()
                mixT, mixk, _ = B.abf(16 * NCMAX, [[NCMAX, 16], [1, NCMAX]])
                sg = [B.af32(512) for _ in range(2)]
                ta = [B.af32(512) for _ in range(2)]
                for jb in range(8):
                    wga, wgak = B.wget(win_spec(OGA + jb * 256))
                    wgc, wgck = B.wget(win_spec(OGC + jb * 256))
                    wao, waok = B.wget((8, 512, [(0, 256, w_ao[:, jb * 256:(jb + 1) * 256]), (256, 256, w_co[:, jb * 256:(jb + 1) * 256])]))
                    for jj in range(2):
                        j = jb * 2 + jj
                        for (c0, n) in own_segs:
                            i = j % 2
                            sgx, sgk, _ = sg[i]
                            tax, tak, _ = ta[i]
                            b = B.bank()
                            for k in range(16):
                                B.mm(ps[:, b, 0:n], wga[:, k, jj * P:(jj + 1) * P], bigT[:, k, c0:c0 + n], k == 0, k == 15, wgak + ["bigT"], B.bk(b))
                            B.act(sgx[:, 0:n], ps[:, b, 0:n], AF.Sigmoid, B.bk(b), sgk)
                            b = B.bank()
                            for k in range(8):
                                B.mm(ps[:, b, 0:n], wao[:, k, jj * P:(jj + 1) * P], attnT[:, k, c0:c0 + n], k == 0, k == 7, waok + ["attnT"], B.bk(b))
                            B.tt("dve", tax[:, 0:n], ps[:, b, 0:n], sgx[:, 0:n], ALU.mult, B.bk(b) + sgk, tak)
                            b = B.bank()
                            for k in range(16):
                                B.mm(ps[:, b, 0:n], wgc[:, k, jj * P:(jj + 1) * P], bigT[:, k, c0:c0 + n], k == 0, k == 15, wgck + ["bigT"], B.bk(b))
                            B.act(sgx[:, 0:n], ps[:, b, 0:n], AF.Sigmoid, B.bk(b), sgk)
                            b = B.bank()
                            for k in range(8):
                                B.mm(ps[:, b, 0:n], wao[:, k, 256 + jj * P:256 + (jj + 1) * P], convT[:, k, c0:c0 + n], k == 0, k == 7, waok + ["convT"], B.bk(b))
                            B.tt("dve", sgx[:, 0:n], ps[:, b, 0:n], sgx[:, 0:n], ALU.mult, B.bk(b) + sgk, sgk)
                            B.tt("dve", mixT[:, j, c0:c0 + n], tax[:, 0:n], sgx[:, 0:n], ALU.add, tak + sgk, mixk)

                for li, (c0, nr, src, kind, tt_) in enumerate(own_tiles):
                    B.dma("sp", "xres%d" % li, hres[0:nr, li, :], src, [], [("hres", li)])
                for n4 in range(4):
                    wlo, wlok = B.wget((8, 512, [(0, 512, w_o[0:1024, n4 * 512:(n4 + 1) * 512])]))
                    whi, whik = B.wget((8, 512, [(0, 512, w_o[1024:2048, n4 * 512:(n4 + 1) * 512])]))
                    for li, (c0, nr, src, kind, tt_) in enumerate(own_tiles):
                        b = B.bank()
                        for k in range(16):
                            w_, wk_ = (wlo, wlok) if k < 8 else (whi, whik)
                            B.mm(ps[0:nr, b, :], mixT[:, k, c0:c0 + nr], w_[:, k % 8, :], k == 0, k == 15, mixk + wk_, B.bk(b))
                        hr = hres[0:nr, li, n4 * 512:(n4 + 1) * 512]
                        B.stt(hr, hr, ALPHA, ps[0:nr, b, :], ALU.mult, ALU.add, [("hres", li)] + B.bk(b), [("hres", li)])
                if B.stop_after == "S3":
                    return

                B.areset()
                if g + 1 < NG:
                    prefetch_x(make_tiles(g + 1))
                lnt = [B.af32(32) for _ in range(2)]
                hbf = [B.abf(D) for _ in range(2)]
                rts = [B.af32(64 * 8) for _ in range(len(own_tiles))]
                actT, actk, _ = B.abf(4 * NCMAX, [[NCMAX, 4], [1, NCMAX]])
                sgs = [B.af32(512) for _ in range(2)]
                wrt, wrk = B.wget((16, 64, [(0, 64, w_r[:, :])]))
                for li, (c0, nr, src, kind, tt_) in enumerate(own_tiles):
                    layer_norm(hres[0:nr, li, :], [("hres", li)], nr, lnt[li % 2])
                    if debug:
                        r0 = TOK if kind == "samp" else tt_ * P
                        B.dma("sp", "dbg0_" + str(DBGC.next()), dbg["h"][r0:r0 + nr, :], hres[0:nr, li, :], [("hres", li)], [])
                    hb, hbk, _ = hbf[li % 2]
                    B.cp("act", fap(hb, 0, [[1, D]], parts=nr), hres[0:nr, li, :], [("hres", li)], hbk)
                    b2 = B.bank(2)
                    for k in range(16):
                        o = fap(psb, (b2 + k // 8) * 1024 + (k % 8) * nr, [[1, nr]])
                        B.tr(o, fap(hb, k * P, [[1, P]], parts=nr), ident[0:nr, 0:nr], hbk + ["ident"], B.bk(b2 + k // 8))
                    for hh in range(2):
                        src_ap = fap(psb, (b2 + hh) * 1024, [[nr, 8], [1, nr]])
                        B.cp("dve" if hh == 0 else "act", bigT[:, hh * 8:(hh + 1) * 8, c0:c0 + nr], src_ap, B.bk(b2 + hh), ["bigT"])
                    tix = NT if kind == "samp" else tt_
                    routing(1, li, c0, nr, tix, wrt, wrk, rts[li], None, None)
                r2_done = 0
                for cb in range(2):
                    wg_, wgk_ = B.wget((16, 256, [(0, 256, w_sg[:, cb * 256:(cb + 1) * 256])]))
                    wu_, wuk_ = B.wget((16, 256, [(0, 256, w_su[:, cb * 256:(cb + 1) * 256])]))
                    for jj in range(2):
                        c = cb * 2 + jj
                        for _ in range(2):
                            if r2_done < len(own_tiles) and (r2_done <= c + 1 or c == 3):
                                li = r2_done
                                (c0_, nr_, src_, kind_, tt2) = own_tiles[li]
                                routing(2, li, c0_, nr_, NT if kind_ == "samp" else tt2, wrt, wrk, rts[li], None, None)
                                r2_done += 1
                        for (c0, n) in own_segs:
                            sx_, sxk, _ = sgs[c % 2]
                            b = B.bank()
                            for k in range(16):
                                B.mm(ps[:, b, 0:n], wg_[:, k, jj * P:(jj + 1) * P], bigT[:, k, c0:c0 + n], k == 0, k == 15, wgk_ + ["bigT"], B.bk(b))
                            B.act(sx_[:, 0:n], ps[:, b, 0:n], AF.Silu, B.bk(b), sxk)
                            b = B.bank()
                            for k in range(16):
                                B.mm(ps[:, b, 0:n], wu_[:, k, jj * P:(jj + 1) * P], bigT[:, k, c0:c0 + n], k == 0, k == 15, wuk_ + ["bigT"], B.bk(b))
                            B.tt("dve", actT[:, c, c0:c0 + n], ps[:, b, 0:n], sx_[:, 0:n], ALU.mult, B.bk(b) + sxk, actk)
                while r2_done < len(own_tiles):
                    li = r2_done
                    (c0_, nr_, src_, kind_, tt2) = own_tiles[li]
                    routing(2, li, c0_, nr_, NT if kind_ == "samp" else tt2, wrt, wrk, rts[li], None, None)
                    r2_done += 1
                for li, (c0, nr, src, kind, tt_) in enumerate(own_tiles):
                    hb, hbk, _ = hbf[li % 2]
                    if kind == "samp":
                        S.op("dve", lambda e, hb=hb: e.memset(hb, 0.0), [], hbk)
                    B.cp("act", fap(hb, 0, [[1, D]], parts=nr), hres[0:nr, li, :], [("hres", li)], hbk)
                    tix = NT if kind == "samp" else tt_
                    routing(3, li, c0, nr, tix, wrt, wrk, rts[li], hb, hbk)
                for nb in range(2):
                    wd_, wdk_ = B.wget((4, 1024, [(0, 1024, w_sd[:, nb * 1024:(nb + 1) * 1024])]))
                    for n2 in range(2):
                        n4 = nb * 2 + n2
                        for li, (c0, nr, src, kind, tt_) in enumerate(own_tiles):
                            b = B.bank()
                            for k in range(4):
                                B.mm(ps[0:nr, b, :], actT[:, k, c0:c0 + nr], wd_[:, k, n2 * 512:(n2 + 1) * 512], k == 0, k == 3, actk + wdk_, B.bk(b))
                            hr = hres[0:nr, li, n4 * 512:(n4 + 1) * 512]
                            B.stt(hr, hr, ALPHA, ps[0:nr, b, :], ALU.mult, ALU.add, [("hres", li)] + B.bk(b), [("hres", li)])
                for li, (c0, nr, src, kind, tt_) in enumerate(own_tiles):
                    r0 = TOK if kind == "samp" else tt_ * P
                    B.dma("sp", "base%d" % li, BASE[r0:r0 + nr, :], hres[0:nr, li, :], [("hres", li)], ["BASE"])

            def layer_norm(xap, xkeys, nr, tmp, out=None, out_keys=None):
                st, stk, _ = tmp
                for q in range(4):
                    S.op("dve", lambda e, q=q: e.bn_stats(out=fap(st, q * 6, [[1, 6]], parts=nr), in_=fap(xap, q * 512, [[1, 512]], parts=nr)), xkeys, stk)
                mv = fap(st, 24, [[1, 2]], parts=nr)
                S.op("dve", lambda e: e.bn_aggr(out=mv, in_=fap(st, 0, [[1, 24]], parts=nr)), stk, stk)
                rs = fap(st, 26, [[1, 1]], parts=nr)
                B.act(rs, fap(st, 25, [[1, 1]], parts=nr), AF.Sqrt, stk + ["epsT"], stk, bias=epsT[0:nr, :])
                S.op("dve", lambda e: e.reciprocal(out=rs, in_=rs), stk, stk)
                o = xap if out is None else out
                ok_ = xkeys if out is None else out_keys
                B.ts("dve", o, xap, fap(st, 24, [[1, 1]], parts=nr), ALU.subtract, xkeys + stk, ok_, s2=rs, op1=ALU.mult)
                B.tt("dve", o, o, lng[0:nr, :], ALU.mult, ok_ + ["lng"], ok_)
                B.tt("dve", o, o, lnb[0:nr, :], ALU.add, ok_ + ["lnb"], ok_)

            def routing(phase, li, c0, nr, tix, wrt, wrk, rt, hb, hbk):
                r, rk, _ = rt
                def rv(i, n=NE, parts=nr):
                    return fap(r, i * 64, [[1, n]], parts=parts)
                sc_ = rv(0)
                ch = rv(1)
                tmp = rv(2)
                sel = rv(3, parts=P)
                wd = rv(4)
                key = rv(5, parts=P)
                sm = fap(r, 6 * 64, [[1, 64]], parts=nr)
                m1 = fap(r, 6 * 64, [[1, 8]], parts=nr)
                m2 = fap(r, 6 * 64 + 8, [[1, 8]], parts=nr)
                gs = fap(r, 6 * 64 + 16, [[1, 8]], parts=nr)
                g8 = fap(r, 6 * 64 + 24, [[1, 8]], parts=nr)
                pen = fap(r, 6 * 64 + 32, [[1, 8]], parts=nr)
                c8 = fap(r, 6 * 64 + 40, [[1, 8]], parts=nr)
                ssum = fap(r, 6 * 64 + 48, [[1, 1]], parts=nr)
                ch3 = fap(r, 1 * 64, [[8, 8], [1, 8]], parts=nr)
                tmp3 = fap(r, 2 * 64, [[8, 8], [1, 8]], parts=nr)
                selb, selbk = rsel_t[:, li, :], [("rsel", li)]
                pos = rv(7, parts=P)
                d8f = fap(r, 6 * 64 + 56, [[1, 8]], parts=P)
                BIGC = float(2 * NSLOT)
                if phase == 1:
                    b = B.bank()
                    for k in range(16):
                        B.mm(ps[0:nr, b, 0:NE], bigT[:, k, c0:c0 + nr], wrt[:, k, :], k == 0, k == 15, ["bigT"] + wrk, B.bk(b))

                    B.act(sc_, ps[0:nr, b, 0:NE], AF.Sigmoid, B.bk(b), rk)
                    return
                if phase == 2:
                    B.tt("dve", ch, sc_, rbias[0:nr, :], ALU.add, rk + ["rbias"], rk)
                    B.red(m1, ch3, ALU.max, rk, rk)
                    B.tt("dve", tmp3, ch3, fap(r, 6 * 64, [[1, 8], [0, 8]], parts=nr), ALU.is_equal, rk, rk)
                    B.stt(tmp, tmp, -1e9, ch, ALU.mult, ALU.add, rk, rk)
                    B.red(m2, tmp3, ALU.max, rk, rk)
                    B.tt("dve", gs, m1, m2, ALU.add, rk, rk)
                    S.op("dve", lambda e: e.max(out=g8, in_=gs), rk, rk)
                    B.ts("dve", pen, gs, fap(r, 6 * 64 + 24 + 3, [[1, 1]], parts=nr), ALU.is_ge, rk, rk, s2=-1.0, op1=ALU.add)
                    B.ts("dve", pen, pen, 1e9, ALU.mult, rk, rk)
                    B.tt("dve", tmp3, ch3, fap(r, 6 * 64 + 32, [[1, 8], [0, 8]], parts=nr), ALU.add, rk, rk)
                    S.op("dve", lambda e: e.max(out=c8, in_=tmp), rk, rk)
                    if nr < P:
                        S.op("dve", lambda e: e.memset(sel, 0.0), rk, rk)
                    B.ts("dve", rv(3), tmp, fap(r, 6 * 64 + 40 + 7, [[1, 1]], parts=nr), ALU.is_ge, rk, rk)
                    B.stt(wd, rv(3), 1.0, sc_, ALU.mult, ALU.mult, rk, rk, accum_out=ssum)
                    S.op("dve", lambda e: e.reciprocal(out=ssum, in_=ssum), rk, rk)
                    B.ts("dve", wd, wd, ssum, ALU.mult, rk, rk, s2=2.5, op1=ALU.mult)
                    B.cp("dve", selb, sel, rk, selbk)
                    return
                bp = B.bank()
                B.mm(ps[:, bp, 0:NE], triU[:], selb, True, True, ["triU"] + selbk, B.bk(bp))
                B.tt("dve", pos, ps[:, bp, 0:NE], cbase[:], ALU.add, B.bk(bp) + ["cbase"], rk)
                bp2 = B.bank()
                B.mm(ps[:, bp2, 0:NE], ones[:], selb, True, True, ["ones"] + selbk, B.bk(bp2))
                B.tt("dve", cbase[:], cbase[:], ps[:, bp2, 0:NE], ALU.add, B.bk(bp2) + ["cbase"], ["cbase"])
                B.ts("dve", key, pos, float(CAP), ALU.is_lt, rk, rk)
                B.tt("dve", key, key, sel, ALU.mult, rk, rk)
                B.tt("dve", pos, pos, c16t[:, 64:128], ALU.add, rk + ["c16t"], rk)
                B.ts("dve", pos, pos, -1.0, ALU.mult, rk, rk, s2=BIGC, op1=ALU.add)
                B.tt("dve", key, key, pos, ALU.mult, rk, rk)
                S.op("dve", lambda e: e.max(out=d8f, in_=key), rk, rk)
                for j in range(8):
                    B.stt(rv(2, parts=nr), fap(r, 5 * 64, [[1, NE]], parts=nr), fap(r, 6 * 64 + 56 + j, [[1, 1]], parts=nr), wd,
                          ALU.is_equal, ALU.mult, rk, rk + [("w8", tix)], accum_out=w8all[0:nr, tix, j:j + 1])
                B.ts("dve", fap(r, 2 * 64, [[1, 8]], parts=nr), fap(r, 6 * 64 + 56, [[1, 8]], parts=nr), 0.0, ALU.is_gt, rk, rk)
                B.tt("dve", w8all[0:nr, tix, :], w8all[0:nr, tix, :], fap(r, 2 * 64, [[1, 8]], parts=nr), ALU.mult, rk + [("w8", tix)], [("w8", tix)])
                B.ts("dve", d8f, d8f, -1.0, ALU.mult, rk, rk, s2=BIGC, op1=ALU.add)
                B.cp("dve", d8all[:, tix, :], d8f, rk, [("d8", tix)])
                for j in range(8):
                    S.dma("pool", "scat%d" % (li % 2), lambda e, j=j: e.indirect_dma_start(
                        out=XG[:, :], out_offset=bass.IndirectOffsetOnAxis(ap=d8all[:, tix, j:j + 1], axis=0),
                        in_=hb, in_offset=None, bounds_check=B.bcreg(e), oob_is_err=False),
                        hbk + [("d8", tix), "XG"], [("XGs", tix, j)])


            def sample_attention(qT, qTk, soff):
                kn, knk, _ = B.af32(256)
                bq = B.bank()
                for kp in range(2):
                    B.tr(ps[0:NS, bq, kp * P:(kp + 1) * P], kT32[:, kp, P:P + NS], ident_f[:], ["kT32", "ident_f"], B.bk(bq))
                B.cp("dve", fap(kn, 0, [[1, 256]], parts=NS), ps[0:NS, bq, 0:256], B.bk(bq), knk)
                B.dma("sp", "sck", sck[:, 0:P - 1, :], ck[:, 1:P, :], [], ["sck"])
                B.dma("sp", "sck", sck[:, P - 1, :], fap(kn, 0, [[1, 256]], parts=NS), knk, ["sck"])
                B.dma("sp", "scv", scvo[:, 0:P - 1, :], cv[:, 1:P, :], [], ["scvo"])
                B.dma("sp", "scv", scvo[:, P - 1, :], v32[0:NS, 1, :], ["v32"], ["scvo"])
                Ke, Kek = fap(hres_bf, 0 * 4096, [[256, NS], [1, 256]]), [("hres", 0)]
                Ve, Vek = fap(hres_bf, 1 * 4096, [[256, NS], [1, 256]]), [("hres", 1)]
                stg = fap(hres, 2 * D, [[256, NS], [1, 256]])
                stgk = [("hres", 2), ("hres", 3)]
                B.dma("sp", "ske", stg, sck.rearrange("b k d -> k b d"), ["sck"], stgk)
                B.cp("act", Ke, stg, stgk, Kek)
                B.dma("sp", "ske", stg, scvo.rearrange("b k d -> k b d"), ["scvo"], stgk)
                B.cp("dve", Ve, stg, stgk, Vek)
                QM, QMk, _ = B.abf(64, [[16, 4], [1, 16]])
                KT2, KT2k, _ = B.abf(4 * P, [[P, 4], [1, P]])
                Ssb, Ssk = fap(hres, 2 * D, [[P, NS], [1, P]]), [("hres", 2)]
                for b_ in range(NS):
                    qin = fap(qT, soff + b_, [[0, 4], [640, 8], [0, 2]])
                    B.tt("dve", fap(QM, 0, [[16, 4], [2, 8], [1, 2]]), qin, fap(c16t, 0, [[16, 4], [2, 8], [1, 2]]), ALU.mult, qTk + ["c16t"], QMk)
                    bt = B.bank()
                    for kv in range(4):
                        for dup in range(2):
                            B.tr(psb[64 * dup:64 * dup + 64, bt, kv * P:(kv + 1) * P], Ke[:, b_, kv * 64:(kv + 1) * 64], ident[:], Kek + ["ident"], B.bk(bt))
                    B.cp("act", KT2, fap(psb, bt * 1024, [[P, 4], [1, P]]), B.bk(bt), KT2k)
                    bsc = B.bank()
                    for kv in range(4):
                        B.mm(ps[0:16, bsc, 0:P], QM[:, kv, :], KT2[:, kv, :], kv == 0, kv == 3, QMk + KT2k, B.bk(bsc))
                    B.cp("dve", fap(Ssb, b_ * P, [[1, P]], parts=16), ps[0:16, bsc, 0:P], B.bk(bsc), Ssk)
                sx, sxk, _ = B.af32(5 * NS)
                S16 = fap(Ssb, 0, [[P, NS], [1, P]], parts=16)
                mx = fap(sx, 0, [[1, NS]], parts=16)
                sm = fap(sx, NS, [[1, NS]], parts=16)
                tq = fap(sx, 2 * NS, [[1, NS]], parts=16)
                rv_ = fap(sx, 3 * NS, [[1, NS]], parts=16)
                B.red(mx, S16, ALU.max, Ssk, sxk)
                B.ts("dve", mx, mx, sinkcol[0:16, 0:1], ALU.max, sxk + ["sinkcol"], sxk)
                B.tt("dve", S16, S16, fap(sx, 0, [[1, NS], [0, P]], parts=16), ALU.subtract, Ssk + sxk, Ssk)
                B.act(S16, S16, AF.Exp, Ssk, Ssk, scale=0.125)
                B.red(sm, S16, ALU.add, Ssk, sxk)
                B.ts("dve", tq, mx, -1.0, ALU.mult, sxk, sxk, s2=sinkcol[0:16, 0:1], op1=ALU.add)
                B.act(tq, tq, AF.Exp, sxk, sxk, scale=0.125)
                B.tt("dve", rv_, sm, tq, ALU.add, sxk, sxk)
                S.op("dve", lambda e: e.reciprocal(out=rv_, in_=rv_), sxk, sxk)
                Pb, Pbk = fap(hres_bf, 4 * 4096, [[P, NS], [1, P]]), [("hres", 4)]
                B.tt("dve", fap(Pb, 0, [[P, NS], [1, P]], parts=16), S16, fap(sx, 3 * NS, [[1, NS], [0, P]], parts=16), ALU.mult, Ssk + sxk, Pbk)
                bt = B.bank()
                for b_ in range(NS):
                    B.tr(psb[:, bt, b_ * 16:(b_ + 1) * 16], fap(Pb, b_ * P, [[1, P]], parts=16), ident[0:16, 0:16], Pbk + ["ident"], B.bk(bt))
                PTs, PTsk, _ = B.abf(NS * 16)
                B.cp("act", PTs, psb[:, bt, 0:NS * 16], B.bk(bt), PTsk)
                As, Ask = fap(hres, 4 * D + 1024, [[64, NS], [1, 64]]), [("hres", 4)]
                tmpv, tmpvk, _ = B.af32(256)
                for b_ in range(NS):
                    bo = B.bank()
                    B.mm(ps[0:16, bo, 0:256], PTs[:, b_ * 16:(b_ + 1) * 16], Ve[:, b_, :], True, True, PTsk + Vek, B.bk(bo))
                    B.tt("dve", fap(tmpv, 0, [[64, 4], [1, 64]], parts=16), fap(ps, bo * 512, [[64, 4], [1, 64]], parts=16),
                         fap(c16t, 192, [[1, 4], [0, 64]], parts=16), ALU.mult, B.bk(bo) + ["c16t"], tmpvk)
                    B.red(fap(As, b_ * 64, [[1, 64]], parts=16), fap(tmpv, 0, [[1, 64], [64, 4]], parts=16), ALU.add, tmpvk, Ask)
                A2, A2k = fap(hres, 3 * D, [[P, NS], [1, P]]), [("hres", 3)]
                B.tt("dve", fap(A2, 0, [[P, NS], [64, 2], [1, 64]], parts=16), fap(As, 0, [[64, NS], [0, 2], [1, 64]], parts=16),
                     fap(c16t, 200, [[0, NS], [1, 2], [0, 64]], parts=16), ALU.mult, Ask + ["c16t"], A2k)
                bt2 = B.bank()
                for b_ in range(NS):
                    B.tr(ps[:, bt2, b_ * 16:(b_ + 1) * 16], fap(A2, b_ * P, [[1, P]], parts=16), ident_f[0:16, 0:16], A2k + ["ident_f"], B.bk(bt2))
                af_, afk, _ = B.af32(8 * NS, [[NS, 8], [1, NS]])
                B.red(af_, fap(ps, bt2 * 512, [[2, 8], [16, NS], [1, 2]]), ALU.add, B.bk(bt2), afk)
                B.cp("dve", fap(attnT, soff, [[640, 8], [1, NS]]), af_, afk, ["attnT"])


            S.planning = True
            phase_a()
            S.planning = False
            B.bank_rr = 0
            phase_a()
        S.barrier()

        if stop_after is None:
            pb = contextlib.ExitStack()
            with pb:
                def sbb(name, shape, dt):
                    return pb.enter_context(nc.sbuf_tensor(name, list(shape), dt))
                wg = [sbb("wg%d" % i, [P, 16, 512], BF16) for i in range(2)]
                wu = [sbb("wu%d" % i, [P, 16, 512], BF16) for i in range(2)]
                wdn = [sbb("wd%d" % i, [P, 4, D], BF16) for i in range(2)]
                xb_ = [sbb("xb%d" % i, [P, NBLK, D], BF16) for i in range(2)]
                xbT = [sbb("xbT%d" % i, [P, 16, CAP], BF16) for i in range(2)]
                acT = [sbb("acT%d" % i, [P, 4, CAP], BF16) for i in range(2)]
                sgt = [sbb("sgt%d" % i, [P, CAP], F32) for i in range(2)]
                yo = [sbb("yo%d" % i, [P, D], BF16) for i in range(2)]

                def load_expert(e):
                    i = e % 2
                    for k0 in range(0, 16, 8):
                        B.dma("pool", "eg%d" % i, wg[i][:, k0:k0 + 8, :], w_eg[e, k0 * P:(k0 + 8) * P, :].rearrange("(k p) n -> p k n", p=P), [], [("wg", i)])
                        B.dma("pool", "eu%d" % i, wu[i][:, k0:k0 + 8, :], w_eu[e, k0 * P:(k0 + 8) * P, :].rearrange("(k p) n -> p k n", p=P), [], [("wu", i)])
                    for k in range(0, 4, 2):
                        B.dma("pool", "ed%d" % i, wdn[i][:, k:k + 2, :], w_ed[e, k * P:(k + 2) * P, :].rearrange("(k p) n -> p k n", p=P), [], [("wdn", i)])
                    B.dma("sp", "xb%d" % i, xb_[i][:], XG[e * CAP:(e + 1) * CAP, :].rearrange("(b p) d -> p b d", p=P), ["XG"] + [("XGs", t, j) for t in range(17) for j in range(8)], [("xb", i)])

                load_expert(0)
                yi = 0
                for e in range(NE):
                    i = e % 2
                    if e + 1 < NE:
                        load_expert(e + 1)
                    for blk in range(NBLK):
                        for h2 in range(2):
                            bt = B.bank()
                            for k8 in range(8):
                                k = h2 * 8 + k8
                                B.tr(psb[:, bt, k8 * P:(k8 + 1) * P], xb_[i][:, blk, k * P:(k + 1) * P], ident[:], [("xb", i), "ident"], B.bk(bt))
                            B.cp("act" if (blk * 2 + h2) % 2 == 0 else "dve", xbT[i][:, h2 * 8:(h2 + 1) * 8, blk * P:(blk + 1) * P],
                                 fap(psb, bt * 1024, [[P, 8], [1, P]]), B.bk(bt), [("xbT", i)])
                    for c in range(4):
                        bg = B.bank()
                        for k in range(16):
                            B.mm(ps[:, bg, 0:CAP], wg[i][:, k, c * P:(c + 1) * P], xbT[i][:, k, :], k == 0, k == 15, [("wg", i), ("xbT", i)], B.bk(bg))
                        B.act(sgt[c % 2][:], ps[:, bg, 0:CAP], AF.Silu, B.bk(bg), [("sgt", c % 2)])
                        bu = B.bank()
                        for k in range(16):
                            B.mm(ps[:, bu, 0:CAP], wu[i][:, k, c * P:(c + 1) * P], xbT[i][:, k, :], k == 0, k == 15, [("wu", i), ("xbT", i)], B.bk(bu))
                        B.tt("dve", acT[i][:, c, :], ps[:, bu, 0:CAP], sgt[c % 2][:], ALU.mult, B.bk(bu) + [("sgt", c % 2)], [("acT", i)])
                    for blk in range(NBLK):
                        y_ = yo[yi % 2]
                        yk = [("yo", yi % 2)]
                        for n4 in range(4):
                            b = B.bank()
                            for k in range(4):
                                B.mm(ps[:, b, :], acT[i][:, k, blk * P:(blk + 1) * P], wdn[i][:, k, n4 * 512:(n4 + 1) * 512], k == 0, k == 3, [("acT", i), ("wdn", i)], B.bk(b))
                            B.cp("act" if n4 % 2 == 0 else "dve", y_[:, n4 * 512:(n4 + 1) * 512], ps[:, b, :], B.bk(b), yk)
                        r0 = e * CAP + blk * P
                        B.dma("sp", "yst%d" % (yi % 2), Y[r0:r0 + P, :], y_[:], yk, ["Y"])
                        yi += 1
            S.barrier()

            pc = contextlib.ExitStack()
            with pc:
                def sbc(name, shape, dt):
                    return pc.enter_context(nc.sbuf_tensor(name, list(shape), dt))
                acc = [sbc("acc%d" % i, [P, D], F32) for i in range(2)]
                yg = [sbc("yg%d" % i, [P, D], BF16) for i in range(6)]
                lnt2 = [sbc("lnt2%d" % i, [P, 32], F32) for i in range(2)]
                B.dma("sp", "cst2", lng[:], fap_dram_bcast(ln2_g, D), ["lng"], ["lng"])
                B.dma("sp", "cst2", lnb[:], fap_dram_bcast(ln2_b, D), ["lnb"], ["lnb"])
                dgt = [sbc("dg%d" % i, [P, 16, P], BF16) for i in range(2)]
                wsm = [sbc("wsm%d" % i, [P, 32], F32) for i in range(2)]
                wbf = [sbc("wbf%d" % i, [P, 8], BF16) for i in range(2)]
                gi = [0]

                def f_ctx(tix):
                    nr = P if tix < NT else NS
                    r0 = tix * P if tix < NT else TOK
                    par = tix % 2
                    bks = [4 * par + n4 for n4 in range(4)]
                    return nr, r0, acc[par], [("acc", par)], dgt[par], [("dg", par)], wsm[par], [("wsm", par)], wbf[par], bks

                def f_prep(tix):
                    nr, r0, a, ak, dg, dgk, ws, wsk, wb16, bks = f_ctx(tix)
                    B.dma("sp", "bl%d" % (tix % 2), a[0:nr, :], BASE[r0:r0 + nr, :], ["BASE"], ak)
                    B.cp("dve", wb16[0:nr, :], w8all[0:nr, tix, :], [("w8", tix)], wsk)
                    B.cp("dve", ws[0:nr, 0:8], wb16[0:nr, :], wsk, wsk)
                    B.tt("dve", ws[0:nr, 8:16], w8all[0:nr, tix, :], ws[0:nr, 0:8], ALU.subtract, wsk + [("w8", tix)], wsk)
                    for j in range(8):
                        B.act(dg[0:nr, 2 * j, 0:nr], ident_f[0:nr, 0:nr], AF.Copy, wsk + ["ident_f"], dgk, scale=ws[0:nr, j:j + 1])
                        B.act(dg[0:nr, 2 * j + 1, 0:nr], ident_f[0:nr, 0:nr], AF.Copy, wsk + ["ident_f"], dgk, scale=ws[0:nr, 8 + j:9 + j])
                    for j in range(8):
                        g_ = yg[gi[0] % 6]
                        gk = [("yg", gi[0] % 6)]
                        S.dma("pool", "yg%d" % (gi[0] % 6), lambda e, g_=g_, tix=tix, j=j: e.indirect_dma_start(
                            out=g_[:], out_offset=None, in_=Y[:, :],
                            in_offset=bass.IndirectOffsetOnAxis(ap=d8all[:, tix, j:j + 1], axis=0),
                            bounds_check=B.bcreg(e), oob_is_err=False), ["Y", ("d8", tix)], gk)
                        for part in range(2):
                            for n4 in range(4):
                                B.mm(ps[0:nr, bks[n4], :], dg[0:nr, 2 * j + part, 0:nr], g_[0:nr, n4 * 512:(n4 + 1) * 512],
                                     j == 0 and part == 0, j == 7 and part == 1, gk + dgk, B.bk(bks[n4]))
                        gi[0] += 1

                def f_finish(tix):
                    nr, r0, a, ak, dg, dgk, ws, wsk, wb16, bks = f_ctx(tix)
                    for n4 in range(4):
                        B.tt("dve", a[0:nr, n4 * 512:(n4 + 1) * 512], a[0:nr, n4 * 512:(n4 + 1) * 512], ps[0:nr, bks[n4], :], ALU.add,
                             ak + B.bk(bks[n4]), ak)
                    layer_norm(a[0:nr, :], ak, nr, (lnt2[tix % 2][:], [("lnt2", tix % 2)], None))
                    if tix < NT:
                        B.dma("sp", "yout%d" % (tix % 2), yp[r0:r0 + nr, :], a[0:nr, :], ak, ["yp"])
                    else:
                        B.dma("sp", "yout%d" % (tix % 2), ys[:, :], a[0:nr, :], ak, ["ys"])

                f_prep(0)
                for tix in range(17):
                    if tix + 1 < 17:
                        f_prep(tix + 1)
                    f_finish(tix)
                if debug:
                    B.dma("sp", "dbg1_" + str(DBGC.next()), dbg["cnt"][:, :], cbase[:], ["cbase"], [])
                    dd = sbc("ddbg", [P, 17, 8], F32)
                    B.cp("dve", dd[:], d8all[:], [("d8", t) for t in range(17)], ["ddbg"])
                    B.dma("sp", "dbg2_" + str(DBGC.next()), dbg["d8"][:, :, :], dd[:], ["ddbg"], [])
                    B.dma("sp", "dbg3_" + str(DBGC.next()), dbg["w8"][:, :, :], w8all[:], [("w8", t) for t in range(17)], [])

        S.finalize(es)
        with nc.Block() as block:
            S.run_block(block)
    return nc


def _consts(core):
    hh = core % 2
    half = 8
    inv_freq = np.power(np.float32(500000.0), -np.arange(half, dtype=np.float32) * np.float32(2.0 / 16)).astype(np.float32)
    pos = np.zeros(2192, np.float32)
    pos[0:P] = hh * TOK - P + np.arange(P)
    pos[P:P + TOK] = hh * TOK + np.arange(TOK)
    pos[P + TOK:] = PAST
    pos = np.maximum(pos, 0).astype(np.float32)
    ang = (pos[:, None] * inv_freq[None, :]).astype(np.float32)
    cosv = np.cos(ang).astype(np.float32)
    sinv = np.sin(ang).astype(np.float32)
    cosT = np.ones((P, 2192), np.float32)
    sinT = np.zeros((P, 2192), np.float32)
    for p in range(P):
        d = p % 64
        if d < 16:
            cosT[p] = cosv[:, d % 8]
            sinT[p] = sinv[:, d % 8]
    ident = np.eye(P, dtype=np.float32)
    R = np.zeros((P, P), np.float32)
    for m in range(P):
        d = m % 64
        if d < 8:
            R[m, m + 8] = -1.0
        elif d < 16:
            R[m, m - 8] = 1.0
    rotT = R.T.copy()
    triU = np.triu(np.ones((P, P), np.float32), 1)
    ones = np.ones((P, P), np.float32)
    cst = np.concatenate([ident, rotT, triU, ones, np.zeros((P, 512), np.float32)], axis=1)
    a = np.arange(P)[:, None]
    c = np.arange(2 * P)[None, :]
    valid = (c > a) & (c <= a + P)
    mask_gen = np.where(valid, 0.0, NEG).astype(np.float32)
    if hh == 0:
        mask_first = np.where(valid & (c >= P), 0.0, NEG).astype(np.float32)
    else:
        mask_first = mask_gen
    msk = np.concatenate([mask_gen, mask_first], axis=1)
    c16 = np.zeros((P, 232), np.float32)
    for p in range(P):
        for kv in range(4):
            for h in range(16):
                c16[p, kv * 16 + h] = 1.0 if ((p // 64) == (h % 2) and (h // 4) == kv) else 0.0
    c16[:, 64:128] = (np.arange(NE) * CAP)[None, :]
    c16[:, 128:192] = np.arange(NE)[None, :]
    for h in range(16):
        for kv in range(4):
            c16[h, 192 + kv] = 1.0 if h // 4 == kv else 0.0
        for par in range(2):
            c16[h, 200 + par] = 1.0 if h % 2 == par else 0.0
    return dict(cosT=cosT, sinT=sinT, cst=cst, msk=msk, c16=c16)


_NC_CACHE = {}


def kernel(x_prompt, x_sample, cache_k, cache_v, state_conv, w_in, attn_sinks, conv_w,
           w_attn_out, w_conv_out, w_o, ln1_g, ln1_b, w_router, router_bias,
           w_exp_gate, w_exp_up, w_exp_down, w_sh_gate, w_sh_up, w_sh_down, ln2_g, ln2_b,
           _stop_after=None, _debug=False, _cores=None):
    f = lambda a: np.ascontiguousarray(np.asarray(a, dtype=np.float32))
    x_prompt, x_sample = f(x_prompt), f(x_sample)
    key = (_stop_after, _debug)
    if key not in _NC_CACHE:
        _NC_CACHE[key] = build_program(_stop_after, _debug)
    nc = _NC_CACHE[key]
    shared = dict(
        w_in=f(w_in[0]), sinks=f(attn_sinks), conv_w=f(conv_w[0]), w_ao=f(w_attn_out[0]), w_co=f(w_conv_out[0]),
        w_o=f(w_o[0]), ln1_g=f(ln1_g), ln1_b=f(ln1_b), w_r=f(w_router[0]), r_bias=f(router_bias),
        w_eg=f(w_exp_gate[0]), w_eu=f(w_exp_up[0]), w_ed=f(w_exp_down[0]), w_sg=f(w_sh_gate[0]),
        w_su=f(w_sh_up[0]), w_sd=f(w_sh_down[0]), ln2_g=f(ln2_g), ln2_b=f(ln2_b))
    if _stop_after is not None:
        for k_ in ("w_eg", "w_eu", "w_ed"):
            shared.pop(k_)
    in_maps = []
    cores = list(range(NCORES)) if _cores is None else list(_cores)
    for c in cores:
        n, hh = c // 2, c % 2
        xp = np.zeros((TOK + P, D), np.float32)
        if hh == 1:
            xp[0:P] = x_prompt[n, TOK - P:TOK]
        xp[P:] = x_prompt[n, hh * TOK:(hh + 1) * TOK]
        m = dict(shared)
        m.update(_consts(c))
        m.update(xp=xp, xs=f(x_sample[c * NS:(c + 1) * NS, 0, :]),
                 ck=f(cache_k[0, c * NS:(c + 1) * NS].reshape(NS, P, 256)),
                 cv=f(cache_v[0, c * NS:(c + 1) * NS].reshape(NS, P, 256)),
                 sc=f(state_conv[0, c * NS:(c + 1) * NS]))
        in_maps.append(m)
    res = run_bass_kernel_spmd(nc, in_maps, core_ids=list(range(len(cores))))
    R = res.results
    if _debug or _stop_after is not None:
        return R
    y_p = np.zeros((4, SEQ, D), np.float32)
    y_s = np.zeros((128, 1, D), np.float32)
    pk = np.zeros((1, 4, P, 4, 64), np.float32)
    pv = np.zeros((1, 4, P, 4, 64), np.float32)
    pc_ = np.zeros((1, 4, 2, 1024), np.float32)
    sk = np.zeros((1, 128, P, 4, 64), np.float32)
    sv = np.zeros((1, 128, P, 4, 64), np.float32)
    ssc_ = np.zeros((1, 128, 2, 1024), np.float32)
    for c in range(NCORES):
        n, hh = c // 2, c % 2
        y_p[n, hh * TOK:(hh + 1) * TOK] = R[c]["yp"]
        y_s[c * NS:(c + 1) * NS, 0] = R[c]["ys"]
        if hh == 1:
            pk[0, n] = R[c]["pck"].reshape(P, 4, 64)
            pv[0, n] = R[c]["pcv"].reshape(P, 4, 64)
            pc_[0, n] = R[c]["psc"]
        sk[0, c * NS:(c + 1) * NS] = R[c]["sck"].reshape(NS, P, 4, 64)
        sv[0, c * NS:(c + 1) * NS] = R[c]["scvo"].reshape(NS, P, 4, 64)
        ssc_[0, c * NS:(c + 1) * NS] = R[c]["ssc"]
    return (y_p, y_s, pk, pv, pc_, sk, sv, ssc_)
```

```python
import contextlib
import numpy as np
import concourse.bass as bass
import concourse.mybir as mybir
from concourse.bass_utils import run_bass_kernel_spmd

F32 = mybir.dt.float32
BF16 = mybir.dt.bfloat16
I32 = mybir.dt.int32
AF = mybir.ActivationFunctionType
ALU = mybir.AluOpType
AX = mybir.AxisListType

ENGS = ("pe", "act", "dve", "pool", "sp")


class _Ctr:
    def __init__(self):
        self.n = 0

    def next(self):
        self.n += 1
        return self.n


DBGC = _Ctr()

P = 128
D = 2048
NCORES = 8
SEQ = 4096
TOK = 2048
NS = 16
NT = 16
GT = 4
NG = NT // GT
NE = 64
CAP = 384
NBLK = CAP // P
NSLOT = NE * CAP
ALPHA = 2.0 ** 0.25
LN_EPS = 1e-5
PAST = 16384
OQ, OK_, OV, OB, OC, OH, OGA, OGC = 0, 1024, 1280, 1536, 2560, 3584, 4608, 6656
NEG = -30000.0


class Op:
    __slots__ = ("eng", "fn", "deps", "is_dma", "chan", "idx", "signal", "semval", "gidx")

    def __init__(self, eng, fn, is_dma, chan):
        self.eng = eng
        self.fn = fn
        self.deps = {}
        self.is_dma = is_dma
        self.chan = chan
        self.signal = False
        self.semval = None


def _slot(op):
    return ("ch", op.chan) if op.is_dma else ("eng", op.eng)


class Sched:
    def __init__(self, nc):
        self.nc = nc
        self.ops = {e: [] for e in ENGS}
        self.last_writer = {}
        self.readers = {}
        self.all_ops = []
        self.planning = False

    def _add(self, eng, fn, reads, writes, is_dma=False, chan=None):
        if self.planning:
            return None
        import os
        mx = int(os.environ.get("KMAXOPS", "0"))
        if mx and len(self.all_ops) >= mx:
            return None
        psr = [k for k in reads if isinstance(k, tuple) and k[0] == "ps"]
        if psr:
            writes = list(writes) + psr
        op = Op(eng, fn, is_dma, chan)
        deps = {}

        def add(d):
            if d is op:
                return
            s = _slot(d)
            o = deps.get(s)
            if o is None or d.gidx > o.gidx:
                deps[s] = d

        for k in reads:
            w = self.last_writer.get(k)
            if w is not None:
                add(w)
        for k in writes:
            w = self.last_writer.get(k)
            if w is not None:
                add(w)
            for r in self.readers.get(k, {}).values():
                add(r)
        op.deps = deps
        op.gidx = len(self.all_ops)
        for k in reads:
            self.readers.setdefault(k, {})[_slot(op)] = op
        for k in writes:
            self.last_writer[k] = op
            self.readers[k] = {}
        self.ops[eng].append(op)
        self.all_ops.append(op)
        return op

    def op(self, eng, fn, reads=(), writes=()):
        return self._add(eng, fn, reads, writes)

    def dma(self, eng, chan, fn, reads=(), writes=()):
        return self._add(eng, fn, reads, writes, is_dma=True, chan=chan)

    def barrier(self):
        if self.planning:
            return
        lasts = {}
        for op in self.all_ops:
            if op.fn is not None:
                lasts[_slot(op)] = op
        for e in ENGS:
            op = Op(e, None, False, None)
            op.deps = {s: d for s, d in lasts.items()}
            op.gidx = len(self.all_ops)
            self.ops[e].append(op)
            self.all_ops.append(op)

    def finalize(self, es):
        nc = self.nc
        for op in self.all_ops:
            for d in op.deps.values():
                if d.is_dma:
                    continue
                if d.eng == "pe" and op.eng == "pe" and not op.is_dma:
                    continue
                d.signal = True
        self.sem = {e: es.enter_context(nc.semaphore("sem_" + e)) for e in ENGS}
        chans = []
        for op in self.all_ops:
            if op.is_dma and op.chan not in chans:
                chans.append(op.chan)
        self.chsem = {c: es.enter_context(nc.semaphore("ch_" + str(c))) for c in chans}
        cnt = {e: 0 for e in ENGS}
        chcnt = {c: 0 for c in chans}
        for op in self.all_ops:
            if op.is_dma:
                chcnt[op.chan] += 16
                op.semval = chcnt[op.chan]
            elif op.signal:
                cnt[op.eng] += 1
                op.semval = cnt[op.eng]
        for op in self.all_ops:
            if op.is_dma and str(op.chan).startswith("cst"):
                op.semval = chcnt[op.chan]
        self.chfinal = chcnt

    def emit(self, ename, eng):
        seen = {}
        for op in self.ops[ename]:
            for s, d in op.deps.items():
                if (not d.is_dma) and d.eng == "pe" and op.eng == "pe" and not op.is_dma:
                    continue
                v = d.semval
                if v is None:
                    continue
                if seen.get(s, 0) >= v:
                    continue
                sem = self.chsem[s[1]] if s[0] == "ch" else self.sem[s[1]]
                eng.wait_ge(sem, v)
                seen[s] = v
            if op.fn is None:
                continue
            inst = op.fn(eng)
            if op.is_dma:
                inst.then_inc(self.chsem[op.chan], 16)
            elif op.signal:
                inst.then_inc(self.sem[op.eng], 1)

    def run_block(self, block):
        S = self

        def mk(ename):
            def body(eng):
                S.emit(ename, eng)
                if ename == "sp":
                    for c, v in S.chfinal.items():
                        if v > 0:
                            eng.wait_ge(S.chsem[c], v)
            return body

        block.tensor(mk("pe"))
        block.scalar(mk("act"))
        block.vector(mk("dve"))
        block.gpsimd(mk("pool"))
        block.sync(mk("sp"))


def fap(t, offset, dims, parts=None, p0=0):
    base = t if isinstance(t, bass.AP) else t[:]
    pst = base.ap[0][0]
    npart = base.ap[0][1] if parts is None else parts
    return bass.AP(tensor=base.tensor, offset=base.offset + p0 * pst + offset,
                   ap=[[pst, npart]] + [list(d) for d in dims])


class Builder:
    def __init__(self, stop_after=None, debug=False):
        self.stop_after = stop_after
        self.debug = debug
        self.nc = bass.Bass("TRN2", target_bir_lowering=False)
        self.S = Sched(self.nc)
        self.bank_rr = 0
        self.wplan = []
        self.wpos = 0
        self.tmp_rr = {}

    def bcreg(self, e):
        if getattr(self, "_bcreg", None) is None:
            self._bcreg = e.to_reg(NSLOT - 1)
        return self._bcreg

    def din(self, name, shape, dt=F32):
        return self.nc.dram_tensor(name, list(shape), dt, kind="ExternalInput").ap()

    def dout(self, name, shape, dt=F32):
        return self.nc.dram_tensor(name, list(shape), dt, kind="ExternalOutput").ap()

    def dscr(self, name, shape, dt=F32):
        return self.nc.dram_tensor(name, list(shape), dt, kind="Internal").ap()

    def bank(self, n=1):
        if n == 2 and self.bank_rr % 2 == 1:
            self.bank_rr += 1
        b = self.bank_rr % 6
        self.bank_rr += n
        return b

    def bk(self, b, n=1):
        return [("ps", b + i) for i in range(n)]

    def mm(self, out, lhsT, rhs, start, stop, reads, writes):
        self.S.op("pe", lambda e: e.matmul(out, lhsT=lhsT, rhs=rhs, start=start, stop=stop), reads, writes)

    def tr(self, out, in_, ident, reads, writes):
        self.S.op("pe", lambda e: e.transpose(out=out, in_=in_, identity=ident), reads, writes)

    def act(self, out, in_, func, reads, writes, scale=None, bias=None, accum_out=None):
        kw = {}
        if scale is not None:
            kw["scale"] = scale
        if bias is not None:
            kw["bias"] = bias
        if accum_out is not None:
            kw["accum_out"] = accum_out
        self.S.op("act", lambda e: e.activation(out=out, in_=in_, func=func, **kw), reads, writes)

    def tt(self, eng, out, in0, in1, op, reads, writes):
        self.S.op(eng, lambda e: e.tensor_tensor(out=out, in0=in0, in1=in1, op=op), reads, writes)

    def ts(self, eng, out, in0, s1, op0, reads, writes, s2=None, op1=None, accum_out=None):
        kw = {}
        if op1 is not None:
            kw["op1"] = op1
        if accum_out is not None:
            kw["accum_out"] = accum_out
        self.S.op(eng, lambda e: e.tensor_scalar(out=out, in0=in0, scalar1=s1, scalar2=s2, op0=op0, **kw), reads, writes)

    def stt(self, out, in0, scalar, in1, op0, op1, reads, writes, accum_out=None):
        kw = {}
        if accum_out is not None:
            kw["accum_out"] = accum_out
        self.S.op("dve", lambda e: e.scalar_tensor_tensor(out=out, in0=in0, scalar=scalar, in1=in1, op0=op0, op1=op1, **kw), reads, writes)

    def red(self, out, in_, op, reads, writes, axis=AX.X):
        self.S.op("dve", lambda e: e.tensor_reduce(out=out, in_=in_, axis=axis, op=op), reads, writes)

    def cp(self, eng, out, in_, reads, writes):
        if eng == "act":
            self.S.op("act", lambda e: e.activation(out=out, in_=in_, func=AF.Copy), reads, writes)
        else:
            self.S.op(eng, lambda e: e.tensor_copy(out=out, in_=in_), reads, writes)

    def dma(self, eng, chan, out, in_, reads, writes, **kw):
        return self.S.dma(eng, chan, lambda e: e.dma_start(out=out, in_=in_, **kw), reads, writes)

    def wget(self, spec):
        S = self.S
        i = self.wpos
        self.wpos += 1
        if S.planning:
            self.wplan.append(spec)
        slot = i % self.NB
        nk, ncols, parts = spec
        while self.wissued < min(len(self.wplan), i + self.NB - 2):
            self._wissue(self.wissued)
            self.wissued += 1
        ap = fap(self.wring, slot * self.WSLOT, [[ncols, nk], [1, ncols]])
        return ap, [("w", slot)]

    def _wissue(self, j):
        if self.S.planning:
            return
        nk, ncols, parts = self.wplan[j]
        slot = j % self.NB
        for (c0, n, src) in parts:
            dst = fap(self.wring, slot * self.WSLOT + c0, [[ncols, nk], [1, n]])
            self.dma("pool", "w%d" % slot, dst, src.rearrange("(k p) n -> p k n", p=P), [], [("w", slot)])

    def areset(self):
        self.apos = 0

    def aalloc(self, nbytes):
        nbytes = (nbytes + 63) // 64 * 64
        off = self.apos
        self.apos += nbytes
        assert self.apos <= self.ARENA_BYTES, ("arena overflow", self.apos)
        keys = [("ar", b) for b in range(off // 2048, (off + nbytes - 1) // 2048 + 1)]
        return off, keys

    def af32(self, n, dims=None):
        off, keys = self.aalloc(n * 4)
        ap = fap(self.arena, off // 4, dims if dims is not None else [[1, n]])
        return ap, keys, off // 4

    def abf(self, n, dims=None):
        off, keys = self.aalloc(n * 2)
        ap = fap(self.arena_bf, off // 2, dims if dims is not None else [[1, n]])
        return ap, keys, off // 2


def build_program(stop_after=None, debug=False):
    B = Builder(stop_after, debug)
    nc = B.nc
    S = B.S

    xp = B.din("xp", [TOK + P, D])
    xs = B.din("xs", [NS, D])
    ck = B.din("ck", [NS, P, 256])
    cv = B.din("cv", [NS, P, 256])
    scv = B.din("sc", [NS, 2, 1024])
    w_in = B.din("w_in", [D, 8704])
    sinks = B.din("sinks", [1, 16])
    conv_w = B.din("conv_w", [3, 1024])
    w_ao = B.din("w_ao", [1024, D])
    w_co = B.din("w_co", [1024, D])
    w_o = B.din("w_o", [D, D])
    ln1_g = B.din("ln1_g", [1, D])
    ln1_b = B.din("ln1_b", [1, D])
    w_r = B.din("w_r", [D, NE])
    r_bias = B.din("r_bias", [1, NE])
    if stop_after is None:
        w_eg = B.din("w_eg", [NE, D, 512])
        w_eu = B.din("w_eu", [NE, D, 512])
        w_ed = B.din("w_ed", [NE, 512, D])
    w_sg = B.din("w_sg", [D, 512])
    w_su = B.din("w_su", [D, 512])
    w_sd = B.din("w_sd", [512, D])
    ln2_g = B.din("ln2_g", [1, D])
    ln2_b = B.din("ln2_b", [1, D])
    cosd = B.din("cosT", [P, 2192])
    sind = B.din("sinT", [P, 2192])
    cst = B.din("cst", [P, 1024])
    mskd = B.din("msk", [P, 512])
    c16 = B.din("c16", [P, 64 + 64 + 64 + 8 + 32])

    yp = B.dout("yp", [TOK, D])
    ys = B.dout("ys", [NS, D])
    pck = B.dout("pck", [P, 256])
    pcv = B.dout("pcv", [P, 256])
    psc = B.dout("psc", [2, 1024])
    sck = B.dout("sck", [NS, P, 256])
    scvo = B.dout("scvo", [NS, P, 256])
    ssc = B.dout("ssc", [NS, 2, 1024])
    if debug:
        dbg = {k: B.dout("dbg_" + k, shp) for k, shp in [("h", [TOK + NS, D]), ("cnt", [P, NE]), ("attn", [P, 8, 640]), ("conv", [P, 8, 640]), ("d8", [P, 17, 8]), ("w8", [P, 17, 8])]}

    XG = B.dscr("XG", [NSLOT + P, D], BF16)
    Y = B.dscr("Y", [NSLOT, D], BF16)
    BASE = B.dscr("BASE", [TOK + P, D], F32)

    es = contextlib.ExitStack()
    with es:
        def sb(name, shape, dt):
            return es.enter_context(nc.sbuf_tensor(name, list(shape), dt))

        ps = es.enter_context(nc.psum_tensor("ps", [P, 8, 512], F32))
        psb = ps.bitcast(BF16)

        ident_f = sb("ident_f", [P, P], F32)
        ident = sb("ident", [P, P], BF16)
        rotT = sb("rotT", [P, P], BF16)
        triU = sb("triU", [P, P], BF16)
        ones = sb("ones", [P, P], BF16)
        msk = sb("mskt", [P, 512], F32)
        c16t = sb("c16t", [P, 232], F32)
        sink8 = sb("sink8", [P, 16], F32)
        sinkraw = sb("sinkraw", [P, 16], F32)
        rbias = sb("rbias", [P, NE], F32)
        convw = sb("convw", [P, 8, 3], F32)
        lng = sb("lng", [P, D], F32)
        lnb = sb("lnb", [P, D], F32)
        d8all = sb("d8all", [P, 17, 8], I32)
        w8all = sb("w8all", [P, 17, 8], F32)
        cbase = sb("cbase", [P, NE], F32)
        epsT = sb("epsT", [P, 1], F32)
        sinkcol = sb("sinkcol", [P, 1], F32)
        rsel_t = sb("rsel", [P, 5, NE], BF16)

        def load_consts():
            B.dma("sp", "cst", ident_f[:], cst[:, 0:128], [], ["ident_f"])
            B.dma("pool", "cstp", ident[:], cst[:, 0:128], [], ["ident"])
            B.dma("pool", "cstp", rotT[:], cst[:, 128:256], [], ["rotT"])
            B.dma("pool", "cstp", triU[:], cst[:, 256:384], [], ["triU"])
            B.dma("pool", "cstp", ones[:], cst[:, 384:512], [], ["ones"])
            B.dma("sp", "cst", msk[:], mskd[:, :], [], ["msk"])
            B.dma("sp", "cst", c16t[:], c16[:, :], [], ["c16t"])
            B.dma("sp", "cst", sinkraw[:], fap_dram_bcast(sinks, 16), [], ["sinkraw"])
            B.dma("sp", "cst", rbias[:], fap_dram_bcast(r_bias, NE), [], ["rbias"])
            for j in range(3):
                B.dma("sp", "cst", convw[:, :, j], conv_w[j, :].rearrange("(c p) -> p c", p=P), [], ["convw%d" % j], allow_slow_non_contiguous=True)
            B.ts("dve", fap(sink8, 0, [[4, 4], [2, 2], [1, 2]]), fap(sinkraw, 0, [[4, 4], [1, 2], [2, 2]]), 8.0, ALU.mult, ["sinkraw"], ["sink8"])
            S.op("dve", lambda e: e.memset(cbase[:], 0.0), [], ["cbase"])
            S.op("dve", lambda e: e.memset(w8all[:], 0.0), [], [("w8", t) for t in range(17)])
            S.op("dve", lambda e: e.memset(epsT[:], LN_EPS), [], ["epsT"])
            B.dma("sp", "cst", sinkcol[0:16, :], sinks.rearrange("a h -> h a"), [], ["sinkcol"], allow_slow_non_contiguous=True)
            B.ts("dve", sinkcol[0:16, :], sinkcol[0:16, :], 8.0, ALU.mult, ["sinkcol"], ["sinkcol"])

        def fap_dram_bcast(src, n):
            return bass.AP(tensor=src.tensor, offset=src.offset, ap=[[0, P], [1, n]])

        load_consts()

        pa = contextlib.ExitStack()
        with pa:
            def sba(name, shape, dt):
                return pa.enter_context(nc.sbuf_tensor(name, list(shape), dt))

            NCMAX = 640
            bigT = sba("bigT", [P, 16, NCMAX], BF16)
            attnT = sba("attnT", [P, 8, NCMAX], BF16)
            convT = sba("convT", [P, 8, NCMAX], BF16)
            kTl = sba("kTl", [P, 4, P + NCMAX], BF16)
            kT32 = sba("kT32", [P, 2, 144], F32)
            vl = sba("vl", [P, 6, 256], BF16)
            v32 = sba("v32", [P, 2, 256], F32)
            cosl = sba("cosl", [P, NCMAX], F32)
            sinl = sba("sinl", [P, NCMAX], F32)
            uprev = sba("uprev", [P, 8, 2], F32)
            hres = sba("hres", [P, 5, D], F32)
            B.NB = 6
            B.WSLOT = 4096
            B.wring = sba("wring", [P, B.NB * B.WSLOT], BF16)
            B.ARENA_BYTES = 38 * 1024
            B.arena = sba("arena", [P, B.ARENA_BYTES // 4], F32)
            B.arena_bf = B.arena.bitcast(BF16)

            hres_bf = hres.bitcast(BF16)
            def zero_fill_xg():
                zt = hres_bf[:, 4, 0:D]
                S.op("dve", lambda e: e.memset(zt, 0.0), [], [("hres", 4)])
                nrow_total = NSLOT + P
                r = 0
                while r < nrow_total:
                    n = min(4 * P, nrow_total - r)
                    B.dma("act", "zf", XG[r:r + n, :].rearrange("(a p) d -> p a d", p=P), fap(hres_bf, 4 * 4096, [[0, n // P], [1, D]]), [("hres", 4)], ["XG"])
                    r += n

            B.dma("sp", "cst", lng[:], fap_dram_bcast(ln1_g, D), [], ["lng"])
            B.dma("sp", "cst", lnb[:], fap_dram_bcast(ln1_b, D), [], ["lnb"])

            if debug:
                S.op("dve", lambda e: e.memset(attnT[:], 0.0), [], ["attnT"])
                S.op("dve", lambda e: e.memset(convT[:], 0.0), [], ["convT"])

            def phase_a():
                B.wpos = 0
                B.wissued = 0
                for g in range(NG):
                    group(g)

            def make_tiles(g):
                halo = (g == 0)
                samp = (g == NG - 1)
                moff = P if halo else 0
                soff = moff + GT * P
                tl = []
                if halo:
                    tl.append((0, P, xp[0:P, :], "halo", None))
                for t in range(GT):
                    tt_ = g * GT + t
                    tl.append((moff + t * P, P, xp[P + tt_ * P:P + (tt_ + 1) * P, :], "main", tt_))
                if samp:
                    tl.append((soff, NS, xs[:, :], "samp", None))
                return tl

            def xpf_loc(i, k):
                if i < 2:
                    return attnT, i * 2048 + k * P, "attnT"
                if i < 4:
                    return convT, (i - 2) * 2048 + k * P, "convT"
                if k < 8:
                    return attnT, 4096 + k * P, "attnT"
                return convT, 4096 + (k - 8) * P, "convT"

            def prefetch_x(tl):
                for i, (c0, nr, src_, kind, tt_) in enumerate(tl):
                    if i < 4:
                        base, off, key = xpf_loc(i, 0)
                        B.dma("pool", "xpf%d" % i, fap(base, off, [[1, D]], parts=nr), src_, [], [key])
                    else:
                        B.dma("pool", "xpf4", fap(attnT, 4096, [[1, 1024]], parts=nr), src_[:, 0:1024], [], ["attnT"])
                        B.dma("pool", "xpf5", fap(convT, 4096, [[1, 1024]], parts=nr), src_[:, 1024:2048], [], ["convT"])

            def group(g):
                halo = (g == 0)
                samp = (g == NG - 1)
                moff = P if halo else 0
                NC = moff + GT * P + (NS if samp else 0)
                soff = moff + GT * P
                absb = (P + GT * P * g) - moff
                own_segs = [(moff, GT * P)] + ([(soff, NS)] if samp else [])
                all_segs = ([(0, P)] if halo else []) + own_segs
                tiles = make_tiles(g)
                own_tiles = [tl for tl in tiles if tl[3] != "halo"]
                if g == 0:
                    prefetch_x(tiles)

                B.areset()
                B.dma("sp", "tabc", cosl[:, 0:NC], cosd[:, absb:absb + NC], [], ["cosl"])
                B.dma("sp", "tabs", sinl[:, 0:NC], sind[:, absb:absb + NC], [], ["sinl"])
                for i, (c0, nr, src, kind, tt_) in enumerate(tiles):
                    b2 = B.bank(2)
                    for k in range(16):
                        base, off, key = xpf_loc(i, k)
                        o = fap(psb, (b2 + k // 8) * 1024 + (k % 8) * nr, [[1, nr]])
                        B.tr(o, fap(base, off, [[1, P]], parts=nr), ident[0:nr, 0:nr], [key, "ident"], B.bk(b2 + k // 8))
                    for hh in range(2):
                        src_ap = fap(psb, (b2 + hh) * 1024, [[nr, 8], [1, nr]])
                        B.cp("dve" if hh == 0 else "act", bigT[:, hh * 8:(hh + 1) * 8, c0:c0 + nr], src_ap, B.bk(b2 + hh), ["bigT"])

                if B.stop_after == "S0":
                    return
                def proj(wt, wk, nk, col_lo, segs, rhs_t, rhs_key, consumer):
                    for (c0, n) in segs:
                        b = B.bank()
                        for k in range(nk):
                            B.mm(ps[:, b, 0:n], wt[:, k, col_lo:col_lo + P], rhs_t[:, k, c0:c0 + n], k == 0, k == nk - 1,
                                 wk + [rhs_key], B.bk(b))
                        consumer(b, c0, n)

                def win_spec(col0, ncols=256):
                    return (16, ncols, [(0, ncols, w_in[:, col0:col0 + ncols])])

                B.areset()
                qT, qTk, _ = B.abf(8 * NCMAX, [[NCMAX, 8], [1, NCMAX]])
                qmark = B.apos
                qb = [B.abf(512) for _ in range(2)]
                t1 = [B.af32(512) for _ in range(2)]
                t2 = [B.af32(512) for _ in range(2)]
                rr = [0]

                def rope(b, c0, n, out_bf, out_keys, out_f32=None, out_f32_keys=None):
                    i = rr[0] % 2
                    rr[0] += 1
                    q_b, qbk, _ = qb[i]
                    a1, a1k, _ = t1[i]
                    a2, a2k, _ = t2[i]
                    B.cp("act", q_b[:, 0:n], ps[:, b, 0:n], B.bk(b), qbk)
                    B.tt("dve", a1[:, 0:n], ps[:, b, 0:n], cosl[:, c0:c0 + n], ALU.mult, B.bk(b) + ["cosl"], a1k)
                    b2 = B.bank()
                    B.mm(ps[:, b2, 0:n], rotT[:], q_b[:, 0:n], True, True, qbk + ["rotT"], B.bk(b2))
                    B.tt("dve", a2[:, 0:n], ps[:, b2, 0:n], sinl[:, c0:c0 + n], ALU.mult, B.bk(b2) + ["sinl"], a2k)
                    B.tt("dve", out_bf, a1[:, 0:n], a2[:, 0:n], ALU.add, a1k + a2k, out_keys)
                    if out_f32 is not None:
                        B.tt("dve", out_f32, a1[:, 0:n], a2[:, 0:n], ALU.add, a1k + a2k, out_f32_keys)

                for qs in range(4):
                    wt, wk = B.wget(win_spec(OQ + qs * 256))
                    for cc in range(2):
                        c = qs * 2 + cc
                        proj(wt, wk, 16, cc * P, own_segs, bigT, "bigT",
                             lambda b, c0, n, c=c: rope(b, c0, n, qT[:, c, c0:c0 + n], qTk))
                if B.stop_after == "S1q":
                    return
                if not halo:
                    ksrc = (P if g == 1 else 0) + GT * P
                    B.cp("act", kTl[:, :, 0:P], kTl[:, :, ksrc:ksrc + P], ["kTl"], ["kTl"])
                    B.cp("act", vl[:, 0, :], vl[:, (GT + 1) if g == 1 else GT, :], ["vl"], ["vl"])
                wt, wk = B.wget(win_spec(OK_))
                krt = [B.af32(512) for _ in range(2)]
                kri = [0]
                ksegs = list(all_segs)
                if samp:
                    ksegs = [(moff, (GT - 1) * P), (moff + (GT - 1) * P, P), (soff, NS)]
                for kp in range(2):
                    def kcons(b, c0, n, kp=kp):
                        kr, krk, _ = krt[kri[0] % 2]
                        kri[0] += 1
                        rope(b, c0, n, kr[:, 0:n], krk)
                        for j in range(2):
                            kv = 2 * kp + j
                            for dup in range(2):
                                B.cp("act" if dup == 0 else "dve", kTl[64 * dup:64 * dup + 64, kv, P + c0:P + c0 + n],
                                     kr[64 * j:64 * j + 64, 0:n], krk, ["kTl"])
                        if samp and c0 == soff:
                            B.cp("act", kT32[:, kp, P:P + NS], kr[:, 0:n], krk, ["kT32"])
                        elif samp and c0 == moff + (GT - 1) * P:
                            B.cp("dve", kT32[:, kp, 0:P], kr[:, 0:n], krk, ["kT32"])
                    proj(wt, wk, 16, kp * P, ksegs, bigT, "bigT", kcons)
                if B.stop_after == "S1k":
                    return
                wt, wk = B.wget(win_spec(OV))
                for li, (c0, nr, src, kind, tt_) in enumerate(tiles):
                    b = B.bank()
                    for k in range(16):
                        B.mm(ps[0:nr, b, 0:256], bigT[:, k, c0:c0 + nr], wt[:, k, :], k == 0, k == 15, wk + ["bigT"], B.bk(b))
                    B.cp("act", vl[0:nr, 1 + li, :], ps[0:nr, b, 0:256], B.bk(b), ["vl"])
                    if samp and kind == "samp":
                        B.cp("dve", v32[0:nr, 1, :], ps[0:nr, b, 0:256], B.bk(b), ["v32"])
                    if samp and kind == "main" and tt_ == NT - 1:
                        B.cp("dve", v32[:, 0, :], ps[:, b, 0:256], B.bk(b), ["v32"])

                if B.stop_after == "S1a":
                    return
                B.apos = qmark
                Sm = [B.af32(1024, [[256, 4], [1, 256]]) for _ in range(2)]
                Pn = [B.abf(1024, [[256, 4], [1, 256]]) for _ in range(2)]
                PTt = [B.abf(1024, [[128, 8], [1, 128]]) for _ in range(2)]
                sm_ = [B.af32(32) for _ in range(2)]
                items = []
                for li, (c0, nr, src_, kind, tt_) in enumerate(tiles):
                    if kind == "main":
                        for kv in range(4):
                            items.append((li, c0, tt_, kv))
                bpv = 6
                sc_bank = {}

                def a_scores(idx):
                    li, c0, tt_, kv = items[idx]
                    b2 = B.bank(2)
                    sc_bank[idx] = b2
                    for hh in range(4):
                        c = 2 * kv + hh // 2
                        half = hh % 2
                        B.mm(ps[:, b2 + half, (hh // 2) * 256:(hh // 2) * 256 + 256],
                             qT[64 * half:64 * half + 64, c, c0:c0 + P],
                             kTl[64 * half:64 * half + 64, kv, c0:c0 + 256], True, True,
                             qTk + ["kTl"], B.bk(b2 + half))

                def a_ctx(idx):
                    li, c0, tt_, kv = items[idx]
                    i = idx % 2
                    Sx, Sk, _ = Sm[i]
                    Px, Pk, _ = Pn[i]
                    PT, PTk, _ = PTt[i]
                    sx, sk_, _ = sm_[i]
                    return li, c0, tt_, kv, Sx, Sk, Px, Pk, PT, PTk, sx, sk_

                def a_A(idx):
                    li, c0, tt_, kv, Sx, Sk, Px, Pk, PT, PTk, sx, sk_ = a_ctx(idx)
                    b2 = sc_bank[idx]
                    mk_off = 256 if tt_ == 0 else 0
                    pin = fap(ps, b2 * 512, [[256, 4], [1, 256]])
                    B.tt("dve", Sx, pin, fap(msk, mk_off, [[0, 4], [1, 256]]), ALU.add, B.bk(b2, 2) + ["msk"], Sk)
                    mx = sx[:, 0:4]
                    m8 = sx[:, 4:8]
                    sm = sx[:, 8:12]
                    tq = sx[:, 12:16]
                    es_ = sx[:, 16:20]
                    rv = sx[:, 20:24]
                    nm = sx[:, 24:28]
                    B.red(mx, Sx, ALU.max, Sk, sk_)
                    B.tt("dve", m8, mx, sink8[:, 4 * kv:4 * kv + 4], ALU.max, sk_ + ["sink8"], sk_)
                    B.ts("dve", nm, m8, -0.125, ALU.mult, sk_, sk_)
                    B.tt("dve", tq, sink8[:, 4 * kv:4 * kv + 4], m8, ALU.subtract, sk_ + ["sink8"], sk_)

                def a_E(idx):
                    li, c0, tt_, kv, Sx, Sk, Px, Pk, PT, PTk, sx, sk_ = a_ctx(idx)
                    for s in range(4):
                        B.act(Sx[:, s, :], Sx[:, s, :], AF.Exp, Sk + sk_, Sk + sk_, scale=0.125, bias=sx[:, 24 + s:25 + s], accum_out=sx[:, 8 + s:9 + s])

                def a_B(idx):
                    li, c0, tt_, kv, Sx, Sk, Px, Pk, PT, PTk, sx, sk_ = a_ctx(idx)
                    sm = sx[:, 8:12]
                    tq = sx[:, 12:16]
                    es_ = sx[:, 16:20]
                    rv = sx[:, 20:24]
                    B.act(es_, tq, AF.Exp, sk_, sk_, scale=0.125)
                    B.tt("dve", rv, sm, es_, ALU.add, sk_, sk_)
                    S.op("dve", lambda e, rv=rv: e.reciprocal(out=rv, in_=rv), sk_, sk_)
                    for s in range(4):
                        B.act(Px[:, s, :], Sx[:, s, :], AF.Copy, Sk + sk_, Pk, scale=sx[:, 20 + s:21 + s])

                def a_C(idx):
                    li, c0, tt_, kv, Sx, Sk, Px, Pk, PT, PTk, sx, sk_ = a_ctx(idx)
                    bt = B.bank()
                    for hh in range(4):
                        for kb in range(2):
                            B.tr(psb[:, bt, (hh * 2 + kb) * P:(hh * 2 + kb + 1) * P], Px[:, (hh % 2) * 2 + hh // 2, kb * P:(kb + 1) * P], ident[:],
                                 Pk + ["ident"], B.bk(bt))
                    B.cp("dve", PT, fap(psb, bt * 1024, [[128, 8], [1, 128]]), B.bk(bt), PTk)
                    for hh in range(4):
                        c = 2 * kv + hh // 2
                        half = hh % 2
                        o = ps[64 * half:64 * half + 64, bpv + c // 4, (c % 4) * P:(c % 4 + 1) * P]
                        for kb in range(2):
                            B.mm(o, vl[:, li + kb, kv * 64:(kv + 1) * 64], PT[:, hh * 2 + kb, :], kb == 0, kb == 1,
                                 ["vl"] + PTk, B.bk(bpv + c // 4))
                    if kv == 3:
                        for hh in range(2):
                            B.cp("act" if hh == 0 else "dve", attnT[:, hh * 4:(hh + 1) * 4, c0:c0 + P],
                                 fap(ps, (bpv + hh) * 512, [[128, 4], [1, 128]]), B.bk(bpv + hh), ["attnT"])

                if items:
                    n_it = len(items)
                    assert n_it % 2 == 0
                    a_scores(0)
                    a_scores(1)
                    a_A(0)
                    a_A(1)
                    for i0_ in range(0, n_it, 2):
                        i1_ = i0_ + 1
                        a_E(i0_)
                        a_E(i1_)
                        if i0_ + 2 < n_it:
                            a_scores(i0_ + 2)
                            a_scores(i1_ + 2)
                        a_B(i0_)
                        a_B(i1_)
                        if i0_ + 2 < n_it:
                            a_A(i0_ + 2)
                            a_A(i1_ + 2)
                        a_C(i0_)
                        a_C(i1_)

                if B.stop_after == "S1b":
                    return
                if samp:
                    B.apos = qmark
                    sample_attention(qT, qTk, soff)

                if B.stop_after == "S1":
                    return

                if g == 0:
                    zero_fill_xg()
                B.areset()
                NU = 2 + NCMAX
                hs = [B.af32(512) for _ in range(2)]
                bs = [B.af32(512) for _ in range(2)]
                ub = [B.af32(NU) for _ in range(2)]
                tb = [B.af32(512) for _ in range(2)]
                if samp:
                    stT, stTk, _ = B.af32(8 * 32, [[32, 8], [1, 32]])
                    st_in, st_ink, _ = B.af32(1024)
                    us_all, us_allk, _ = B.af32(8 * NS, [[NS, 8], [1, NS]])
                    ul_all, ul_allk, _ = B.af32(8 * 2, [[2, 8], [1, 2]])
                    B.dma("sp", "st", fap(st_in, 0, [[1, 1024]], parts=32), scv.rearrange("b j c -> (b j) c"), [], st_ink)
                    bq = B.bank()
                    for cc in range(8):
                        B.tr(ps[:, bq, cc * 32:(cc + 1) * 32], fap(st_in, cc * P, [[1, P]], parts=32), ident_f[0:32, 0:32],
                             st_ink + ["ident_f"], B.bk(bq))
                    B.cp("dve", stT, fap(ps, bq * 512, [[32, 8], [1, 32]]), B.bk(bq), stTk)
                for cc in range(8):
                    if cc % 2 == 0:
                        wb_, wbk_ = B.wget(win_spec(OB + (cc // 2) * 256))
                        wc_, wck_ = B.wget(win_spec(OC + (cc // 2) * 256))
                        wh_, whk_ = B.wget(win_spec(OH + (cc // 2) * 256))
                    cj = (cc % 2) * P
                    i = cc % 2
                    u, uk, _ = ub[i]
                    if not halo:
                        B.cp("act", u[:, 0:2], uprev[:, cc, :], ["uprev"], uk)
                    for (c0, n) in all_segs:
                        hx, hk, _ = hs[i]
                        bx, bxk, _ = bs[i]
                        tx, txk, _ = tb[i]
                        is_own = (c0, n) in own_segs
                        bh = B.bank()
                        for k in range(16):
                            B.mm(ps[:, bh, 0:n], wh_[:, k, cj:cj + P], bigT[:, k, c0:c0 + n], k == 0, k == 15, whk_ + ["bigT"], B.bk(bh))
                        B.cp("act", hx[:, 0:n], ps[:, bh, 0:n], B.bk(bh), hk)
                        bc = B.bank()
                        for k in range(16):
                            B.mm(ps[:, bc, 0:n], wc_[:, k, cj:cj + P], bigT[:, k, c0:c0 + n], k == 0, k == 15, wck_ + ["bigT"], B.bk(bc))
                        B.tt("dve", u[:, 2 + c0:2 + c0 + n], ps[:, bc, 0:n], hx[:, 0:n], ALU.mult, B.bk(bc) + hk, uk)
                        if not is_own:
                            continue
                        bb = B.bank()
                        for k in range(16):
                            B.mm(ps[:, bb, 0:n], wb_[:, k, cj:cj + P], bigT[:, k, c0:c0 + n], k == 0, k == 15, wbk_ + ["bigT"], B.bk(bb))
                        B.cp("act", bx[:, 0:n], ps[:, bb, 0:n], B.bk(bb), bxk)
                        if c0 == soff and samp:
                            s0 = fap(stT, cc * 32, [[2, NS]])
                            s1 = fap(stT, cc * 32 + 1, [[2, NS]])
                            B.ts("dve", tx[:, 0:n], u[:, 2 + c0:2 + c0 + n], convw[:, cc, 2:3], ALU.mult, uk + ["convw0", "convw1", "convw2"], txk)
                            B.stt(tx[:, 0:n], s1, convw[:, cc, 1:2], tx[:, 0:n], ALU.mult, ALU.add, stTk + ["convw0", "convw1", "convw2"] + txk, txk)
                            B.stt(tx[:, 0:n], s0, convw[:, cc, 0:1], tx[:, 0:n], ALU.mult, ALU.add, stTk + ["convw0", "convw1", "convw2"] + txk, txk)
                            B.cp("act", us_all[:, cc, :], u[:, 2 + c0:2 + c0 + n], uk, us_allk)
                        else:
                            B.ts("dve", tx[:, 0:n], u[:, 2 + c0:2 + c0 + n], convw[:, cc, 2:3], ALU.mult, uk + ["convw0", "convw1", "convw2"], txk)
                            B.stt(tx[:, 0:n], u[:, 1 + c0:1 + c0 + n], convw[:, cc, 1:2], tx[:, 0:n], ALU.mult, ALU.add, uk + ["convw0", "convw1", "convw2"] + txk, txk)
                            B.stt(tx[:, 0:n], u[:, c0:c0 + n], convw[:, cc, 0:1], tx[:, 0:n], ALU.mult, ALU.add, uk + ["convw0", "convw1", "convw2"] + txk, txk)
                            B.cp("act", uprev[:, cc, :], u[:, 2 + c0 + n - 2:2 + c0 + n], uk, ["uprev"])
                            if samp:
                                B.cp("act", ul_all[:, cc, :], u[:, 2 + c0 + n - 2:2 + c0 + n], uk, ul_allk)
                        B.tt("dve", convT[:, cc, c0:c0 + n], tx[:, 0:n], bx[:, 0:n], ALU.mult, txk + bxk, ["convT"])
                if samp:
                    def fm_to_tm(src3, srck, n):
                        b2 = B.bank(2)
                        for cc in range(8):
                            B.tr(ps[0:n, b2 + cc // 4, (cc % 4) * P:(cc % 4 + 1) * P], src3[:, cc, :], ident_f[:], srck + ["ident_f"], B.bk(b2 + cc // 4))
                        o, ok_, _ = B.af32(1024)
                        B.cp("dve", fap(o, 0, [[1, 1024]], parts=n), fap(ps, b2 * 512, [[1, 1024]], parts=n), B.bk(b2, 2), ok_)
                        return o, ok_
                    uo, uok = fm_to_tm(us_all, us_allk, NS)
                    B.dma("sp", "o_ssc", ssc[:, 1, :], fap(uo, 0, [[1, 1024]], parts=NS), uok, ["ssc"])
                    B.dma("sp", "o_ssc0", ssc[:, 0, :], scv[:, 1, :], [], ["ssc0"])
                    ul, ulk = fm_to_tm(ul_all, ul_allk, 2)
                    B.dma("sp", "o_psc", psc[:, :], fap(ul, 0, [[1, 1024]], parts=2), ulk, ["psc"])
                    kc, kck, _ = B.af32(256)
                    bq5 = B.bank()
                    for kp in range(2):
                        B.tr(ps[:, bq5, kp * P:(kp + 1) * P], kT32[:, kp, 0:P], ident_f[:], ["kT32", "ident_f"], B.bk(bq5))
                    B.cp("dve", kc, ps[:, bq5, 0:256], B.bk(bq5), kck)
                    B.dma("sp", "o_pck", pck[:, :], kc, kck, ["pck"])
                    B.dma("sp", "o_pcv", pcv[:, :], v32[:, 0, :], ["v32"], ["pcv"])

                if debug and g == 0:
                    B.dma("pool", "dbgp0_" + str(DBGC.next()), dbg["attn"][:, :, :], attnT[:], ["attnT"], [])
                    B.dma("pool", "dbgp1_" + str(DBGC.next()), dbg["conv"][:, :, :], convT[:], ["convT"], [])
                if B.stop_after == "S2":
                    return

                B.areset()
                mixT, mixk, _ = B.abf(16 * NCMAX, [[NCMAX, 16], [1, NCMAX]])
                sg = [B.af32(512) for _ in range(2)]
                ta = [B.af32(512) for _ in range(2)]
                for jb in range(8):
                    wga, wgak = B.wget(win_spec(OGA + jb * 256))
                    wgc, wgck = B.wget(win_spec(OGC + jb * 256))
                    wao, waok = B.wget((8, 512, [(0, 256, w_ao[:, jb * 256:(jb + 1) * 256]), (256, 256, w_co[:, jb * 256:(jb + 1) * 256])]))
                    for jj in range(2):
                        j = jb * 2 + jj
                        for (c0, n) in own_segs:
                            i = j % 2
                            sgx, sgk, _ = sg[i]
                            tax, tak, _ = ta[i]
                            b = B.bank()
                            for k in range(16):
                                B.mm(ps[:, b, 0:n], wga[:, k, jj * P:(jj + 1) * P], bigT[:, k, c0:c0 + n], k == 0, k == 15, wgak + ["bigT"], B.bk(b))
                            B.act(sgx[:, 0:n], ps[:, b, 0:n], AF.Sigmoid, B.bk(b), sgk)
                            b = B.bank()
                            for k in range(8):
                                B.mm(ps[:, b, 0:n], wao[:, k, jj * P:(jj + 1) * P], attnT[:, k, c0:c0 + n], k == 0, k == 7, waok + ["attnT"], B.bk(b))
                            B.tt("dve", tax[:, 0:n], ps[:, b, 0:n], sgx[:, 0:n], ALU.mult, B.bk(b) + sgk, tak)
                            b = B.bank()
                            for k in range(16):
                                B.mm(ps[:, b, 0:n], wgc[:, k, jj * P:(jj + 1) * P], bigT[:, k, c0:c0 + n], k == 0, k == 15, wgck + ["bigT"], B.bk(b))
                            B.act(sgx[:, 0:n], ps[:, b, 0:n], AF.Sigmoid, B.bk(b), sgk)
                            b = B.bank()
                            for k in range(8):
                                B.mm(ps[:, b, 0:n], wao[:, k, 256 + jj * P:256 + (jj + 1) * P], convT[:, k, c0:c0 + n], k == 0, k == 7, waok + ["convT"], B.bk(b))
                            B.tt("dve", sgx[:, 0:n], ps[:, b, 0:n], sgx[:, 0:n], ALU.mult, B.bk(b) + sgk, sgk)
                            B.tt("dve", mixT[:, j, c0:c0 + n], tax[:, 0:n], sgx[:, 0:n], ALU.add, tak + sgk, mixk)

                for li, (c0, nr, src, kind, tt_) in enumerate(own_tiles):
                    B.dma("sp", "xres%d" % li, hres[0:nr, li, :], src, [], [("hres", li)])
                for n4 in range(4):
                    wlo, wlok = B.wget((8, 512, [(0, 512, w_o[0:1024, n4 * 512:(n4 + 1) * 512])]))
                    whi, whik = B.wget((8, 512, [(0, 512, w_o[1024:2048, n4 * 512:(n4 + 1) * 512])]))
                    for li, (c0, nr, src, kind, tt_) in enumerate(own_tiles):
                        b = B.bank()
                        for k in range(16):
                            w_, wk_ = (wlo, wlok) if k < 8 else (whi, whik)
                            B.mm(ps[0:nr, b, :], mixT[:, k, c0:c0 + nr], w_[:, k % 8, :], k == 0, k == 15, mixk + wk_, B.bk(b))
                        hr = hres[0:nr, li, n4 * 512:(n4 + 1) * 512]
                        B.stt(hr, hr, ALPHA, ps[0:nr, b, :], ALU.mult, ALU.add, [("hres", li)] + B.bk(b), [("hres", li)])
                if B.stop_after == "S3":
                    return

                B.areset()
                if g + 1 < NG:
                    prefetch_x(make_tiles(g + 1))
                lnt = [B.af32(32) for _ in range(2)]
                hbf = [B.abf(D) for _ in range(2)]
                rts = [B.af32(64 * 8) for _ in range(len(own_tiles))]
                actT, actk, _ = B.abf(4 * NCMAX, [[NCMAX, 4], [1, NCMAX]])
                sgs = [B.af32(512) for _ in range(2)]
                wrt, wrk = B.wget((16, 64, [(0, 64, w_r[:, :])]))
                for li, (c0, nr, src, kind, tt_) in enumerate(own_tiles):
                    layer_norm(hres[0:nr, li, :], [("hres", li)], nr, lnt[li % 2])
                    if debug:
                        r0 = TOK if kind == "samp" else tt_ * P
                        B.dma("sp", "dbg0_" + str(DBGC.next()), dbg["h"][r0:r0 + nr, :], hres[0:nr, li, :], [("hres", li)], [])
                    hb, hbk, _ = hbf[li % 2]
                    B.cp("act", fap(hb, 0, [[1, D]], parts=nr), hres[0:nr, li, :], [("hres", li)], hbk)
                    b2 = B.bank(2)
                    for k in range(16):
                        o = fap(psb, (b2 + k // 8) * 1024 + (k % 8) * nr, [[1, nr]])
                        B.tr(o, fap(hb, k * P, [[1, P]], parts=nr), ident[0:nr, 0:nr], hbk + ["ident"], B.bk(b2 + k // 8))
                    for hh in range(2):
                        src_ap = fap(psb, (b2 + hh) * 1024, [[nr, 8], [1, nr]])
                        B.cp("dve" if hh == 0 else "act", bigT[:, hh * 8:(hh + 1) * 8, c0:c0 + nr], src_ap, B.bk(b2 + hh), ["bigT"])
                    tix = NT if kind == "samp" else tt_
                    routing(1, li, c0, nr, tix, wrt, wrk, rts[li], None, None)
                r2_done = 0
                for cb in range(2):
                    wg_, wgk_ = B.wget((16, 256, [(0, 256, w_sg[:, cb * 256:(cb + 1) * 256])]))
                    wu_, wuk_ = B.wget((16, 256, [(0, 256, w_su[:, cb * 256:(cb + 1) * 256])]))
                    for jj in range(2):
                        c = cb * 2 + jj
                        for _ in range(2):
                            if r2_done < len(own_tiles) and (r2_done <= c + 1 or c == 3):
                                li = r2_done
                                (c0_, nr_, src_, kind_, tt2) = own_tiles[li]
                                routing(2, li, c0_, nr_, NT if kind_ == "samp" else tt2, wrt, wrk, rts[li], None, None)
                                r2_done += 1
                        for (c0, n) in own_segs:
                            sx_, sxk, _ = sgs[c % 2]
                            b = B.bank()
                            for k in range(16):
                                B.mm(ps[:, b, 0:n], wg_[:, k, jj * P:(jj + 1) * P], bigT[:, k, c0:c0 + n], k == 0, k == 15, wgk_ + ["bigT"], B.bk(b))
                            B.act(sx_[:, 0:n], ps[:, b, 0:n], AF.Silu, B.bk(b), sxk)
                            b = B.bank()
                            for k in range(16):
                                B.mm(ps[:, b, 0:n], wu_[:, k, jj * P:(jj + 1) * P], bigT[:, k, c0:c0 + n], k == 0, k == 15, wuk_ + ["bigT"], B.bk(b))
                            B.tt("dve", actT[:, c, c0:c0 + n], ps[:, b, 0:n], sx_[:, 0:n], ALU.mult, B.bk(b) + sxk, actk)
                while r2_done < len(own_tiles):
                    li = r2_done
                    (c0_, nr_, src_, kind_, tt2) = own_tiles[li]
                    routing(2, li, c0_, nr_, NT if kind_ == "samp" else tt2, wrt, wrk, rts[li], None, None)
                    r2_done += 1
                for li, (c0, nr, src, kind, tt_) in enumerate(own_tiles):
                    tix = NT if kind == "samp" else tt_
                    routing(3, li, c0, nr, tix, wrt, wrk, rts[li], None, None)
                for li, (c0, nr, src, kind, tt_) in enumerate(own_tiles):
                    hb, hbk, _ = hbf[li % 2]
                    if kind == "samp":
                        S.op("dve", lambda e, hb=hb: e.memset(hb, 0.0), [], hbk)
                    B.cp("act", fap(hb, 0, [[1, D]], parts=nr), hres[0:nr, li, :], [("hres", li)], hbk)
                    tix = NT if kind == "samp" else tt_
                    routing4(li, c0, nr, tix, rts[li], hb, hbk)
                for nb in range(2):
                    wd_, wdk_ = B.wget((4, 1024, [(0, 1024, w_sd[:, nb * 1024:(nb + 1) * 1024])]))
                    for n2 in range(2):
                        n4 = nb * 2 + n2
                        for li, (c0, nr, src, kind, tt_) in enumerate(own_tiles):
                            b = B.bank()
                            for k in range(4):
                                B.mm(ps[0:nr, b, :], actT[:, k, c0:c0 + nr], wd_[:, k, n2 * 512:(n2 + 1) * 512], k == 0, k == 3, actk + wdk_, B.bk(b))
                            hr = hres[0:nr, li, n4 * 512:(n4 + 1) * 512]
                            B.stt(hr, hr, ALPHA, ps[0:nr, b, :], ALU.mult, ALU.add, [("hres", li)] + B.bk(b), [("hres", li)])
                for li, (c0, nr, src, kind, tt_) in enumerate(own_tiles):
                    r0 = TOK if kind == "samp" else tt_ * P
                    B.dma("sp", "base%d" % li, BASE[r0:r0 + nr, :], hres[0:nr, li, :], [("hres", li)], ["BASE"])

            def layer_norm(xap, xkeys, nr, tmp, out=None, out_keys=None):
                st, stk, _ = tmp
                for q in range(4):
                    S.op("dve", lambda e, q=q: e.bn_stats(out=fap(st, q * 6, [[1, 6]], parts=nr), in_=fap(xap, q * 512, [[1, 512]], parts=nr)), xkeys, stk)
                mv = fap(st, 24, [[1, 2]], parts=nr)
                S.op("dve", lambda e: e.bn_aggr(out=mv, in_=fap(st, 0, [[1, 24]], parts=nr)), stk, stk)
                rs = fap(st, 26, [[1, 1]], parts=nr)
                B.act(rs, fap(st, 25, [[1, 1]], parts=nr), AF.Sqrt, stk + ["epsT"], stk, bias=epsT[0:nr, :])
                S.op("dve", lambda e: e.reciprocal(out=rs, in_=rs), stk, stk)
                o = xap if out is None else out
                ok_ = xkeys if out is None else out_keys
                nb = fap(st, 27, [[1, 1]], parts=nr)
                B.ts("dve", nb, fap(st, 24, [[1, 1]], parts=nr), rs, ALU.mult, stk, stk, s2=-1.0, op1=ALU.mult)
                B.act(o, xap, AF.Identity, xkeys + stk, ok_, scale=rs, bias=nb)
                B.tt("dve", o, o, lng[0:nr, :], ALU.mult, ok_ + ["lng"], ok_)
                B.tt("dve", o, o, lnb[0:nr, :], ALU.add, ok_ + ["lnb"], ok_)

            def routing(phase, li, c0, nr, tix, wrt, wrk, rt, hb, hbk):
                r, rk, _ = rt
                def rv(i, n=NE, parts=nr):
                    return fap(r, i * 64, [[1, n]], parts=parts)
                sc_ = rv(0)
                ch = rv(1)
                tmp = rv(2)
                sel = rv(3, parts=P)
                wd = rv(4)
                key = rv(5, parts=P)
                sm = fap(r, 6 * 64, [[1, 64]], parts=nr)
                m1 = fap(r, 6 * 64, [[1, 8]], parts=nr)
                m2 = fap(r, 6 * 64 + 8, [[1, 8]], parts=nr)
                gs = fap(r, 6 * 64 + 16, [[1, 8]], parts=nr)
                g8 = fap(r, 6 * 64 + 24, [[1, 8]], parts=nr)
                pen = fap(r, 6 * 64 + 32, [[1, 8]], parts=nr)
                c8 = fap(r, 6 * 64 + 40, [[1, 8]], parts=nr)
                ssum = fap(r, 6 * 64 + 48, [[1, 1]], parts=nr)
                ch3 = fap(r, 1 * 64, [[8, 8], [1, 8]], parts=nr)
                tmp3 = fap(r, 2 * 64, [[8, 8], [1, 8]], parts=nr)
                selb, selbk = rsel_t[:, li, :], [("rsel", li)]
                pos = rv(7, parts=P)
                d8f = fap(r, 6 * 64 + 56, [[1, 8]], parts=P)
                BIGC = float(2 * NSLOT)
                if phase == 1:
                    b = B.bank()
                    for k in range(16):
                        B.mm(ps[0:nr, b, 0:NE], bigT[:, k, c0:c0 + nr], wrt[:, k, :], k == 0, k == 15, ["bigT"] + wrk, B.bk(b))

                    B.act(sc_, ps[0:nr, b, 0:NE], AF.Sigmoid, B.bk(b), rk)
                    return
                if phase == 2:
                    B.tt("dve", ch, sc_, rbias[0:nr, :], ALU.add, rk + ["rbias"], rk)
                    B.red(m1, ch3, ALU.max, rk, rk)
                    B.tt("dve", tmp3, ch3, fap(r, 6 * 64, [[1, 8], [0, 8]], parts=nr), ALU.is_equal, rk, rk)
                    B.stt(tmp, tmp, -1e9, ch, ALU.mult, ALU.add, rk, rk)
                    B.red(m2, tmp3, ALU.max, rk, rk)
                    B.tt("dve", gs, m1, m2, ALU.add, rk, rk)
                    S.op("dve", lambda e: e.max(out=g8, in_=gs), rk, rk)
                    B.ts("dve", pen, gs, fap(r, 6 * 64 + 24 + 3, [[1, 1]], parts=nr), ALU.is_ge, rk, rk, s2=-1.0, op1=ALU.add)
                    B.ts("dve", pen, pen, 1e9, ALU.mult, rk, rk)
                    B.tt("dve", tmp3, ch3, fap(r, 6 * 64 + 32, [[1, 8], [0, 8]], parts=nr), ALU.add, rk, rk)
                    S.op("dve", lambda e: e.max(out=c8, in_=tmp), rk, rk)
                    if nr < P:
                        S.op("dve", lambda e: e.memset(sel, 0.0), rk, rk)
                    B.ts("dve", rv(3), tmp, fap(r, 6 * 64 + 40 + 7, [[1, 1]], parts=nr), ALU.is_ge, rk, rk)
                    B.stt(wd, rv(3), 1.0, sc_, ALU.mult, ALU.mult, rk, rk, accum_out=ssum)
                    S.op("dve", lambda e: e.reciprocal(out=ssum, in_=ssum), rk, rk)
                    B.ts("dve", wd, wd, ssum, ALU.mult, rk, rk, s2=2.5, op1=ALU.mult)
                    B.cp("dve", selb, sel, rk, selbk)
                    return
                bp = B.bank()
                B.mm(ps[:, bp, 0:NE], triU[:], selb, True, True, ["triU"] + selbk, B.bk(bp))
                B.tt("dve", pos, ps[:, bp, 0:NE], cbase[:], ALU.add, B.bk(bp) + ["cbase"], rk)
                bp2 = B.bank()
                B.mm(ps[:, bp2, 0:NE], ones[:], selb, True, True, ["ones"] + selbk, B.bk(bp2))
                B.tt("dve", cbase[:], cbase[:], ps[:, bp2, 0:NE], ALU.add, B.bk(bp2) + ["cbase"], ["cbase"])
                return

            def routing4(li, c0, nr, tix, rt, hb, hbk):
                r, rk, _ = rt

                def rv(i, n=NE, parts=nr):
                    return fap(r, i * 64, [[1, n]], parts=parts)
                sel = rv(3, parts=P)
                wd = rv(4)
                key = rv(5, parts=P)
                pos = rv(7, parts=P)
                d8f = fap(r, 6 * 64 + 56, [[1, 8]], parts=P)
                BIGC = float(2 * NSLOT)
                B.ts("dve", key, pos, float(CAP), ALU.is_lt, rk, rk)
                B.tt("dve", key, key, sel, ALU.mult, rk, rk)
                B.tt("dve", pos, pos, c16t[:, 64:128], ALU.add, rk + ["c16t"], rk)
                B.ts("dve", pos, pos, -1.0, ALU.mult, rk, rk, s2=BIGC, op1=ALU.add)
                B.tt("dve", key, key, pos, ALU.mult, rk, rk)
                S.op("dve", lambda e: e.max(out=d8f, in_=key), rk, rk)
                for j in range(8):
                    B.stt(rv(2, parts=nr), fap(r, 5 * 64, [[1, NE]], parts=nr), fap(r, 6 * 64 + 56 + j, [[1, 1]], parts=nr), wd,
                          ALU.is_equal, ALU.mult, rk, rk + [("w8", tix)], accum_out=w8all[0:nr, tix, j:j + 1])
                B.ts("dve", fap(r, 2 * 64, [[1, 8]], parts=nr), fap(r, 6 * 64 + 56, [[1, 8]], parts=nr), 0.0, ALU.is_gt, rk, rk)
                B.tt("dve", w8all[0:nr, tix, :], w8all[0:nr, tix, :], fap(r, 2 * 64, [[1, 8]], parts=nr), ALU.mult, rk + [("w8", tix)], [("w8", tix)])
                B.ts("dve", d8f, d8f, -1.0, ALU.mult, rk, rk, s2=BIGC, op1=ALU.add)
                B.cp("dve", d8all[:, tix, :], d8f, rk, [("d8", tix)])
                for j in range(8):
                    S.dma("pool", "scat%d" % (li % 2), lambda e, j=j: e.indirect_dma_start(
                        out=XG[:, :], out_offset=bass.IndirectOffsetOnAxis(ap=d8all[:, tix, j:j + 1], axis=0),
                        in_=hb, in_offset=None, bounds_check=B.bcreg(e), oob_is_err=False),
                        hbk + [("d8", tix), "XG"], [("XGs", tix, j)])


            def sample_attention(qT, qTk, soff):
                kn, knk, _ = B.af32(256)
                bq = B.bank()
                for kp in range(2):
                    B.tr(ps[0:NS, bq, kp * P:(kp + 1) * P], kT32[:, kp, P:P + NS], ident_f[:], ["kT32", "ident_f"], B.bk(bq))
                B.cp("dve", fap(kn, 0, [[1, 256]], parts=NS), ps[0:NS, bq, 0:256], B.bk(bq), knk)
                B.dma("sp", "sck", sck[:, 0:P - 1, :], ck[:, 1:P, :], [], ["sck"])
                B.dma("sp", "sck", sck[:, P - 1, :], fap(kn, 0, [[1, 256]], parts=NS), knk, ["sck"])
                B.dma("sp", "scv", scvo[:, 0:P - 1, :], cv[:, 1:P, :], [], ["scvo"])
                B.dma("sp", "scv", scvo[:, P - 1, :], v32[0:NS, 1, :], ["v32"], ["scvo"])
                Ke, Kek = fap(hres_bf, 0 * 4096, [[256, NS], [1, 256]]), [("hres", 0)]
                Ve, Vek = fap(hres_bf, 1 * 4096, [[256, NS], [1, 256]]), [("hres", 1)]
                stg = fap(hres, 2 * D, [[256, NS], [1, 256]])
                stgk = [("hres", 2), ("hres", 3)]
                B.dma("sp", "ske", stg, sck.rearrange("b k d -> k b d"), ["sck"], stgk)
                B.cp("act", Ke, stg, stgk, Kek)
                B.dma("sp", "ske", stg, scvo.rearrange("b k d -> k b d"), ["scvo"], stgk)
                B.cp("dve", Ve, stg, stgk, Vek)
                QM, QMk, _ = B.abf(64, [[16, 4], [1, 16]])
                KT2, KT2k, _ = B.abf(4 * P, [[P, 4], [1, P]])
                Ssb, Ssk = fap(hres, 2 * D, [[P, NS], [1, P]]), [("hres", 2)]
                for b_ in range(NS):
                    qin = fap(qT, soff + b_, [[0, 4], [640, 8], [0, 2]])
                    B.tt("dve", fap(QM, 0, [[16, 4], [2, 8], [1, 2]]), qin, fap(c16t, 0, [[16, 4], [2, 8], [1, 2]]), ALU.mult, qTk + ["c16t"], QMk)
                    bt = B.bank()
                    for kv in range(4):
                        for dup in range(2):
                            B.tr(psb[64 * dup:64 * dup + 64, bt, kv * P:(kv + 1) * P], Ke[:, b_, kv * 64:(kv + 1) * 64], ident[:], Kek + ["ident"], B.bk(bt))
                    B.cp("act", KT2, fap(psb, bt * 1024, [[P, 4], [1, P]]), B.bk(bt), KT2k)
                    bsc = B.bank()
                    for kv in range(4):
                        B.mm(ps[0:16, bsc, 0:P], QM[:, kv, :], KT2[:, kv, :], kv == 0, kv == 3, QMk + KT2k, B.bk(bsc))
                    B.cp("dve", fap(Ssb, b_ * P, [[1, P]], parts=16), ps[0:16, bsc, 0:P], B.bk(bsc), Ssk)
                sx, sxk, _ = B.af32(5 * NS)
                S16 = fap(Ssb, 0, [[P, NS], [1, P]], parts=16)
                mx = fap(sx, 0, [[1, NS]], parts=16)
                sm = fap(sx, NS, [[1, NS]], parts=16)
                tq = fap(sx, 2 * NS, [[1, NS]], parts=16)
                rv_ = fap(sx, 3 * NS, [[1, NS]], parts=16)
                B.red(mx, S16, ALU.max, Ssk, sxk)
                B.ts("dve", mx, mx, sinkcol[0:16, 0:1], ALU.max, sxk + ["sinkcol"], sxk)
                B.tt("dve", S16, S16, fap(sx, 0, [[1, NS], [0, P]], parts=16), ALU.subtract, Ssk + sxk, Ssk)
                B.act(S16, S16, AF.Exp, Ssk, Ssk, scale=0.125)
                B.red(sm, S16, ALU.add, Ssk, sxk)
                B.ts("dve", tq, mx, -1.0, ALU.mult, sxk, sxk, s2=sinkcol[0:16, 0:1], op1=ALU.add)
                B.act(tq, tq, AF.Exp, sxk, sxk, scale=0.125)
                B.tt("dve", rv_, sm, tq, ALU.add, sxk, sxk)
                S.op("dve", lambda e: e.reciprocal(out=rv_, in_=rv_), sxk, sxk)
                Pb, Pbk = fap(hres_bf, 4 * 4096, [[P, NS], [1, P]]), [("hres", 4)]
                B.tt("dve", fap(Pb, 0, [[P, NS], [1, P]], parts=16), S16, fap(sx, 3 * NS, [[1, NS], [0, P]], parts=16), ALU.mult, Ssk + sxk, Pbk)
                bt = B.bank()
                for b_ in range(NS):
                    B.tr(psb[:, bt, b_ * 16:(b_ + 1) * 16], fap(Pb, b_ * P, [[1, P]], parts=16), ident[0:16, 0:16], Pbk + ["ident"], B.bk(bt))
                PTs, PTsk, _ = B.abf(NS * 16)
                B.cp("act", PTs, psb[:, bt, 0:NS * 16], B.bk(bt), PTsk)
                As, Ask = fap(hres, 4 * D + 1024, [[64, NS], [1, 64]]), [("hres", 4)]
                tmpv, tmpvk, _ = B.af32(256)
                for b_ in range(NS):
                    bo = B.bank()
                    B.mm(ps[0:16, bo, 0:256], PTs[:, b_ * 16:(b_ + 1) * 16], Ve[:, b_, :], True, True, PTsk + Vek, B.bk(bo))
                    B.tt("dve", fap(tmpv, 0, [[64, 4], [1, 64]], parts=16), fap(ps, bo * 512, [[64, 4], [1, 64]], parts=16),
                         fap(c16t, 192, [[1, 4], [0, 64]], parts=16), ALU.mult, B.bk(bo) + ["c16t"], tmpvk)
                    B.red(fap(As, b_ * 64, [[1, 64]], parts=16), fap(tmpv, 0, [[1, 64], [64, 4]], parts=16), ALU.add, tmpvk, Ask)
                A2, A2k = fap(hres, 3 * D, [[P, NS], [1, P]]), [("hres", 3)]
                B.tt("dve", fap(A2, 0, [[P, NS], [64, 2], [1, 64]], parts=16), fap(As, 0, [[64, NS], [0, 2], [1, 64]], parts=16),
                     fap(c16t, 200, [[0, NS], [1, 2], [0, 64]], parts=16), ALU.mult, Ask + ["c16t"], A2k)
                bt2 = B.bank()
                for b_ in range(NS):
                    B.tr(ps[:, bt2, b_ * 16:(b_ + 1) * 16], fap(A2, b_ * P, [[1, P]], parts=16), ident_f[0:16, 0:16], A2k + ["ident_f"], B.bk(bt2))
                af_, afk, _ = B.af32(8 * NS, [[NS, 8], [1, NS]])
                B.red(af_, fap(ps, bt2 * 512, [[2, 8], [16, NS], [1, 2]]), ALU.add, B.bk(bt2), afk)
                B.cp("dve", fap(attnT, soff, [[640, 8], [1, NS]]), af_, afk, ["attnT"])


            S.planning = True
            phase_a()
            S.planning = False
            B.bank_rr = 0
            phase_a()
        S.barrier()

        if stop_after is None:
            pb = contextlib.ExitStack()
            with pb:
                def sbb(name, shape, dt):
                    return pb.enter_context(nc.sbuf_tensor(name, list(shape), dt))
                wg = [sbb("wg%d" % i, [P, 16, 512], BF16) for i in range(2)]
                wu = [sbb("wu%d" % i, [P, 16, 512], BF16) for i in range(2)]
                wdn = [sbb("wd%d" % i, [P, 4, D], BF16) for i in range(2)]
                xb_ = [sbb("xb%d" % i, [P, NBLK, D], BF16) for i in range(2)]
                xbT = [sbb("xbT%d" % i, [P, 16, CAP], BF16) for i in range(2)]
                acT = [sbb("acT%d" % i, [P, 4, CAP], BF16) for i in range(2)]
                sgt = [sbb("sgt%d" % i, [P, CAP], F32) for i in range(2)]
                yo = [sbb("yo%d" % i, [P, D], BF16) for i in range(2)]

                def load_expert(e):
                    i = e % 2
                    for k0 in range(0, 16, 8):
                        B.dma("pool", "eg%d" % i, wg[i][:, k0:k0 + 8, :], w_eg[e, k0 * P:(k0 + 8) * P, :].rearrange("(k p) n -> p k n", p=P), [], [("wg", i)])
                        B.dma("pool", "eu%d" % i, wu[i][:, k0:k0 + 8, :], w_eu[e, k0 * P:(k0 + 8) * P, :].rearrange("(k p) n -> p k n", p=P), [], [("wu", i)])
                    for k in range(0, 4, 2):
                        B.dma("pool", "ed%d" % i, wdn[i][:, k:k + 2, :], w_ed[e, k * P:(k + 2) * P, :].rearrange("(k p) n -> p k n", p=P), [], [("wdn", i)])
                    B.dma("sp", "xb%d" % i, xb_[i][:], XG[e * CAP:(e + 1) * CAP, :].rearrange("(b p) d -> p b d", p=P), ["XG"] + [("XGs", t, j) for t in range(17) for j in range(8)], [("xb", i)])

                load_expert(0)
                yi = 0
                for e in range(NE):
                    i = e % 2
                    if e + 1 < NE:
                        load_expert(e + 1)
                    for blk in range(NBLK):
                        for h2 in range(2):
                            bt = B.bank()
                            for k8 in range(8):
                                k = h2 * 8 + k8
                                B.tr(psb[:, bt, k8 * P:(k8 + 1) * P], xb_[i][:, blk, k * P:(k + 1) * P], ident[:], [("xb", i), "ident"], B.bk(bt))
                            B.cp("act" if (blk * 2 + h2) % 2 == 0 else "dve", xbT[i][:, h2 * 8:(h2 + 1) * 8, blk * P:(blk + 1) * P],
                                 fap(psb, bt * 1024, [[P, 8], [1, P]]), B.bk(bt), [("xbT", i)])
                    for c in range(4):
                        bg = B.bank()
                        for k in range(16):
                            B.mm(ps[:, bg, 0:CAP], wg[i][:, k, c * P:(c + 1) * P], xbT[i][:, k, :], k == 0, k == 15, [("wg", i), ("xbT", i)], B.bk(bg))
                        B.act(sgt[c % 2][:], ps[:, bg, 0:CAP], AF.Silu, B.bk(bg), [("sgt", c % 2)])
                        bu = B.bank()
                        for k in range(16):
                            B.mm(ps[:, bu, 0:CAP], wu[i][:, k, c * P:(c + 1) * P], xbT[i][:, k, :], k == 0, k == 15, [("wu", i), ("xbT", i)], B.bk(bu))
                        B.tt("dve", acT[i][:, c, :], ps[:, bu, 0:CAP], sgt[c % 2][:], ALU.mult, B.bk(bu) + [("sgt", c % 2)], [("acT", i)])
                    for blk in range(NBLK):
                        y_ = yo[yi % 2]
                        yk = [("yo", yi % 2)]
                        for n4 in range(4):
                            b = B.bank()
                            for k in range(4):
                                B.mm(ps[:, b, :], acT[i][:, k, blk * P:(blk + 1) * P], wdn[i][:, k, n4 * 512:(n4 + 1) * 512], k == 0, k == 3, [("acT", i), ("wdn", i)], B.bk(b))
                            B.cp("act" if n4 % 2 == 0 else "dve", y_[:, n4 * 512:(n4 + 1) * 512], ps[:, b, :], B.bk(b), yk)
                        r0 = e * CAP + blk * P
                        B.dma("sp", "yst%d" % (yi % 2), Y[r0:r0 + P, :], y_[:], yk, ["Y"])
                        yi += 1
            S.barrier()

            pc = contextlib.ExitStack()
            with pc:
                def sbc(name, shape, dt):
                    return pc.enter_context(nc.sbuf_tensor(name, list(shape), dt))
                acc = [sbc("acc%d" % i, [P, D], F32) for i in range(2)]
                yg = [sbc("yg%d" % i, [P, D], BF16) for i in range(6)]
                lnt2 = [sbc("lnt2%d" % i, [P, 32], F32) for i in range(2)]
                B.dma("sp", "cst2", lng[:], fap_dram_bcast(ln2_g, D), ["lng"], ["lng"])
                B.dma("sp", "cst2", lnb[:], fap_dram_bcast(ln2_b, D), ["lnb"], ["lnb"])
                dgt = [sbc("dg%d" % i, [P, 16, P], BF16) for i in range(2)]
                wsm = [sbc("wsm%d" % i, [P, 32], F32) for i in range(2)]
                wbf = [sbc("wbf%d" % i, [P, 8], BF16) for i in range(2)]
                gi = [0]

                def f_ctx(tix):
                    nr = P if tix < NT else NS
                    r0 = tix * P if tix < NT else TOK
                    par = tix % 2
                    bks = [4 * par + n4 for n4 in range(4)]
                    return nr, r0, acc[par], [("acc", par)], dgt[par], [("dg", par)], wsm[par], [("wsm", par)], wbf[par], bks

                def f_prep(tix):
                    nr, r0, a, ak, dg, dgk, ws, wsk, wb16, bks = f_ctx(tix)
                    B.dma("sp", "bl%d" % (tix % 2), a[0:nr, :], BASE[r0:r0 + nr, :], ["BASE"], ak)
                    B.cp("dve", wb16[0:nr, :], w8all[0:nr, tix, :], [("w8", tix)], wsk)
                    B.cp("dve", ws[0:nr, 0:8], wb16[0:nr, :], wsk, wsk)
                    B.tt("dve", ws[0:nr, 8:16], w8all[0:nr, tix, :], ws[0:nr, 0:8], ALU.subtract, wsk + [("w8", tix)], wsk)
                    for j in range(8):
                        B.act(dg[0:nr, 2 * j, 0:nr], ident_f[0:nr, 0:nr], AF.Copy, wsk + ["ident_f"], dgk, scale=ws[0:nr, j:j + 1])
                        B.act(dg[0:nr, 2 * j + 1, 0:nr], ident_f[0:nr, 0:nr], AF.Copy, wsk + ["ident_f"], dgk, scale=ws[0:nr, 8 + j:9 + j])
                    for j in range(8):
                        g_ = yg[gi[0] % 6]
                        gk = [("yg", gi[0] % 6)]
                        S.dma("pool", "yg%d" % (gi[0] % 6), lambda e, g_=g_, tix=tix, j=j: e.indirect_dma_start(
                            out=g_[:], out_offset=None, in_=Y[:, :],
                            in_offset=bass.IndirectOffsetOnAxis(ap=d8all[:, tix, j:j + 1], axis=0),
                            bounds_check=B.bcreg(e), oob_is_err=False), ["Y", ("d8", tix)], gk)
                        for part in range(2):
                            for n4 in range(4):
                                B.mm(ps[0:nr, bks[n4], :], dg[0:nr, 2 * j + part, 0:nr], g_[0:nr, n4 * 512:(n4 + 1) * 512],
                                     j == 0 and part == 0, j == 7 and part == 1, gk + dgk, B.bk(bks[n4]))
                        gi[0] += 1

                def f_finish(tix):
                    nr, r0, a, ak, dg, dgk, ws, wsk, wb16, bks = f_ctx(tix)
                    for n4 in range(4):
                        B.tt("dve", a[0:nr, n4 * 512:(n4 + 1) * 512], a[0:nr, n4 * 512:(n4 + 1) * 512], ps[0:nr, bks[n4], :], ALU.add,
                             ak + B.bk(bks[n4]), ak)
                    layer_norm(a[0:nr, :], ak, nr, (lnt2[tix % 2][:], [("lnt2", tix % 2)], None))
                    if tix < NT:
                        B.dma("sp", "yout%d" % (tix % 2), yp[r0:r0 + nr, :], a[0:nr, :], ak, ["yp"])
                    else:
                        B.dma("sp", "yout%d" % (tix % 2), ys[:, :], a[0:nr, :], ak, ["ys"])

                f_prep(0)
                for tix in range(17):
                    if tix + 1 < 17:
                        f_prep(tix + 1)
                    f_finish(tix)
                if debug:
                    B.dma("sp", "dbg1_" + str(DBGC.next()), dbg["cnt"][:, :], cbase[:], ["cbase"], [])
                    dd = sbc("ddbg", [P, 17, 8], F32)
                    B.cp("dve", dd[:], d8all[:], [("d8", t) for t in range(17)], ["ddbg"])
                    B.dma("sp", "dbg2_" + str(DBGC.next()), dbg["d8"][:, :, :], dd[:], ["ddbg"], [])
                    B.dma("sp", "dbg3_" + str(DBGC.next()), dbg["w8"][:, :, :], w8all[:], [("w8", t) for t in range(17)], [])

        S.finalize(es)
        with nc.Block() as block:
            S.run_block(block)
    return nc


def _consts(core):
    hh = core % 2
    half = 8
    inv_freq = np.power(np.float32(500000.0), -np.arange(half, dtype=np.float32) * np.float32(2.0 / 16)).astype(np.float32)
    pos = np.zeros(2192, np.float32)
    pos[0:P] = hh * TOK - P + np.arange(P)
    pos[P:P + TOK] = hh * TOK + np.arange(TOK)
    pos[P + TOK:] = PAST
    pos = np.maximum(pos, 0).astype(np.float32)
    ang = (pos[:, None] * inv_freq[None, :]).astype(np.float32)
    cosv = np.cos(ang).astype(np.float32)
    sinv = np.sin(ang).astype(np.float32)
    cosT = np.ones((P, 2192), np.float32)
    sinT = np.zeros((P, 2192), np.float32)
    for p in range(P):
        d = p % 64
        if d < 16:
            cosT[p] = cosv[:, d % 8]
            sinT[p] = sinv[:, d % 8]
    ident = np.eye(P, dtype=np.float32)
    R = np.zeros((P, P), np.float32)
    for m in range(P):
        d = m % 64
        if d < 8:
            R[m, m + 8] = -1.0
        elif d < 16:
            R[m, m - 8] = 1.0
    rotT = R.T.copy()
    triU = np.triu(np.ones((P, P), np.float32), 1)
    ones = np.ones((P, P), np.float32)
    cst = np.concatenate([ident, rotT, triU, ones, np.zeros((P, 512), np.float32)], axis=1)
    a = np.arange(P)[:, None]
    c = np.arange(2 * P)[None, :]
    valid = (c > a) & (c <= a + P)
    mask_gen = np.where(valid, 0.0, NEG).astype(np.float32)
    if hh == 0:
        mask_first = np.where(valid & (c >= P), 0.0, NEG).astype(np.float32)
    else:
        mask_first = mask_gen
    msk = np.concatenate([mask_gen, mask_first], axis=1)
    c16 = np.zeros((P, 232), np.float32)
    for p in range(P):
        for kv in range(4):
            for h in range(16):
                c16[p, kv * 16 + h] = 1.0 if ((p // 64) == (h % 2) and (h // 4) == kv) else 0.0
    c16[:, 64:128] = (np.arange(NE) * CAP)[None, :]
    c16[:, 128:192] = np.arange(NE)[None, :]
    for h in range(16):
        for kv in range(4):
            c16[h, 192 + kv] = 1.0 if h // 4 == kv else 0.0
        for par in range(2):
            c16[h, 200 + par] = 1.0 if h % 2 == par else 0.0
    return dict(cosT=cosT, sinT=sinT, cst=cst, msk=msk, c16=c16)


_NC_CACHE = {}


def kernel(x_prompt, x_sample, cache_k, cache_v, state_conv, w_in, attn_sinks, conv_w,
           w_attn_out, w_conv_out, w_o, ln1_g, ln1_b, w_router, router_bias,
           w_exp_gate, w_exp_up, w_exp_down, w_sh_gate, w_sh_up, w_sh_down, ln2_g, ln2_b,
           _stop_after=None, _debug=False, _cores=None):
    f = lambda a: np.ascontiguousarray(np.asarray(a, dtype=np.float32))
    x_prompt, x_sample = f(x_prompt), f(x_sample)
    key = (_stop_after, _debug)
    if key not in _NC_CACHE:
        _NC_CACHE[key] = build_program(_stop_after, _debug)
    nc = _NC_CACHE[key]
    shared = dict(
        w_in=f(w_in[0]), sinks=f(attn_sinks), conv_w=f(conv_w[0]), w_ao=f(w_attn_out[0]), w_co=f(w_conv_out[0]),
        w_o=f(w_o[0]), ln1_g=f(ln1_g), ln1_b=f(ln1_b), w_r=f(w_router[0]), r_bias=f(router_bias),
        w_eg=f(w_exp_gate[0]), w_eu=f(w_exp_up[0]), w_ed=f(w_exp_down[0]), w_sg=f(w_sh_gate[0]),
        w_su=f(w_sh_up[0]), w_sd=f(w_sh_down[0]), ln2_g=f(ln2_g), ln2_b=f(ln2_b))
    if _stop_after is not None:
        for k_ in ("w_eg", "w_eu", "w_ed"):
            shared.pop(k_)
    in_maps = []
    cores = list(range(NCORES)) if _cores is None else list(_cores)
    for c in cores:
        n, hh = c // 2, c % 2
        xp = np.zeros((TOK + P, D), np.float32)
        if hh == 1:
            xp[0:P] = x_prompt[n, TOK - P:TOK]
        xp[P:] = x_prompt[n, hh * TOK:(hh + 1) * TOK]
        m = dict(shared)
        m.update(_consts(c))
        m.update(xp=xp, xs=f(x_sample[c * NS:(c + 1) * NS, 0, :]),
                 ck=f(cache_k[0, c * NS:(c + 1) * NS].reshape(NS, P, 256)),
                 cv=f(cache_v[0, c * NS:(c + 1) * NS].reshape(NS, P, 256)),
                 sc=f(state_conv[0, c * NS:(c + 1) * NS]))
        in_maps.append(m)
    res = run_bass_kernel_spmd(nc, in_maps, core_ids=list(range(len(cores))))
    R = res.results
    if _debug or _stop_after is not None:
        return R
    y_p = np.zeros((4, SEQ, D), np.float32)
    y_s = np.zeros((128, 1, D), np.float32)
    pk = np.zeros((1, 4, P, 4, 64), np.float32)
    pv = np.zeros((1, 4, P, 4, 64), np.float32)
    pc_ = np.zeros((1, 4, 2, 1024), np.float32)
    sk = np.zeros((1, 128, P, 4, 64), np.float32)
    sv = np.zeros((1, 128, P, 4, 64), np.float32)
    ssc_ = np.zeros((1, 128, 2, 1024), np.float32)
    for c in range(NCORES):
        n, hh = c // 2, c % 2
        y_p[n, hh * TOK:(hh + 1) * TOK] = R[c]["yp"]
        y_s[c * NS:(c + 1) * NS, 0] = R[c]["ys"]
        if hh == 1:
            pk[0, n] = R[c]["pck"].reshape(P, 4, 64)
            pv[0, n] = R[c]["pcv"].reshape(P, 4, 64)
            pc_[0, n] = R[c]["psc"]
        sk[0, c * NS:(c + 1) * NS] = R[c]["sck"].reshape(NS, P, 4, 64)
        sv[0, c * NS:(c + 1) * NS] = R[c]["scvo"].reshape(NS, P, 4, 64)
        ssc_[0, c * NS:(c + 1) * NS] = R[c]["ssc"]
    return (y_p, y_s, pk, pv, pc_, sk, sv, ssc_)
```

```python
import contextlib
import numpy as np
import concourse.bass as bass
import concourse.mybir as mybir
from concourse.bass_utils import run_bass_kernel_spmd

F32 = mybir.dt.float32
BF16 = mybir.dt.bfloat16
I32 = mybir.dt.int32
AF = mybir.ActivationFunctionType
ALU = mybir.AluOpType
AX = mybir.AxisListType

ENGS = ("pe", "act", "dve", "pool", "sp")


class _Ctr:
    def __init__(self):
        self.n = 0

    def next(self):
        self.n += 1
        return self.n


DBGC = _Ctr()

P = 128
D = 2048
NCORES = 8
SEQ = 4096
TOK = 2048
NS = 16
NT = 16
GT = 4
NG = NT // GT
NE = 64
CAP = 384
NBLK = CAP // P
NSLOT = NE * CAP
ALPHA = 2.0 ** 0.25
LN_EPS = 1e-5
PAST = 16384
OQ, OK_, OV, OB, OC, OH, OGA, OGC = 0, 1024, 1280, 1536, 2560, 3584, 4608, 6656
NEG = -30000.0


class Op:
    __slots__ = ("eng", "fn", "deps", "is_dma", "chan", "idx", "signal", "semval", "gidx")

    def __init__(self, eng, fn, is_dma, chan):
        self.eng = eng
        self.fn = fn
        self.deps = {}
        self.is_dma = is_dma
        self.chan = chan
        self.signal = False
        self.semval = None


def _slot(op):
    return ("ch", op.chan) if op.is_dma else ("eng", op.eng)


class Sched:
    def __init__(self, nc):
        self.nc = nc
        self.ops = {e: [] for e in ENGS}
        self.last_writer = {}
        self.readers = {}
        self.all_ops = []
        self.planning = False

    def _add(self, eng, fn, reads, writes, is_dma=False, chan=None):
        if self.planning:
            return None
        import os
        mx = int(os.environ.get("KMAXOPS", "0"))
        if mx and len(self.all_ops) >= mx:
            return None
        psr = [k for k in reads if isinstance(k, tuple) and k[0] == "ps"]
        if psr:
            writes = list(writes) + psr
        op = Op(eng, fn, is_dma, chan)
        deps = {}

        def add(d):
            if d is op:
                return
            s = _slot(d)
            o = deps.get(s)
            if o is None or d.gidx > o.gidx:
                deps[s] = d

        for k in reads:
            w = self.last_writer.get(k)
            if w is not None:
                add(w)
        for k in writes:
            w = self.last_writer.get(k)
            if w is not None:
                add(w)
            for r in self.readers.get(k, {}).values():
                add(r)
        op.deps = deps
        op.gidx = len(self.all_ops)
        for k in reads:
            self.readers.setdefault(k, {})[_slot(op)] = op
        for k in writes:
            self.last_writer[k] = op
            self.readers[k] = {}
        self.ops[eng].append(op)
        self.all_ops.append(op)
        return op

    def op(self, eng, fn, reads=(), writes=()):
        return self._add(eng, fn, reads, writes)

    def dma(self, eng, chan, fn, reads=(), writes=()):
        return self._add(eng, fn, reads, writes, is_dma=True, chan=chan)

    def barrier(self):
        if self.planning:
            return
        lasts = {}
        for op in self.all_ops:
            if op.fn is not None:
                lasts[_slot(op)] = op
        for e in ENGS:
            op = Op(e, None, False, None)
            op.deps = {s: d for s, d in lasts.items()}
            op.gidx = len(self.all_ops)
            self.ops[e].append(op)
            self.all_ops.append(op)

    def finalize(self, es):
        nc = self.nc
        for op in self.all_ops:
            for d in op.deps.values():
                if d.is_dma:
                    continue
                if d.eng == "pe" and op.eng == "pe" and not op.is_dma:
                    continue
                d.signal = True
        self.sem = {e: es.enter_context(nc.semaphore("sem_" + e)) for e in ENGS}
        chans = []
        for op in self.all_ops:
            if op.is_dma and op.chan not in chans:
                chans.append(op.chan)
        self.chsem = {c: es.enter_context(nc.semaphore("ch_" + str(c))) for c in chans}
        cnt = {e: 0 for e in ENGS}
        chcnt = {c: 0 for c in chans}
        for op in self.all_ops:
            if op.is_dma:
                chcnt[op.chan] += 16
                op.semval = chcnt[op.chan]
            elif op.signal:
                cnt[op.eng] += 1
                op.semval = cnt[op.eng]
        for op in self.all_ops:
            if op.is_dma and str(op.chan).startswith("cst"):
                op.semval = chcnt[op.chan]
        self.chfinal = chcnt

    def emit(self, ename, eng):
        seen = {}
        for op in self.ops[ename]:
            for s, d in op.deps.items():
                if (not d.is_dma) and d.eng == "pe" and op.eng == "pe" and not op.is_dma:
                    continue
                v = d.semval
                if v is None:
                    continue
                if seen.get(s, 0) >= v:
                    continue
                sem = self.chsem[s[1]] if s[0] == "ch" else self.sem[s[1]]
                eng.wait_ge(sem, v)
                seen[s] = v
            if op.fn is None:
                continue
            inst = op.fn(eng)
            if op.is_dma:
                inst.then_inc(self.chsem[op.chan], 16)
            elif op.signal:
                inst.then_inc(self.sem[op.eng], 1)

    def run_block(self, block):
        S = self

        def mk(ename):
            def body(eng):
                S.emit(ename, eng)
                if ename == "sp":
                    for c, v in S.chfinal.items():
                        if v > 0:
                            eng.wait_ge(S.chsem[c], v)
            return body

        block.tensor(mk("pe"))
        block.scalar(mk("act"))
        block.vector(mk("dve"))
        block.gpsimd(mk("pool"))
        block.sync(mk("sp"))


def fap(t, offset, dims, parts=None, p0=0):
    base = t if isinstance(t, bass.AP) else t[:]
    pst = base.ap[0][0]
    npart = base.ap[0][1] if parts is None else parts
    return bass.AP(tensor=base.tensor, offset=base.offset + p0 * pst + offset,
                   ap=[[pst, npart]] + [list(d) for d in dims])


class Builder:
    def __init__(self, stop_after=None, debug=False):
        self.stop_after = stop_after
        self.debug = debug
        self.nc = bass.Bass("TRN2", target_bir_lowering=False)
        self.S = Sched(self.nc)
        self.bank_rr = 0
        self.wplan = []
        self.wpos = 0
        self.tmp_rr = {}

    def bcreg(self, e):
        if getattr(self, "_bcreg", None) is None:
            self._bcreg = e.to_reg(NSLOT - 1)
        return self._bcreg

    def din(self, name, shape, dt=F32):
        return self.nc.dram_tensor(name, list(shape), dt, kind="ExternalInput").ap()

    def dout(self, name, shape, dt=F32):
        return self.nc.dram_tensor(name, list(shape), dt, kind="ExternalOutput").ap()

    def dscr(self, name, shape, dt=F32):
        return self.nc.dram_tensor(name, list(shape), dt, kind="Internal").ap()

    def bank(self, n=1):
        if n == 2 and self.bank_rr % 2 == 1:
            self.bank_rr += 1
        b = self.bank_rr % 6
        self.bank_rr += n
        return b

    def bk(self, b, n=1):
        return [("ps", b + i) for i in range(n)]

    def mm(self, out, lhsT, rhs, start, stop, reads, writes):
        self.S.op("pe", lambda e: e.matmul(out, lhsT=lhsT, rhs=rhs, start=start, stop=stop), reads, writes)

    def tr(self, out, in_, ident, reads, writes):
        self.S.op("pe", lambda e: e.transpose(out=out, in_=in_, identity=ident), reads, writes)

    def act(self, out, in_, func, reads, writes, scale=None, bias=None, accum_out=None):
        kw = {}
        if scale is not None:
            kw["scale"] = scale
        if bias is not None:
            kw["bias"] = bias
        if accum_out is not None:
            kw["accum_out"] = accum_out
        self.S.op("act", lambda e: e.activation(out=out, in_=in_, func=func, **kw), reads, writes)

    def tt(self, eng, out, in0, in1, op, reads, writes):
        self.S.op(eng, lambda e: e.tensor_tensor(out=out, in0=in0, in1=in1, op=op), reads, writes)

    def ts(self, eng, out, in0, s1, op0, reads, writes, s2=None, op1=None, accum_out=None):
        kw = {}
        if op1 is not None:
            kw["op1"] = op1
        if accum_out is not None:
            kw["accum_out"] = accum_out
        self.S.op(eng, lambda e: e.tensor_scalar(out=out, in0=in0, scalar1=s1, scalar2=s2, op0=op0, **kw), reads, writes)

    def stt(self, out, in0, scalar, in1, op0, op1, reads, writes, accum_out=None):
        kw = {}
        if accum_out is not None:
            kw["accum_out"] = accum_out
        self.S.op("dve", lambda e: e.scalar_tensor_tensor(out=out, in0=in0, scalar=scalar, in1=in1, op0=op0, op1=op1, **kw), reads, writes)

    def red(self, out, in_, op, reads, writes, axis=AX.X):
        self.S.op("dve", lambda e: e.tensor_reduce(out=out, in_=in_, axis=axis, op=op), reads, writes)

    def cp(self, eng, out, in_, reads, writes):
        if eng == "act":
            self.S.op("act", lambda e: e.activation(out=out, in_=in_, func=AF.Copy), reads, writes)
        else:
            self.S.op(eng, lambda e: e.tensor_copy(out=out, in_=in_), reads, writes)

    def dma(self, eng, chan, out, in_, reads, writes, **kw):
        return self.S.dma(eng, chan, lambda e: e.dma_start(out=out, in_=in_, **kw), reads, writes)

    def wget(self, spec):
        S = self.S
        i = self.wpos
        self.wpos += 1
        if S.planning:
            self.wplan.append(spec)
        slot = i % self.NB
        nk, ncols, parts = spec
        while self.wissued < min(len(self.wplan), i + self.NB - 2):
            self._wissue(self.wissued)
            self.wissued += 1
        ap = fap(self.wring, slot * self.WSLOT, [[ncols, nk], [1, ncols]])
        return ap, [("w", slot)]

    def _wissue(self, j):
        if self.S.planning:
            return
        nk, ncols, parts = self.wplan[j]
        slot = j % self.NB
        for (c0, n, src) in parts:
            dst = fap(self.wring, slot * self.WSLOT + c0, [[ncols, nk], [1, n]])
            self.dma("pool", "w%d" % slot, dst, src.rearrange("(k p) n -> p k n", p=P), [], [("w", slot)])

    def areset(self):
        self.apos = 0

    def aalloc(self, nbytes):
        nbytes = (nbytes + 63) // 64 * 64
        off = self.apos
        self.apos += nbytes
        assert self.apos <= self.ARENA_BYTES, ("arena overflow", self.apos)
        keys = [("ar", b) for b in range(off // 2048, (off + nbytes - 1) // 2048 + 1)]
        return off, keys

    def af32(self, n, dims=None):
        off, keys = self.aalloc(n * 4)
        ap = fap(self.arena, off // 4, dims if dims is not None else [[1, n]])
        return ap, keys, off // 4

    def abf(self, n, dims=None):
        off, keys = self.aalloc(n * 2)
        ap = fap(self.arena_bf, off // 2, dims if dims is not None else [[1, n]])
        return ap, keys, off // 2


def build_program(stop_after=None, debug=False):
    B = Builder(stop_after, debug)
    nc = B.nc
    S = B.S

    xp = B.din("xp", [TOK + P, D])
    xs = B.din("xs", [NS, D])
    ck = B.din("ck", [NS, P, 256])
    cv = B.din("cv", [NS, P, 256])
    scv = B.din("sc", [NS, 2, 1024])
    w_in = B.din("w_in", [D, 8704])
    sinks = B.din("sinks", [1, 16])
    conv_w = B.din("conv_w", [3, 1024])
    w_ao = B.din("w_ao", [1024, D])
    w_co = B.din("w_co", [1024, D])
    w_o = B.din("w_o", [D, D])
    ln1_g = B.din("ln1_g", [1, D])
    ln1_b = B.din("ln1_b", [1, D])
    w_r = B.din("w_r", [D, NE])
    r_bias = B.din("r_bias", [1, NE])
    if stop_after is None:
        w_eg = B.din("w_eg", [NE, D, 512])
        w_eu = B.din("w_eu", [NE, D, 512])
        w_ed = B.din("w_ed", [NE, 512, D])
    w_sg = B.din("w_sg", [D, 512])
    w_su = B.din("w_su", [D, 512])
    w_sd = B.din("w_sd", [512, D])
    ln2_g = B.din("ln2_g", [1, D])
    ln2_b = B.din("ln2_b", [1, D])
    cosd = B.din("cosT", [P, 2192])
    sind = B.din("sinT", [P, 2192])
    cst = B.din("cst", [P, 1024])
    mskd = B.din("msk", [P, 512])
    c16 = B.din("c16", [P, 64 + 64 + 64 + 8 + 32])

    yp = B.dout("yp", [TOK, D])
    ys = B.dout("ys", [NS, D])
    pck = B.dout("pck", [P, 256])
    pcv = B.dout("pcv", [P, 256])
    psc = B.dout("psc", [2, 1024])
    sck = B.dout("sck", [NS, P, 256])
    scvo = B.dout("scvo", [NS, P, 256])
    ssc = B.dout("ssc", [NS, 2, 1024])
    if debug:
        dbg = {k: B.dout("dbg_" + k, shp) for k, shp in [("h", [TOK + NS, D]), ("cnt", [P, NE]), ("attn", [P, 8, 640]), ("conv", [P, 8, 640]), ("d8", [P, 17, 8]), ("w8", [P, 17, 8])]}

    XG = B.dscr("XG", [NSLOT + P, D], BF16)
    Y = B.dscr("Y", [NSLOT, D], BF16)
    BASE = B.dscr("BASE", [TOK + P, D], F32)

    es = contextlib.ExitStack()
    with es:
        def sb(name, shape, dt):
            return es.enter_context(nc.sbuf_tensor(name, list(shape), dt))

        ps = es.enter_context(nc.psum_tensor("ps", [P, 8, 512], F32))
        psb = ps.bitcast(BF16)

        ident_f = sb("ident_f", [P, P], F32)
        ident = sb("ident", [P, P], BF16)
        rotT = sb("rotT", [P, P], BF16)
        triU = sb("triU", [P, P], BF16)
        ones = sb("ones", [P, P], BF16)
        msk = sb("mskt", [P, 512], F32)
        c16t = sb("c16t", [P, 232], F32)
        sink8 = sb("sink8", [P, 16], F32)
        sinkraw = sb("sinkraw", [P, 16], F32)
        rbias = sb("rbias", [P, NE], F32)
        convw = sb("convw", [P, 8, 3], F32)
        lng = sb("lng", [P, D], F32)
        lnb = sb("lnb", [P, D], F32)
        d8all = sb("d8all", [P, 17, 8], I32)
        w8all = sb("w8all", [P, 17, 8], F32)
        cbase = sb("cbase", [P, NE], F32)
        epsT = sb("epsT", [P, 1], F32)
        sinkcol = sb("sinkcol", [P, 1], F32)
        rsel_t = sb("rsel", [P, 5, NE], BF16)

        def load_consts():
            B.dma("sp", "cst", ident_f[:], cst[:, 0:128], [], ["ident_f"])
            B.dma("pool", "cstp", ident[:], cst[:, 0:128], [], ["ident"])
            B.dma("pool", "cstp", rotT[:], cst[:, 128:256], [], ["rotT"])
            B.dma("pool", "cstp", triU[:], cst[:, 256:384], [], ["triU"])
            B.dma("pool", "cstp", ones[:], cst[:, 384:512], [], ["ones"])
            B.dma("sp", "cst", msk[:], mskd[:, :], [], ["msk"])
            B.dma("sp", "cst", c16t[:], c16[:, :], [], ["c16t"])
            B.dma("sp", "cst", sinkraw[:], fap_dram_bcast(sinks, 16), [], ["sinkraw"])
            B.dma("sp", "cst", rbias[:], fap_dram_bcast(r_bias, NE), [], ["rbias"])
            for j in range(3):
                B.dma("sp", "cst", convw[:, :, j], conv_w[j, :].rearrange("(c p) -> p c", p=P), [], ["convw%d" % j], allow_slow_non_contiguous=True)
            B.ts("dve", fap(sink8, 0, [[4, 4], [2, 2], [1, 2]]), fap(sinkraw, 0, [[4, 4], [1, 2], [2, 2]]), 8.0, ALU.mult, ["sinkraw"], ["sink8"])
            S.op("dve", lambda e: e.memset(cbase[:], 0.0), [], ["cbase"])
            S.op("dve", lambda e: e.memset(w8all[:], 0.0), [], [("w8", t) for t in range(17)])
            S.op("dve", lambda e: e.memset(epsT[:], LN_EPS), [], ["epsT"])
            B.dma("sp", "cst", sinkcol[0:16, :], sinks.rearrange("a h -> h a"), [], ["sinkcol"], allow_slow_non_contiguous=True)
            B.ts("dve", sinkcol[0:16, :], sinkcol[0:16, :], 8.0, ALU.mult, ["sinkcol"], ["sinkcol"])

        def fap_dram_bcast(src, n):
            return bass.AP(tensor=src.tensor, offset=src.offset, ap=[[0, P], [1, n]])

        load_consts()

        pa = contextlib.ExitStack()
        with pa:
            def sba(name, shape, dt):
                return pa.enter_context(nc.sbuf_tensor(name, list(shape), dt))

            NCMAX = 640
            bigT = sba("bigT", [P, 16, NCMAX], BF16)
            attnT = sba("attnT", [P, 8, NCMAX], BF16)
            convT = sba("convT", [P, 8, NCMAX], BF16)
            kTl = sba("kTl", [P, 4, P + NCMAX], BF16)
            kT32 = sba("kT32", [P, 2, 144], F32)
            vl = sba("vl", [P, 6, 256], BF16)
            v32 = sba("v32", [P, 2, 256], F32)
            cosl = sba("cosl", [P, NCMAX], F32)
            sinl = sba("sinl", [P, NCMAX], F32)
            uprev = sba("uprev", [P, 8, 2], F32)
            hres = sba("hres", [P, 5, D], F32)
            B.NB = 6
            B.WSLOT = 4096
            B.wring = sba("wring", [P, B.NB * B.WSLOT], BF16)
            B.ARENA_BYTES = 38 * 1024
            B.arena = sba("arena", [P, B.ARENA_BYTES // 4], F32)
            B.arena_bf = B.arena.bitcast(BF16)

            hres_bf = hres.bitcast(BF16)
            def zero_fill_xg():
                zt = hres_bf[:, 4, 0:D]
                S.op("dve", lambda e: e.memset(zt, 0.0), [], [("hres", 4)])
                nrow_total = NSLOT + P
                r = 0
                while r < nrow_total:
                    n = min(4 * P, nrow_total - r)
                    B.dma("act", "zf", XG[r:r + n, :].rearrange("(a p) d -> p a d", p=P), fap(hres_bf, 4 * 4096, [[0, n // P], [1, D]]), [("hres", 4)], ["XG"])
                    r += n

            B.dma("sp", "cst", lng[:], fap_dram_bcast(ln1_g, D), [], ["lng"])
            B.dma("sp", "cst", lnb[:], fap_dram_bcast(ln1_b, D), [], ["lnb"])

            if debug:
                S.op("dve", lambda e: e.memset(attnT[:], 0.0), [], ["attnT"])
                S.op("dve", lambda e: e.memset(convT[:], 0.0), [], ["convT"])

            def phase_a():
                B.wpos = 0
                B.wissued = 0
                for g in range(NG):
                    group(g)

            def make_tiles(g):
                halo = (g == 0)
                samp = (g == NG - 1)
                moff = P if halo else 0
                soff = moff + GT * P
                tl = []
                if halo:
                    tl.append((0, P, xp[0:P, :], "halo", None))
                for t in range(GT):
                    tt_ = g * GT + t
                    tl.append((moff + t * P, P, xp[P + tt_ * P:P + (tt_ + 1) * P, :], "main", tt_))
                if samp:
                    tl.append((soff, NS, xs[:, :], "samp", None))
                return tl

            def xpf_loc(i, k):
                if i < 2:
                    return attnT, i * 2048 + k * P, "attnT"
                if i < 4:
                    return convT, (i - 2) * 2048 + k * P, "convT"
                if k < 8:
                    return attnT, 4096 + k * P, "attnT"
                return convT, 4096 + (k - 8) * P, "convT"

            def prefetch_x(tl):
                for i, (c0, nr, src_, kind, tt_) in enumerate(tl):
                    if i < 4:
                        base, off, key = xpf_loc(i, 0)
                        B.dma("pool", "xpf%d" % i, fap(base, off, [[1, D]], parts=nr), src_, [], [key])
                    else:
                        B.dma("pool", "xpf4", fap(attnT, 4096, [[1, 1024]], parts=nr), src_[:, 0:1024], [], ["attnT"])
                        B.dma("pool", "xpf5", fap(convT, 4096, [[1, 1024]], parts=nr), src_[:, 1024:2048], [], ["convT"])

            def group(g):
                halo = (g == 0)
                samp = (g == NG - 1)
                moff = P if halo else 0
                NC = moff + GT * P + (NS if samp else 0)
                soff = moff + GT * P
                absb = (P + GT * P * g) - moff
                own_segs = [(moff, GT * P)] + ([(soff, NS)] if samp else [])
                all_segs = ([(0, P)] if halo else []) + own_segs
                tiles = make_tiles(g)
                own_tiles = [tl for tl in tiles if tl[3] != "halo"]
                if g == 0:
                    prefetch_x(tiles)

                B.areset()
                B.dma("sp", "tabc", cosl[:, 0:NC], cosd[:, absb:absb + NC], [], ["cosl"])
                B.dma("sp", "tabs", sinl[:, 0:NC], sind[:, absb:absb + NC], [], ["sinl"])
                for i, (c0, nr, src, kind, tt_) in enumerate(tiles):
                    b2 = B.bank(2)
                    for k in range(16):
                        base, off, key = xpf_loc(i, k)
                        o = fap(psb, (b2 + k // 8) * 1024 + (k % 8) * nr, [[1, nr]])
                        B.tr(o, fap(base, off, [[1, P]], parts=nr), ident[0:nr, 0:nr], [key, "ident"], B.bk(b2 + k // 8))
                    for hh in range(2):
                        src_ap = fap(psb, (b2 + hh) * 1024, [[nr, 8], [1, nr]])
                        B.cp("dve" if hh == 0 else "act", bigT[:, hh * 8:(hh + 1) * 8, c0:c0 + nr], src_ap, B.bk(b2 + hh), ["bigT"])

                if B.stop_after == "S0":
                    return
                def proj(wt, wk, nk, col_lo, segs, rhs_t, rhs_key, consumer):
                    for (c0, n) in segs:
                        b = B.bank()
                        for k in range(nk):
                            B.mm(ps[:, b, 0:n], wt[:, k, col_lo:col_lo + P], rhs_t[:, k, c0:c0 + n], k == 0, k == nk - 1,
                                 wk + [rhs_key], B.bk(b))
                        consumer(b, c0, n)

                def win_spec(col0, ncols=256):
                    return (16, ncols, [(0, ncols, w_in[:, col0:col0 + ncols])])

                B.areset()
                qT, qTk, _ = B.abf(8 * NCMAX, [[NCMAX, 8], [1, NCMAX]])
                qmark = B.apos
                qb = [B.abf(512) for _ in range(2)]
                t1 = [B.af32(512) for _ in range(2)]
                t2 = [B.af32(512) for _ in range(2)]
                rr = [0]

                def rope(b, c0, n, out_bf, out_keys, out_f32=None, out_f32_keys=None):
                    i = rr[0] % 2
                    rr[0] += 1
                    q_b, qbk, _ = qb[i]
                    a1, a1k, _ = t1[i]
                    a2, a2k, _ = t2[i]
                    B.cp("act", q_b[:, 0:n], ps[:, b, 0:n], B.bk(b), qbk)
                    B.tt("dve", a1[:, 0:n], ps[:, b, 0:n], cosl[:, c0:c0 + n], ALU.mult, B.bk(b) + ["cosl"], a1k)
                    b2 = B.bank()
                    B.mm(ps[:, b2, 0:n], rotT[:], q_b[:, 0:n], True, True, qbk + ["rotT"], B.bk(b2))
                    B.tt("dve", a2[:, 0:n], ps[:, b2, 0:n], sinl[:, c0:c0 + n], ALU.mult, B.bk(b2) + ["sinl"], a2k)
                    B.tt("dve", out_bf, a1[:, 0:n], a2[:, 0:n], ALU.add, a1k + a2k, out_keys)
                    if out_f32 is not None:
                        B.tt("dve", out_f32, a1[:, 0:n], a2[:, 0:n], ALU.add, a1k + a2k, out_f32_keys)

                for qs in range(4):
                    wt, wk = B.wget(win_spec(OQ + qs * 256))
                    for cc in range(2):
                        c = qs * 2 + cc
                        proj(wt, wk, 16, cc * P, own_segs, bigT, "bigT",
                             lambda b, c0, n, c=c: rope(b, c0, n, qT[:, c, c0:c0 + n], qTk))
                if B.stop_after == "S1q":
                    return
                if not halo:
                    ksrc = (P if g == 1 else 0) + GT * P
                    B.cp("act", kTl[:, :, 0:P], kTl[:, :, ksrc:ksrc + P], ["kTl"], ["kTl"])
                    B.cp("act", vl[:, 0, :], vl[:, (GT + 1) if g == 1 else GT, :], ["vl"], ["vl"])
                wt, wk = B.wget(win_spec(OK_))
                krt = [B.af32(512) for _ in range(2)]
                kri = [0]
                ksegs = list(all_segs)
                if samp:
                    ksegs = [(moff, (GT - 1) * P), (moff + (GT - 1) * P, P), (soff, NS)]
                for kp in range(2):
                    def kcons(b, c0, n, kp=kp):
                        kr, krk, _ = krt[kri[0] % 2]
                        kri[0] += 1
                        rope(b, c0, n, kr[:, 0:n], krk)
                        for j in range(2):
                            kv = 2 * kp + j
                            for dup in range(2):
                                B.cp("act" if dup == 0 else "dve", kTl[64 * dup:64 * dup + 64, kv, P + c0:P + c0 + n],
                                     kr[64 * j:64 * j + 64, 0:n], krk, ["kTl"])
                        if samp and c0 == soff:
                            B.cp("act", kT32[:, kp, P:P + NS], kr[:, 0:n], krk, ["kT32"])
                        elif samp and c0 == moff + (GT - 1) * P:
                            B.cp("dve", kT32[:, kp, 0:P], kr[:, 0:n], krk, ["kT32"])
                    proj(wt, wk, 16, kp * P, ksegs, bigT, "bigT", kcons)
                if B.stop_after == "S1k":
                    return
                wt, wk = B.wget(win_spec(OV))
                for li, (c0, nr, src, kind, tt_) in enumerate(tiles):
                    b = B.bank()
                    for k in range(16):
                        B.mm(ps[0:nr, b, 0:256], bigT[:, k, c0:c0 + nr], wt[:, k, :], k == 0, k == 15, wk + ["bigT"], B.bk(b))
                    B.cp("act", vl[0:nr, 1 + li, :], ps[0:nr, b, 0:256], B.bk(b), ["vl"])
                    if samp and kind == "samp":
                        B.cp("dve", v32[0:nr, 1, :], ps[0:nr, b, 0:256], B.bk(b), ["v32"])
                    if samp and kind == "main" and tt_ == NT - 1:
                        B.cp("dve", v32[:, 0, :], ps[:, b, 0:256], B.bk(b), ["v32"])

                if B.stop_after == "S1a":
                    return
                B.apos = qmark
                Sm = [B.af32(1024, [[256, 4], [1, 256]]) for _ in range(2)]
                Pn = [B.abf(1024, [[256, 4], [1, 256]]) for _ in range(2)]
                PTt = [B.abf(1024, [[128, 8], [1, 128]]) for _ in range(2)]
                sm_ = [B.af32(32) for _ in range(2)]
                items = []
                for li, (c0, nr, src_, kind, tt_) in enumerate(tiles):
                    if kind == "main":
                        for kv in range(4):
                            items.append((li, c0, tt_, kv))
                bpv = 6
                sc_bank = {}

                def a_scores(idx):
                    li, c0, tt_, kv = items[idx]
                    b2 = B.bank(2)
                    sc_bank[idx] = b2
                    for hh in range(4):
                        c = 2 * kv + hh // 2
                        half = hh % 2
                        B.mm(ps[:, b2 + half, (hh // 2) * 256:(hh // 2) * 256 + 256],
                             qT[64 * half:64 * half + 64, c, c0:c0 + P],
                             kTl[64 * half:64 * half + 64, kv, c0:c0 + 256], True, True,
                             qTk + ["kTl"], B.bk(b2 + half))

                def a_ctx(idx):
                    li, c0, tt_, kv = items[idx]
                    i = idx % 2
                    Sx, Sk, _ = Sm[i]
                    Px, Pk, _ = Pn[i]
                    PT, PTk, _ = PTt[i]
                    sx, sk_, _ = sm_[i]
                    return li, c0, tt_, kv, Sx, Sk, Px, Pk, PT, PTk, sx, sk_

                def a_A(idx):
                    li, c0, tt_, kv, Sx, Sk, Px, Pk, PT, PTk, sx, sk_ = a_ctx(idx)
                    b2 = sc_bank[idx]
                    mk_off = 256 if tt_ == 0 else 0
                    pin = fap(ps, b2 * 512, [[256, 4], [1, 256]])
                    B.tt("dve", Sx, pin, fap(msk, mk_off, [[0, 4], [1, 256]]), ALU.add, B.bk(b2, 2) + ["msk"], Sk)
                    mx = sx[:, 0:4]
                    m8 = sx[:, 4:8]
                    sm = sx[:, 8:12]
                    tq = sx[:, 12:16]
                    es_ = sx[:, 16:20]
                    rv = sx[:, 20:24]
                    nm = sx[:, 24:28]
                    B.red(mx, Sx, ALU.max, Sk, sk_)
                    B.tt("dve", m8, mx, sink8[:, 4 * kv:4 * kv + 4], ALU.max, sk_ + ["sink8"], sk_)
                    B.ts("dve", nm, m8, -0.125, ALU.mult, sk_, sk_)
                    B.tt("dve", tq, sink8[:, 4 * kv:4 * kv + 4], m8, ALU.subtract, sk_ + ["sink8"], sk_)

                def a_E(idx):
                    li, c0, tt_, kv, Sx, Sk, Px, Pk, PT, PTk, sx, sk_ = a_ctx(idx)
                    for s in range(4):
                        B.act(Sx[:, s, :], Sx[:, s, :], AF.Exp, Sk + sk_, Sk + sk_, scale=0.125, bias=sx[:, 24 + s:25 + s], accum_out=sx[:, 8 + s:9 + s])

                def a_B(idx):
                    li, c0, tt_, kv, Sx, Sk, Px, Pk, PT, PTk, sx, sk_ = a_ctx(idx)
                    sm = sx[:, 8:12]
                    tq = sx[:, 12:16]
                    es_ = sx[:, 16:20]
                    rv = sx[:, 20:24]
                    B.act(es_, tq, AF.Exp, sk_, sk_, scale=0.125)
                    B.tt("dve", rv, sm, es_, ALU.add, sk_, sk_)
                    S.op("dve", lambda e, rv=rv: e.reciprocal(out=rv, in_=rv), sk_, sk_)
                    for s in range(4):
                        B.act(Px[:, s, :], Sx[:, s, :], AF.Copy, Sk + sk_, Pk, scale=sx[:, 20 + s:21 + s])

                def a_C(idx):
                    li, c0, tt_, kv, Sx, Sk, Px, Pk, PT, PTk, sx, sk_ = a_ctx(idx)
                    bt = B.bank()
                    for hh in range(4):
                        for kb in range(2):
                            B.tr(psb[:, bt, (hh * 2 + kb) * P:(hh * 2 + kb + 1) * P], Px[:, (hh % 2) * 2 + hh // 2, kb * P:(kb + 1) * P], ident[:],
                                 Pk + ["ident"], B.bk(bt))
                    B.cp("act", PT, fap(psb, bt * 1024, [[128, 8], [1, 128]]), B.bk(bt), PTk)
                    for hh in range(4):
                        c = 2 * kv + hh // 2
                        half = hh % 2
                        o = ps[64 * half:64 * half + 64, bpv + c // 4, (c % 4) * P:(c % 4 + 1) * P]
                        for kb in range(2):
                            B.mm(o, vl[:, li + kb, kv * 64:(kv + 1) * 64], PT[:, hh * 2 + kb, :], kb == 0, kb == 1,
                                 ["vl"] + PTk, B.bk(bpv + c // 4))
                    if kv == 3:
                        for hh in range(2):
                            B.cp("act" if hh == 0 else "dve", attnT[:, hh * 4:(hh + 1) * 4, c0:c0 + P],
                                 fap(ps, (bpv + hh) * 512, [[128, 4], [1, 128]]), B.bk(bpv + hh), ["attnT"])

                if items:
                    n_it = len(items)
                    a_scores(0)
                    if n_it > 1:
                        a_scores(1)
                    a_A(0)
                    for idx in range(n_it):
                        a_E(idx)
                        if idx + 2 < n_it:
                            a_scores(idx + 2)
                        if idx + 1 < n_it:
                            a_A(idx + 1)
                        a_B(idx)
                        a_C(idx)

                if B.stop_after == "S1b":
                    return
                if samp:
                    B.apos = qmark
                    sample_attention(qT, qTk, soff)

                if B.stop_after == "S1":
                    return

                if g == 0:
                    zero_fill_xg()
                B.areset()
                NU = 2 + NCMAX
                hs = [B.af32(512) for _ in range(2)]
                bs = [B.af32(512) for _ in range(2)]
                ub = [B.af32(NU) for _ in range(2)]
                tb = [B.af32(512) for _ in range(2)]
                if samp:
                    stT, stTk, _ = B.af32(8 * 32, [[32, 8], [1, 32]])
                    st_in, st_ink, _ = B.af32(1024)
                    us_all, us_allk, _ = B.af32(8 * NS, [[NS, 8], [1, NS]])
                    ul_all, ul_allk, _ = B.af32(8 * 2, [[2, 8], [1, 2]])
                    B.dma("sp", "st", fap(st_in, 0, [[1, 1024]], parts=32), scv.rearrange("b j c -> (b j) c"), [], st_ink)
                    bq = B.bank()
                    for cc in range(8):
                        B.tr(ps[:, bq, cc * 32:(cc + 1) * 32], fap(st_in, cc * P, [[1, P]], parts=32), ident_f[0:32, 0:32],
                             st_ink + ["ident_f"], B.bk(bq))
                    B.cp("dve", stT, fap(ps, bq * 512, [[32, 8], [1, 32]]), B.bk(bq), stTk)
                for cc in range(8):
                    if cc % 2 == 0:
                        wb_, wbk_ = B.wget(win_spec(OB + (cc // 2) * 256))
                        wc_, wck_ = B.wget(win_spec(OC + (cc // 2) * 256))
                        wh_, whk_ = B.wget(win_spec(OH + (cc // 2) * 256))
                    cj = (cc % 2) * P
                    i = cc % 2
                    u, uk, _ = ub[i]
                    if not halo:
                        B.cp("act", u[:, 0:2], uprev[:, cc, :], ["uprev"], uk)
                    for (c0, n) in all_segs:
                        hx, hk, _ = hs[i]
                        bx, bxk, _ = bs[i]
                        tx, txk, _ = tb[i]
                        is_own = (c0, n) in own_segs
                        bh = B.bank()
                        for k in range(16):
                            B.mm(ps[:, bh, 0:n], wh_[:, k, cj:cj + P], bigT[:, k, c0:c0 + n], k == 0, k == 15, whk_ + ["bigT"], B.bk(bh))
                        B.cp("act", hx[:, 0:n], ps[:, bh, 0:n], B.bk(bh), hk)
                        bc = B.bank()
                        for k in range(16):
                            B.mm(ps[:, bc, 0:n], wc_[:, k, cj:cj + P], bigT[:, k, c0:c0 + n], k == 0, k == 15, wck_ + ["bigT"], B.bk(bc))
                        B.tt("dve", u[:, 2 + c0:2 + c0 + n], ps[:, bc, 0:n], hx[:, 0:n], ALU.mult, B.bk(bc) + hk, uk)
                        if not is_own:
                            continue
                        bb = B.bank()
                        for k in range(16):
                            B.mm(ps[:, bb, 0:n], wb_[:, k, cj:cj + P], bigT[:, k, c0:c0 + n], k == 0, k == 15, wbk_ + ["bigT"], B.bk(bb))
                        B.cp("act", bx[:, 0:n], ps[:, bb, 0:n], B.bk(bb), bxk)
                        if c0 == soff and samp:
                            s0 = fap(stT, cc * 32, [[2, NS]])
                            s1 = fap(stT, cc * 32 + 1, [[2, NS]])
                            B.ts("dve", tx[:, 0:n], u[:, 2 + c0:2 + c0 + n], convw[:, cc, 2:3], ALU.mult, uk + ["convw0", "convw1", "convw2"], txk)
                            B.stt(tx[:, 0:n], s1, convw[:, cc, 1:2], tx[:, 0:n], ALU.mult, ALU.add, stTk + ["convw0", "convw1", "convw2"] + txk, txk)
                            B.stt(tx[:, 0:n], s0, convw[:, cc, 0:1], tx[:, 0:n], ALU.mult, ALU.add, stTk + ["convw0", "convw1", "convw2"] + txk, txk)
                            B.cp("act", us_all[:, cc, :], u[:, 2 + c0:2 + c0 + n], uk, us_allk)
                        else:
                            B.ts("dve", tx[:, 0:n], u[:, 2 + c0:2 + c0 + n], convw[:, cc, 2:3], ALU.mult, uk + ["convw0", "convw1", "convw2"], txk)
                            B.stt(tx[:, 0:n], u[:, 1 + c0:1 + c0 + n], convw[:, cc, 1:2], tx[:, 0:n], ALU.mult, ALU.add, uk + ["convw0", "convw1", "convw2"] + txk, txk)
                            B.stt(tx[:, 0:n], u[:, c0:c0 + n], convw[:, cc, 0:1], tx[:, 0:n], ALU.mult, ALU.add, uk + ["convw0", "convw1", "convw2"] + txk, txk)
                            B.cp("act", uprev[:, cc, :], u[:, 2 + c0 + n - 2:2 + c0 + n], uk, ["uprev"])
                            if samp:
                                B.cp("act", ul_all[:, cc, :], u[:, 2 + c0 + n - 2:2 + c0 + n], uk, ul_allk)
                        B.tt("dve", convT[:, cc, c0:c0 + n], tx[:, 0:n], bx[:, 0:n], ALU.mult, txk + bxk, ["convT"])
                if samp:
                    def fm_to_tm(src3, srck, n):
                        b2 = B.bank(2)
                        for cc in range(8):
                            B.tr(ps[0:n, b2 + cc // 4, (cc % 4) * P:(cc % 4 + 1) * P], src3[:, cc, :], ident_f[:], srck + ["ident_f"], B.bk(b2 + cc // 4))
                        o, ok_, _ = B.af32(1024)
                        B.cp("dve", fap(o, 0, [[1, 1024]], parts=n), fap(ps, b2 * 512, [[1, 1024]], parts=n), B.bk(b2, 2), ok_)
                        return o, ok_
                    uo, uok = fm_to_tm(us_all, us_allk, NS)
                    B.dma("sp", "o_ssc", ssc[:, 1, :], fap(uo, 0, [[1, 1024]], parts=NS), uok, ["ssc"])
                    B.dma("sp", "o_ssc0", ssc[:, 0, :], scv[:, 1, :], [], ["ssc0"])
                    ul, ulk = fm_to_tm(ul_all, ul_allk, 2)
                    B.dma("sp", "o_psc", psc[:, :], fap(ul, 0, [[1, 1024]], parts=2), ulk, ["psc"])
                    kc, kck, _ = B.af32(256)
                    bq5 = B.bank()
                    for kp in range(2):
                        B.tr(ps[:, bq5, kp * P:(kp + 1) * P], kT32[:, kp, 0:P], ident_f[:], ["kT32", "ident_f"], B.bk(bq5))
                    B.cp("dve", kc, ps[:, bq5, 0:256], B.bk(bq5), kck)
                    B.dma("sp", "o_pck", pck[:, :], kc, kck, ["pck"])
                    B.dma("sp", "o_pcv", pcv[:, :], v32[:, 0, :], ["v32"], ["pcv"])

                if debug and g == 0:
                    B.dma("pool", "dbgp0_" + str(DBGC.next()), dbg["attn"][:, :, :], attnT[:], ["attnT"], [])
                    B.dma("pool", "dbgp1_" + str(DBGC.next()), dbg["conv"][:, :, :], convT[:], ["convT"], [])
                if B.stop_after == "S2":
                    return

                B.areset()
                for li, (c0, nr, src, kind, tt_) in enumerate(own_tiles):
                    B.dma("sp", "xres%d" % li, hres[0:nr, li, :], src, [], [("hres", li)])
                mixT, mixk, _ = B.abf(16 * NCMAX, [[NCMAX, 16], [1, NCMAX]])
                sg = [B.af32(512) for _ in range(2)]
                ta = [B.af32(512) for _ in range(2)]
                for jb in range(8):
                    wga, wgak = B.wget(win_spec(OGA + jb * 256))
                    wgc, wgck = B.wget(win_spec(OGC + jb * 256))
                    wao, waok = B.wget((8, 512, [(0, 256, w_ao[:, jb * 256:(jb + 1) * 256]), (256, 256, w_co[:, jb * 256:(jb + 1) * 256])]))
                    for jj in range(2):
                        j = jb * 2 + jj
                        for (c0, n) in own_segs:
                            i = j % 2
                            sgx, sgk, _ = sg[i]
                            tax, tak, _ = ta[i]
                            b = B.bank()
                            for k in range(16):
                                B.mm(ps[:, b, 0:n], wga[:, k, jj * P:(jj + 1) * P], bigT[:, k, c0:c0 + n], k == 0, k == 15, wgak + ["bigT"], B.bk(b))
                            B.act(sgx[:, 0:n], ps[:, b, 0:n], AF.Sigmoid, B.bk(b), sgk)
                            b = B.bank()
                            for k in range(8):
                                B.mm(ps[:, b, 0:n], wao[:, k, jj * P:(jj + 1) * P], attnT[:, k, c0:c0 + n], k == 0, k == 7, waok + ["attnT"], B.bk(b))
                            B.tt("dve", tax[:, 0:n], ps[:, b, 0:n], sgx[:, 0:n], ALU.mult, B.bk(b) + sgk, tak)
                            b = B.bank()
                            for k in range(16):
                                B.mm(ps[:, b, 0:n], wgc[:, k, jj * P:(jj + 1) * P], bigT[:, k, c0:c0 + n], k == 0, k == 15, wgck + ["bigT"], B.bk(b))
                            B.act(sgx[:, 0:n], ps[:, b, 0:n], AF.Sigmoid, B.bk(b), sgk)
                            b = B.bank()
                            for k in range(8):
                                B.mm(ps[:, b, 0:n], wao[:, k, 256 + jj * P:256 + (jj + 1) * P], convT[:, k, c0:c0 + n], k == 0, k == 7, waok + ["convT"], B.bk(b))
                            B.tt("dve", sgx[:, 0:n], ps[:, b, 0:n], sgx[:, 0:n], ALU.mult, B.bk(b) + sgk, sgk)
                            B.tt("dve", mixT[:, j, c0:c0 + n], tax[:, 0:n], sgx[:, 0:n], ALU.add, tak + sgk, mixk)

                for n4 in range(4):
                    wlo, wlok = B.wget((8, 512, [(0, 512, w_o[0:1024, n4 * 512:(n4 + 1) * 512])]))
                    whi, whik = B.wget((8, 512, [(0, 512, w_o[1024:2048, n4 * 512:(n4 + 1) * 512])]))
                    for li, (c0, nr, src, kind, tt_) in enumerate(own_tiles):
                        b = B.bank()
                        for k in range(16):
                            w_, wk_ = (wlo, wlok) if k < 8 else (whi, whik)
                            B.mm(ps[0:nr, b, :], mixT[:, k, c0:c0 + nr], w_[:, k % 8, :], k == 0, k == 15, mixk + wk_, B.bk(b))
                        hr = hres[0:nr, li, n4 * 512:(n4 + 1) * 512]
                        B.stt(hr, hr, ALPHA, ps[0:nr, b, :], ALU.mult, ALU.add, [("hres", li)] + B.bk(b), [("hres", li)])
                if B.stop_after == "S3":
                    return

                B.areset()
                if g + 1 < NG:
                    prefetch_x(make_tiles(g + 1))
                lnt = [B.af32(32) for _ in range(2)]
                hbf = [B.abf(D) for _ in range(2)]
                rts = [B.af32(64 * 8) for _ in range(len(own_tiles))]
                actT, actk, _ = B.abf(4 * NCMAX, [[NCMAX, 4], [1, NCMAX]])
                sgs = [B.af32(512) for _ in range(2)]
                wrt, wrk = B.wget((16, 64, [(0, 64, w_r[:, :])]))
                for li, (c0, nr, src, kind, tt_) in enumerate(own_tiles):
                    layer_norm(hres[0:nr, li, :], [("hres", li)], nr, lnt[li % 2])
                    if debug:
                        r0 = TOK if kind == "samp" else tt_ * P
                        B.dma("sp", "dbg0_" + str(DBGC.next()), dbg["h"][r0:r0 + nr, :], hres[0:nr, li, :], [("hres", li)], [])
                    hb, hbk, _ = hbf[li % 2]
                    B.cp("act", fap(hb, 0, [[1, D]], parts=nr), hres[0:nr, li, :], [("hres", li)], hbk)
                    b2 = B.bank(2)
                    for k in range(16):
                        o = fap(psb, (b2 + k // 8) * 1024 + (k % 8) * nr, [[1, nr]])
                        B.tr(o, fap(hb, k * P, [[1, P]], parts=nr), ident[0:nr, 0:nr], hbk + ["ident"], B.bk(b2 + k // 8))
                    for hh in range(2):
                        src_ap = fap(psb, (b2 + hh) * 1024, [[nr, 8], [1, nr]])
                        B.cp("dve" if hh == 0 else "act", bigT[:, hh * 8:(hh + 1) * 8, c0:c0 + nr], src_ap, B.bk(b2 + hh), ["bigT"])
                    tix = NT if kind == "samp" else tt_
                    routing(1, li, c0, nr, tix, wrt, wrk, rts[li], None, None)
                r2_done = 0
                for cb in range(2):
                    wg_, wgk_ = B.wget((16, 256, [(0, 256, w_sg[:, cb * 256:(cb + 1) * 256])]))
                    wu_, wuk_ = B.wget((16, 256, [(0, 256, w_su[:, cb * 256:(cb + 1) * 256])]))
                    for jj in range(2):
                        c = cb * 2 + jj
                        for _ in range(2):
                            if r2_done < len(own_tiles) and (r2_done <= c + 1 or c == 3):
                                li = r2_done
                                (c0_, nr_, src_, kind_, tt2) = own_tiles[li]
                                routing(2, li, c0_, nr_, NT if kind_ == "samp" else tt2, wrt, wrk, rts[li], None, None)
                                r2_done += 1
                        for (c0, n) in own_segs:
                            sx_, sxk, _ = sgs[c % 2]
                            b = B.bank()
                            for k in range(16):
                                B.mm(ps[:, b, 0:n], wg_[:, k, jj * P:(jj + 1) * P], bigT[:, k, c0:c0 + n], k == 0, k == 15, wgk_ + ["bigT"], B.bk(b))
                            B.act(sx_[:, 0:n], ps[:, b, 0:n], AF.Silu, B.bk(b), sxk)
                            b = B.bank()
                            for k in range(16):
                                B.mm(ps[:, b, 0:n], wu_[:, k, jj * P:(jj + 1) * P], bigT[:, k, c0:c0 + n], k == 0, k == 15, wuk_ + ["bigT"], B.bk(b))
                            B.tt("dve", actT[:, c, c0:c0 + n], ps[:, b, 0:n], sx_[:, 0:n], ALU.mult, B.bk(b) + sxk, actk)
                while r2_done < len(own_tiles):
                    li = r2_done
                    (c0_, nr_, src_, kind_, tt2) = own_tiles[li]
                    routing(2, li, c0_, nr_, NT if kind_ == "samp" else tt2, wrt, wrk, rts[li], None, None)
                    r2_done += 1
                for li, (c0, nr, src, kind, tt_) in enumerate(own_tiles):
                    hb, hbk, _ = hbf[li % 2]
                    if kind == "samp":
                        S.op("dve", lambda e, hb=hb: e.memset(hb, 0.0), [], hbk)
                    B.cp("act", fap(hb, 0, [[1, D]], parts=nr), hres[0:nr, li, :], [("hres", li)], hbk)
                    tix = NT if kind == "samp" else tt_
                    routing(3, li, c0, nr, tix, wrt, wrk, rts[li], hb, hbk)
                for nb in range(2):
                    wd_, wdk_ = B.wget((4, 1024, [(0, 1024, w_sd[:, nb * 1024:(nb + 1) * 1024])]))
                    for n2 in range(2):
                        n4 = nb * 2 + n2
                        for li, (c0, nr, src, kind, tt_) in enumerate(own_tiles):
                            b = B.bank()
                            for k in range(4):
                                B.mm(ps[0:nr, b, :], actT[:, k, c0:c0 + nr], wd_[:, k, n2 * 512:(n2 + 1) * 512], k == 0, k == 3, actk + wdk_, B.bk(b))
                            hr = hres[0:nr, li, n4 * 512:(n4 + 1) * 512]
                            B.stt(hr, hr, ALPHA, ps[0:nr, b, :], ALU.mult, ALU.add, [("hres", li)] + B.bk(b), [("hres", li)])
                for li, (c0, nr, src, kind, tt_) in enumerate(own_tiles):
                    r0 = TOK if kind == "samp" else tt_ * P
                    B.dma("sp", "base%d" % li, BASE[r0:r0 + nr, :], hres[0:nr, li, :], [("hres", li)], ["BASE"])

            def layer_norm(xap, xkeys, nr, tmp, out=None, out_keys=None):
                st, stk, _ = tmp
                for q in range(4):
                    S.op("dve", lambda e, q=q: e.bn_stats(out=fap(st, q * 6, [[1, 6]], parts=nr), in_=fap(xap, q * 512, [[1, 512]], parts=nr)), xkeys, stk)
                mv = fap(st, 24, [[1, 2]], parts=nr)
                S.op("dve", lambda e: e.bn_aggr(out=mv, in_=fap(st, 0, [[1, 24]], parts=nr)), stk, stk)
                rs = fap(st, 26, [[1, 1]], parts=nr)
                B.act(rs, fap(st, 25, [[1, 1]], parts=nr), AF.Sqrt, stk + ["epsT"], stk, bias=epsT[0:nr, :])
                S.op("dve", lambda e: e.reciprocal(out=rs, in_=rs), stk, stk)
                o = xap if out is None else out
                ok_ = xkeys if out is None else out_keys
                B.ts("dve", o, xap, fap(st, 24, [[1, 1]], parts=nr), ALU.subtract, xkeys + stk, ok_, s2=rs, op1=ALU.mult)
                B.tt("dve", o, o, lng[0:nr, :], ALU.mult, ok_ + ["lng"], ok_)
                B.tt("dve", o, o, lnb[0:nr, :], ALU.add, ok_ + ["lnb"], ok_)

            def routing(phase, li, c0, nr, tix, wrt, wrk, rt, hb, hbk):
                r, rk, _ = rt
                def rv(i, n=NE, parts=nr):
                    return fap(r, i * 64, [[1, n]], parts=parts)
                sc_ = rv(0)
                ch = rv(1)
                tmp = rv(2)
                sel = rv(3, parts=P)
                wd = rv(4)
                key = rv(5, parts=P)
                sm = fap(r, 6 * 64, [[1, 64]], parts=nr)
                m1 = fap(r, 6 * 64, [[1, 8]], parts=nr)
                m2 = fap(r, 6 * 64 + 8, [[1, 8]], parts=nr)
                gs = fap(r, 6 * 64 + 16, [[1, 8]], parts=nr)
                g8 = fap(r, 6 * 64 + 24, [[1, 8]], parts=nr)
                pen = fap(r, 6 * 64 + 32, [[1, 8]], parts=nr)
                c8 = fap(r, 6 * 64 + 40, [[1, 8]], parts=nr)
                ssum = fap(r, 6 * 64 + 48, [[1, 1]], parts=nr)
                ch3 = fap(r, 1 * 64, [[8, 8], [1, 8]], parts=nr)
                tmp3 = fap(r, 2 * 64, [[8, 8], [1, 8]], parts=nr)
                selb, selbk = rsel_t[:, li, :], [("rsel", li)]
                pos = rv(7, parts=P)
                d8f = fap(r, 6 * 64 + 56, [[1, 8]], parts=P)
                BIGC = float(2 * NSLOT)
                if phase == 1:
                    b = B.bank()
                    for k in range(16):
                        B.mm(ps[0:nr, b, 0:NE], bigT[:, k, c0:c0 + nr], wrt[:, k, :], k == 0, k == 15, ["bigT"] + wrk, B.bk(b))

                    B.act(sc_, ps[0:nr, b, 0:NE], AF.Sigmoid, B.bk(b), rk)
                    return
                if phase == 2:
                    B.tt("dve", ch, sc_, rbias[0:nr, :], ALU.add, rk + ["rbias"], rk)
                    B.red(m1, ch3, ALU.max, rk, rk)
                    B.tt("dve", tmp3, ch3, fap(r, 6 * 64, [[1, 8], [0, 8]], parts=nr), ALU.is_equal, rk, rk)
                    B.stt(tmp, tmp, -1e9, ch, ALU.mult, ALU.add, rk, rk)
                    B.red(m2, tmp3, ALU.max, rk, rk)
                    B.tt("dve", gs, m1, m2, ALU.add, rk, rk)
                    S.op("dve", lambda e: e.max(out=g8, in_=gs), rk, rk)
                    B.ts("dve", pen, gs, fap(r, 6 * 64 + 24 + 3, [[1, 1]], parts=nr), ALU.is_ge, rk, rk, s2=-1.0, op1=ALU.add)
                    B.ts("dve", pen, pen, 1e9, ALU.mult, rk, rk)
                    B.tt("dve", tmp3, ch3, fap(r, 6 * 64 + 32, [[1, 8], [0, 8]], parts=nr), ALU.add, rk, rk)
                    S.op("dve", lambda e: e.max(out=c8, in_=tmp), rk, rk)
                    if nr < P:
                        S.op("dve", lambda e: e.memset(sel, 0.0), rk, rk)
                    B.ts("dve", rv(3), tmp, fap(r, 6 * 64 + 40 + 7, [[1, 1]], parts=nr), ALU.is_ge, rk, rk)
                    B.stt(wd, rv(3), 1.0, sc_, ALU.mult, ALU.mult, rk, rk, accum_out=ssum)
                    S.op("dve", lambda e: e.reciprocal(out=ssum, in_=ssum), rk, rk)
                    B.ts("dve", wd, wd, ssum, ALU.mult, rk, rk, s2=2.5, op1=ALU.mult)
                    B.cp("dve", selb, sel, rk, selbk)
                    return
                bp = B.bank()
                B.mm(ps[:, bp, 0:NE], triU[:], selb, True, True, ["triU"] + selbk, B.bk(bp))
                B.tt("dve", pos, ps[:, bp, 0:NE], cbase[:], ALU.add, B.bk(bp) + ["cbase"], rk)
                bp2 = B.bank()
                B.mm(ps[:, bp2, 0:NE], ones[:], selb, True, True, ["ones"] + selbk, B.bk(bp2))
                B.tt("dve", cbase[:], cbase[:], ps[:, bp2, 0:NE], ALU.add, B.bk(bp2) + ["cbase"], ["cbase"])
                B.ts("dve", key, pos, float(CAP), ALU.is_lt, rk, rk)
                B.tt("dve", key, key, sel, ALU.mult, rk, rk)
                B.tt("dve", pos, pos, c16t[:, 64:128], ALU.add, rk + ["c16t"], rk)
                B.ts("dve", pos, pos, -1.0, ALU.mult, rk, rk, s2=BIGC, op1=ALU.add)
                B.tt("dve", key, key, pos, ALU.mult, rk, rk)
                S.op("dve", lambda e: e.max(out=d8f, in_=key), rk, rk)
                for j in range(8):
                    B.stt(rv(2, parts=nr), fap(r, 5 * 64, [[1, NE]], parts=nr), fap(r, 6 * 64 + 56 + j, [[1, 1]], parts=nr), wd,
                          ALU.is_equal, ALU.mult, rk, rk + [("w8", tix)], accum_out=w8all[0:nr, tix, j:j + 1])
                B.ts("dve", fap(r, 2 * 64, [[1, 8]], parts=nr), fap(r, 6 * 64 + 56, [[1, 8]], parts=nr), 0.0, ALU.is_gt, rk, rk)
                B.tt("dve", w8all[0:nr, tix, :], w8all[0:nr, tix, :], fap(r, 2 * 64, [[1, 8]], parts=nr), ALU.mult, rk + [("w8", tix)], [("w8", tix)])
                B.ts("dve", d8f, d8f, -1.0, ALU.mult, rk, rk, s2=BIGC, op1=ALU.add)
                B.cp("dve", d8all[:, tix, :], d8f, rk, [("d8", tix)])
                for j in range(8):
                    S.dma("pool", "scat%d" % (li % 2), lambda e, j=j: e.indirect_dma_start(
                        out=XG[:, :], out_offset=bass.IndirectOffsetOnAxis(ap=d8all[:, tix, j:j + 1], axis=0),
                        in_=hb, in_offset=None, bounds_check=B.bcreg(e), oob_is_err=False),
                        hbk + [("d8", tix), "XG"], [("XGs", tix, j)])


            def sample_attention(qT, qTk, soff):
                kn, knk, _ = B.af32(256)
                bq = B.bank()
                for kp in range(2):
                    B.tr(ps[0:NS, bq, kp * P:(kp + 1) * P], kT32[:, kp, P:P + NS], ident_f[:], ["kT32", "ident_f"], B.bk(bq))
                B.cp("dve", fap(kn, 0, [[1, 256]], parts=NS), ps[0:NS, bq, 0:256], B.bk(bq), knk)
                B.dma("sp", "sck", sck[:, 0:P - 1, :], ck[:, 1:P, :], [], ["sck"])
                B.dma("sp", "sck", sck[:, P - 1, :], fap(kn, 0, [[1, 256]], parts=NS), knk, ["sck"])
                B.dma("sp", "scv", scvo[:, 0:P - 1, :], cv[:, 1:P, :], [], ["scvo"])
                B.dma("sp", "scv", scvo[:, P - 1, :], v32[0:NS, 1, :], ["v32"], ["scvo"])
                Ke, Kek = fap(hres_bf, 0 * 4096, [[256, NS], [1, 256]]), [("hres", 0)]
                Ve, Vek = fap(hres_bf, 1 * 4096, [[256, NS], [1, 256]]), [("hres", 1)]
                stg = fap(hres, 2 * D, [[256, NS], [1, 256]])
                stgk = [("hres", 2), ("hres", 3)]
                B.dma("sp", "ske", stg, sck.rearrange("b k d -> k b d"), ["sck"], stgk)
                B.cp("act", Ke, stg, stgk, Kek)
                B.dma("sp", "ske", stg, scvo.rearrange("b k d -> k b d"), ["scvo"], stgk)
                B.cp("dve", Ve, stg, stgk, Vek)
                QM, QMk, _ = B.abf(64, [[16, 4], [1, 16]])
                KT2, KT2k, _ = B.abf(4 * P, [[P, 4], [1, P]])
                Ssb, Ssk = fap(hres, 2 * D, [[P, NS], [1, P]]), [("hres", 2)]
                for b_ in range(NS):
                    qin = fap(qT, soff + b_, [[0, 4], [640, 8], [0, 2]])
                    B.tt("dve", fap(QM, 0, [[16, 4], [2, 8], [1, 2]]), qin, fap(c16t, 0, [[16, 4], [2, 8], [1, 2]]), ALU.mult, qTk + ["c16t"], QMk)
                    bt = B.bank()
                    for kv in range(4):
                        for dup in range(2):
                            B.tr(psb[64 * dup:64 * dup + 64, bt, kv * P:(kv + 1) * P], Ke[:, b_, kv * 64:(kv + 1) * 64], ident[:], Kek + ["ident"], B.bk(bt))
                    B.cp("act", KT2, fap(psb, bt * 1024, [[P, 4], [1, P]]), B.bk(bt), KT2k)
                    bsc = B.bank()
                    for kv in range(4):
                        B.mm(ps[0:16, bsc, 0:P], QM[:, kv, :], KT2[:, kv, :], kv == 0, kv == 3, QMk + KT2k, B.bk(bsc))
                    B.cp("dve", fap(Ssb, b_ * P, [[1, P]], parts=16), ps[0:16, bsc, 0:P], B.bk(bsc), Ssk)
                sx, sxk, _ = B.af32(5 * NS)
                S16 = fap(Ssb, 0, [[P, NS], [1, P]], parts=16)
                mx = fap(sx, 0, [[1, NS]], parts=16)
                sm = fap(sx, NS, [[1, NS]], parts=16)
                tq = fap(sx, 2 * NS, [[1, NS]], parts=16)
                rv_ = fap(sx, 3 * NS, [[1, NS]], parts=16)
                B.red(mx, S16, ALU.max, Ssk, sxk)
                B.ts("dve", mx, mx, sinkcol[0:16, 0:1], ALU.max, sxk + ["sinkcol"], sxk)
                B.tt("dve", S16, S16, fap(sx, 0, [[1, NS], [0, P]], parts=16), ALU.subtract, Ssk + sxk, Ssk)
                B.act(S16, S16, AF.Exp, Ssk, Ssk, scale=0.125)
                B.red(sm, S16, ALU.add, Ssk, sxk)
                B.ts("dve", tq, mx, -1.0, ALU.mult, sxk, sxk, s2=sinkcol[0:16, 0:1], op1=ALU.add)
                B.act(tq, tq, AF.Exp, sxk, sxk, scale=0.125)
                B.tt("dve", rv_, sm, tq, ALU.add, sxk, sxk)
                S.op("dve", lambda e: e.reciprocal(out=rv_, in_=rv_), sxk, sxk)
                Pb, Pbk = fap(hres_bf, 4 * 4096, [[P, NS], [1, P]]), [("hres", 4)]
                B.tt("dve", fap(Pb, 0, [[P, NS], [1, P]], parts=16), S16, fap(sx, 3 * NS, [[1, NS], [0, P]], parts=16), ALU.mult, Ssk + sxk, Pbk)
                bt = B.bank()
                for b_ in range(NS):
                    B.tr(psb[:, bt, b_ * 16:(b_ + 1) * 16], fap(Pb, b_ * P, [[1, P]], parts=16), ident[0:16, 0:16], Pbk + ["ident"], B.bk(bt))
                PTs, PTsk, _ = B.abf(NS * 16)
                B.cp("act", PTs, psb[:, bt, 0:NS * 16], B.bk(bt), PTsk)
                As, Ask = fap(hres, 4 * D + 1024, [[64, NS], [1, 64]]), [("hres", 4)]
                tmpv, tmpvk, _ = B.af32(256)
                for b_ in range(NS):
                    bo = B.bank()
                    B.mm(ps[0:16, bo, 0:256], PTs[:, b_ * 16:(b_ + 1) * 16], Ve[:, b_, :], True, True, PTsk + Vek, B.bk(bo))
                    B.tt("dve", fap(tmpv, 0, [[64, 4], [1, 64]], parts=16), fap(ps, bo * 512, [[64, 4], [1, 64]], parts=16),
                         fap(c16t, 192, [[1, 4], [0, 64]], parts=16), ALU.mult, B.bk(bo) + ["c16t"], tmpvk)
                    B.red(fap(As, b_ * 64, [[1, 64]], parts=16), fap(tmpv, 0, [[1, 64], [64, 4]], parts=16), ALU.add, tmpvk, Ask)
                A2, A2k = fap(hres, 3 * D, [[P, NS], [1, P]]), [("hres", 3)]
                B.tt("dve", fap(A2, 0, [[P, NS], [64, 2], [1, 64]], parts=16), fap(As, 0, [[64, NS], [0, 2], [1, 64]], parts=16),
                     fap(c16t, 200, [[0, NS], [1, 2], [0, 64]], parts=16), ALU.mult, Ask + ["c16t"], A2k)
                bt2 = B.bank()
                for b_ in range(NS):
                    B.tr(ps[:, bt2, b_ * 16:(b_ + 1) * 16], fap(A2, b_ * P, [[1, P]], parts=16), ident_f[0:16, 0:16], A2k + ["ident_f"], B.bk(bt2))
                af_, afk, _ = B.af32(8 * NS, [[NS, 8], [1, NS]])
                B.red(af_, fap(ps, bt2 * 512, [[2, 8], [16, NS], [1, 2]]), ALU.add, B.bk(bt2), afk)
                B.cp("dve", fap(attnT, soff, [[640, 8], [1, NS]]), af_, afk, ["attnT"])


            S.planning = True
            phase_a()
            S.planning = False
            B.bank_rr = 0
            phase_a()
        S.barrier()

        if stop_after is None:
            pb = contextlib.ExitStack()
            with pb:
                def sbb(name, shape, dt):
                    return pb.enter_context(nc.sbuf_tensor(name, list(shape), dt))
                wg = [sbb("wg%d" % i, [P, 16, 512], BF16) for i in range(2)]
                wu = [sbb("wu%d" % i, [P, 16, 512], BF16) for i in range(2)]
                wdn = [sbb("wd%d" % i, [P, 4, D], BF16) for i in range(2)]
                xb_ = [sbb("xb%d" % i, [P, NBLK, D], BF16) for i in range(2)]
                xbT = [sbb("xbT%d" % i, [P, 16, CAP], BF16) for i in range(2)]
                acT = [sbb("acT%d" % i, [P, 4, CAP], BF16) for i in range(2)]
                sgt = [sbb("sgt%d" % i, [P, CAP], F32) for i in range(2)]
                yo = [sbb("yo%d" % i, [P, D], BF16) for i in range(2)]

                def load_expert(e):
                    i = e % 2
                    for k0 in range(0, 16, 8):
                        B.dma("pool", "eg%d" % i, wg[i][:, k0:k0 + 8, :], w_eg[e, k0 * P:(k0 + 8) * P, :].rearrange("(k p) n -> p k n", p=P), [], [("wg", i)])
                        B.dma("pool", "eu%d" % i, wu[i][:, k0:k0 + 8, :], w_eu[e, k0 * P:(k0 + 8) * P, :].rearrange("(k p) n -> p k n", p=P), [], [("wu", i)])
                    for k in range(0, 4, 2):
                        B.dma("pool", "ed%d" % i, wdn[i][:, k:k + 2, :], w_ed[e, k * P:(k + 2) * P, :].rearrange("(k p) n -> p k n", p=P), [], [("wdn", i)])
                    B.dma("sp", "xb%d" % i, xb_[i][:], XG[e * CAP:(e + 1) * CAP, :].rearrange("(b p) d -> p b d", p=P), ["XG"] + [("XGs", t, j) for t in range(17) for j in range(8)], [("xb", i)])

                load_expert(0)
                yi = 0
                for e in range(NE):
                    i = e % 2
                    if e + 1 < NE:
                        load_expert(e + 1)
                    for blk in range(NBLK):
                        for h2 in range(2):
                            bt = B.bank()
                            for k8 in range(8):
                                k = h2 * 8 + k8
                                B.tr(psb[:, bt, k8 * P:(k8 + 1) * P], xb_[i][:, blk, k * P:(k + 1) * P], ident[:], [("xb", i), "ident"], B.bk(bt))
                            B.cp("act" if (blk * 2 + h2) % 2 == 0 else "dve", xbT[i][:, h2 * 8:(h2 + 1) * 8, blk * P:(blk + 1) * P],
                                 fap(psb, bt * 1024, [[P, 8], [1, P]]), B.bk(bt), [("xbT", i)])
                    for c in range(4):
                        bg = B.bank()
                        for k in range(16):
                            B.mm(ps[:, bg, 0:CAP], wg[i][:, k, c * P:(c + 1) * P], xbT[i][:, k, :], k == 0, k == 15, [("wg", i), ("xbT", i)], B.bk(bg))
                        B.act(sgt[c % 2][:], ps[:, bg, 0:CAP], AF.Silu, B.bk(bg), [("sgt", c % 2)])
                        bu = B.bank()
                        for k in range(16):
                            B.mm(ps[:, bu, 0:CAP], wu[i][:, k, c * P:(c + 1) * P], xbT[i][:, k, :], k == 0, k == 15, [("wu", i), ("xbT", i)], B.bk(bu))
                        B.tt("dve", acT[i][:, c, :], ps[:, bu, 0:CAP], sgt[c % 2][:], ALU.mult, B.bk(bu) + [("sgt", c % 2)], [("acT", i)])
                    for blk in range(NBLK):
                        y_ = yo[yi % 2]
                        yk = [("yo", yi % 2)]
                        for n4 in range(4):
                            b = B.bank()
                            for k in range(4):
                                B.mm(ps[:, b, :], acT[i][:, k, blk * P:(blk + 1) * P], wdn[i][:, k, n4 * 512:(n4 + 1) * 512], k == 0, k == 3, [("acT", i), ("wdn", i)], B.bk(b))
                            B.cp("act" if n4 % 2 == 0 else "dve", y_[:, n4 * 512:(n4 + 1) * 512], ps[:, b, :], B.bk(b), yk)
                        r0 = e * CAP + blk * P
                        B.dma("sp", "yst%d" % (yi % 2), Y[r0:r0 + P, :], y_[:], yk, ["Y"])
                        yi += 1
            S.barrier()

            pc = contextlib.ExitStack()
            with pc:
                def sbc(name, shape, dt):
                    return pc.enter_context(nc.sbuf_tensor(name, list(shape), dt))
                acc = [sbc("acc%d" % i, [P, D], F32) for i in range(2)]
                yg = [sbc("yg%d" % i, [P, D], BF16) for i in range(6)]
                lnt2 = [sbc("lnt2%d" % i, [P, 32], F32) for i in range(2)]
                B.dma("sp", "cst2", lng[:], fap_dram_bcast(ln2_g, D), ["lng"], ["lng"])
                B.dma("sp", "cst2", lnb[:], fap_dram_bcast(ln2_b, D), ["lnb"], ["lnb"])
                dgt = [sbc("dg%d" % i, [P, 16, P], BF16) for i in range(2)]
                wsm = [sbc("wsm%d" % i, [P, 32], F32) for i in range(2)]
                wbf = [sbc("wbf%d" % i, [P, 8], BF16) for i in range(2)]
                gi = [0]

                def f_ctx(tix):
                    nr = P if tix < NT else NS
                    r0 = tix * P if tix < NT else TOK
                    par = tix % 2
                    bks = [4 * par + n4 for n4 in range(4)]
                    return nr, r0, acc[par], [("acc", par)], dgt[par], [("dg", par)], wsm[par], [("wsm", par)], wbf[par], bks

                def f_prep(tix):
                    nr, r0, a, ak, dg, dgk, ws, wsk, wb16, bks = f_ctx(tix)
                    B.dma("sp", "bl%d" % (tix % 2), a[0:nr, :], BASE[r0:r0 + nr, :], ["BASE"], ak)
                    B.cp("dve", wb16[0:nr, :], w8all[0:nr, tix, :], [("w8", tix)], wsk)
                    B.cp("dve", ws[0:nr, 0:8], wb16[0:nr, :], wsk, wsk)
                    B.tt("dve", ws[0:nr, 8:16], w8all[0:nr, tix, :], ws[0:nr, 0:8], ALU.subtract, wsk + [("w8", tix)], wsk)
                    for j in range(8):
                        B.act(dg[0:nr, 2 * j, 0:nr], ident_f[0:nr, 0:nr], AF.Copy, wsk + ["ident_f"], dgk, scale=ws[0:nr, j:j + 1])
                        B.act(dg[0:nr, 2 * j + 1, 0:nr], ident_f[0:nr, 0:nr], AF.Copy, wsk + ["ident_f"], dgk, scale=ws[0:nr, 8 + j:9 + j])
                    for j in range(8):
                        g_ = yg[gi[0] % 6]
                        gk = [("yg", gi[0] % 6)]
                        S.dma("pool", "yg%d" % (gi[0] % 6), lambda e, g_=g_, tix=tix, j=j: e.indirect_dma_start(
                            out=g_[:], out_offset=None, in_=Y[:, :],
                            in_offset=bass.IndirectOffsetOnAxis(ap=d8all[:, tix, j:j + 1], axis=0),
                            bounds_check=B.bcreg(e), oob_is_err=False), ["Y", ("d8", tix)], gk)
                        for part in range(2):
                            for n4 in range(4):
                                B.mm(ps[0:nr, bks[n4], :], dg[0:nr, 2 * j + part, 0:nr], g_[0:nr, n4 * 512:(n4 + 1) * 512],
                                     j == 0 and part == 0, j == 7 and part == 1, gk + dgk, B.bk(bks[n4]))
                        gi[0] += 1

                def f_finish(tix):
                    nr, r0, a, ak, dg, dgk, ws, wsk, wb16, bks = f_ctx(tix)
                    for n4 in range(4):
                        B.tt("dve", a[0:nr, n4 * 512:(n4 + 1) * 512], a[0:nr, n4 * 512:(n4 + 1) * 512], ps[0:nr, bks[n4], :], ALU.add,
                             ak + B.bk(bks[n4]), ak)
                    layer_norm(a[0:nr, :], ak, nr, (lnt2[tix % 2][:], [("lnt2", tix % 2)], None))
                    if tix < NT:
                        B.dma("sp", "yout%d" % (tix % 2), yp[r0:r0 + nr, :], a[0:nr, :], ak, ["yp"])
                    else:
                        B.dma("sp", "yout%d" % (tix % 2), ys[:, :], a[0:nr, :], ak, ["ys"])

                f_prep(0)
                for tix in range(17):
                    if tix + 1 < 17:
                        f_prep(tix + 1)
                    f_finish(tix)
                if debug:
                    B.dma("sp", "dbg1_" + str(DBGC.next()), dbg["cnt"][:, :], cbase[:], ["cbase"], [])
                    dd = sbc("ddbg", [P, 17, 8], F32)
                    B.cp("dve", dd[:], d8all[:], [("d8", t) for t in range(17)], ["ddbg"])
                    B.dma("sp", "dbg2_" + str(DBGC.next()), dbg["d8"][:, :, :], dd[:], ["ddbg"], [])
                    B.dma("sp", "dbg3_" + str(DBGC.next()), dbg["w8"][:, :, :], w8all[:], [("w8", t) for t in range(17)], [])

        S.finalize(es)
        with nc.Block() as block:
            S.run_block(block)
    return nc


def _consts(core):
    hh = core % 2
    half = 8
    inv_freq = np.power(np.float32(500000.0), -np.arange(half, dtype=np.float32) * np.float32(2.0 / 16)).astype(np.float32)
    pos = np.zeros(2192, np.float32)
    pos[0:P] = hh * TOK - P + np.arange(P)
    pos[P:P + TOK] = hh * TOK + np.arange(TOK)
    pos[P + TOK:] = PAST
    pos = np.maximum(pos, 0).astype(np.float32)
    ang = (pos[:, None] * inv_freq[None, :]).astype(np.float32)
    cosv = np.cos(ang).astype(np.float32)
    sinv = np.sin(ang).astype(np.float32)
    cosT = np.ones((P, 2192), np.float32)
    sinT = np.zeros((P, 2192), np.float32)
    for p in range(P):
        d = p % 64
        if d < 16:
            cosT[p] = cosv[:, d % 8]
            sinT[p] = sinv[:, d % 8]
    ident = np.eye(P, dtype=np.float32)
    R = np.zeros((P, P), np.float32)
    for m in range(P):
        d = m % 64
        if d < 8:
            R[m, m + 8] = -1.0
        elif d < 16:
            R[m, m - 8] = 1.0
    rotT = R.T.copy()
    triU = np.triu(np.ones((P, P), np.float32), 1)
    ones = np.ones((P, P), np.float32)
    cst = np.concatenate([ident, rotT, triU, ones, np.zeros((P, 512), np.float32)], axis=1)
    a = np.arange(P)[:, None]
    c = np.arange(2 * P)[None, :]
    valid = (c > a) & (c <= a + P)
    mask_gen = np.where(valid, 0.0, NEG).astype(np.float32)
    if hh == 0:
        mask_first = np.where(valid & (c >= P), 0.0, NEG).astype(np.float32)
    else:
        mask_first = mask_gen
    msk = np.concatenate([mask_gen, mask_first], axis=1)
    c16 = np.zeros((P, 232), np.float32)
    for p in range(P):
        for kv in range(4):
            for h in range(16):
                c16[p, kv * 16 + h] = 1.0 if ((p // 64) == (h % 2) and (h // 4) == kv) else 0.0
    c16[:, 64:128] = (np.arange(NE) * CAP)[None, :]
    c16[:, 128:192] = np.arange(NE)[None, :]
    for h in range(16):
        for kv in range(4):
            c16[h, 192 + kv] = 1.0 if h // 4 == kv else 0.0
        for par in range(2):
            c16[h, 200 + par] = 1.0 if h % 2 == par else 0.0
    return dict(cosT=cosT, sinT=sinT, cst=cst, msk=msk, c16=c16)


_NC_CACHE = {}


def kernel(x_prompt, x_sample, cache_k, cache_v, state_conv, w_in, attn_sinks, conv_w,
           w_attn_out, w_conv_out, w_o, ln1_g, ln1_b, w_router, router_bias,
           w_exp_gate, w_exp_up, w_exp_down, w_sh_gate, w_sh_up, w_sh_down, ln2_g, ln2_b,
           _stop_after=None, _debug=False, _cores=None):
    f = lambda a: np.ascontiguousarray(np.asarray(a, dtype=np.float32))
    x_prompt, x_sample = f(x_prompt), f(x_sample)
    key = (_stop_after, _debug)
    if key not in _NC_CACHE:
        _NC_CACHE[key] = build_program(_stop_after, _debug)
    nc = _NC_CACHE[key]
    shared = dict(
        w_in=f(w_in[0]), sinks=f(attn_sinks), conv_w=f(conv_w[0]), w_ao=f(w_attn_out[0]), w_co=f(w_conv_out[0]),
        w_o=f(w_o[0]), ln1_g=f(ln1_g), ln1_b=f(ln1_b), w_r=f(w_router[0]), r_bias=f(router_bias),
        w_eg=f(w_exp_gate[0]), w_eu=f(w_exp_up[0]), w_ed=f(w_exp_down[0]), w_sg=f(w_sh_gate[0]),
        w_su=f(w_sh_up[0]), w_sd=f(w_sh_down[0]), ln2_g=f(ln2_g), ln2_b=f(ln2_b))
    if _stop_after is not None:
        for k_ in ("w_eg", "w_eu", "w_ed"):
            shared.pop(k_)
    in_maps = []
    cores = list(range(NCORES)) if _cores is None else list(_cores)
    for c in cores:
        n, hh = c // 2, c % 2
        xp = np.zeros((TOK + P, D), np.float32)
        if hh == 1:
            xp[0:P] = x_prompt[n, TOK - P:TOK]
        xp[P:] = x_prompt[n, hh * TOK:(hh + 1) * TOK]
        m = dict(shared)
        m.update(_consts(c))
        m.update(xp=xp, xs=f(x_sample[c * NS:(c + 1) * NS, 0, :]),
                 ck=f(cache_k[0, c * NS:(c + 1) * NS].reshape(NS, P, 256)),
                 cv=f(cache_v[0, c * NS:(c + 1) * NS].reshape(NS, P, 256)),
                 sc=f(state_conv[0, c * NS:(c + 1) * NS]))
        in_maps.append(m)
    res = run_bass_kernel_spmd(nc, in_maps, core_ids=list(range(len(cores))))
    R = res.results
    if _debug or _stop_after is not None:
        return R
    y_p = np.zeros((4, SEQ, D), np.float32)
    y_s = np.zeros((128, 1, D), np.float32)
    pk = np.zeros((1, 4, P, 4, 64), np.float32)
    pv = np.zeros((1, 4, P, 4, 64), np.float32)
    pc_ = np.zeros((1, 4, 2, 1024), np.float32)
    sk = np.zeros((1, 128, P, 4, 64), np.float32)
    sv = np.zeros((1, 128, P, 4, 64), np.float32)
    ssc_ = np.zeros((1, 128, 2, 1024), np.float32)
    for c in range(NCORES):
        n, hh = c // 2, c % 2
        y_p[n, hh * TOK:(hh + 1) * TOK] = R[c]["yp"]
        y_s[c * NS:(c + 1) * NS, 0] = R[c]["ys"]
        if hh == 1:
            pk[0, n] = R[c]["pck"].reshape(P, 4, 64)
            pv[0, n] = R[c]["pcv"].reshape(P, 4, 64)
            pc_[0, n] = R[c]["psc"]
        sk[0, c * NS:(c + 1) * NS] = R[c]["sck"].reshape(NS, P, 4, 64)
        sv[0, c * NS:(c + 1) * NS] = R[c]["scvo"].reshape(NS, P, 4, 64)
        ssc_[0, c * NS:(c + 1) * NS] = R[c]["ssc"]
    return (y_p, y_s, pk, pv, pc_, sk, sv, ssc_)
```
